# Optimizing a Trainium2 kernel written in Bass

```python
import math
import jax, jax.numpy as jnp
from jax import lax
import numpy as np

D_MODEL = 1024
BATCH = 32
SEQ = 2048
DEPTH = 4

GRID_W = 64
CTX_LEN = 256
EPS = 1e-6
ROPE_BASE = 10000.0
ROT_DIM = 64
Q_BLOCK = 128
CHUNK = 64
N_BRANCH = 4
BRANCH_WIDTH = 512

MLA_HEADS = 4
MLA_NOPE = 128
MLA_ROPE = ROT_DIM
MLA_V = BRANCH_WIDTH // MLA_HEADS
MLA_Q_LORA = 384
MLA_KV_LORA = 256
GLA_HEADS = 4
GLA_DK = 64
GLA_DV = BRANCH_WIDTH // GLA_HEADS
GLA_GATE_RANK = 16
GLA_TAU = 16.0
ML_HEADS = 4
ML_DQK = 64
ML_DV = BRANCH_WIDTH // ML_HEADS
ML_CONV = 3
ML_F_BIAS = 3.0
DF_HEADS = 4
DF_DQK = ROT_DIM
DF_DV = BRANCH_WIDTH // DF_HEADS

IN_SPLITS = (
    ('mla_cq', MLA_Q_LORA), ('mla_ckv', MLA_KV_LORA), ('mla_kr', MLA_ROPE), ('mla_z', BRANCH_WIDTH),
    ('gla_q', GLA_HEADS * GLA_DK), ('gla_k', GLA_HEADS * GLA_DK), ('gla_v', BRANCH_WIDTH),
    ('gla_af', GLA_GATE_RANK), ('gla_ab', GLA_GATE_RANK), ('gla_z', BRANCH_WIDTH),
    ('ml_x', BRANCH_WIDTH), ('ml_v', BRANCH_WIDTH), ('ml_o', BRANCH_WIDTH), ('ml_if', 4 * ML_HEADS),
    ('ml_z', BRANCH_WIDTH),
    ('df_q', DF_HEADS * 2 * DF_DQK), ('df_k', DF_HEADS * 2 * DF_DQK), ('df_v', BRANCH_WIDTH),
    ('df_z', BRANCH_WIDTH),
    ('merge', N_BRANCH * D_MODEL),
)
D_IN = sum(w for _, w in IN_SPLITS)

kernel_name = 'hybrid_mla_gla_mlstm_diffattn_dit_block'

F32 = jnp.float32


def rmsnorm(x, g):
    xf = x.astype(F32)
    y = xf * lax.rsqrt(jnp.mean(xf * xf, axis=-1, keepdims=True) + EPS)
    return (y * g.astype(F32)).astype(x.dtype)


def split_cols(proj):
    out, off = {}, 0
    for name, w in IN_SPLITS:
        out[name] = proj[..., off:off + w]
        off += w
    return out


def axial_rope_tables(rows, n_ctx, dtype):
    quarter = ROT_DIM // 4
    inv_freq = ROPE_BASE ** (-jnp.arange(quarter, dtype=F32) / quarter)
    row = jnp.repeat(jnp.arange(rows, dtype=F32), GRID_W)
    col = jnp.tile(jnp.arange(GRID_W, dtype=F32), rows)
    ar = row[:, None] * inv_freq
    ac = col[:, None] * inv_freq
    ang = jnp.concatenate([ar, ar, ac, ac], axis=-1)
    cos = jnp.concatenate([jnp.ones((n_ctx, ROT_DIM), F32), jnp.cos(ang)], axis=0)
    sin = jnp.concatenate([jnp.zeros((n_ctx, ROT_DIM), F32), jnp.sin(ang)], axis=0)
    return cos.astype(dtype), sin.astype(dtype)


def apply_rope(x, cos, sin):
    a1, a2, b1, b2 = jnp.split(x, 4, axis=-1)
    rot = jnp.concatenate([-a2, a1, -b2, b1], axis=-1)
    return x * cos + rot * sin


def flip_segments(a, n_ctx, axis):
    c_part = lax.slice_in_dim(a, 0, n_ctx, axis=axis)
    l_part = lax.slice_in_dim(a, n_ctx, a.shape[axis], axis=axis)
    return jnp.concatenate([jnp.flip(c_part, axis), jnp.flip(l_part, axis)], axis=axis)


def centred_dwconv(x, w, b):
    pad = w.shape[0] // 2
    y = lax.conv_general_dilated(x, w[:, None, :].astype(x.dtype), window_strides=(1,),
                                 padding=[(pad, pad)], dimension_numbers=('NWC', 'WIO', 'NWC'),
                                 feature_group_count=x.shape[-1])
    return y + b


def _chunks(a):
    B, H, T = a.shape[:3]
    a = a.reshape((B, H, T // CHUNK, CHUNK) + a.shape[3:])
    return jnp.moveaxis(a, 2, 0)


def _unchunks(a):
    a = jnp.moveaxis(a, 0, 2)
    return a.reshape(a.shape[:2] + (-1,) + a.shape[4:])


def mixed_softmax_attention(q, k, v, w, n_ctx, with_ctx):
    B, H, P, T, dq = q.shape
    scale = dq ** -0.5
    wf = w.astype(F32)

    def attend(qb, kb, vb):
        s = jnp.einsum('bhpqd,bhpkd->bhpqk', qb, kb).astype(F32) * scale
        pr = jax.nn.softmax(s, axis=-1)
        pw = jnp.einsum('bhpqk,hp->bhqk', pr, wf)
        return jnp.einsum('bhqk,bhkd->bhqd', pw.astype(vb.dtype), vb)

    n_lat = T - n_ctx
    nb = n_lat // Q_BLOCK
    ql = q[:, :, :, n_ctx:].reshape(B, H, P, nb, Q_BLOCK, dq)
    ql = jnp.moveaxis(ql, 3, 0)
    ol = lax.map(lambda qb: attend(qb, k, v), ql)
    ol = jnp.moveaxis(ol, 0, 2).reshape(B, H, n_lat, v.shape[-1])
    if not with_ctx:
        return ol
    oc = attend(q[:, :, :, :n_ctx], k[:, :, :, :n_ctx], v[:, :, :n_ctx])
    return jnp.concatenate([oc, ol], axis=2)


def gla_chunk_scan(q, k, v, log_a):
    B, H, T, dk = q.shape
    dv = v.shape[-1]
    mask = jnp.tril(jnp.ones((CHUNK, CHUNK), bool))[:, :, None]

    def step(S, inp):
        qc, kc, vc, lac = inp
        b = jnp.cumsum(lac, axis=-2)
        o_inter = jnp.einsum('bhld,bhde->bhle', qc * jnp.exp(b), S)
        diff = jnp.where(mask, b[:, :, :, None, :] - b[:, :, None, :, :], -jnp.inf)
        A = jnp.einsum('bhtd,bhsd,bhtsd->bhts', qc, kc, jnp.exp(diff))
        o_intra = jnp.einsum('bhts,bhse->bhte', A, vc)
        bl = b[:, :, -1:, :]
        S_new = jnp.exp(bl[:, :, 0, :])[..., None] * S + jnp.einsum('bhsd,bhse->bhde', kc * jnp.exp(bl - b), vc)
        return S_new, o_inter + o_intra

    S0 = jnp.zeros((B, H, dk, dv), F32)
    _, o = lax.scan(step, S0, (_chunks(q), _chunks(k), _chunks(v), _chunks(log_a)))
    return _unchunks(o)


def mlstm_chunk_scan(q, k, v, log_i, log_f):
    B, H, T, dk = q.shape
    dv = v.shape[-1]
    mask = jnp.tril(jnp.ones((CHUNK, CHUNK), bool))

    def step(carry, inp):
        C, nv, m = carry
        qc, kc, vc, lic, lfc = inp
        b = jnp.cumsum(lfc, axis=-1)
        log_inter = b + m[..., None]
        log_D = jnp.where(mask, b[..., :, None] - b[..., None, :] + lic[..., None, :], -jnp.inf)
        m_t = jnp.maximum(log_inter, jnp.max(log_D, axis=-1))
        inter_w = jnp.exp(log_inter - m_t)
        s = jnp.einsum('bhtd,bhsd->bhts', qc, kc) * jnp.exp(log_D - m_t[..., None])
        num = inter_w[..., None] * jnp.einsum('bhtd,bhde->bhte', qc, C) + jnp.einsum('bhts,bhse->bhte', s, vc)
        den = inter_w * jnp.einsum('bhtd,bhd->bht', qc, nv) + jnp.sum(s, axis=-1)
        h = num / jnp.maximum(jnp.abs(den), jnp.exp(-m_t))[..., None]
        bL = b[..., -1]
        log_upd = bL[..., None] - b + lic
        m_new = jnp.maximum(bL + m, jnp.max(log_upd, axis=-1))
        decay = jnp.exp(bL + m - m_new)
        wu = jnp.exp(log_upd - m_new[..., None])
        C_new = decay[..., None, None] * C + jnp.einsum('bhs,bhsd,bhse->bhde', wu, kc, vc)
        n_new = decay[..., None] * nv + jnp.einsum('bhs,bhsd->bhd', wu, kc)
        return (C_new, n_new, m_new), h

    init = (jnp.zeros((B, H, dk, dv), F32), jnp.zeros((B, H, dk), F32), jnp.zeros((B, H), F32))
    _, h = lax.scan(step, init, (_chunks(q), _chunks(k), _chunks(v), _chunks(log_i), _chunks(log_f)))
    return _unchunks(h)


def mla_branch(p, cq_g, ckv_g, wuq, wukv, q_g, k_g, cos, sin, n_ctx, t0):
    B, T, _ = p['mla_cq'].shape
    H = MLA_HEADS
    q = (rmsnorm(p['mla_cq'], cq_g) @ wuq).reshape(B, T, H, MLA_NOPE + MLA_ROPE)
    kv = (rmsnorm(p['mla_ckv'], ckv_g) @ wukv).reshape(B, T, H, MLA_NOPE + MLA_V)
    q_nope = rmsnorm(q[..., :MLA_NOPE], q_g[:MLA_NOPE])
    q_rope = apply_rope(rmsnorm(q[..., MLA_NOPE:], q_g[MLA_NOPE:]), cos[:, None], sin[:, None])
    k_nope = rmsnorm(kv[..., :MLA_NOPE], k_g[:MLA_NOPE])
    k_rope = apply_rope(rmsnorm(p['mla_kr'], k_g[MLA_NOPE:]), cos, sin)
    k_rope = jnp.broadcast_to(k_rope[:, :, None, :], (B, T, H, MLA_ROPE))
    qf = jnp.concatenate([q_nope, q_rope], axis=-1).transpose(0, 2, 1, 3)[:, :, None]
    kf = jnp.concatenate([k_nope, k_rope], axis=-1).transpose(0, 2, 1, 3)[:, :, None]
    vf = kv[..., MLA_NOPE:].transpose(0, 2, 1, 3)
    o = mixed_softmax_attention(qf, kf, vf, jnp.ones((H, 1), F32), n_ctx, t0 == 0)
    o = o.transpose(0, 2, 1, 3).reshape(B, -1, BRANCH_WIDTH)
    return o * jax.nn.silu(p['mla_z'][:, t0:])


def gla_branch(p, a_w, a_b, out_g, n_ctx, t0):
    B, T, _ = p['gla_q'].shape
    H = GLA_HEADS

    def heads(a, d):
        return a.reshape(B, T, H, d).transpose(0, 2, 1, 3).astype(F32)

    q = heads(p['gla_q'], GLA_DK) * GLA_DK ** -0.5
    k = heads(p['gla_k'], GLA_DK)
    v = heads(p['gla_v'], GLA_DV)
    la_f = heads(jax.nn.log_sigmoid((p['gla_af'] @ a_w[0] + a_b[0]).astype(F32)) / GLA_TAU, GLA_DK)
    la_b = heads(jax.nn.log_sigmoid((p['gla_ab'] @ a_w[1] + a_b[1]).astype(F32)) / GLA_TAU, GLA_DK)
    o_f = gla_chunk_scan(q, k, v, la_f)
    fl = lambda a: flip_segments(a, n_ctx, 2)
    o_b = fl(gla_chunk_scan(fl(q), fl(k), fl(v), fl(la_b)))
    o = (o_f + o_b).transpose(0, 2, 1, 3)
    o = rmsnorm(o, out_g.reshape(H, GLA_DV)).reshape(B, T, BRANCH_WIDTH).astype(p['gla_z'].dtype)
    return (o * jax.nn.silu(p['gla_z']))[:, t0:]


def mlstm_branch(p, conv_w, conv_b, wq, wk, gate_b, out_g, skip, n_ctx, t0):
    B, T, _ = p['ml_x'].shape
    H = ML_HEADS
    xm = p['ml_x']
    xc = jnp.concatenate([centred_dwconv(xm[:, :n_ctx], conv_w, conv_b),
                          centred_dwconv(xm[:, n_ctx:], conv_w, conv_b)], axis=1)
    xc = jax.nn.silu(xc)
    xh = xc.reshape(B, T, H, ML_DV)
    q = jnp.einsum('bthc,hcd->bhtd', xh, wq).astype(F32)
    k = jnp.einsum('bthc,hcd->bhtd', xh, wk).astype(F32) * ML_DQK ** -0.5
    v = p['ml_v'].reshape(B, T, H, ML_DV).transpose(0, 2, 1, 3).astype(F32)
    g = (p['ml_if'].reshape(B, T, 4, H) + gate_b).astype(F32).transpose(2, 0, 3, 1)
    h_f = mlstm_chunk_scan(q, k, v, g[0], jax.nn.log_sigmoid(g[1]))
    fl = lambda a, ax=2: flip_segments(a, n_ctx, ax)
    h_b = fl(mlstm_chunk_scan(fl(q), fl(k), fl(v), fl(g[2], 2), fl(jax.nn.log_sigmoid(g[3]), 2)))
    h = (h_f + h_b).transpose(0, 2, 1, 3)
    h = jax.nn.sigmoid(p['ml_o'].astype(F32)).reshape(B, T, H, ML_DV) * h
    h = rmsnorm(h, out_g.reshape(H, ML_DV)).reshape(B, T, BRANCH_WIDTH).astype(xm.dtype)
    y = (h + skip * xc) * jax.nn.silu(p['ml_z'])
    return y[:, t0:]


def diff_branch(p, qk_g, lam_p, out_g, lam_init, cos, sin, n_ctx, t0):
    B, T, _ = p['df_q'].shape
    H = DF_HEADS
    cs, sn = cos[:, None, None], sin[:, None, None]
    q = apply_rope(rmsnorm(p['df_q'].reshape(B, T, H, 2, DF_DQK), qk_g[0]), cs, sn)
    k = apply_rope(rmsnorm(p['df_k'].reshape(B, T, H, 2, DF_DQK), qk_g[1]), cs, sn)
    q = q.transpose(0, 2, 3, 1, 4)
    k = k.transpose(0, 2, 3, 1, 4)
    v = p['df_v'].reshape(B, T, H, DF_DV).transpose(0, 2, 1, 3)
    lp = lam_p.astype(F32)
    lam = jnp.exp(jnp.sum(lp[0] * lp[1])) - jnp.exp(jnp.sum(lp[2] * lp[3])) + lam_init
    w = jnp.broadcast_to(jnp.stack([jnp.ones_like(lam), -lam]), (H, 2))
    o = mixed_softmax_attention(q, k, v, w, n_ctx, t0 == 0)
    o = rmsnorm(o, out_g) * (1.0 - lam_init)
    o = o.transpose(0, 2, 1, 3).reshape(B, -1, BRANCH_WIDTH)
    return o * jax.nn.silu(p['df_z'][:, t0:])


def setup_inputs(seed: int = 0) -> dict:
    key = jax.random.key(seed)
    ks = iter(jax.random.split(key, 40))
    L, D = DEPTH, D_MODEL

    def nrm(shape, scale):
        return scale * jax.random.normal(next(ks), shape, F32)

    def gain(shape):
        return 1.0 + nrm(shape, 0.02)

    out = {}
    out['x'] = nrm((BATCH, SEQ, D), 1.0)
    out['c'] = nrm((BATCH, D), 1.0)
    out['ctx'] = nrm((BATCH, CTX_LEN, D), 1.0)
    out['c_ctx'] = nrm((D,), 1.0)
    out['ada_w'] = nrm((L, D, 3 * D), D ** -0.5)
    out['ada_b'] = nrm((L, 3 * D), 0.01)
    out['norm_g'] = gain((L, D))
    out['w_in'] = nrm((L, D, D_IN), D ** -0.5)
    out['mla_cq_g'] = gain((L, MLA_Q_LORA))
    out['mla_ckv_g'] = gain((L, MLA_KV_LORA))
    out['mla_wuq'] = nrm((L, MLA_Q_LORA, MLA_HEADS * (MLA_NOPE + MLA_ROPE)), MLA_Q_LORA ** -0.5)
    out['mla_wukv'] = nrm((L, MLA_KV_LORA, MLA_HEADS * (MLA_NOPE + MLA_V)), MLA_KV_LORA ** -0.5)
    out['mla_q_g'] = gain((L, MLA_NOPE + MLA_ROPE))
    out['mla_k_g'] = gain((L, MLA_NOPE + MLA_ROPE))
    out['gla_a_w'] = nrm((L, 2, GLA_GATE_RANK, GLA_HEADS * GLA_DK), GLA_GATE_RANK ** -0.5)
    out['gla_a_b'] = nrm((L, 2, GLA_HEADS * GLA_DK), 0.01)
    out['gla_out_g'] = gain((L, BRANCH_WIDTH))
    out['ml_conv_w'] = nrm((L, ML_CONV, BRANCH_WIDTH), ML_CONV ** -0.5)
    out['ml_conv_b'] = nrm((L, BRANCH_WIDTH), 0.01)
    out['ml_wq'] = nrm((L, ML_HEADS, ML_DV, ML_DQK), ML_DV ** -0.5)
    out['ml_wk'] = nrm((L, ML_HEADS, ML_DV, ML_DQK), ML_DV ** -0.5)
    out['ml_gate_b'] = jnp.array([0.0, ML_F_BIAS, 0.0, ML_F_BIAS], F32)[None, :, None] + nrm((L, 4, ML_HEADS), 0.1)
    out['ml_out_g'] = gain((L, BRANCH_WIDTH))
    out['ml_skip'] = gain((L, BRANCH_WIDTH))
    out['df_qk_g'] = gain((L, 2, DF_DQK))
    out['df_lambda'] = nrm((L, 4, DF_DQK), 0.1)
    out['df_out_g'] = gain((L, DF_DV))
    out['br_w'] = nrm((L, N_BRANCH, BRANCH_WIDTH, D), BRANCH_WIDTH ** -0.5)
    out['w_out'] = nrm((L, D, D), D ** -0.5)
    return out


def reference(x, c, ctx, c_ctx, ada_w, ada_b, norm_g, w_in, mla_cq_g, mla_ckv_g, mla_wuq, mla_wukv,
              mla_q_g, mla_k_g, gla_a_w, gla_a_b, gla_out_g, ml_conv_w, ml_conv_b, ml_wq, ml_wk,
              ml_gate_b, ml_out_g, ml_skip, df_qk_g, df_lambda, df_out_g, br_w, w_out):
    B, S, D = x.shape
    n_ctx = ctx.shape[1]
    ROWS = S // GRID_W
    cos, sin = axial_rope_tables(ROWS, n_ctx, x.dtype)
    s_c = jax.nn.silu(c)
    s_cc = jax.nn.silu(c_ctx)
    for l in range(DEPTH):
        last = l == DEPTH - 1
        t0 = n_ctx if last else 0
        shift, scale, gate = jnp.split(s_c @ ada_w[l] + ada_b[l], 3, axis=-1)
        shift_c, scale_c, gate_c = jnp.split(s_cc @ ada_w[l] + ada_b[l], 3, axis=-1)
        h = jnp.concatenate([rmsnorm(ctx, norm_g[l]) * (1 + scale_c) + shift_c,
                             rmsnorm(x, norm_g[l]) * (1 + scale[:, None]) + shift[:, None]], axis=1)
        p = split_cols(h @ w_in[l])
        lam_init = 0.8 - 0.6 * math.exp(-0.3 * l)
        ys = (
            mla_branch(p, mla_cq_g[l], mla_ckv_g[l], mla_wuq[l], mla_wukv[l], mla_q_g[l], mla_k_g[l],
                       cos, sin, n_ctx, t0),
            gla_branch(p, gla_a_w[l], gla_a_b[l], gla_out_g[l], n_ctx, t0),
            mlstm_branch(p, ml_conv_w[l], ml_conv_b[l], ml_wq[l], ml_wk[l], ml_gate_b[l], ml_out_g[l],
                         ml_skip[l], n_ctx, t0),
            diff_branch(p, df_qk_g[l], df_lambda[l], df_out_g[l], lam_init, cos, sin, n_ctx, t0),
        )
        gl = p['merge'][:, t0:]
        merged = jax.nn.sigmoid(gl[..., :D]) * (ys[0] @ br_w[l, 0])
        for i in range(1, N_BRANCH):
            merged = merged + jax.nn.sigmoid(gl[..., i * D:(i + 1) * D]) * (ys[i] @ br_w[l, i])
        out = merged @ w_out[l]
        x = x + gate[:, None] * out[:, n_ctx - t0:]
        if not last:
            ctx = ctx + gate_c * out[:, :n_ctx]
    return x
```

```python
import math
import contextlib
import numpy as np
import concourse.bass as bass
import concourse.mybir as mybir
from concourse.bass_utils import run_bass_kernel_spmd

F32 = mybir.dt.float32
BF16 = mybir.dt.bfloat16
AF = mybir.ActivationFunctionType
ALU = mybir.AluOpType
AX = mybir.AxisListType

T = 2304
NCTX = 256
NT = 18
D = 1024
DIN = 10992
NL = 4
EPS = 1e-6
BLKS = [(0, 256), (256, 512), (768, 512), (1280, 512), (1792, 512)]
OFF = dict(mla_cq=0, mla_ckv=384, mla_kr=640, mla_z=704, gla_q=1216, gla_k=1472, gla_v=1728, gla_af=2240,
           gla_ab=2256, gla_z=2272, ml_x=2784, ml_v=3296, ml_o=3808, ml_if=4320, ml_z=4336, df_q=4848,
           df_k=5360, df_v=5872, df_z=6384, merge=6896)
PC_NORMG, PC_CQG, PC_CKVG, PC_QGN, PC_QGR, PC_KGN, PC_KGR, PC_DFQG, PC_DFKG, PC_DFOG = 0, 8, 11, 13, 14, 15, 16, 17, 18, 19
PC_GLAOG, PC_MLOG, PC_MLSKIP, PC_CONVW, PC_CONVB, PC_GLAAB, NPC = 20, 24, 28, 32, 44, 48, 52
NPR = 16 + 256
DC_CQG, DC_CKVG, DC_QGN, DC_QGR, DC_KGN, DC_KGR, DC_DFQG, DC_DFKG, DC_DFOG, DC_GLAOG, DC_MLOG, DC_NAB, DC_LAM, NDC = 0, 3, 5, 6, 7, 8, 9, 10, 11, 12, 16, 20, 24, 28

import os
PE, ACT, DVE, POOL, SP = range(5)
SERIAL = os.environ.get("SERIAL", "0") == "1"
PEL = DVE if os.environ.get("NOPOOL", "1") == "1" else POOL
NDS = 24


class V:
    __slots__ = ("tile", "ap", "subs")

    def __init__(self, tile, ap, subs):
        self.tile, self.ap, self.subs = tile, ap, subs


class _Sel:
    def __init__(self, tile, subs):
        self.tile, self.subs = tile, subs

    def __getitem__(self, idx):
        return V(self.tile, self.tile.t[idx], self.subs)


class Tile:
    def __init__(self, t, nsub=1):
        self.t = t
        self.nsub = nsub
        self.w = [None] * nsub
        self.r = [dict() for _ in range(nsub)]
        self.excl = False

    def __getitem__(self, idx):
        return V(self, self.t[idx], None)

    def s(self, subs):
        if isinstance(subs, int):
            subs = [subs]
        return _Sel(self, list(subs))


def tk(a, b):
    return list(range(a // 128, (b + 127) // 128))


class Ctx:
    def __init__(self, nc):
        self.nc = nc
        self.engs = [nc.tensor, nc.scalar, nc.vector, nc.gpsimd, nc.sync]
        self.es = contextlib.ExitStack()
        self.esem = [self.es.enter_context(nc.semaphore(f"e{i}")) for i in range(5)]
        self.dsem = [self.es.enter_context(nc.semaphore(f"d{i}")) for i in range(NDS)]
        self.cnt = [0] * 5
        self.dval = [0] * NDS
        self.dk = 0
        self.seen = [dict() for _ in range(5)]
        self.scopes = [self.es]
        self.scope_tiles = [[]]
        self.freed = {}
        self.uid = 0
        self.ninstr = 0
        self.last = None

    def push(self):
        st = contextlib.ExitStack()
        self.scopes.append(st)
        self.scope_tiles.append([])
        return st

    def pop(self):
        for tl in self.scope_tiles.pop():
            for s in range(tl.nsub):
                w = tl.w[s]
                if w is not None and self.freed.get(w[0], 0) < w[1]:
                    self.freed[w[0]] = w[1]
                for key, val in tl.r[s].items():
                    if self.freed.get(key, 0) < val:
                        self.freed[key] = val
        self.scopes.pop().close()

    def sb(self, shape, dt, nsub=1, name=None):
        self.uid += 1
        t = self.scopes[-1].enter_context(self.nc.sbuf_tensor(f"{name or 't'}{self.uid}", list(shape), dt))
        tl = Tile(t, nsub)
        if self.freed:
            for s in range(nsub):
                tl.r[s] = dict(self.freed)
        self.scope_tiles[-1].append(tl)
        return tl

    def psum(self, shape, dt):
        self.uid += 1
        t = self.scopes[-1].enter_context(self.nc.psum_tensor(f"ps{self.uid}", list(shape), dt))
        tl = Tile(t, 1)
        tl.excl = True
        return tl

    def _sync(self, eng, reads, writes, extra=None):
        need = {}

        def add(key, val):
            if key[0] == "E" and key[1] == eng and eng == PE:
                return
            if need.get(key, 0) < val:
                need[key] = val

        for v in reads:
            if v.tile is None:
                continue
            for s in (v.subs if v.subs is not None else range(v.tile.nsub)):
                w = v.tile.w[s]
                if w is not None:
                    add(w[0], w[1])
                if v.tile.excl:
                    for key, val in v.tile.r[s].items():
                        if not (key[0] == "E" and key[1] == eng):
                            add(key, val)
        for v in writes:
            if v.tile is None:
                continue
            for s in (v.subs if v.subs is not None else range(v.tile.nsub)):
                w = v.tile.w[s]
                if w is not None:
                    add(w[0], w[1])
                for key, val in v.tile.r[s].items():
                    add(key, val)
        if extra is not None and extra[1] > 0:
            add(extra[0], extra[1])
        if SERIAL and self.last is not None:
            if not (self.last[0][0] == "E" and self.last[0][1] == eng and eng == PE):
                if need.get(self.last[0], 0) < self.last[1]:
                    need[self.last[0]] = self.last[1]
        seen = self.seen[eng]
        for key, val in need.items():
            if seen.get(key, 0) >= val:
                continue
            seen[key] = val
            sem = self.esem[key[1]] if key[0] == "E" else self.dsem[key[1]]
            self.engs[eng].wait_ge(sem, val)
            self.ninstr += 1

    def _commit(self, tok, reads, writes):
        key, val = tok
        self.last = tok
        for v in reads:
            if v.tile is None:
                continue
            for s in (v.subs if v.subs is not None else range(v.tile.nsub)):
                r = v.tile.r[s]
                if r.get(key, 0) < val:
                    r[key] = val
        for v in writes:
            if v.tile is None:
                continue
            for s in (v.subs if v.subs is not None else range(v.tile.nsub)):
                v.tile.w[s] = tok
                v.tile.r[s] = {}

    def op(self, eng, name, **kw):
        reads, writes, args = [], [], {}
        for k, v in kw.items():
            if isinstance(v, V):
                (writes if k in ("out", "accum_out") else reads).append(v)
                args[k] = v.ap
            else:
                args[k] = v
        self._sync(eng, reads, writes)
        ins = getattr(self.engs[eng], name)(**args)
        self.cnt[eng] += 1
        ins.then_inc(self.esem[eng], 1)
        self.ninstr += 1
        self._commit((("E", eng), self.cnt[eng]), reads, writes)

    def dma(self, q, out, in_, **kw):
        k = self.dk
        self.dk = (k + 1) % NDS
        prev = self.dval[k]
        self._sync(q, [in_], [out], extra=(("D", k), prev))
        ins = self.engs[q].dma_start(out=out.ap, in_=in_.ap, **kw)
        ins.then_inc(self.dsem[k], 16)
        self.ninstr += 1
        self.dval[k] = prev + 16
        self._commit((("D", k), prev + 16), [in_], [out])

    def finish(self):
        for k in range(NDS):
            if self.dval[k] > 0:
                self.nc.sync.wait_ge(self.dsem[k], self.dval[k])
        for e in range(4):
            if self.cnt[e] > 0:
                self.nc.sync.wait_ge(self.esem[e], self.cnt[e])

    def mm(self, out, lhsT, rhs, start=True, stop=True):
        self.op(PE, "matmul", out=out, lhsT=lhsT, rhs=rhs, start=start, stop=stop, skip_group_check=True)

    def tr(self, out, in_, identity):
        self.op(PE, "transpose", out=out, in_=in_, identity=identity)

    def act(self, out, in_, func, **kw):
        self.op(ACT, "activation", out=out, in_=in_, func=func, **kw)


def U(ap):
    return V(None, ap, None)


class K:
    pass


def build(nseq, nlayers, dbg=False):
    nc = bass.Bass("TRN2", target_bir_lowering=False)
    cx = Ctx(nc)
    k = K()
    k.nc, k.cx, k.dbg = nc, cx, dbg

    def din(name, shape, dt=F32):
        return nc.dram_tensor(name, list(shape), dt, kind="ExternalInput").ap()

    k.x = din("x", [4, 2048, D]); k.ctxin = din("ctx", [4, NCTX, D]); k.c5 = din("c5", [5, D])
    k.ada_w = din("ada_w", [NL, D, 3 * D]); k.ada_b = din("ada_b", [NL, 3 * D])
    k.w_in = din("w_in", [NL, D, DIN]); k.wuq = din("wuq", [NL, 384, 768]); k.wukv = din("wukv", [NL, 256, 1024])
    k.gla_a_w = din("gla_a_w", [NL, 2, 16, 256]); k.ml_wq = din("ml_wq", [NL, 4, 128, 64]); k.ml_wk = din("ml_wk", [NL, 4, 128, 64])
    k.br_w = din("br_w", [NL, 4, 512, D]); k.w_out = din("w_out", [NL, D, D])
    k.pcols = din("pcols", [128, NL, NPC]); k.prow = din("prow", [NL, NPR])
    k.cident = din("cident", [128, 128]); k.cbd64 = din("cbd64", [128, 128]); k.crm2 = din("crm2", [128, 128])
    k.ctrif = din("ctrif", [128, 128]); k.ctrib = din("ctrib", [128, 128])
    k.ccos = din("ccos", [128, T]); k.csin = din("csin", [128, T])
    k.out = nc.dram_tensor("out", [4, 2048, D], F32, kind="ExternalOutput").ap()
    k.xres_ap = nc.dram_tensor("xres", [4, T, D], F32, kind="Internal").ap()
    k.gscr_ap = nc.dram_tensor("gscr", [NL, 5, D], F32, kind="Internal").ap()
    k.xres = [Tile(k.xres_ap[s], NT) for s in range(4)]
    k.gscr = Tile(k.gscr_ap, NL)
    if dbg:
        k.dbg_y = nc.dram_tensor("dbg_y", [4, 128, 4, T], F32, kind="ExternalOutput").ap()
        k.dbg_acc = nc.dram_tensor("dbg_acc", [128, 8, T], F32, kind="ExternalOutput").ap()
        k.dbg_h = nc.dram_tensor("dbg_h", [128, 8, T], F32, kind="ExternalOutput").ap()
        k.dbg_x = nc.dram_tensor("dbg_x", [T, D], F32, kind="ExternalOutput").ap()

    def cload(src, dt, shape=(128, 128), q=POOL):
        t = cx.sb(shape, dt)
        cx.dma(q, t[:], U(src))
        return t

    k.identb = cload(k.cident, BF16); k.identf = cload(k.cident, F32, q=SP)
    k.bd64 = cload(k.cbd64, BF16); k.rm2 = cload(k.crm2, BF16)
    k.trif = cload(k.ctrif, F32, q=SP); k.trib = cload(k.ctrib, F32, q=SP)
    k.trifb = cload(k.ctrif, BF16); k.tribb = cload(k.ctrib, BF16)
    k.cos = cload(k.ccos, BF16, (128, T)); k.sin = cload(k.csin, BF16, (128, T))
    k.onesb = cx.sb((128, 128), BF16)
    k.onesf = cx.sb((128, 128), F32)
    _memset(cx, k.onesb[:], 1.0); _memset(cx, k.onesf[:], 1.0)
    k.epsc = cx.sb((128, 1), F32); _memset(cx, k.epsc[:], EPS)
    k.onec = cx.sb((128, 1), F32); _memset(cx, k.onec[:], 1.0)
    k.pc = cx.sb((128, NL, NPC), F32)
    cx.dma(SP, k.pc[:], U(k.pcols))
    k.AB = cx.sb((128, NL, 5, 16), F32)
    k.ps = [cx.psum((128, 512), F32) for _ in range(8)]
    k.psi = 0
    k.wpool = [cx.sb((128, 8, 512), BF16) for _ in range(3)]
    k.wi = 0

    for s in range(nseq):
        cx.dma(SP, k.xres[s].s([0, 1])[0:NCTX, :], U(k.ctxin[s]))
        for j in range(4):
            cx.dma(SP, k.xres[s].s(tk(NCTX + j * 512, NCTX + (j + 1) * 512))[NCTX + j * 512:NCTX + (j + 1) * 512, :],
                   U(k.x[s, j * 512:(j + 1) * 512, :]))

    prologue(k)
    for s in range(nseq):
        for l in range(nlayers):
            layer(k, s, l, last=(l == NL - 1))
    cx.finish()
    cx.es.close()
    return nc, cx


def _memset(cx, v, val, eng=DVE):
    cx._sync(eng, [], [v])
    ins = cx.engs[eng].memset(v.ap, val)
    cx.cnt[eng] += 1
    ins.then_inc(cx.esem[eng], 1)
    cx._commit((("E", eng), cx.cnt[eng]), [], [v])


def nps(k):
    p = k.ps[k.psi]
    k.psi = (k.psi + 1) % 8
    return p


def wload(k, src2d, kc, ncols):
    wt = k.wpool[k.wi]
    k.wi = (k.wi + 1) % len(k.wpool)
    k.cx.dma(POOL, wt[:, 0:kc, 0:ncols], U(src2d.rearrange("(kc p) n -> p kc n", p=128)))
    return wt


def prologue(k):
    cx, nc = k.cx, k.nc
    cx.push()
    c5t = cx.sb((5, D), F32)
    cx.dma(SP, c5t[:], U(k.c5))
    cx.act(c5t[:], c5t[:], AF.Silu)
    scT = cx.sb((128, 8, 5), F32)
    p = nps(k)
    for kc in range(8):
        cx.tr(p[:, kc * 8:kc * 8 + 5], c5t[0:5, kc * 128:(kc + 1) * 128], k.identf[0:5, 0:5])
    cx.op(DVE, "tensor_copy", out=scT[:], in_=V(p, p.t[:, 0:64].rearrange("p (a b) -> p a b", b=8)[:, :, 0:5], None))
    wf = [cx.sb((128, 8, 512), F32) for _ in range(2)]
    adab = cx.sb((5, 3 * D), F32)
    modrow = cx.sb((5, 3 * D), F32)
    modcol = cx.sb((128, 16, 5), F32)
    for l in range(NL):
        cx.dma(SP, adab[:], U(k.ada_b[l].partition_broadcast(5)))
        for nb in range(6):
            w = wf[nb % 2]
            cx.dma(SP, w[:], U(k.ada_w[l][:, nb * 512:(nb + 1) * 512].rearrange("(kc p) n -> p kc n", p=128)))
            p = nps(k)
            for kc in range(8):
                cx.mm(p[0:5, :], scT[:, kc, :], w[:, kc, :], start=(kc == 0), stop=(kc == 7))
            cx.op(DVE, "tensor_tensor", out=modrow[:, nb * 512:(nb + 1) * 512], in0=p[0:5, :], in1=adab[:, nb * 512:(nb + 1) * 512], op=ALU.add)
        p = nps(k)
        for j in range(16):
            cx.tr(p[:, j * 8:j * 8 + 5], modrow[0:5, j * 128:(j + 1) * 128], k.identf[0:5, 0:5])
        cx.op(DVE, "tensor_copy", out=modcol[:], in_=V(p, p.t[:, 0:128].rearrange("p (a b) -> p a b", b=8)[:, :, 0:5], None))
        for r in range(5):
            cx.op(DVE, "scalar_tensor_tensor", out=k.AB[:, l, r, 0:8], in0=modcol[:, 8:16, r], scalar=1.0, in1=k.pc[:, l, PC_NORMG:PC_NORMG + 8], op0=ALU.add, op1=ALU.mult)
            cx.op(DVE, "tensor_copy", out=k.AB[:, l, r, 8:16], in_=modcol[:, 0:8, r])
        cx.dma(SP, k.gscr.s(l)[l], modrow[0:5, 2 * D:3 * D])
    cx.pop()


def layer_params(k, l):
    cx = k.cx
    dc = cx.sb((128, NDC), F32)
    pc = k.pc

    def sc(dst, src, n, f):
        cx.op(DVE, "tensor_scalar", out=dc[:, dst:dst + n], in0=pc[:, l, src:src + n], scalar1=float(f), scalar2=None, op0=ALU.mult)

    lam_init = 0.8 - 0.6 * math.exp(-0.3 * l)
    sc(DC_CQG, PC_CQG, 3, 1.0); sc(DC_CKVG, PC_CKVG, 2, 1.0)
    sc(DC_QGN, PC_QGN, 1, 192 ** -0.5); sc(DC_QGR, PC_QGR, 1, 192 ** -0.5)
    sc(DC_KGN, PC_KGN, 1, 1.0); sc(DC_KGR, PC_KGR, 1, 1.0)
    sc(DC_DFQG, PC_DFQG, 1, 0.125); sc(DC_DFKG, PC_DFKG, 1, 1.0)
    sc(DC_DFOG, PC_DFOG, 1, (1.0 - lam_init))
    sc(DC_GLAOG, PC_GLAOG, 4, 1.0); sc(DC_MLOG, PC_MLOG, 4, 1.0)
    sc(DC_NAB, PC_GLAAB, 4, -1.0)
    pr = cx.sb((128, NPR), F32)
    cx.dma(SP, pr[:], U(k.prow[l].partition_broadcast(128)))
    junk = cx.sb((128, 64), F32)
    s2 = cx.sb((128, 2), F32)
    for i in range(2):
        cx.op(DVE, "tensor_tensor", out=junk[:], in0=pr[:, 16 + 128 * i:16 + 128 * i + 64], in1=pr[:, 16 + 128 * i + 64:16 + 128 * i + 128], op=ALU.mult)
        cx.op(DVE, "reduce_sum", out=s2[:, i:i + 1], in_=junk[:], axis=AX.X)
    cx.act(s2[:], s2[:], AF.Exp)
    cx.op(DVE, "scalar_tensor_tensor", out=dc[:, DC_LAM:DC_LAM + 1], in0=s2[:, 1:2], scalar=-lam_init, in1=s2[:, 0:1], op0=ALU.add, op1=ALU.subtract)
    return dc, pr


def stage_h(k, s, l, hT):
    cx = k.cx
    cx.push()
    xts = [cx.sb((128, D), F32) for _ in range(2)]
    xns = [cx.sb((128, D), BF16) for _ in range(2)]
    junk = cx.sb((128, D), BF16)
    ss = cx.sb((128, NT), F32)
    rs = cx.sb((128, NT), F32)
    for tt in range(NT):
        xt, xn = xts[tt % 2], xns[tt % 2]
        cx.dma(SP, xt[:], k.xres[s].s(tt)[tt * 128:(tt + 1) * 128, :])
        cx.act(junk[:], xt[:], AF.Square, accum_out=ss[:, tt:tt + 1])
        cx.act(rs[:, tt:tt + 1], ss[:, tt:tt + 1], AF.Sqrt, scale=1.0 / D, bias=k.epsc[:, 0:1])
        cx.op(DVE, "reciprocal", out=rs[:, tt:tt + 1], in_=rs[:, tt:tt + 1])
        cx.op(DVE, "tensor_scalar", out=xn[:], in0=xt[:], scalar1=rs[:, tt:tt + 1], scalar2=None, op0=ALU.mult)
        p = nps(k)
        pb = V(p, p.t[:].bitcast(BF16), None)
        for kc in range(8):
            cx.tr(V(p, pb.ap[:, kc * 128:(kc + 1) * 128], None), xn[:, kc * 128:(kc + 1) * 128], k.identb[:])
        r = 4 if tt < 2 else s
        for kc in range(8):
            src = V(p, pb.ap[:, kc * 128:(kc + 1) * 128], None)
            dst = hT.s(tt)[:, kc, tt * 128:(tt + 1) * 128]
            if kc % 2 == 0:
                cx.act(dst, src, AF.Identity, scale=k.AB[:, l, r, kc:kc + 1], bias=k.AB[:, l, r, 8 + kc:9 + kc])
            else:
                cx.op(DVE, "tensor_scalar", out=dst, in0=src, scalar1=k.AB[:, l, r, kc:kc + 1], scalar2=k.AB[:, l, r, 8 + kc:9 + kc], op0=ALU.mult, op1=ALU.add)
    cx.pop()


def proj_fm(k, src, wt, chunks, evac, blks, kcs=8):
    cx = k.cx
    for (b0, n) in blks:
        for ci, grp in enumerate(chunks):
            p = nps(k)
            for (co, m, r0) in grp:
                for kc in range(kcs):
                    cx.mm(p[r0:r0 + m, 0:n], wt[:, kc, co:co + m], src.s(tk(b0, b0 + n))[:, kc, b0:b0 + n], start=(kc == 0), stop=(kc == kcs - 1))
            evac(ci, p, b0, n)


def norm_group(k, pss, n, gmat, neps, gcols, dsts, tmp):
    cx = k.cx
    nchunk = len(pss)
    for c, p in enumerate(pss):
        cx.act(tmp["sq"][:, c, 0:n], p[:, 0:n], AF.Square)
        cx.op(DVE, "tensor_copy", out=tmp["raw"][:, c, 0:n], in_=p[:, 0:n])
    pq = nps(k)
    for c in range(nchunk):
        cx.mm(pq[:, 0:n], gmat[:], tmp["sq"][:, c, 0:n], start=(c == 0), stop=(c == nchunk - 1))
    cx.act(tmp["rs"][:, 0:n], pq[:, 0:n], AF.Sqrt, scale=EPS / float(neps), bias=k.epsc[:, 0:1])
    cx.op(DVE, "reciprocal", out=tmp["rs"][:, 0:n], in_=tmp["rs"][:, 0:n])
    for c in range(nchunk):
        cx.op(DVE, "scalar_tensor_tensor", out=dsts[c], in0=tmp["raw"][:, c, 0:n], scalar=gcols[c], in1=tmp["rs"][:, 0:n], op0=ALU.mult, op1=ALU.mult)


def rope(k, xn, dst, b0, n, tmp):
    cx = k.cx
    p = nps(k)
    cx.mm(p[:, 0:n], k.rm2[:], xn)
    cx.op(DVE, "tensor_tensor", out=tmp["t1"][:, 0:n], in0=p[:, 0:n], in1=k.sin[:, b0:b0 + n], op=ALU.mult)
    cx.op(PEL, "tensor_tensor", out=tmp["t2"][:, 0:n], in0=xn, in1=k.cos[:, b0:b0 + n], op=ALU.mult)
    cx.op(DVE, "tensor_tensor", out=dst, in0=tmp["t1"][:, 0:n], in1=tmp["t2"][:, 0:n], op=ALU.add)


def mk_tmp(cx, nchunk=3):
    return dict(raw=cx.sb((128, nchunk, 512), BF16), sq=cx.sb((128, nchunk, 512), BF16), rs=cx.sb((128, 512), F32),
                t1=cx.sb((128, 512), F32), t2=cx.sb((128, 512), F32), xn=cx.sb((128, 512), BF16))


def merge(k, l, i, y, hT, acc, blks):
    cx = k.cx
    cx.push()
    sg = [cx.sb((128, 512), BF16) for _ in range(2)]
    tmps = [cx.sb((128, 512), BF16) for _ in range(2)]
    j = 0
    for mg in range(2):
        c0 = OFF["merge"] + i * D + mg * 512
        wg = wload(k, k.w_in[l][:, c0:c0 + 512], 8, 512)
        wb = wload(k, k.br_w[l, i][:, mg * 512:(mg + 1) * 512], 4, 512)
        for mc4 in range(4):
            mc = mg * 4 + mc4
            for (b0, n) in blks:
                sb_ = tk(b0, b0 + n)
                pg = nps(k)
                for kc in range(8):
                    cx.mm(pg[:, 0:n], wg[:, kc, mc4 * 128:(mc4 + 1) * 128], hT.s(sb_)[:, kc, b0:b0 + n], start=(kc == 0), stop=(kc == 7))
                g = sg[j % 2]
                cx.act(g[:, 0:n], pg[:, 0:n], AF.Sigmoid)
                py = nps(k)
                for kc in range(4):
                    cx.mm(py[:, 0:n], wb[:, kc, mc4 * 128:(mc4 + 1) * 128], y.s(sb_)[:, kc, b0:b0 + n], start=(kc == 0), stop=(kc == 3))
                dst = acc.s(sb_)[:, mc, b0:b0 + n]
                if i == 0:
                    cx.op(DVE, "tensor_tensor", out=dst, in0=py[:, 0:n], in1=g[:, 0:n], op=ALU.mult)
                else:
                    t = tmps[j % 2]
                    cx.op(DVE, "tensor_tensor", out=t[:, 0:n], in0=py[:, 0:n], in1=g[:, 0:n], op=ALU.mult)
                    cx.op(PEL, "tensor_tensor", out=dst, in0=acc.s(sb_)[:, mc, b0:b0 + n], in1=t[:, 0:n], op=ALU.add)
                j += 1
    cx.pop()


def final(k, s, l, acc, last):
    cx = k.cx
    cx.push()
    w0 = wload(k, k.w_out[l][:, 0:512], 8, 512)
    w1 = wload(k, k.w_out[l][:, 512:1024], 8, 512)
    gb = [cx.sb((128, D), F32) for _ in range(2)]
    cx.dma(SP, gb[0][:], V(k.gscr, k.gscr.t[l, 4].partition_broadcast(128), [l]))
    cx.dma(SP, gb[1][:], V(k.gscr, k.gscr.t[l, s].partition_broadcast(128), [l]))
    xo = [cx.sb((128, D), F32) for _ in range(2)]
    tm = [cx.sb((128, 512), F32) for _ in range(2)]
    for tt in range(2 if last else 0, NT):
        x_ = xo[tt % 2]
        cx.dma(SP, x_[:], k.xres[s].s(tt)[tt * 128:(tt + 1) * 128, :])
        g = gb[0] if tt < 2 else gb[1]
        for nh, w in enumerate((w0, w1)):
            p = nps(k)
            for kc in range(8):
                cx.mm(p[:, :], acc.s(tt)[:, kc, tt * 128:(tt + 1) * 128], w[:, kc, :], start=(kc == 0), stop=(kc == 7))
            t = tm[nh]
            cx.op(DVE, "tensor_tensor", out=t[:], in0=p[:, :], in1=g[:, nh * 512:(nh + 1) * 512], op=ALU.mult)
            cx.op(PEL if nh else DVE, "tensor_tensor", out=x_[:, nh * 512:(nh + 1) * 512], in0=x_[:, nh * 512:(nh + 1) * 512], in1=t[:], op=ALU.add)
        if k.dbg and s == 0 and l == 0:
            cx.dma(SP, U(k.dbg_x[tt * 128:(tt + 1) * 128, :]), x_[:])
        if last:
            cx.dma(SP, U(k.out[s, (tt - 2) * 128:(tt - 1) * 128, :]), x_[:])
        else:
            cx.dma(SP, k.xres[s].s(tt)[tt * 128:(tt + 1) * 128, :], x_[:])
    cx.pop()


def attn_core(k, nheads, nmaps, qk_fn, v_fn, out_fn, last, blk_fn=None):
    cx = k.cx
    cx.push()
    pts = [cx.sb((128, 512), BF16) for _ in range(4)]
    pj = 0
    sbank = [k.ps[0], k.ps[1]]
    abanks = [k.ps[2], k.ps[3], k.ps[4], k.ps[5]]
    sj = 0
    for qb, (q0, nq) in enumerate(BLKS):
        if last and qb == 0:
            continue
        kts = [0, 1] if qb == 0 else list(range(NT))
        nqt = nq // 128
        if blk_fn is not None:
            blk_fn(qb, q0, nq)
        for h in range(nheads):
            def accv(m, qi):
                idx = m * 4 + qi
                b = abanks[idx // 2] if nmaps == 2 else abanks[qi // 2]
                o = (idx % 2) * 129
                return b, V(b, b.t[:, o:o + 129], None)
            started = set()
            pend = []

            def do_pv(kt_, items):
                vv = v_fn(h, kt_)
                for (m, pt) in items:
                    for qi in range(nqt):
                        b, av = accv(m, qi)
                        first = id(b) not in started
                        started.add(id(b))
                        cx.mm(av, pt[:, qi * 128:(qi + 1) * 128], vv, start=first, stop=(kt_ == kts[-1]))

            for kt in kts:
                items = []
                for m in range(nmaps):
                    sp_ = sbank[sj % 2]; sj += 1
                    qk_fn(h, m, kt, q0, nq, sp_)
                    pt = pts[pj % 4]; pj += 1
                    cx.act(pt[:, 0:nq], sp_[:, 0:nq], AF.Exp)
                    items.append((m, pt))
                if pend:
                    do_pv(*pend.pop())
                pend.append((kt, items))
            do_pv(*pend.pop())
            for qi in range(nqt):
                out_fn(h, qb, q0 + qi * 128, [accv(m, qi)[1] for m in range(nmaps)])
    cx.pop()


def mla(k, s, l, hT, acc, dc, last):
    cx = k.cx
    cx.push()
    qn = cx.sb((128, 4, T), BF16, NT); qr = cx.sb((128, 2, T), BF16, NT)
    kn = cx.sb((128, 4, T), BF16, NT); kr2 = cx.sb((128, T), BF16, NT)
    vaug = cx.sb((128, NT, 4, 129), BF16, NT)
    _memset(cx, vaug[:], 1.0)
    W = k.w_in[l]
    cx.push()
    cqn = cx.sb((128, 3, T), BF16, NT)
    tmp = mk_tmp(cx)
    wA = wload(k, W[:, 0:384], 8, 384)
    wq1 = wload(k, k.wuq[l][:, 0:512], 3, 512)
    wq2 = wload(k, k.wuq[l][:, 512:768], 3, 256)
    for (b0, n) in BLKS:
        sb_ = tk(b0, b0 + n)
        pss = []
        for c in range(3):
            p = nps(k)
            for kc in range(8):
                cx.mm(p[:, 0:n], wA[:, kc, c * 128:(c + 1) * 128], hT.s(sb_)[:, kc, b0:b0 + n], start=(kc == 0), stop=(kc == 7))
            pss.append(p)
        ksub = int(os.environ.get("KSUB", "9"))
        if ksub < 1:
            for c in range(3):
                cx.op(DVE, "tensor_copy", out=cqn.s(sb_)[:, c, b0:b0 + n], in_=pss[c][:, 0:n])
            continue
        norm_group(k, pss, n, k.onesb, 384 * EPS, [dc[:, DC_CQG + c:DC_CQG + c + 1] for c in range(3)],
                   [cqn.s(sb_)[:, c, b0:b0 + n] for c in range(3)], tmp)
        if ksub < 2:
            continue
        for h in range(4):
            p = nps(k)
            for kc in range(3):
                kv = os.environ.get("KV", "0")
                lw = wA if kv == "2" else wq1
                rr = hT if kv == "1" else cqn
                cx.mm(p[:, 0:n], lw[:, kc, h * 128:(h + 1) * 128], rr.s(sb_)[:, kc, b0:b0 + n], start=(kc == 0), stop=(kc == 2))
            if os.environ.get("KQ", "1") == "0":
                cx.op(DVE, "tensor_copy", out=qn.s(sb_)[:, h, b0:b0 + n], in_=p[:, 0:n])
            else:
                norm_group(k, [p], n, k.onesb, 128 * EPS, [dc[:, DC_QGN:DC_QGN + 1]], [qn.s(sb_)[:, h, b0:b0 + n]], tmp)
        if ksub < 3:
            continue
        for c in range(2):
            p = nps(k)
            for kc in range(3):
                cx.mm(p[:, 0:n], wq2[:, kc, c * 128:(c + 1) * 128], cqn.s(sb_)[:, kc, b0:b0 + n], start=(kc == 0), stop=(kc == 2))
            norm_group(k, [p], n, k.bd64, 64 * EPS, [dc[:, DC_QGR:DC_QGR + 1]], [tmp["xn"][:, 0:n]], tmp)
            rope(k, tmp["xn"][:, 0:n], qr.s(sb_)[:, c, b0:b0 + n], b0, n, tmp)
    cx.pop()
    stg = int(os.environ.get("KSTAGE", "9"))
    if stg < 4:
        cx.pop(); return
    cx.push()
    ckvn = cx.sb((128, 2, T), BF16, NT)
    tmp = mk_tmp(cx)
    wB = wload(k, W[:, 384:704], 8, 320)
    wk1 = wload(k, k.wukv[l][:, 0:512], 2, 512)
    for (b0, n) in BLKS:
        sb_ = tk(b0, b0 + n)
        pss = []
        for c in range(2):
            p = nps(k)
            for kc in range(8):
                cx.mm(p[:, 0:n], wB[:, kc, c * 128:(c + 1) * 128], hT.s(sb_)[:, kc, b0:b0 + n], start=(kc == 0), stop=(kc == 7))
            pss.append(p)
        norm_group(k, pss, n, k.onesb, 256 * EPS, [dc[:, DC_CKVG + c:DC_CKVG + c + 1] for c in range(2)],
                   [ckvn.s(sb_)[:, c, b0:b0 + n] for c in range(2)], tmp)
        p = nps(k)
        for r0 in (0, 64):
            for kc in range(8):
                cx.mm(p[r0:r0 + 64, 0:n], wB[:, kc, 256:320], hT.s(sb_)[:, kc, b0:b0 + n], start=(kc == 0), stop=(kc == 7))
        norm_group(k, [p], n, k.bd64, 64 * EPS, [dc[:, DC_KGR:DC_KGR + 1]], [tmp["xn"][:, 0:n]], tmp)
        rope(k, tmp["xn"][:, 0:n], kr2.s(sb_)[:, b0:b0 + n], b0, n, tmp)
        for h in range(4):
            p = nps(k)
            for kc in range(2):
                cx.mm(p[:, 0:n], wk1[:, kc, h * 128:(h + 1) * 128], ckvn.s(sb_)[:, kc, b0:b0 + n], start=(kc == 0), stop=(kc == 1))
            norm_group(k, [p], n, k.onesb, 128 * EPS, [dc[:, DC_KGN:DC_KGN + 1]], [kn.s(sb_)[:, h, b0:b0 + n]], tmp)
    wv1 = wload(k, k.wukv[l][:, 512:1024], 2, 512)
    for tt in range(NT):
        p = nps(k)
        for kc in range(2):
            cx.mm(p[:, :], ckvn.s(tt)[:, kc, tt * 128:(tt + 1) * 128], wv1[:, kc, :], start=(kc == 0), stop=(kc == 1))
        cx.op(DVE, "tensor_copy", out=vaug.s(tt)[:, tt, :, 0:128], in_=V(p, p.t[:, :].rearrange("p (h e) -> p h e", e=128), None))
    cx.pop()
    if stg < 5:
        cx.pop(); return
    if k.dbg and s == 0 and l == 0:
        dumpv(k, "d_kr2", kr2[:, :], T)
        dumpv(k, "d_qr0", qr[:, 0, :], T)
        dumpv(k, "d_qn1", qn[:, 1, :], T)
        dumpv(k, "d_kn1", kn[:, 1, :], T)
    y = cx.sb((128, 4, T), BF16, NT)
    sz = cx.sb((128, 4, 512), BF16)
    wz = wload(k, W[:, OFF["mla_z"]:OFF["mla_z"] + 512], 8, 512)

    def blk_fn(qb, b0, n):
        for c in range(4):
            p = k.ps[6 + c % 2]
            for kc in range(8):
                cx.mm(p[:, 0:n], wz[:, kc, c * 128:(c + 1) * 128], hT.s(tk(b0, b0 + n))[:, kc, b0:b0 + n], start=(kc == 0), stop=(kc == 7))
            cx.act(sz[:, c, 0:n], p[:, 0:n], AF.Silu)

    ob = [cx.sb((128, 128), BF16) for _ in range(2)]
    rc = [cx.sb((128, 1), F32) for _ in range(2)]
    oj = [0]

    def qk_fn(h, m, kt, q0, nq, sp_):
        r0 = (h % 2) * 64
        cx.mm(sp_[:, 0:nq], kn.s(kt)[:, h, kt * 128:(kt + 1) * 128], qn.s(tk(q0, q0 + nq))[:, h, q0:q0 + nq], start=True, stop=False)
        for c0 in range(0, nq, 256):
            cx.mm(sp_[:, c0:c0 + 256], kr2.s(kt)[r0:r0 + 64, kt * 128:(kt + 1) * 128], qr.s(tk(q0, q0 + nq))[r0:r0 + 64, h // 2, q0 + c0:q0 + c0 + 256], start=False, stop=True)

    def v_fn(h, kt):
        return vaug.s(kt)[:, kt, h, :]

    def out_fn(h, qb, q0, accs):
        a = accs[0]
        j = oj[0]; oj[0] += 1
        o_, r_ = ob[j % 2], rc[j % 2]
        cx.op(DVE, "reciprocal", out=r_[:], in_=V(a.tile, a.ap[:, 128:129], None))
        cx.op(DVE, "tensor_scalar", out=o_[:], in0=V(a.tile, a.ap[:, 0:128], None), scalar1=r_[:, 0:1], scalar2=None, op0=ALU.mult)
        p = k.ps[6 + j % 2]
        pb = V(p, p.t[:].bitcast(BF16)[:, 0:128], None)
        cx.tr(pb, o_[:], k.identb[:])
        tt = q0 // 128
        qoff = q0 - BLKS[qb][0]
        cx.op(DVE, "tensor_tensor", out=y.s(tt)[:, h, q0:q0 + 128], in0=pb, in1=sz[:, h, qoff:qoff + 128], op=ALU.mult)

    attn_core(k, 4, 1, qk_fn, v_fn, out_fn, last, blk_fn)
    if k.dbg and s == 0 and l == 0:
        dump(k, y, k.dbg_y[0], 4)
    if stg < 6:
        cx.pop(); return
    merge(k, l, 0, y, hT, acc, BLKS[1:] if last else BLKS)
    cx.pop()


def sz_block(k, wz, hT, sz, b0, n):
    cx = k.cx
    for c in range(4):
        p = k.ps[6 + c % 2]
        for kc in range(8):
            cx.mm(p[:, 0:n], wz[:, kc, c * 128:(c + 1) * 128], hT.s(tk(b0, b0 + n))[:, kc, b0:b0 + n], start=(kc == 0), stop=(kc == 7))
        cx.act(sz[:, c, 0:n], p[:, 0:n], AF.Silu)


def proj_tm(k, hT, w, dst_fn, ncols=512):
    cx = k.cx
    for tt in range(NT):
        p = nps(k)
        for kc in range(8):
            cx.mm(p[:, 0:ncols], hT.s(tt)[:, kc, tt * 128:(tt + 1) * 128], w[:, kc, 0:ncols], start=(kc == 0), stop=(kc == 7))
        dst_fn(tt, p)


def diffattn(k, s, l, hT, acc, dc, last):
    cx = k.cx
    cx.push()
    qd = cx.sb((128, 4, T), BF16, NT); kd = cx.sb((128, 4, T), BF16, NT)
    vaug = cx.sb((128, NT, 4, 129), BF16, NT)
    _memset(cx, vaug[:], 1.0)
    W = k.w_in[l]
    cx.push()
    tmp = mk_tmp(cx, 1)
    for (dst, off, gcol) in ((qd, OFF["df_q"], DC_DFQG), (kd, OFF["df_k"], DC_DFKG)):
        w = wload(k, W[:, off:off + 512], 8, 512)
        for (b0, n) in BLKS:
            sb_ = tk(b0, b0 + n)
            for h in range(4):
                p = nps(k)
                for kc in range(8):
                    cx.mm(p[:, 0:n], w[:, kc, h * 128:(h + 1) * 128], hT.s(sb_)[:, kc, b0:b0 + n], start=(kc == 0), stop=(kc == 7))
                norm_group(k, [p], n, k.bd64, 64 * EPS, [dc[:, gcol:gcol + 1]], [tmp["xn"][:, 0:n]], tmp)
                rope(k, tmp["xn"][:, 0:n], dst.s(sb_)[:, h, b0:b0 + n], b0, n, tmp)
    wv = wload(k, W[:, OFF["df_v"]:OFF["df_v"] + 512], 8, 512)
    proj_tm(k, hT, wv, lambda tt, p: cx.op(DVE, "tensor_copy", out=vaug.s(tt)[:, tt, :, 0:128],
                                             in_=V(p, p.t[:, :].rearrange("p (h e) -> p h e", e=128), None)))
    cx.pop()
    y = cx.sb((128, 4, T), BF16, NT)
    sz = cx.sb((128, 4, 512), BF16)
    wz = wload(k, W[:, OFF["df_z"]:OFF["df_z"] + 512], 8, 512)
    o1 = [cx.sb((128, 128), F32) for _ in range(2)]
    o2 = [cx.sb((128, 128), F32) for _ in range(2)]
    ob = [cx.sb((128, 128), BF16) for _ in range(2)]
    junk = cx.sb((128, 128), BF16)
    rc = [cx.sb((128, 4), F32) for _ in range(2)]
    oj = [0]

    def qk_fn(h, m, kt, q0, nq, sp_):
        r0 = m * 64
        for c0 in range(0, nq, 256):
            cx.mm(sp_[:, c0:c0 + 256], kd.s(kt)[r0:r0 + 64, h, kt * 128:(kt + 1) * 128], qd.s(tk(q0, q0 + nq))[r0:r0 + 64, h, q0 + c0:q0 + c0 + 256], start=True, stop=True)

    def v_fn(h, kt):
        return vaug.s(kt)[:, kt, h, :]

    def out_fn(h, qb, q0, accs):
        a0, a1 = accs
        j = oj[0]; oj[0] += 1
        r_ = rc[j % 2]
        cx.op(DVE, "reciprocal", out=r_[:, 0:1], in_=V(a0.tile, a0.ap[:, 128:129], None))
        cx.op(DVE, "reciprocal", out=r_[:, 1:2], in_=V(a1.tile, a1.ap[:, 128:129], None))
        cx.op(DVE, "tensor_tensor", out=r_[:, 1:2], in0=r_[:, 1:2], in1=dc[:, DC_LAM:DC_LAM + 1], op=ALU.mult)
        cx.op(DVE, "tensor_scalar", out=o1[j % 2][:], in0=V(a0.tile, a0.ap[:, 0:128], None), scalar1=r_[:, 0:1], scalar2=None, op0=ALU.mult)
        cx.op(DVE, "scalar_tensor_tensor", out=o2[j % 2][:], in0=V(a1.tile, a1.ap[:, 0:128], None), scalar=r_[:, 1:2], in1=o1[j % 2][:], op0=ALU.mult, op1=ALU.add)
        cx.act(junk[:], o2[j % 2][:], AF.Square, accum_out=r_[:, 2:3])
        cx.act(r_[:, 3:4], r_[:, 2:3], AF.Sqrt, scale=1.0 / 128, bias=k.epsc[:, 0:1])
        cx.op(DVE, "reciprocal", out=r_[:, 3:4], in_=r_[:, 3:4])
        cx.op(DVE, "tensor_scalar", out=ob[j % 2][:], in0=o2[j % 2][:], scalar1=r_[:, 3:4], scalar2=None, op0=ALU.mult)
        p = k.ps[6 + j % 2]
        pb = V(p, p.t[:].bitcast(BF16)[:, 0:128], None)
        cx.tr(pb, ob[j % 2][:], k.identb[:])
        tt = q0 // 128
        qoff = q0 - BLKS[qb][0]
        cx.op(DVE, "scalar_tensor_tensor", out=y.s(tt)[:, h, q0:q0 + 128], in0=pb, scalar=dc[:, DC_DFOG:DC_DFOG + 1], in1=sz[:, h, qoff:qoff + 128], op0=ALU.mult, op1=ALU.mult)

    attn_core(k, 4, 2, qk_fn, v_fn, out_fn, last, lambda qb, b0, n: sz_block(k, wz, hT, sz, b0, n))
    if k.dbg and s == 0 and l == 0:
        dump(k, y, k.dbg_y[3], 4)
    merge(k, l, 3, y, hT, acc, BLKS[1:] if last else BLKS)
    cx.pop()


def out_stage(k, l, i, hT, osrc_fn, y, dc, gcol0, zoff, last, extra_fn=None):
    cx = k.cx
    cx.push()
    sz = cx.sb((128, 4, 512), BF16)
    wz = wload(k, k.w_in[l][:, zoff:zoff + 512], 8, 512)
    on = [cx.sb((128, 512), BF16) for _ in range(2)]
    junk = cx.sb((128, 128), BF16)
    ss = [cx.sb((128, 4), F32) for _ in range(2)]
    for (b0, n) in (BLKS[1:] if last else BLKS):
        sz_block(k, wz, hT, sz, b0, n)
        for tt in tk(b0, b0 + n):
            src = osrc_fn(tt)
            s_, o_ = ss[tt % 2], on[tt % 2]
            for h in range(4):
                cx.act(junk[:], V(src.tile, src.ap[:, h * 128:(h + 1) * 128], src.subs), AF.Square, accum_out=s_[:, h:h + 1])
            cx.act(s_[:], s_[:], AF.Sqrt, scale=1.0 / 128, bias=k.epsc[:, 0:1])
            cx.op(DVE, "reciprocal", out=s_[:], in_=s_[:])
            for h in range(4):
                cx.op(DVE, "tensor_scalar", out=o_[:, h * 128:(h + 1) * 128], in0=V(src.tile, src.ap[:, h * 128:(h + 1) * 128], src.subs), scalar1=s_[:, h:h + 1], scalar2=None, op0=ALU.mult)
            p = nps(k)
            pb = p.t[:].bitcast(BF16)
            for h in range(4):
                cx.tr(V(p, pb[:, h * 128:(h + 1) * 128], None), o_[:, h * 128:(h + 1) * 128], k.identb[:])
            t0 = tt * 128
            for h in range(4):
                pv = V(p, pb[:, h * 128:(h + 1) * 128], None)
                if extra_fn is None:
                    cx.op(DVE, "scalar_tensor_tensor", out=y.s(tt)[:, h, t0:t0 + 128], in0=pv, scalar=dc[:, gcol0 + h:gcol0 + h + 1], in1=sz[:, h, t0 - b0:t0 - b0 + 128], op0=ALU.mult, op1=ALU.mult)
                else:
                    extra_fn(tt, h, pv, sz[:, h, t0 - b0:t0 - b0 + 128])
    cx.pop()


def gla(k, s, l, hT, acc, dc, last):
    cx = k.cx
    cx.push()
    W = k.w_in[l]
    ogl = cx.sb((128, NT, 512), BF16, NT)
    cx.push()
    qT = cx.sb((128, 2, T), BF16, NT); kT = cx.sb((128, 2, T), BF16, NT)
    afT = cx.sb((16, 2, T), BF16, NT)
    vtm = cx.sb((128, NT, 512), BF16, NT)
    aw = cx.sb((16, 2, 256), BF16)
    cx.dma(POOL, aw[:], U(k.gla_a_w[l].rearrange("d r n -> r d n")))
    wqk = wload(k, W[:, OFF["gla_q"]:OFF["gla_q"] + 512], 8, 512)
    waf = wload(k, W[:, OFF["gla_af"]:OFF["gla_af"] + 32], 8, 32)
    for (b0, n) in BLKS:
        sb_ = tk(b0, b0 + n)
        for c in range(4):
            p = nps(k)
            for kc in range(8):
                cx.mm(p[:, 0:n], wqk[:, kc, c * 128:(c + 1) * 128], hT.s(sb_)[:, kc, b0:b0 + n], start=(kc == 0), stop=(kc == 7))
            if c < 2:
                cx.act(qT.s(sb_)[:, c, b0:b0 + n], p[:, 0:n], AF.Copy, scale=0.125)
            else:
                cx.op(DVE, "tensor_copy", out=kT.s(sb_)[:, c - 2, b0:b0 + n], in_=p[:, 0:n])
        for d in range(2):
            p = nps(k)
            for kc in range(8):
                cx.mm(p[0:16, 0:n], waf[:, kc, d * 16:(d + 1) * 16], hT.s(sb_)[:, kc, b0:b0 + n], start=(kc == 0), stop=(kc == 7))
            cx.op(DVE, "tensor_copy", out=afT.s(sb_)[0:16, d, b0:b0 + n], in_=p[0:16, 0:n])
    wv = wload(k, W[:, OFF["gla_v"]:OFF["gla_v"] + 512], 8, 512)
    proj_tm(k, hT, wv, lambda tt, p: cx.act(vtm.s(tt)[:, tt, :], p[:, :], AF.Copy))
    S32 = cx.sb((128, 2, 128), F32); Sb = cx.sb((128, 2, 128), BF16)
    R = lambda shape, dt, nb=2: [cx.sb(shape, dt) for _ in range(nb)]
    e_ = R((128, 2, 128), F32); lp_ = R((128, 2, 128), F32); Bp_ = R((128, 2, 128), F32); tR_ = R((128, 2, 128), F32)
    E_ = R((128, 2, 128), BF16, 3); qe_ = R((128, 2, 128), BF16); ke_ = R((128, 2, 128), BF16); kl_ = R((128, 2, 128), BF16)
    bt_ = R((128, 2, 4), F32); kltm_ = R((128, 256), BF16); AT_ = R((128, 128), BF16, 3)
    it = 0
    for d in range(2):
        _memset(cx, S32[:], 0.0); _memset(cx, Sb[:], 0.0)
        order = list(range(NT)) if d == 0 else [1, 0] + list(range(NT - 1, 1, -1))
        mask = k.trifb if d == 0 else k.tribb
        for tt in order:
            j = it % 2; it += 1
            t0 = tt * 128
            e, lp, Bp, tR, qe, ke, kl, bt, kltm = e_[j], lp_[j], Bp_[j], tR_[j], qe_[j], ke_[j], kl_[j], bt_[j], kltm_[j]
            pl = nps(k)
            for c in range(2):
                cx.mm(pl[:, c * 128:(c + 1) * 128], aw[0:16, d, c * 128:(c + 1) * 128], afT.s(tt)[0:16, d, t0:t0 + 128])
            for c in range(2):
                cx.act(e[:, c, :], pl[:, c * 128:(c + 1) * 128], AF.Exp, scale=-1.0, bias=dc[:, DC_NAB + 2 * d + c:DC_NAB + 2 * d + c + 1])
            cx.act(lp[:], e[:], AF.Ln, bias=k.onec[:, 0:1])
            for c in range(2):
                cx.op(DVE, "tensor_tensor_scan", out=Bp[:, c, :], data0=k.onesf[:, 0:128], data1=lp[:, c, :], initial=0.0, op0=ALU.mult, op1=ALU.add)
            E1, E2, E3 = E_[0], E_[1], E_[2]
            if d == 0:
                cx.op(DVE, "tensor_scalar", out=bt[:, :, 0:1], in0=Bp[:, :, 127:128], scalar1=-1.0 / 16, scalar2=None, op0=ALU.mult)
                cx.act(E1[:], Bp[:], AF.Exp, scale=-1.0 / 16)
                cx.act(E2[:], Bp[:], AF.Exp, scale=1.0 / 16)
                for c in range(2):
                    cx.act(E3[:, c, :], Bp[:, c, :], AF.Exp, scale=1.0 / 16, bias=bt[:, c, 0:1])
            else:
                cx.op(DVE, "tensor_tensor", out=tR[:], in0=lp[:], in1=Bp[:], op=ALU.subtract)
                cx.op(DVE, "tensor_scalar", out=bt[:, :, 0:1], in0=Bp[:, :, 127:128], scalar1=-1.0 / 16, scalar2=None, op0=ALU.mult)
                cx.op(DVE, "tensor_scalar", out=bt[:, :, 1:2], in0=Bp[:, :, 127:128], scalar1=1.0 / 16, scalar2=None, op0=ALU.mult)
                for c in range(2):
                    cx.act(E1[:, c, :], tR[:, c, :], AF.Exp, scale=-1.0 / 16, bias=bt[:, c, 0:1])
                    cx.act(E2[:, c, :], tR[:, c, :], AF.Exp, scale=1.0 / 16, bias=bt[:, c, 1:2])
                cx.act(E3[:], tR[:], AF.Exp, scale=1.0 / 16)
            cx.act(bt[:, :, 2:3], bt[:, :, 0:1], AF.Exp)
            cx.op(DVE, "tensor_tensor", out=qe[:], in0=qT.s(tt)[:, :, t0:t0 + 128], in1=E1[:], op=ALU.mult)
            cx.op(DVE, "tensor_tensor", out=ke[:], in0=kT.s(tt)[:, :, t0:t0 + 128], in1=E2[:], op=ALU.mult)
            cx.op(DVE, "tensor_tensor", out=kl[:], in0=kT.s(tt)[:, :, t0:t0 + 128], in1=E3[:], op=ALU.mult)
            ptr = nps(k)
            pb = ptr.t[:].bitcast(BF16)
            for c in range(2):
                cx.tr(V(ptr, pb[:, c * 128:(c + 1) * 128], None), kl[:, c, :], k.identb[:])
            cx.op(DVE, "tensor_copy", out=kltm[:], in_=V(ptr, pb[:, 0:256], None))
            po = nps(k)
            for h in range(4):
                c, r0 = h // 2, (h % 2) * 64
                pa = nps(k)
                cx.mm(pa[:, 0:128], ke[r0:r0 + 64, c, :], qe[r0:r0 + 64, c, :])
                AT = AT_[h % 3]
                cx.op(DVE, "tensor_tensor", out=AT[:], in0=pa[:, 0:128], in1=mask[:], op=ALU.mult)
                cx.mm(po[:, h * 128:(h + 1) * 128], AT[:], vtm.s(tt)[:, tt, h * 128:(h + 1) * 128], start=True, stop=False)
                cx.mm(po[:, h * 128:(h + 1) * 128], qe[r0:r0 + 64, c, :], Sb[r0:r0 + 64, c, :], start=False, stop=True)
            pu = nps(k)
            for h in range(4):
                c, r0 = h // 2, (h % 2) * 64
                cx.mm(pu[r0:r0 + 64, c * 128:(c + 1) * 128], kltm[:, h * 64:(h + 1) * 64], vtm.s(tt)[:, tt, h * 128:(h + 1) * 128], start=True, stop=True)
            if d == 0:
                cx.act(ogl.s(tt)[:, tt, :], po[:, :], AF.Copy)
            else:
                cx.op(DVE, "tensor_tensor", out=ogl.s(tt)[:, tt, :], in0=po[:, :], in1=ogl.s(tt)[:, tt, :], op=ALU.add)
            for c in range(2):
                cx.op(DVE, "scalar_tensor_tensor", out=S32[:, c, :], in0=S32[:, c, :], scalar=bt[:, c, 2:3], in1=pu[:, c * 128:(c + 1) * 128], op0=ALU.mult, op1=ALU.add)
            cx.act(Sb[:], S32[:], AF.Copy)
    cx.pop()
    y = cx.sb((128, 4, T), BF16, NT)
    out_stage(k, l, 1, hT, lambda tt: ogl.s(tt)[:, tt, :], y, dc, DC_GLAOG, OFF["gla_z"], last)
    if k.dbg and s == 0 and l == 0:
        dump(k, y, k.dbg_y[1], 4)
    merge(k, l, 1, y, hT, acc, BLKS[1:] if last else BLKS)
    cx.pop()


def mlstm(k, s, l, hT, acc, dc, pr, last):
    cx = k.cx
    cx.push()
    W = k.w_in[l]
    xcT = cx.sb((128, 4, T), BF16, NT)
    hml = cx.sb((128, NT, 512), BF16, NT)
    cx.push()
    xraw = cx.sb((128, 4, T), BF16, NT)
    cacc = cx.sb((128, T), F32)
    wx = wload(k, W[:, OFF["ml_x"]:OFF["ml_x"] + 512], 8, 512)
    for (b0, n) in BLKS:
        sb_ = tk(b0, b0 + n)
        for c in range(4):
            p = nps(k)
            for kc in range(8):
                cx.mm(p[:, 0:n], wx[:, kc, c * 128:(c + 1) * 128], hT.s(sb_)[:, kc, b0:b0 + n], start=(kc == 0), stop=(kc == 7))
            if c % 2:
                cx.act(xraw.s(sb_)[:, c, b0:b0 + n], p[:, 0:n], AF.Copy)
            else:
                cx.op(DVE, "tensor_copy", out=xraw.s(sb_)[:, c, b0:b0 + n], in_=p[:, 0:n])
    pc = k.pc
    for c in range(4):
        w0, w1, w2 = [pc[:, l, PC_CONVW + 4 * j + c:PC_CONVW + 4 * j + c + 1] for j in range(3)]
        cx.op(DVE, "tensor_scalar", out=cacc[:], in0=xraw[:, c, :], scalar1=w1, scalar2=pc[:, l, PC_CONVB + c:PC_CONVB + c + 1], op0=ALU.mult, op1=ALU.add)
        for (a_, b_) in ((0, NCTX), (NCTX, T)):
            cx.op(DVE, "scalar_tensor_tensor", out=cacc[:, a_ + 1:b_], in0=xraw[:, c, a_:b_ - 1], scalar=w0, in1=cacc[:, a_ + 1:b_], op0=ALU.mult, op1=ALU.add)
            cx.op(DVE, "scalar_tensor_tensor", out=cacc[:, a_:b_ - 1], in0=xraw[:, c, a_ + 1:b_], scalar=w2, in1=cacc[:, a_:b_ - 1], op0=ALU.mult, op1=ALU.add)
        cx.act(xcT[:, c, :], cacc[:], AF.Silu)
    cx.pop()
    cx.push()
    qT = cx.sb((128, 2, T), BF16, NT); kT = cx.sb((128, 2, T), BF16, NT)
    vaug = cx.sb((128, NT, 4, 129), BF16, NT)
    _memset(cx, vaug[:], 1.0)
    gt = cx.sb((128, NT, 16), F32); lpt = cx.sb((128, NT, 16), F32)
    wqb = cx.sb((128, 4, 64), BF16); wkb = cx.sb((128, 4, 64), BF16)
    cx.dma(POOL, wqb[:], U(k.ml_wq[l].rearrange("h c d -> c h d")))
    cx.dma(POOL, wkb[:], U(k.ml_wk[l].rearrange("h c d -> c h d")))
    for (b0, n) in BLKS:
        sb_ = tk(b0, b0 + n)
        for (dst, wb, scl) in ((qT, wqb, 1.0), (kT, wkb, 0.125)):
            for c in range(2):
                p = nps(k)
                for hh in range(2):
                    h = 2 * c + hh
                    cx.mm(p[hh * 64:(hh + 1) * 64, 0:n], wb[:, h, :], xcT.s(sb_)[:, h, b0:b0 + n], start=True, stop=True)
                cx.act(dst.s(sb_)[:, c, b0:b0 + n], p[:, 0:n], AF.Copy, scale=scl)
    wv = wload(k, W[:, OFF["ml_v"]:OFF["ml_v"] + 512], 8, 512)
    proj_tm(k, hT, wv, lambda tt, p: cx.op(DVE, "tensor_copy", out=vaug.s(tt)[:, tt, :, 0:128],
                                             in_=V(p, p.t[:, :].rearrange("p (h e) -> p h e", e=128), None)))
    wif = wload(k, W[:, OFF["ml_if"]:OFF["ml_if"] + 16], 8, 16)
    proj_tm(k, hT, wif, lambda tt, p: cx.op(DVE, "tensor_tensor", out=gt[:, tt, :], in0=p[:, 0:16], in1=pr[:, 0:16], op=ALU.add), ncols=16)
    cx.act(lpt[:], gt[:], AF.Exp, scale=-1.0)
    cx.act(lpt[:], lpt[:], AF.Ln, bias=k.onec[:, 0:1])
    Bp = cx.sb((128, 2, NT, 4), F32); Bt = cx.sb((128, 2, NT, 4), F32)
    fa = cx.sb((128, 2, NT, 4), F32); fg = cx.sb((128, 2, NT, 4), F32); fgk = cx.sb((128, 2, NT, 4), F32); fdec = cx.sb((128, 2, NT, 4), F32)
    for d in range(2):
        g0 = (1 + 2 * d) * 4
        rhs = lpt[:, :, g0:g0 + 4]
        p = nps(k)
        cx.mm(p[:, 0:72], (k.trif if d == 0 else k.trib)[:], rhs)
        cx.op(DVE, "tensor_copy", out=Bp[:, d], in_=V(p, p.t[:, 0:72].rearrange("p (a b) -> p a b", b=4), None))
        p = nps(k)
        cx.mm(p[:, 0:72], k.onesf[:], rhs)
        cx.op(DVE, "tensor_copy", out=Bt[:, d], in_=V(p, p.t[:, 0:72].rearrange("p (a b) -> p a b", b=4), None))
        li = gt[:, :, 8 * d:8 * d + 4]
        cx.act(fa[:, d], Bp[:, d], AF.Exp, scale=-1.0)
        cx.act(fdec[:, d], Bt[:, d], AF.Exp, scale=-1.0)
        cx.op(DVE, "tensor_tensor", out=Bp[:, d], in0=Bp[:, d], in1=li, op=ALU.add)
        cx.act(fg[:, d], Bp[:, d], AF.Exp)
        cx.op(DVE, "tensor_tensor", out=Bp[:, d], in0=Bp[:, d], in1=Bt[:, d], op=ALU.subtract)
        cx.act(fgk[:, d], Bp[:, d], AF.Exp)
    C32 = cx.sb((128, 2, 129), F32); Cb = cx.sb((128, 2, 129), BF16)
    PT_ = [cx.sb((128, 128), BF16) for _ in range(3)]
    kg_ = [cx.sb((128, 256), BF16) for _ in range(2)]
    dn_ = [cx.sb((128, 4), F32) for _ in range(2)]
    it = 0
    for d in range(2):
        _memset(cx, C32[:], 0.0); _memset(cx, Cb[:], 0.0)
        order = list(range(NT)) if d == 0 else [1, 0] + list(range(NT - 1, 1, -1))
        mask = k.trifb if d == 0 else k.tribb
        for tt in order:
            j = it % 2; it += 1
            t0 = tt * 128
            kg, dn = kg_[j], dn_[j]
            pk = nps(k)
            for h in range(4):
                cx.mm(pk[:, h * 64:(h + 1) * 64], xcT.s(tt)[:, h, t0:t0 + 128], wkb[:, h, :], start=True, stop=True)
            for h in range(4):
                cx.op(DVE, "tensor_scalar", out=kg[:, h * 64:(h + 1) * 64], in0=pk[:, h * 64:(h + 1) * 64], scalar1=fgk[:, d, tt, h:h + 1], scalar2=0.125, op0=ALU.mult, op1=ALU.mult)
            pos = [nps(k), nps(k)]
            for h in range(4):
                c, r0 = h // 2, (h % 2) * 64
                pa = nps(k)
                cx.mm(pa[:, 0:128], kT.s(tt)[r0:r0 + 64, c, t0:t0 + 128], qT.s(tt)[r0:r0 + 64, c, t0:t0 + 128])
                PT = PT_[h % 3]
                cx.op(DVE, "scalar_tensor_tensor", out=PT[:], in0=pa[:, 0:128], scalar=fg[:, d, tt, h:h + 1], in1=mask[:], op0=ALU.mult, op1=ALU.mult)
                o = (h % 2) * 129
                cx.mm(pos[c][:, o:o + 129], PT[:], vaug.s(tt)[:, tt, h, :], start=True, stop=False)
                cx.mm(pos[c][:, o:o + 129], qT.s(tt)[r0:r0 + 64, c, t0:t0 + 128], Cb[r0:r0 + 64, c, :], start=False, stop=True)
            pu = nps(k)
            for h in range(4):
                c, r0 = h // 2, (h % 2) * 64
                cx.mm(pu[r0:r0 + 64, c * 129:(c + 1) * 129], kg[:, h * 64:(h + 1) * 64], vaug.s(tt)[:, tt, h, :], start=True, stop=True)
            for c in range(2):
                po = pos[c]
                den = V(po, po.t[:, 0:258].rearrange("p (a b) -> p a b", b=129)[:, :, 128], None)
                cx.op(DVE, "tensor_tensor", out=dn[:, 0:2], in0=den, in1=fa[:, d, tt, 2 * c:2 * c + 2], op=ALU.mult)
                cx.act(dn[:, 0:2], dn[:, 0:2], AF.Abs)
                cx.op(DVE, "tensor_scalar", out=dn[:, 0:2], in0=dn[:, 0:2], scalar1=1.0, scalar2=None, op0=ALU.max)
                cx.op(DVE, "reciprocal", out=dn[:, 0:2], in_=dn[:, 0:2])
                cx.op(DVE, "tensor_tensor", out=dn[:, 2:4], in0=dn[:, 0:2], in1=fa[:, d, tt, 2 * c:2 * c + 2], op=ALU.mult)
                for hh in range(2):
                    h = 2 * c + hh
                    dst = hml.s(tt)[:, tt, h * 128:(h + 1) * 128]
                    if d == 0:
                        cx.op(DVE, "tensor_scalar", out=dst, in0=po[:, hh * 129:hh * 129 + 128], scalar1=dn[:, 2 + hh:3 + hh], scalar2=None, op0=ALU.mult)
                    else:
                        cx.op(DVE, "scalar_tensor_tensor", out=dst, in0=po[:, hh * 129:hh * 129 + 128], scalar=dn[:, 2 + hh:3 + hh], in1=dst, op0=ALU.mult, op1=ALU.add)
            for h in range(4):
                c, r0 = h // 2, (h % 2) * 64
                cx.op(DVE, "scalar_tensor_tensor", out=C32[r0:r0 + 64, c, :], in0=C32[r0:r0 + 64, c, :], scalar=fdec[r0:r0 + 64, d, tt, h:h + 1], in1=pu[r0:r0 + 64, c * 129:(c + 1) * 129], op0=ALU.mult, op1=ALU.add)
            cx.act(Cb[:], C32[:], AF.Copy)
    cx.pop()
    y = cx.sb((128, 4, T), BF16, NT)
    cx.push()
    wo = wload(k, W[:, OFF["ml_o"]:OFF["ml_o"] + 512], 8, 512)
    sgo = [cx.sb((128, 512), BF16) for _ in range(2)]

    def og(tt, p):
        cx.act(sgo[tt % 2][:], p[:, :], AF.Sigmoid)
        cx.op(DVE, "tensor_tensor", out=hml.s(tt)[:, tt, :], in0=hml.s(tt)[:, tt, :], in1=sgo[tt % 2][:], op=ALU.mult)
    proj_tm(k, hT, wo, og)
    t1_ = [cx.sb((128, 128), F32) for _ in range(2)]
    t2_ = [cx.sb((128, 128), F32) for _ in range(2)]
    ej = [0]

    def extra(tt, h, pv, szv):
        j = ej[0] % 2; ej[0] += 1
        t0 = tt * 128
        cx.act(t1_[j][:], pv, AF.Identity, scale=dc[:, DC_MLOG + h:DC_MLOG + h + 1])
        cx.op(DVE, "scalar_tensor_tensor", out=t2_[j][:], in0=xcT.s(tt)[:, h, t0:t0 + 128], scalar=k.pc[:, l, PC_MLSKIP + h:PC_MLSKIP + h + 1], in1=t1_[j][:], op0=ALU.mult, op1=ALU.add)
        cx.op(DVE, "tensor_tensor", out=y.s(tt)[:, h, t0:t0 + 128], in0=t2_[j][:], in1=szv, op=ALU.mult)
    out_stage(k, l, 2, hT, lambda tt: hml.s(tt)[:, tt, :], y, dc, DC_MLOG, OFF["ml_z"], last, extra)
    cx.pop()
    if k.dbg and s == 0 and l == 0:
        dump(k, y, k.dbg_y[2], 4)
    merge(k, l, 2, y, hT, acc, BLKS[1:] if last else BLKS)
    cx.pop()


def dumpv(k, name, v, ncols):
    cx = k.cx
    dst = k.nc.dram_tensor(name, [128, ncols], F32, kind="ExternalOutput").ap()
    cx.push()
    f = cx.sb((128, 256), F32)
    for j in range(0, ncols, 256):
        w = min(256, ncols - j)
        cx.op(DVE, "tensor_copy", out=f[:, 0:w], in_=V(v.tile, v.ap[:, j:j + w], v.subs))
        cx.dma(SP, U(dst[:, j:j + w]), f[:, 0:w])
    cx.pop()


def dump(k, t, dst, nch):
    cx = k.cx
    cx.push()
    f = cx.sb((128, 256), F32)
    for c in range(nch):
        for j in range(T // 256):
            cx.op(DVE, "tensor_copy", out=f[:], in_=t[:, c, j * 256:(j + 1) * 256])
            cx.dma(SP, U(dst[:, c, j * 256:(j + 1) * 256]), f[:])
    cx.pop()


def layer(k, s, l, last):
    cx = k.cx
    cx.push()
    hT = cx.sb((128, 8, T), BF16, NT)
    acc = cx.sb((128, 8, T), BF16, NT)
    stg = int(os.environ.get("KSTAGE", "9"))
    if stg < 1:
        cx.pop(); return
    dc, pr = layer_params(k, l)
    if stg < 2:
        cx.pop(); return
    stage_h(k, s, l, hT)
    if stg < 3:
        if k.dbg:
            dump(k, hT, k.dbg_h, 8)
        cx.pop(); return
    if k.dbg and s == 0 and l == 0:
        dump(k, hT, k.dbg_h, 8)
    mla(k, s, l, hT, acc, dc, last)
    if stg < 7:
        cx.pop(); return
    mix = os.environ.get("KMIX", "123")
    if "1" in mix:
        gla(k, s, l, hT, acc, dc, last)
    if "2" in mix:
        mlstm(k, s, l, hT, acc, dc, pr, last)
    if "3" in mix:
        diffattn(k, s, l, hT, acc, dc, last)
    if k.dbg and s == 0 and l == 0:
        dump(k, acc, k.dbg_acc, 8)
    final(k, s, l, acc, last)
    cx.pop()


def _consts():
    ident = np.eye(128, dtype=np.float32)
    bd = np.zeros((128, 128), np.float32); bd[:64, :64] = 1; bd[64:, 64:] = 1
    rm = np.zeros((64, 64), np.float32)
    for i in range(16):
        rm[16 + i, i] = -1; rm[i, 16 + i] = 1; rm[48 + i, 32 + i] = -1; rm[32 + i, 48 + i] = 1
    rm2 = np.zeros((128, 128), np.float32); rm2[:64, :64] = rm; rm2[64:, 64:] = rm
    si, ti = np.meshgrid(np.arange(128), np.arange(128), indexing="ij")
    trif = (si <= ti).astype(np.float32); trib = (si >= ti).astype(np.float32)
    quarter = 16
    inv_freq = (10000.0 ** (-np.arange(quarter, dtype=np.float32) / quarter)).astype(np.float32)
    row = np.repeat(np.arange(32, dtype=np.float32), 64); col = np.tile(np.arange(64, dtype=np.float32), 32)
    ar = row[:, None] * inv_freq; ac = col[:, None] * inv_freq
    ang = np.concatenate([ar, ar, ac, ac], axis=-1).astype(np.float32)
    cos = np.concatenate([np.ones((NCTX, 64), np.float32), np.cos(ang)], 0)
    sin = np.concatenate([np.zeros((NCTX, 64), np.float32), np.sin(ang)], 0)
    cosT = np.ascontiguousarray(np.concatenate([cos.T, cos.T], 0)); sinT = np.ascontiguousarray(np.concatenate([sin.T, sin.T], 0))
    return dict(cident=ident, cbd64=bd, crm2=rm2, ctrif=trif, ctrib=trib, ccos=cosT.astype(np.float32), csin=sinT.astype(np.float32))


def _cols(v):
    v = np.asarray(v, np.float32).reshape(-1)
    return v.reshape(-1, 128).T


def _pack(inp):
    pcols = np.zeros((128, NL, NPC), np.float32)
    prow = np.zeros((NL, NPR), np.float32)
    for l in range(NL):
        P = pcols[:, l]
        P[:, PC_NORMG:PC_NORMG + 8] = _cols(inp["norm_g"][l])
        P[:, PC_CQG:PC_CQG + 3] = _cols(inp["mla_cq_g"][l]); P[:, PC_CKVG:PC_CKVG + 2] = _cols(inp["mla_ckv_g"][l])
        qg, kg = inp["mla_q_g"][l], inp["mla_k_g"][l]
        P[:, PC_QGN] = qg[:128]; P[:, PC_QGR] = np.concatenate([qg[128:], qg[128:]])
        P[:, PC_KGN] = kg[:128]; P[:, PC_KGR] = np.concatenate([kg[128:], kg[128:]])
        P[:, PC_DFQG] = np.concatenate([inp["df_qk_g"][l, 0]] * 2); P[:, PC_DFKG] = np.concatenate([inp["df_qk_g"][l, 1]] * 2)
        P[:, PC_DFOG] = inp["df_out_g"][l]
        P[:, PC_GLAOG:PC_GLAOG + 4] = _cols(inp["gla_out_g"][l]); P[:, PC_MLOG:PC_MLOG + 4] = _cols(inp["ml_out_g"][l])
        P[:, PC_MLSKIP:PC_MLSKIP + 4] = _cols(inp["ml_skip"][l])
        for j in range(3):
            P[:, PC_CONVW + 4 * j:PC_CONVW + 4 * j + 4] = _cols(inp["ml_conv_w"][l, j])
        P[:, PC_CONVB:PC_CONVB + 4] = _cols(inp["ml_conv_b"][l])
        for d_ in range(2):
            P[:, PC_GLAAB + 2 * d_:PC_GLAAB + 2 * d_ + 2] = _cols(inp["gla_a_b"][l, d_])
        prow[l, 0:16] = inp["ml_gate_b"][l].reshape(-1)
        prow[l, 16:272] = inp["df_lambda"][l].reshape(-1)
    return pcols, prow


def _perm_wuq(w):
    w = w.reshape(NL, 384, 4, 192)
    return np.ascontiguousarray(np.concatenate([w[..., :128].reshape(NL, 384, 512), w[..., 128:].reshape(NL, 384, 256)], -1))


def _perm_wukv(w):
    w = w.reshape(NL, 256, 4, 256)
    return np.ascontiguousarray(np.concatenate([w[..., :128].reshape(NL, 256, 512), w[..., 128:].reshape(NL, 256, 512)], -1))


_CACHE = {}


def host_maps(inp, ncores=8):
    inp = {k_: np.asarray(v, np.float32) for k_, v in inp.items()}
    pcols, prow = _pack(inp)
    shared = dict(ada_w=inp["ada_w"], ada_b=inp["ada_b"], w_in=inp["w_in"], wuq=_perm_wuq(inp["mla_wuq"]),
                  wukv=_perm_wukv(inp["mla_wukv"]), gla_a_w=inp["gla_a_w"], ml_wq=inp["ml_wq"], ml_wk=inp["ml_wk"],
                  br_w=inp["br_w"], w_out=inp["w_out"], pcols=pcols, prow=prow, **_consts())
    maps = []
    for c in range(ncores):
        m = dict(shared)
        m["x"] = np.ascontiguousarray(inp["x"][4 * c:4 * c + 4]); m["ctx"] = np.ascontiguousarray(inp["ctx"][4 * c:4 * c + 4])
        m["c5"] = np.ascontiguousarray(np.concatenate([inp["c"][4 * c:4 * c + 4], inp["c_ctx"][None]], 0))
        maps.append(m)
    return maps


def kernel(**inputs):
    if "nc" not in _CACHE:
        _CACHE["nc"] = build(4, NL)[0]
    nc = _CACHE["nc"]
    maps = host_maps(inputs)
    res = run_bass_kernel_spmd(nc, maps, core_ids=list(range(8)))
    return np.concatenate([np.asarray(r["out"], np.float32) for r in res.results], 0)
```

```python
import math
import contextlib
import numpy as np
import concourse.bass as bass
import concourse.mybir as mybir
from concourse.bass_utils import run_bass_kernel_spmd

F32 = mybir.dt.float32
BF16 = mybir.dt.bfloat16
AF = mybir.ActivationFunctionType
ALU = mybir.AluOpType
AX = mybir.AxisListType

T = 2304
NCTX = 256
NT = 18
D = 1024
DIN = 10992
NL = 4
EPS = 1e-6
BLKS = [(0, 256), (256, 512), (768, 512), (1280, 512), (1792, 512)]
OFF = dict(mla_cq=0, mla_ckv=384, mla_kr=640, mla_z=704, gla_q=1216, gla_k=1472, gla_v=1728, gla_af=2240,
           gla_ab=2256, gla_z=2272, ml_x=2784, ml_v=3296, ml_o=3808, ml_if=4320, ml_z=4336, df_q=4848,
           df_k=5360, df_v=5872, df_z=6384, merge=6896)
PC_NORMG, PC_CQG, PC_CKVG, PC_QGN, PC_QGR, PC_KGN, PC_KGR, PC_DFQG, PC_DFKG, PC_DFOG = 0, 8, 11, 13, 14, 15, 16, 17, 18, 19
PC_GLAOG, PC_MLOG, PC_MLSKIP, PC_CONVW, PC_CONVB, PC_GLAAB, NPC = 20, 24, 28, 32, 44, 48, 52
NPR = 16 + 256
DC_CQG, DC_CKVG, DC_QGN, DC_QGR, DC_KGN, DC_KGR, DC_DFQG, DC_DFKG, DC_DFOG, DC_GLAOG, DC_MLOG, DC_NAB, DC_LAM, NDC = 0, 3, 5, 6, 7, 8, 9, 10, 11, 12, 16, 20, 24, 28

import os
PE, ACT, DVE, POOL, SP = range(5)
SERIAL = os.environ.get("SERIAL", "0") == "1"
PEL = DVE if os.environ.get("NOPOOL", "0") == "1" else POOL
NDS = 24


class V:
    __slots__ = ("tile", "ap", "subs")

    def __init__(self, tile, ap, subs):
        self.tile, self.ap, self.subs = tile, ap, subs


class _Sel:
    def __init__(self, tile, subs):
        self.tile, self.subs = tile, subs

    def __getitem__(self, idx):
        return V(self.tile, self.tile.t[idx], self.subs)


class Tile:
    def __init__(self, t, nsub=1):
        self.t = t
        self.nsub = nsub
        self.w = [None] * nsub
        self.r = [dict() for _ in range(nsub)]
        self.excl = False

    def __getitem__(self, idx):
        return V(self, self.t[idx], None)

    def s(self, subs):
        if isinstance(subs, int):
            subs = [subs]
        return _Sel(self, list(subs))


def tk(a, b):
    return list(range(a // 128, (b + 127) // 128))


class Ctx:
    def __init__(self, nc):
        self.nc = nc
        self.engs = [nc.tensor, nc.scalar, nc.vector, nc.gpsimd, nc.sync]
        self.es = contextlib.ExitStack()
        self.esem = [self.es.enter_context(nc.semaphore(f"e{i}")) for i in range(5)]
        self.dsem = [self.es.enter_context(nc.semaphore(f"d{i}")) for i in range(NDS)]
        self.cnt = [0] * 5
        self.dval = [0] * NDS
        self.dk = 0
        self.dkp = 0
        self.seen = [dict() for _ in range(5)]
        self.scopes = [self.es]
        self.scope_tiles = [[]]
        self.freed = {}
        self.uid = 0
        self.ninstr = 0
        self.last = None

    def push(self):
        st = contextlib.ExitStack()
        self.scopes.append(st)
        self.scope_tiles.append([])
        return st

    def pop(self):
        for tl in self.scope_tiles.pop():
            for s in range(tl.nsub):
                w = tl.w[s]
                if w is not None and self.freed.get(w[0], 0) < w[1]:
                    self.freed[w[0]] = w[1]
                for key, val in tl.r[s].items():
                    if self.freed.get(key, 0) < val:
                        self.freed[key] = val
        self.scopes.pop().close()

    def sb(self, shape, dt, nsub=1, name=None):
        self.uid += 1
        t = self.scopes[-1].enter_context(self.nc.sbuf_tensor(f"{name or 't'}{self.uid}", list(shape), dt))
        tl = Tile(t, nsub)
        if self.freed:
            for s in range(nsub):
                tl.r[s] = dict(self.freed)
        self.scope_tiles[-1].append(tl)
        return tl

    def psum(self, shape, dt):
        self.uid += 1
        t = self.scopes[-1].enter_context(self.nc.psum_tensor(f"ps{self.uid}", list(shape), dt))
        tl = Tile(t, 1)
        tl.excl = True
        return tl

    def _sync(self, eng, reads, writes, extra=None):
        need = {}

        def add(key, val):
            if key[0] == "E" and key[1] == eng and eng == PE:
                return
            if need.get(key, 0) < val:
                need[key] = val

        for v in reads:
            if v.tile is None:
                continue
            for s in (v.subs if v.subs is not None else range(v.tile.nsub)):
                w = v.tile.w[s]
                if w is not None:
                    add(w[0], w[1])
                if v.tile.excl:
                    for key, val in v.tile.r[s].items():
                        if not (key[0] == "E" and key[1] == eng):
                            add(key, val)
        for v in writes:
            if v.tile is None:
                continue
            for s in (v.subs if v.subs is not None else range(v.tile.nsub)):
                w = v.tile.w[s]
                if w is not None:
                    add(w[0], w[1])
                for key, val in v.tile.r[s].items():
                    add(key, val)
        if extra is not None and extra[1] > 0:
            add(extra[0], extra[1])
        if SERIAL and self.last is not None:
            if not (self.last[0][0] == "E" and self.last[0][1] == eng and eng == PE):
                if need.get(self.last[0], 0) < self.last[1]:
                    need[self.last[0]] = self.last[1]
        seen = self.seen[eng]
        for key, val in need.items():
            if seen.get(key, 0) >= val:
                continue
            seen[key] = val
            sem = self.esem[key[1]] if key[0] == "E" else self.dsem[key[1]]
            self.engs[eng].wait_ge(sem, val)
            self.ninstr += 1

    def _commit(self, tok, reads, writes):
        key, val = tok
        self.last = tok
        for v in reads:
            if v.tile is None:
                continue
            for s in (v.subs if v.subs is not None else range(v.tile.nsub)):
                r = v.tile.r[s]
                if r.get(key, 0) < val:
                    r[key] = val
        for v in writes:
            if v.tile is None:
                continue
            for s in (v.subs if v.subs is not None else range(v.tile.nsub)):
                v.tile.w[s] = tok
                v.tile.r[s] = {}

    def op(self, eng, name, **kw):
        reads, writes, args = [], [], {}
        for k, v in kw.items():
            if isinstance(v, V):
                (writes if k in ("out", "accum_out") else reads).append(v)
                args[k] = v.ap
            else:
                args[k] = v
        self._sync(eng, reads, writes)
        ins = getattr(self.engs[eng], name)(**args)
        self.cnt[eng] += 1
        ins.then_inc(self.esem[eng], 1)
        self.ninstr += 1
        self._commit((("E", eng), self.cnt[eng]), reads, writes)

    def dma(self, q, out, in_, **kw):
        if q == POOL:
            k = 16 + self.dkp
            self.dkp = (self.dkp + 1) % (NDS - 16)
        else:
            k = self.dk
            self.dk = (self.dk + 1) % 16
        prev = self.dval[k]
        self._sync(q, [in_], [out], extra=(("D", k), prev))
        ins = self.engs[q].dma_start(out=out.ap, in_=in_.ap, **kw)
        ins.then_inc(self.dsem[k], 16)
        self.ninstr += 1
        self.dval[k] = prev + 16
        self._commit((("D", k), prev + 16), [in_], [out])

    def finish(self):
        for k in range(NDS):
            if self.dval[k] > 0:
                self.nc.sync.wait_ge(self.dsem[k], self.dval[k])
        for e in range(4):
            if self.cnt[e] > 0:
                self.nc.sync.wait_ge(self.esem[e], self.cnt[e])

    def mm(self, out, lhsT, rhs, start=True, stop=True):
        self.op(PE, "matmul", out=out, lhsT=lhsT, rhs=rhs, start=start, stop=stop, skip_group_check=True)

    def tr(self, out, in_, identity):
        self.op(PE, "transpose", out=out, in_=in_, identity=identity)

    def act(self, out, in_, func, **kw):
        self.op(ACT, "activation", out=out, in_=in_, func=func, **kw)


def U(ap):
    return V(None, ap, None)


class K:
    pass


def build(nseq, nlayers, dbg=False):
    nc = bass.Bass("TRN2", target_bir_lowering=False)
    cx = Ctx(nc)
    k = K()
    k.nc, k.cx, k.dbg = nc, cx, dbg

    def din(name, shape, dt=F32):
        return nc.dram_tensor(name, list(shape), dt, kind="ExternalInput").ap()

    k.x = din("x", [4, 2048, D]); k.ctxin = din("ctx", [4, NCTX, D]); k.c5 = din("c5", [5, D])
    k.ada_w = din("ada_w", [NL, D, 3 * D]); k.ada_b = din("ada_b", [NL, 3 * D])
    k.w_in = din("w_in", [NL, D, DIN]); k.wuq = din("wuq", [NL, 384, 768]); k.wukv = din("wukv", [NL, 256, 1024])
    k.gla_a_w = din("gla_a_w", [NL, 2, 16, 256]); k.ml_wq = din("ml_wq", [NL, 4, 128, 64]); k.ml_wk = din("ml_wk", [NL, 4, 128, 64])
    k.br_w = din("br_w", [NL, 4, 512, D]); k.w_out = din("w_out", [NL, D, D])
    k.pcols = din("pcols", [128, NL, NPC]); k.prow = din("prow", [NL, NPR])
    k.cident = din("cident", [128, 128]); k.cbd64 = din("cbd64", [128, 128]); k.crm2 = din("crm2", [128, 128])
    k.ctrif = din("ctrif", [128, 128]); k.ctrib = din("ctrib", [128, 128])
    k.ccos = din("ccos", [128, T]); k.csin = din("csin", [128, T])
    k.out = nc.dram_tensor("out", [4, 2048, D], F32, kind="ExternalOutput").ap()
    k.xres_ap = nc.dram_tensor("xres", [4, T, D], F32, kind="Internal").ap()
    k.gscr_ap = nc.dram_tensor("gscr", [NL, 5, D], F32, kind="Internal").ap()
    k.xres = [Tile(k.xres_ap[s], NT) for s in range(4)]
    k.gscr = Tile(k.gscr_ap, NL)
    if dbg:
        k.dbg_y = nc.dram_tensor("dbg_y", [4, 128, 4, T], F32, kind="ExternalOutput").ap()
        k.dbg_acc = nc.dram_tensor("dbg_acc", [128, 8, T], F32, kind="ExternalOutput").ap()
        k.dbg_h = nc.dram_tensor("dbg_h", [128, 8, T], F32, kind="ExternalOutput").ap()
        k.dbg_x = nc.dram_tensor("dbg_x", [T, D], F32, kind="ExternalOutput").ap()

    def cload(src, dt, shape=(128, 128), q=POOL):
        t = cx.sb(shape, dt)
        cx.dma(q, t[:], U(src))
        return t

    k.identb = cload(k.cident, BF16); k.identf = cload(k.cident, F32, q=SP)
    k.bd64 = cload(k.cbd64, BF16); k.rm2 = cload(k.crm2, BF16)
    k.trif = cload(k.ctrif, F32, q=SP); k.trib = cload(k.ctrib, F32, q=SP)
    k.trifb = cload(k.ctrif, BF16); k.tribb = cload(k.ctrib, BF16)
    k.cos = cload(k.ccos, BF16, (128, T)); k.sin = cload(k.csin, BF16, (128, T))
    k.onesb = cx.sb((128, 128), BF16)
    k.onesf = cx.sb((128, 128), F32)
    _memset(cx, k.onesb[:], 1.0); _memset(cx, k.onesf[:], 1.0)
    k.epsc = cx.sb((128, 1), F32); _memset(cx, k.epsc[:], EPS)
    k.onec = cx.sb((128, 1), F32); _memset(cx, k.onec[:], 1.0)
    k.pc = cx.sb((128, NL, NPC), F32)
    cx.dma(SP, k.pc[:], U(k.pcols))
    k.AB = cx.sb((128, NL, 5, 16), F32)
    k.ps = [cx.psum((128, 512), F32) for _ in range(8)]
    k.psi = 0
    k.wpool = [cx.sb((128, 8, 512), BF16) for _ in range(3)]
    k.wi = 0

    for s in range(nseq):
        cx.dma(SP, k.xres[s].s([0, 1])[0:NCTX, :], U(k.ctxin[s]))
        for j in range(4):
            cx.dma(SP, k.xres[s].s(tk(NCTX + j * 512, NCTX + (j + 1) * 512))[NCTX + j * 512:NCTX + (j + 1) * 512, :],
                   U(k.x[s, j * 512:(j + 1) * 512, :]))

    prologue(k)
    for s in range(nseq):
        for l in range(nlayers):
            layer(k, s, l, last=(l == NL - 1))
    cx.finish()
    cx.es.close()
    return nc, cx


def _memset(cx, v, val, eng=DVE):
    cx._sync(eng, [], [v])
    ins = cx.engs[eng].memset(v.ap, val)
    cx.cnt[eng] += 1
    ins.then_inc(cx.esem[eng], 1)
    cx._commit((("E", eng), cx.cnt[eng]), [], [v])


def nps(k):
    p = k.ps[k.psi]
    k.psi = (k.psi + 1) % 8
    return p


def wload(k, src2d, kc, ncols):
    wt = k.wpool[k.wi]
    k.wi = (k.wi + 1) % len(k.wpool)
    k.cx.dma(POOL, wt[:, 0:kc, 0:ncols], U(src2d.rearrange("(kc p) n -> p kc n", p=128)))
    return wt


def prologue(k):
    cx, nc = k.cx, k.nc
    cx.push()
    c5t = cx.sb((5, D), F32)
    cx.dma(SP, c5t[:], U(k.c5))
    cx.act(c5t[:], c5t[:], AF.Silu)
    scT = cx.sb((128, 8, 5), F32)
    p = nps(k)
    for kc in range(8):
        cx.tr(p[:, kc * 8:kc * 8 + 5], c5t[0:5, kc * 128:(kc + 1) * 128], k.identf[0:5, 0:5])
    cx.op(DVE, "tensor_copy", out=scT[:], in_=V(p, p.t[:, 0:64].rearrange("p (a b) -> p a b", b=8)[:, :, 0:5], None))
    wf = [cx.sb((128, 8, 512), F32) for _ in range(2)]
    adab = cx.sb((5, 3 * D), F32)
    modrow = cx.sb((5, 3 * D), F32)
    modcol = cx.sb((128, 16, 5), F32)
    for l in range(NL):
        cx.dma(SP, adab[:], U(k.ada_b[l].partition_broadcast(5)))
        for nb in range(6):
            w = wf[nb % 2]
            cx.dma(SP, w[:], U(k.ada_w[l][:, nb * 512:(nb + 1) * 512].rearrange("(kc p) n -> p kc n", p=128)))
            p = nps(k)
            for kc in range(8):
                cx.mm(p[0:5, :], scT[:, kc, :], w[:, kc, :], start=(kc == 0), stop=(kc == 7))
            cx.op(DVE, "tensor_tensor", out=modrow[:, nb * 512:(nb + 1) * 512], in0=p[0:5, :], in1=adab[:, nb * 512:(nb + 1) * 512], op=ALU.add)
        p = nps(k)
        for j in range(16):
            cx.tr(p[:, j * 8:j * 8 + 5], modrow[0:5, j * 128:(j + 1) * 128], k.identf[0:5, 0:5])
        cx.op(DVE, "tensor_copy", out=modcol[:], in_=V(p, p.t[:, 0:128].rearrange("p (a b) -> p a b", b=8)[:, :, 0:5], None))
        for r in range(5):
            cx.op(DVE, "scalar_tensor_tensor", out=k.AB[:, l, r, 0:8], in0=modcol[:, 8:16, r], scalar=1.0, in1=k.pc[:, l, PC_NORMG:PC_NORMG + 8], op0=ALU.add, op1=ALU.mult)
            cx.op(DVE, "tensor_copy", out=k.AB[:, l, r, 8:16], in_=modcol[:, 0:8, r])
        cx.dma(SP, k.gscr.s(l)[l], modrow[0:5, 2 * D:3 * D])
    cx.pop()


def layer_params(k, l):
    cx = k.cx
    dc = cx.sb((128, NDC), F32)
    pc = k.pc

    def sc(dst, src, n, f):
        cx.op(DVE, "tensor_scalar", out=dc[:, dst:dst + n], in0=pc[:, l, src:src + n], scalar1=float(f), scalar2=None, op0=ALU.mult)

    lam_init = 0.8 - 0.6 * math.exp(-0.3 * l)
    sc(DC_CQG, PC_CQG, 3, 1.0); sc(DC_CKVG, PC_CKVG, 2, 1.0)
    sc(DC_QGN, PC_QGN, 1, 192 ** -0.5); sc(DC_QGR, PC_QGR, 1, 192 ** -0.5)
    sc(DC_KGN, PC_KGN, 1, 1.0); sc(DC_KGR, PC_KGR, 1, 1.0)
    sc(DC_DFQG, PC_DFQG, 1, 0.125); sc(DC_DFKG, PC_DFKG, 1, 1.0)
    sc(DC_DFOG, PC_DFOG, 1, (1.0 - lam_init))
    sc(DC_GLAOG, PC_GLAOG, 4, 1.0); sc(DC_MLOG, PC_MLOG, 4, 1.0)
    sc(DC_NAB, PC_GLAAB, 4, -1.0)
    pr = cx.sb((128, NPR), F32)
    cx.dma(SP, pr[:], U(k.prow[l].partition_broadcast(128)))
    junk = cx.sb((128, 64), F32)
    s2 = cx.sb((128, 2), F32)
    for i in range(2):
        cx.op(DVE, "tensor_tensor", out=junk[:], in0=pr[:, 16 + 128 * i:16 + 128 * i + 64], in1=pr[:, 16 + 128 * i + 64:16 + 128 * i + 128], op=ALU.mult)
        cx.op(DVE, "reduce_sum", out=s2[:, i:i + 1], in_=junk[:], axis=AX.X)
    cx.act(s2[:], s2[:], AF.Exp)
    cx.op(DVE, "scalar_tensor_tensor", out=dc[:, DC_LAM:DC_LAM + 1], in0=s2[:, 1:2], scalar=-lam_init, in1=s2[:, 0:1], op0=ALU.add, op1=ALU.subtract)
    return dc, pr


def stage_h(k, s, l, hT):
    cx = k.cx
    cx.push()
    xts = [cx.sb((128, D), F32) for _ in range(2)]
    xns = [cx.sb((128, D), BF16) for _ in range(2)]
    junk = cx.sb((128, D), BF16)
    ss = cx.sb((128, NT), F32)
    rs = cx.sb((128, NT), F32)
    for tt in range(NT):
        xt, xn = xts[tt % 2], xns[tt % 2]
        cx.dma(SP, xt[:], k.xres[s].s(tt)[tt * 128:(tt + 1) * 128, :])
        cx.act(junk[:], xt[:], AF.Square, accum_out=ss[:, tt:tt + 1])
        cx.act(rs[:, tt:tt + 1], ss[:, tt:tt + 1], AF.Sqrt, scale=1.0 / D, bias=k.epsc[:, 0:1])
        cx.op(DVE, "reciprocal", out=rs[:, tt:tt + 1], in_=rs[:, tt:tt + 1])
        cx.op(DVE, "tensor_scalar", out=xn[:], in0=xt[:], scalar1=rs[:, tt:tt + 1], scalar2=None, op0=ALU.mult)
        p = nps(k)
        pb = V(p, p.t[:].bitcast(BF16), None)
        for kc in range(8):
            cx.tr(V(p, pb.ap[:, kc * 128:(kc + 1) * 128], None), xn[:, kc * 128:(kc + 1) * 128], k.identb[:])
        r = 4 if tt < 2 else s
        for kc in range(8):
            src = V(p, pb.ap[:, kc * 128:(kc + 1) * 128], None)
            dst = hT.s(tt)[:, kc, tt * 128:(tt + 1) * 128]
            if kc % 2 == 0:
                cx.act(dst, src, AF.Identity, scale=k.AB[:, l, r, kc:kc + 1], bias=k.AB[:, l, r, 8 + kc:9 + kc])
            else:
                cx.op(DVE, "tensor_scalar", out=dst, in0=src, scalar1=k.AB[:, l, r, kc:kc + 1], scalar2=k.AB[:, l, r, 8 + kc:9 + kc], op0=ALU.mult, op1=ALU.add)
    cx.pop()


def proj_fm(k, src, wt, chunks, evac, blks, kcs=8):
    cx = k.cx
    for (b0, n) in blks:
        for ci, grp in enumerate(chunks):
            p = nps(k)
            for (co, m, r0) in grp:
                for kc in range(kcs):
                    cx.mm(p[r0:r0 + m, 0:n], wt[:, kc, co:co + m], src.s(tk(b0, b0 + n))[:, kc, b0:b0 + n], start=(kc == 0), stop=(kc == kcs - 1))
            evac(ci, p, b0, n)


def norm_group(k, pss, n, gmat, neps, gcols, dsts, tmp):
    cx = k.cx
    nchunk = len(pss)
    for c, p in enumerate(pss):
        cx.act(tmp["sq"][:, c, 0:n], p[:, 0:n], AF.Square)
        cx.op(DVE, "tensor_copy", out=tmp["raw"][:, c, 0:n], in_=p[:, 0:n])
    pq = nps(k)
    for c in range(nchunk):
        cx.mm(pq[:, 0:n], gmat[:], tmp["sq"][:, c, 0:n], start=(c == 0), stop=(c == nchunk - 1))
    cx.act(tmp["rs"][:, 0:n], pq[:, 0:n], AF.Ln, scale=EPS / float(neps), bias=k.epsc[:, 0:1])
    cx.act(tmp["rs"][:, 0:n], tmp["rs"][:, 0:n], AF.Exp, scale=-0.5)
    for c in range(nchunk):
        cx.op(DVE, "scalar_tensor_tensor", out=dsts[c], in0=tmp["raw"][:, c, 0:n], scalar=gcols[c], in1=tmp["rs"][:, 0:n], op0=ALU.mult, op1=ALU.mult)


def rope(k, xn, dst, b0, n, tmp):
    cx = k.cx
    p = nps(k)
    cx.mm(p[:, 0:n], k.rm2[:], xn)
    cx.op(DVE, "tensor_tensor", out=tmp["t1"][:, 0:n], in0=p[:, 0:n], in1=k.sin[:, b0:b0 + n], op=ALU.mult)
    cx.op(PEL, "tensor_tensor", out=tmp["t2"][:, 0:n], in0=xn, in1=k.cos[:, b0:b0 + n], op=ALU.mult)
    cx.op(DVE, "tensor_tensor", out=dst, in0=tmp["t1"][:, 0:n], in1=tmp["t2"][:, 0:n], op=ALU.add)


def mk_tmp(cx, nchunk=3):
    return dict(raw=cx.sb((128, nchunk, 512), BF16), sq=cx.sb((128, nchunk, 512), BF16), rs=cx.sb((128, 512), F32),
                t1=cx.sb((128, 512), F32), t2=cx.sb((128, 512), F32), xn=cx.sb((128, 512), BF16))


def merge(k, l, i, y, hT, acc, blks):
    cx = k.cx
    cx.push()
    sg = [cx.sb((128, 512), BF16) for _ in range(2)]
    tmps = [cx.sb((128, 512), BF16) for _ in range(2)]
    j = 0
    for mg in range(2):
        c0 = OFF["merge"] + i * D + mg * 512
        wg = wload(k, k.w_in[l][:, c0:c0 + 512], 8, 512)
        wb = wload(k, k.br_w[l, i][:, mg * 512:(mg + 1) * 512], 4, 512)
        for mc4 in range(4):
            mc = mg * 4 + mc4
            for (b0, n) in blks:
                sb_ = tk(b0, b0 + n)
                pg = nps(k)
                for kc in range(8):
                    cx.mm(pg[:, 0:n], wg[:, kc, mc4 * 128:(mc4 + 1) * 128], hT.s(sb_)[:, kc, b0:b0 + n], start=(kc == 0), stop=(kc == 7))
                g = sg[j % 2]
                cx.act(g[:, 0:n], pg[:, 0:n], AF.Sigmoid)
                py = nps(k)
                for kc in range(4):
                    cx.mm(py[:, 0:n], wb[:, kc, mc4 * 128:(mc4 + 1) * 128], y.s(sb_)[:, kc, b0:b0 + n], start=(kc == 0), stop=(kc == 3))
                dst = acc.s(sb_)[:, mc, b0:b0 + n]
                if i == 0:
                    cx.op(DVE, "tensor_tensor", out=dst, in0=py[:, 0:n], in1=g[:, 0:n], op=ALU.mult)
                else:
                    t = tmps[j % 2]
                    cx.op(DVE, "tensor_tensor", out=t[:, 0:n], in0=py[:, 0:n], in1=g[:, 0:n], op=ALU.mult)
                    cx.op(PEL, "tensor_tensor", out=dst, in0=acc.s(sb_)[:, mc, b0:b0 + n], in1=t[:, 0:n], op=ALU.add)
                j += 1
    cx.pop()


def final(k, s, l, acc, last):
    cx = k.cx
    cx.push()
    w0 = wload(k, k.w_out[l][:, 0:512], 8, 512)
    w1 = wload(k, k.w_out[l][:, 512:1024], 8, 512)
    gb = [cx.sb((128, D), F32) for _ in range(2)]
    cx.dma(SP, gb[0][:], V(k.gscr, k.gscr.t[l, 4].partition_broadcast(128), [l]))
    cx.dma(SP, gb[1][:], V(k.gscr, k.gscr.t[l, s].partition_broadcast(128), [l]))
    xo = [cx.sb((128, D), F32) for _ in range(2)]
    tm = [cx.sb((128, 512), F32) for _ in range(2)]
    for tt in range(2 if last else 0, NT):
        x_ = xo[tt % 2]
        cx.dma(SP, x_[:], k.xres[s].s(tt)[tt * 128:(tt + 1) * 128, :])
        g = gb[0] if tt < 2 else gb[1]
        for nh, w in enumerate((w0, w1)):
            p = nps(k)
            for kc in range(8):
                cx.mm(p[:, :], acc.s(tt)[:, kc, tt * 128:(tt + 1) * 128], w[:, kc, :], start=(kc == 0), stop=(kc == 7))
            t = tm[nh]
            cx.op(DVE, "tensor_tensor", out=t[:], in0=p[:, :], in1=g[:, nh * 512:(nh + 1) * 512], op=ALU.mult)
            cx.op(PEL if nh else DVE, "tensor_tensor", out=x_[:, nh * 512:(nh + 1) * 512], in0=x_[:, nh * 512:(nh + 1) * 512], in1=t[:], op=ALU.add)
        if k.dbg and s == 0 and l == 0:
            cx.dma(SP, U(k.dbg_x[tt * 128:(tt + 1) * 128, :]), x_[:])
        if last:
            cx.dma(SP, U(k.out[s, (tt - 2) * 128:(tt - 1) * 128, :]), x_[:])
        else:
            cx.dma(SP, k.xres[s].s(tt)[tt * 128:(tt + 1) * 128, :], x_[:])
    cx.pop()


def attn_core(k, nheads, nmaps, qk_fn, v_fn, out_fn, last, blk_fn=None):
    cx = k.cx
    cx.push()
    pts = [cx.sb((128, 512), BF16) for _ in range(4)]
    pj = 0
    sbank = [k.ps[0], k.ps[1]]
    abanks = [k.ps[2], k.ps[3], k.ps[4], k.ps[5]]
    sj = 0
    for qb, (q0, nq) in enumerate(BLKS):
        if last and qb == 0:
            continue
        kts = [0, 1] if qb == 0 else list(range(NT))
        nqt = nq // 128
        if blk_fn is not None:
            blk_fn(qb, q0, nq)
        for h in range(nheads):
            def accv(m, qi):
                idx = m * 4 + qi
                b = abanks[idx // 2] if nmaps == 2 else abanks[qi // 2]
                o = (idx % 2) * 129
                return b, V(b, b.t[:, o:o + 129], None)
            started = set()
            pend = []

            def do_pv(kt_, items):
                vv = v_fn(h, kt_)
                for (m, pt) in items:
                    for qi in range(nqt):
                        b, av = accv(m, qi)
                        first = id(b) not in started
                        started.add(id(b))
                        cx.mm(av, pt[:, qi * 128:(qi + 1) * 128], vv, start=first, stop=(kt_ == kts[-1]))

            for kt in kts:
                items = []
                for m in range(nmaps):
                    sp_ = sbank[sj % 2]; sj += 1
                    qk_fn(h, m, kt, q0, nq, sp_)
                    pt = pts[pj % 4]; pj += 1
                    cx.act(pt[:, 0:nq], sp_[:, 0:nq], AF.Exp)
                    items.append((m, pt))
                if pend:
                    do_pv(*pend.pop())
                pend.append((kt, items))
            do_pv(*pend.pop())
            for qi in range(nqt):
                out_fn(h, qb, q0 + qi * 128, [accv(m, qi)[1] for m in range(nmaps)])
    cx.pop()


def mla(k, s, l, hT, acc, dc, last):
    cx = k.cx
    cx.push()
    qn = cx.sb((128, 4, T), BF16, NT); qr = cx.sb((128, 2, T), BF16, NT)
    kn = cx.sb((128, 4, T), BF16, NT); kr2 = cx.sb((128, T), BF16, NT)
    vaug = cx.sb((128, NT, 4, 129), BF16, NT)
    _memset(cx, vaug[:], 1.0)
    W = k.w_in[l]
    cx.push()
    cqn = cx.sb((128, 3, T), BF16, NT)
    tmp = mk_tmp(cx)
    wA = wload(k, W[:, 0:384], 8, 384)
    wq1 = wload(k, k.wuq[l][:, 0:512], 3, 512)
    wq2 = wload(k, k.wuq[l][:, 512:768], 3, 256)
    for (b0, n) in BLKS:
        sb_ = tk(b0, b0 + n)
        pss = []
        for c in range(3):
            p = nps(k)
            for kc in range(8):
                cx.mm(p[:, 0:n], wA[:, kc, c * 128:(c + 1) * 128], hT.s(sb_)[:, kc, b0:b0 + n], start=(kc == 0), stop=(kc == 7))
            pss.append(p)
        ksub = int(os.environ.get("KSUB", "9"))
        if ksub < 1:
            for c in range(3):
                cx.op(DVE, "tensor_copy", out=cqn.s(sb_)[:, c, b0:b0 + n], in_=pss[c][:, 0:n])
            continue
        norm_group(k, pss, n, k.onesb, 384 * EPS, [dc[:, DC_CQG + c:DC_CQG + c + 1] for c in range(3)],
                   [cqn.s(sb_)[:, c, b0:b0 + n] for c in range(3)], tmp)
        if ksub < 2:
            continue
        for h in range(4):
            p = nps(k)
            for kc in range(3):
                kv = os.environ.get("KV", "0")
                lw = wA if kv == "2" else wq1
                rr = hT if kv == "1" else cqn
                cx.mm(p[:, 0:n], lw[:, kc, h * 128:(h + 1) * 128], rr.s(sb_)[:, kc, b0:b0 + n], start=(kc == 0), stop=(kc == 2))
            if os.environ.get("KQ", "1") == "0":
                cx.op(DVE, "tensor_copy", out=qn.s(sb_)[:, h, b0:b0 + n], in_=p[:, 0:n])
            else:
                norm_group(k, [p], n, k.onesb, 128 * EPS, [dc[:, DC_QGN:DC_QGN + 1]], [qn.s(sb_)[:, h, b0:b0 + n]], tmp)
        if ksub < 3:
            continue
        for c in range(2):
            p = nps(k)
            for kc in range(3):
                cx.mm(p[:, 0:n], wq2[:, kc, c * 128:(c + 1) * 128], cqn.s(sb_)[:, kc, b0:b0 + n], start=(kc == 0), stop=(kc == 2))
            norm_group(k, [p], n, k.bd64, 64 * EPS, [dc[:, DC_QGR:DC_QGR + 1]], [tmp["xn"][:, 0:n]], tmp)
            rope(k, tmp["xn"][:, 0:n], qr.s(sb_)[:, c, b0:b0 + n], b0, n, tmp)
    cx.pop()
    stg = int(os.environ.get("KSTAGE", "9"))
    if stg < 4:
        cx.pop(); return
    cx.push()
    ckvn = cx.sb((128, 2, T), BF16, NT)
    tmp = mk_tmp(cx)
    wB = wload(k, W[:, 384:704], 8, 320)
    wk1 = wload(k, k.wukv[l][:, 0:512], 2, 512)
    for (b0, n) in BLKS:
        sb_ = tk(b0, b0 + n)
        pss = []
        for c in range(2):
            p = nps(k)
            for kc in range(8):
                cx.mm(p[:, 0:n], wB[:, kc, c * 128:(c + 1) * 128], hT.s(sb_)[:, kc, b0:b0 + n], start=(kc == 0), stop=(kc == 7))
            pss.append(p)
        norm_group(k, pss, n, k.onesb, 256 * EPS, [dc[:, DC_CKVG + c:DC_CKVG + c + 1] for c in range(2)],
                   [ckvn.s(sb_)[:, c, b0:b0 + n] for c in range(2)], tmp)
        p = nps(k)
        for r0 in (0, 64):
            for kc in range(8):
                cx.mm(p[r0:r0 + 64, 0:n], wB[:, kc, 256:320], hT.s(sb_)[:, kc, b0:b0 + n], start=(kc == 0), stop=(kc == 7))
        norm_group(k, [p], n, k.bd64, 64 * EPS, [dc[:, DC_KGR:DC_KGR + 1]], [tmp["xn"][:, 0:n]], tmp)
        rope(k, tmp["xn"][:, 0:n], kr2.s(sb_)[:, b0:b0 + n], b0, n, tmp)
        for h in range(4):
            p = nps(k)
            for kc in range(2):
                cx.mm(p[:, 0:n], wk1[:, kc, h * 128:(h + 1) * 128], ckvn.s(sb_)[:, kc, b0:b0 + n], start=(kc == 0), stop=(kc == 1))
            norm_group(k, [p], n, k.onesb, 128 * EPS, [dc[:, DC_KGN:DC_KGN + 1]], [kn.s(sb_)[:, h, b0:b0 + n]], tmp)
    wv1 = wload(k, k.wukv[l][:, 512:1024], 2, 512)
    for tt in range(NT):
        p = nps(k)
        for kc in range(2):
            cx.mm(p[:, :], ckvn.s(tt)[:, kc, tt * 128:(tt + 1) * 128], wv1[:, kc, :], start=(kc == 0), stop=(kc == 1))
        cx.op(DVE, "tensor_copy", out=vaug.s(tt)[:, tt, :, 0:128], in_=V(p, p.t[:, :].rearrange("p (h e) -> p h e", e=128), None))
    cx.pop()
    if stg < 5:
        cx.pop(); return
    if k.dbg and s == 0 and l == 0:
        dumpv(k, "d_kr2", kr2[:, :], T)
        dumpv(k, "d_qr0", qr[:, 0, :], T)
        dumpv(k, "d_qn1", qn[:, 1, :], T)
        dumpv(k, "d_kn1", kn[:, 1, :], T)
    y = cx.sb((128, 4, T), BF16, NT)
    sz = cx.sb((128, 4, 512), BF16)
    wz = wload(k, W[:, OFF["mla_z"]:OFF["mla_z"] + 512], 8, 512)

    def blk_fn(qb, b0, n):
        for c in range(4):
            p = k.ps[6 + c % 2]
            for kc in range(8):
                cx.mm(p[:, 0:n], wz[:, kc, c * 128:(c + 1) * 128], hT.s(tk(b0, b0 + n))[:, kc, b0:b0 + n], start=(kc == 0), stop=(kc == 7))
            cx.act(sz[:, c, 0:n], p[:, 0:n], AF.Silu)

    ob = [cx.sb((128, 128), BF16) for _ in range(2)]
    rc = [cx.sb((128, 1), F32) for _ in range(2)]
    oj = [0]

    def qk_fn(h, m, kt, q0, nq, sp_):
        r0 = (h % 2) * 64
        cx.mm(sp_[:, 0:nq], kn.s(kt)[:, h, kt * 128:(kt + 1) * 128], qn.s(tk(q0, q0 + nq))[:, h, q0:q0 + nq], start=True, stop=False)
        cx.mm(sp_[:, 0:nq], kr2.s(kt)[r0:r0 + 64, kt * 128:(kt + 1) * 128], qr.s(tk(q0, q0 + nq))[r0:r0 + 64, h // 2, q0:q0 + nq], start=False, stop=True)

    def v_fn(h, kt):
        return vaug.s(kt)[:, kt, h, :]

    def out_fn(h, qb, q0, accs):
        a = accs[0]
        j = oj[0]; oj[0] += 1
        o_, r_ = ob[j % 2], rc[j % 2]
        cx.op(DVE, "reciprocal", out=r_[:], in_=V(a.tile, a.ap[:, 128:129], None))
        cx.op(DVE, "tensor_scalar", out=o_[:], in0=V(a.tile, a.ap[:, 0:128], None), scalar1=r_[:, 0:1], scalar2=None, op0=ALU.mult)
        p = k.ps[6 + j % 2]
        pb = V(p, p.t[:].bitcast(BF16)[:, 0:128], None)
        cx.tr(pb, o_[:], k.identb[:])
        tt = q0 // 128
        qoff = q0 - BLKS[qb][0]
        cx.op(DVE, "tensor_tensor", out=y.s(tt)[:, h, q0:q0 + 128], in0=pb, in1=sz[:, h, qoff:qoff + 128], op=ALU.mult)

    attn_core(k, 4, 1, qk_fn, v_fn, out_fn, last, blk_fn)
    if k.dbg and s == 0 and l == 0:
        dump(k, y, k.dbg_y[0], 4)
    if stg < 6:
        cx.pop(); return
    merge(k, l, 0, y, hT, acc, BLKS[1:] if last else BLKS)
    cx.pop()


def sz_block(k, wz, hT, sz, b0, n):
    cx = k.cx
    for c in range(4):
        p = k.ps[6 + c % 2]
        for kc in range(8):
            cx.mm(p[:, 0:n], wz[:, kc, c * 128:(c + 1) * 128], hT.s(tk(b0, b0 + n))[:, kc, b0:b0 + n], start=(kc == 0), stop=(kc == 7))
        cx.act(sz[:, c, 0:n], p[:, 0:n], AF.Silu)


def proj_tm(k, hT, w, dst_fn, ncols=512):
    cx = k.cx
    for tt in range(NT):
        p = nps(k)
        for kc in range(8):
            cx.mm(p[:, 0:ncols], hT.s(tt)[:, kc, tt * 128:(tt + 1) * 128], w[:, kc, 0:ncols], start=(kc == 0), stop=(kc == 7))
        dst_fn(tt, p)


def diffattn(k, s, l, hT, acc, dc, last):
    cx = k.cx
    cx.push()
    qd = cx.sb((128, 4, T), BF16, NT); kd = cx.sb((128, 4, T), BF16, NT)
    vaug = cx.sb((128, NT, 4, 129), BF16, NT)
    _memset(cx, vaug[:], 1.0)
    W = k.w_in[l]
    cx.push()
    tmp = mk_tmp(cx, 1)
    for (dst, off, gcol) in ((qd, OFF["df_q"], DC_DFQG), (kd, OFF["df_k"], DC_DFKG)):
        w = wload(k, W[:, off:off + 512], 8, 512)
        for (b0, n) in BLKS:
            sb_ = tk(b0, b0 + n)
            for h in range(4):
                p = nps(k)
                for kc in range(8):
                    cx.mm(p[:, 0:n], w[:, kc, h * 128:(h + 1) * 128], hT.s(sb_)[:, kc, b0:b0 + n], start=(kc == 0), stop=(kc == 7))
                norm_group(k, [p], n, k.bd64, 64 * EPS, [dc[:, gcol:gcol + 1]], [tmp["xn"][:, 0:n]], tmp)
                rope(k, tmp["xn"][:, 0:n], dst.s(sb_)[:, h, b0:b0 + n], b0, n, tmp)
    wv = wload(k, W[:, OFF["df_v"]:OFF["df_v"] + 512], 8, 512)
    proj_tm(k, hT, wv, lambda tt, p: cx.op(DVE, "tensor_copy", out=vaug.s(tt)[:, tt, :, 0:128],
                                             in_=V(p, p.t[:, :].rearrange("p (h e) -> p h e", e=128), None)))
    cx.pop()
    y = cx.sb((128, 4, T), BF16, NT)
    sz = cx.sb((128, 4, 512), BF16)
    wz = wload(k, W[:, OFF["df_z"]:OFF["df_z"] + 512], 8, 512)
    o1 = [cx.sb((128, 128), F32) for _ in range(2)]
    o2 = [cx.sb((128, 128), F32) for _ in range(2)]
    ob = [cx.sb((128, 128), BF16) for _ in range(2)]
    junk = cx.sb((128, 128), BF16)
    rc = [cx.sb((128, 4), F32) for _ in range(2)]
    oj = [0]

    def qk_fn(h, m, kt, q0, nq, sp_):
        r0 = m * 64
        cx.mm(sp_[:, 0:nq], kd.s(kt)[r0:r0 + 64, h, kt * 128:(kt + 1) * 128], qd.s(tk(q0, q0 + nq))[r0:r0 + 64, h, q0:q0 + nq], start=True, stop=True)

    def v_fn(h, kt):
        return vaug.s(kt)[:, kt, h, :]

    def out_fn(h, qb, q0, accs):
        a0, a1 = accs
        j = oj[0]; oj[0] += 1
        r_ = rc[j % 2]
        cx.op(DVE, "reciprocal", out=r_[:, 0:1], in_=V(a0.tile, a0.ap[:, 128:129], None))
        cx.op(DVE, "reciprocal", out=r_[:, 1:2], in_=V(a1.tile, a1.ap[:, 128:129], None))
        cx.op(DVE, "tensor_tensor", out=r_[:, 1:2], in0=r_[:, 1:2], in1=dc[:, DC_LAM:DC_LAM + 1], op=ALU.mult)
        cx.op(DVE, "tensor_scalar", out=o1[j % 2][:], in0=V(a0.tile, a0.ap[:, 0:128], None), scalar1=r_[:, 0:1], scalar2=None, op0=ALU.mult)
        cx.op(DVE, "scalar_tensor_tensor", out=o2[j % 2][:], in0=V(a1.tile, a1.ap[:, 0:128], None), scalar=r_[:, 1:2], in1=o1[j % 2][:], op0=ALU.mult, op1=ALU.add)
        cx.act(junk[:], o2[j % 2][:], AF.Square, accum_out=r_[:, 2:3])
        cx.act(r_[:, 3:4], r_[:, 2:3], AF.Sqrt, scale=1.0 / 128, bias=k.epsc[:, 0:1])
        cx.op(DVE, "reciprocal", out=r_[:, 3:4], in_=r_[:, 3:4])
        cx.op(DVE, "tensor_scalar", out=ob[j % 2][:], in0=o2[j % 2][:], scalar1=r_[:, 3:4], scalar2=None, op0=ALU.mult)
        p = k.ps[6 + j % 2]
        pb = V(p, p.t[:].bitcast(BF16)[:, 0:128], None)
        cx.tr(pb, ob[j % 2][:], k.identb[:])
        tt = q0 // 128
        qoff = q0 - BLKS[qb][0]
        cx.op(DVE, "scalar_tensor_tensor", out=y.s(tt)[:, h, q0:q0 + 128], in0=pb, scalar=dc[:, DC_DFOG:DC_DFOG + 1], in1=sz[:, h, qoff:qoff + 128], op0=ALU.mult, op1=ALU.mult)

    attn_core(k, 4, 2, qk_fn, v_fn, out_fn, last, lambda qb, b0, n: sz_block(k, wz, hT, sz, b0, n))
    if k.dbg and s == 0 and l == 0:
        dump(k, y, k.dbg_y[3], 4)
    merge(k, l, 3, y, hT, acc, BLKS[1:] if last else BLKS)
    cx.pop()


def out_stage(k, l, i, hT, osrc_fn, y, dc, gcol0, zoff, last, extra_fn=None):
    cx = k.cx
    cx.push()
    sz = cx.sb((128, 4, 512), BF16)
    wz = wload(k, k.w_in[l][:, zoff:zoff + 512], 8, 512)
    on = [cx.sb((128, 512), BF16) for _ in range(2)]
    junk = cx.sb((128, 128), BF16)
    ss = [cx.sb((128, 4), F32) for _ in range(2)]
    for (b0, n) in (BLKS[1:] if last else BLKS):
        sz_block(k, wz, hT, sz, b0, n)
        for tt in tk(b0, b0 + n):
            src = osrc_fn(tt)
            s_, o_ = ss[tt % 2], on[tt % 2]
            for h in range(4):
                cx.act(junk[:], V(src.tile, src.ap[:, h * 128:(h + 1) * 128], src.subs), AF.Square, accum_out=s_[:, h:h + 1])
            cx.act(s_[:], s_[:], AF.Sqrt, scale=1.0 / 128, bias=k.epsc[:, 0:1])
            cx.op(DVE, "reciprocal", out=s_[:], in_=s_[:])
            for h in range(4):
                cx.op(DVE, "tensor_scalar", out=o_[:, h * 128:(h + 1) * 128], in0=V(src.tile, src.ap[:, h * 128:(h + 1) * 128], src.subs), scalar1=s_[:, h:h + 1], scalar2=None, op0=ALU.mult)
            p = nps(k)
            pb = p.t[:].bitcast(BF16)
            for h in range(4):
                cx.tr(V(p, pb[:, h * 128:(h + 1) * 128], None), o_[:, h * 128:(h + 1) * 128], k.identb[:])
            t0 = tt * 128
            for h in range(4):
                pv = V(p, pb[:, h * 128:(h + 1) * 128], None)
                if extra_fn is None:
                    cx.op(DVE, "scalar_tensor_tensor", out=y.s(tt)[:, h, t0:t0 + 128], in0=pv, scalar=dc[:, gcol0 + h:gcol0 + h + 1], in1=sz[:, h, t0 - b0:t0 - b0 + 128], op0=ALU.mult, op1=ALU.mult)
                else:
                    extra_fn(tt, h, pv, sz[:, h, t0 - b0:t0 - b0 + 128])
    cx.pop()


def gla(k, s, l, hT, acc, dc, last):
    cx = k.cx
    cx.push()
    W = k.w_in[l]
    ogl = cx.sb((128, NT, 512), BF16, NT)
    cx.push()
    qT = cx.sb((128, 2, T), BF16, NT); kT = cx.sb((128, 2, T), BF16, NT)
    afT = cx.sb((16, 2, T), BF16, NT)
    vtm = cx.sb((128, NT, 512), BF16, NT)
    aw = cx.sb((16, 2, 256), BF16)
    cx.dma(POOL, aw[:], U(k.gla_a_w[l].rearrange("d r n -> r d n")))
    wqk = wload(k, W[:, OFF["gla_q"]:OFF["gla_q"] + 512], 8, 512)
    waf = wload(k, W[:, OFF["gla_af"]:OFF["gla_af"] + 32], 8, 32)
    for (b0, n) in BLKS:
        sb_ = tk(b0, b0 + n)
        for c in range(4):
            p = nps(k)
            for kc in range(8):
                cx.mm(p[:, 0:n], wqk[:, kc, c * 128:(c + 1) * 128], hT.s(sb_)[:, kc, b0:b0 + n], start=(kc == 0), stop=(kc == 7))
            if c < 2:
                cx.act(qT.s(sb_)[:, c, b0:b0 + n], p[:, 0:n], AF.Copy, scale=0.125)
            else:
                cx.op(DVE, "tensor_copy", out=kT.s(sb_)[:, c - 2, b0:b0 + n], in_=p[:, 0:n])
        for d in range(2):
            p = nps(k)
            for kc in range(8):
                cx.mm(p[0:16, 0:n], waf[:, kc, d * 16:(d + 1) * 16], hT.s(sb_)[:, kc, b0:b0 + n], start=(kc == 0), stop=(kc == 7))
            cx.op(DVE, "tensor_copy", out=afT.s(sb_)[0:16, d, b0:b0 + n], in_=p[0:16, 0:n])
    wv = wload(k, W[:, OFF["gla_v"]:OFF["gla_v"] + 512], 8, 512)
    proj_tm(k, hT, wv, lambda tt, p: cx.act(vtm.s(tt)[:, tt, :], p[:, :], AF.Copy))
    S32 = cx.sb((128, 2, 128), F32); Sb = cx.sb((128, 2, 128), BF16)
    R = lambda shape, dt, nb=2: [cx.sb(shape, dt) for _ in range(nb)]
    e_ = R((128, 2, 128), F32); lp_ = R((128, 2, 128), F32); Bp_ = R((128, 2, 128), F32); tR_ = R((128, 2, 128), F32)
    E_ = R((128, 2, 128), BF16, 3); qe_ = R((128, 2, 128), BF16); ke_ = R((128, 2, 128), BF16); kl_ = R((128, 2, 128), BF16)
    bt_ = R((128, 2, 4), F32); kltm_ = R((128, 256), BF16); AT_ = R((128, 128), BF16, 3)
    for d in range(2):
        _memset(cx, S32[:], 0.0); _memset(cx, Sb[:], 0.0)
        order = list(range(NT)) if d == 0 else [1, 0] + list(range(NT - 1, 1, -1))
        mask = k.trifb if d == 0 else k.tribb

        def prep(tt, j, d=d):
            t0 = tt * 128
            e, lp, Bp, tR, qe, ke, kl, bt, kltm = e_[j], lp_[j], Bp_[j], tR_[j], qe_[j], ke_[j], kl_[j], bt_[j], kltm_[j]
            pl = nps(k)
            for c in range(2):
                cx.mm(pl[:, c * 128:(c + 1) * 128], aw[0:16, d, c * 128:(c + 1) * 128], afT.s(tt)[0:16, d, t0:t0 + 128])
            for c in range(2):
                cx.act(e[:, c, :], pl[:, c * 128:(c + 1) * 128], AF.Exp, scale=-1.0, bias=dc[:, DC_NAB + 2 * d + c:DC_NAB + 2 * d + c + 1])
            cx.act(lp[:], e[:], AF.Ln, bias=k.onec[:, 0:1])
            for c in range(2):
                cx.op(DVE, "tensor_tensor_scan", out=Bp[:, c, :], data0=k.onesf[:, 0:128], data1=lp[:, c, :], initial=0.0, op0=ALU.mult, op1=ALU.add)
            E1, E2, E3 = E_[0], E_[1], E_[2]
            if d == 0:
                cx.op(DVE, "tensor_scalar", out=bt[:, :, 0:1], in0=Bp[:, :, 127:128], scalar1=-1.0 / 16, scalar2=None, op0=ALU.mult)
                cx.act(E1[:], Bp[:], AF.Exp, scale=-1.0 / 16)
                cx.act(E2[:], Bp[:], AF.Exp, scale=1.0 / 16)
                for c in range(2):
                    cx.act(E3[:, c, :], Bp[:, c, :], AF.Exp, scale=1.0 / 16, bias=bt[:, c, 0:1])
            else:
                cx.op(DVE, "tensor_tensor", out=tR[:], in0=lp[:], in1=Bp[:], op=ALU.subtract)
                cx.op(DVE, "tensor_scalar", out=bt[:, :, 0:1], in0=Bp[:, :, 127:128], scalar1=-1.0 / 16, scalar2=None, op0=ALU.mult)
                cx.op(DVE, "tensor_scalar", out=bt[:, :, 1:2], in0=Bp[:, :, 127:128], scalar1=1.0 / 16, scalar2=None, op0=ALU.mult)
                for c in range(2):
                    cx.act(E1[:, c, :], tR[:, c, :], AF.Exp, scale=-1.0 / 16, bias=bt[:, c, 0:1])
                    cx.act(E2[:, c, :], tR[:, c, :], AF.Exp, scale=1.0 / 16, bias=bt[:, c, 1:2])
                cx.act(E3[:], tR[:], AF.Exp, scale=1.0 / 16)
            cx.act(bt[:, :, 2:3], bt[:, :, 0:1], AF.Exp)
            cx.op(DVE, "tensor_tensor", out=qe[:], in0=qT.s(tt)[:, :, t0:t0 + 128], in1=E1[:], op=ALU.mult)
            cx.op(DVE, "tensor_tensor", out=ke[:], in0=kT.s(tt)[:, :, t0:t0 + 128], in1=E2[:], op=ALU.mult)
            cx.op(DVE, "tensor_tensor", out=kl[:], in0=kT.s(tt)[:, :, t0:t0 + 128], in1=E3[:], op=ALU.mult)
            ptr = nps(k)
            pb = ptr.t[:].bitcast(BF16)
            for c in range(2):
                cx.tr(V(ptr, pb[:, c * 128:(c + 1) * 128], None), kl[:, c, :], k.identb[:])
            cx.op(DVE, "tensor_copy", out=kltm[:], in_=V(ptr, pb[:, 0:256], None))

        def body(tt, j, d=d, mask=mask):
            qe, ke, bt, kltm = qe_[j], ke_[j], bt_[j], kltm_[j]
            po = nps(k)
            for h in range(4):
                c, r0 = h // 2, (h % 2) * 64
                pa = nps(k)
                cx.mm(pa[:, 0:128], ke[r0:r0 + 64, c, :], qe[r0:r0 + 64, c, :])
                AT = AT_[h % 3]
                cx.op(DVE, "tensor_tensor", out=AT[:], in0=pa[:, 0:128], in1=mask[:], op=ALU.mult)
                cx.mm(po[:, h * 128:(h + 1) * 128], AT[:], vtm.s(tt)[:, tt, h * 128:(h + 1) * 128], start=True, stop=False)
                cx.mm(po[:, h * 128:(h + 1) * 128], qe[r0:r0 + 64, c, :], Sb[r0:r0 + 64, c, :], start=False, stop=True)
            pu = nps(k)
            for h in range(4):
                c, r0 = h // 2, (h % 2) * 64
                cx.mm(pu[r0:r0 + 64, c * 128:(c + 1) * 128], kltm[:, h * 64:(h + 1) * 64], vtm.s(tt)[:, tt, h * 128:(h + 1) * 128], start=True, stop=True)
            if d == 0:
                cx.act(ogl.s(tt)[:, tt, :], po[:, :], AF.Copy)
            else:
                cx.op(DVE, "tensor_tensor", out=ogl.s(tt)[:, tt, :], in0=po[:, :], in1=ogl.s(tt)[:, tt, :], op=ALU.add)
            for c in range(2):
                cx.op(DVE, "scalar_tensor_tensor", out=S32[:, c, :], in0=S32[:, c, :], scalar=bt[:, c, 2:3], in1=pu[:, c * 128:(c + 1) * 128], op0=ALU.mult, op1=ALU.add)
            cx.act(Sb[:], S32[:], AF.Copy)

        prep(order[0], 0)
        for i, tt in enumerate(order):
            if i + 1 < len(order):
                prep(order[i + 1], (i + 1) % 2)
            body(tt, i % 2)
    cx.pop()
    y = cx.sb((128, 4, T), BF16, NT)
    out_stage(k, l, 1, hT, lambda tt: ogl.s(tt)[:, tt, :], y, dc, DC_GLAOG, OFF["gla_z"], last)
    if k.dbg and s == 0 and l == 0:
        dump(k, y, k.dbg_y[1], 4)
    merge(k, l, 1, y, hT, acc, BLKS[1:] if last else BLKS)
    cx.pop()


def mlstm(k, s, l, hT, acc, dc, pr, last):
    cx = k.cx
    cx.push()
    W = k.w_in[l]
    xcT = cx.sb((128, 4, T), BF16, NT)
    hml = cx.sb((128, NT, 512), BF16, NT)
    cx.push()
    xraw = cx.sb((128, 4, T), BF16, NT)
    cacc = cx.sb((128, T), F32)
    wx = wload(k, W[:, OFF["ml_x"]:OFF["ml_x"] + 512], 8, 512)
    for (b0, n) in BLKS:
        sb_ = tk(b0, b0 + n)
        for c in range(4):
            p = nps(k)
            for kc in range(8):
                cx.mm(p[:, 0:n], wx[:, kc, c * 128:(c + 1) * 128], hT.s(sb_)[:, kc, b0:b0 + n], start=(kc == 0), stop=(kc == 7))
            if c % 2:
                cx.act(xraw.s(sb_)[:, c, b0:b0 + n], p[:, 0:n], AF.Copy)
            else:
                cx.op(DVE, "tensor_copy", out=xraw.s(sb_)[:, c, b0:b0 + n], in_=p[:, 0:n])
    pc = k.pc
    for c in range(4):
        w0, w1, w2 = [pc[:, l, PC_CONVW + 4 * j + c:PC_CONVW + 4 * j + c + 1] for j in range(3)]
        cx.op(DVE, "tensor_scalar", out=cacc[:], in0=xraw[:, c, :], scalar1=w1, scalar2=pc[:, l, PC_CONVB + c:PC_CONVB + c + 1], op0=ALU.mult, op1=ALU.add)
        for (a_, b_) in ((0, NCTX), (NCTX, T)):
            cx.op(DVE, "scalar_tensor_tensor", out=cacc[:, a_ + 1:b_], in0=xraw[:, c, a_:b_ - 1], scalar=w0, in1=cacc[:, a_ + 1:b_], op0=ALU.mult, op1=ALU.add)
            cx.op(DVE, "scalar_tensor_tensor", out=cacc[:, a_:b_ - 1], in0=xraw[:, c, a_ + 1:b_], scalar=w2, in1=cacc[:, a_:b_ - 1], op0=ALU.mult, op1=ALU.add)
        cx.act(xcT[:, c, :], cacc[:], AF.Silu)
    cx.pop()
    cx.push()
    qT = cx.sb((128, 2, T), BF16, NT); kT = cx.sb((128, 2, T), BF16, NT)
    vaug = cx.sb((128, NT, 4, 129), BF16, NT)
    _memset(cx, vaug[:], 1.0)
    gt = cx.sb((128, NT, 16), F32); lpt = cx.sb((128, NT, 16), F32)
    wqb = cx.sb((128, 4, 64), BF16); wkb = cx.sb((128, 4, 64), BF16)
    cx.dma(POOL, wqb[:], U(k.ml_wq[l].rearrange("h c d -> c h d")))
    cx.dma(POOL, wkb[:], U(k.ml_wk[l].rearrange("h c d -> c h d")))
    for (b0, n) in BLKS:
        sb_ = tk(b0, b0 + n)
        for (dst, wb, scl) in ((qT, wqb, 1.0), (kT, wkb, 0.125)):
            for c in range(2):
                p = nps(k)
                for hh in range(2):
                    h = 2 * c + hh
                    cx.mm(p[hh * 64:(hh + 1) * 64, 0:n], wb[:, h, :], xcT.s(sb_)[:, h, b0:b0 + n], start=True, stop=True)
                cx.act(dst.s(sb_)[:, c, b0:b0 + n], p[:, 0:n], AF.Copy, scale=scl)
    wv = wload(k, W[:, OFF["ml_v"]:OFF["ml_v"] + 512], 8, 512)
    proj_tm(k, hT, wv, lambda tt, p: cx.op(DVE, "tensor_copy", out=vaug.s(tt)[:, tt, :, 0:128],
                                             in_=V(p, p.t[:, :].rearrange("p (h e) -> p h e", e=128), None)))
    wif = wload(k, W[:, OFF["ml_if"]:OFF["ml_if"] + 16], 8, 16)
    proj_tm(k, hT, wif, lambda tt, p: cx.op(DVE, "tensor_tensor", out=gt[:, tt, :], in0=p[:, 0:16], in1=pr[:, 0:16], op=ALU.add), ncols=16)
    cx.act(lpt[:], gt[:], AF.Exp, scale=-1.0)
    cx.act(lpt[:], lpt[:], AF.Ln, bias=k.onec[:, 0:1])
    Bp = cx.sb((128, 2, NT, 4), F32); Bt = cx.sb((128, 2, NT, 4), F32)
    fa = cx.sb((128, 2, NT, 4), F32); fg = cx.sb((128, 2, NT, 4), F32); fgk = cx.sb((128, 2, NT, 4), F32); fdec = cx.sb((128, 2, NT, 4), F32)
    for d in range(2):
        g0 = (1 + 2 * d) * 4
        rhs = lpt[:, :, g0:g0 + 4]
        p = nps(k)
        cx.mm(p[:, 0:72], (k.trif if d == 0 else k.trib)[:], rhs)
        cx.op(DVE, "tensor_copy", out=Bp[:, d], in_=V(p, p.t[:, 0:72].rearrange("p (a b) -> p a b", b=4), None))
        p = nps(k)
        cx.mm(p[:, 0:72], k.onesf[:], rhs)
        cx.op(DVE, "tensor_copy", out=Bt[:, d], in_=V(p, p.t[:, 0:72].rearrange("p (a b) -> p a b", b=4), None))
        li = gt[:, :, 8 * d:8 * d + 4]
        cx.act(fa[:, d], Bp[:, d], AF.Exp, scale=-1.0)
        cx.act(fdec[:, d], Bt[:, d], AF.Exp, scale=-1.0)
        cx.op(DVE, "tensor_tensor", out=Bp[:, d], in0=Bp[:, d], in1=li, op=ALU.add)
        cx.act(fg[:, d], Bp[:, d], AF.Exp)
        cx.op(DVE, "tensor_tensor", out=Bp[:, d], in0=Bp[:, d], in1=Bt[:, d], op=ALU.subtract)
        cx.act(fgk[:, d], Bp[:, d], AF.Exp)
    C32 = cx.sb((128, 2, 129), F32); Cb = cx.sb((128, 2, 129), BF16)
    PT_ = [cx.sb((128, 128), BF16) for _ in range(3)]
    kg_ = [cx.sb((128, 256), BF16) for _ in range(2)]
    dn_ = [cx.sb((128, 4), F32) for _ in range(2)]
    for d in range(2):
        _memset(cx, C32[:], 0.0); _memset(cx, Cb[:], 0.0)
        order = list(range(NT)) if d == 0 else [1, 0] + list(range(NT - 1, 1, -1))
        mask = k.trifb if d == 0 else k.tribb

        def prep(tt, j, d=d):
            t0 = tt * 128
            kg = kg_[j]
            pk = nps(k)
            for h in range(4):
                cx.mm(pk[:, h * 64:(h + 1) * 64], xcT.s(tt)[:, h, t0:t0 + 128], wkb[:, h, :], start=True, stop=True)
            for h in range(4):
                cx.op(DVE, "tensor_scalar", out=kg[:, h * 64:(h + 1) * 64], in0=pk[:, h * 64:(h + 1) * 64], scalar1=fgk[:, d, tt, h:h + 1], scalar2=0.125, op0=ALU.mult, op1=ALU.mult)

        def body(tt, j, d=d, mask=mask):
            t0 = tt * 128
            kg, dn = kg_[j], dn_[j]
            pos = [nps(k), nps(k)]
            for h in range(4):
                c, r0 = h // 2, (h % 2) * 64
                pa = nps(k)
                cx.mm(pa[:, 0:128], kT.s(tt)[r0:r0 + 64, c, t0:t0 + 128], qT.s(tt)[r0:r0 + 64, c, t0:t0 + 128])
                PT = PT_[h % 3]
                cx.op(DVE, "scalar_tensor_tensor", out=PT[:], in0=pa[:, 0:128], scalar=fg[:, d, tt, h:h + 1], in1=mask[:], op0=ALU.mult, op1=ALU.mult)
                o = (h % 2) * 129
                cx.mm(pos[c][:, o:o + 129], PT[:], vaug.s(tt)[:, tt, h, :], start=True, stop=False)
                cx.mm(pos[c][:, o:o + 129], qT.s(tt)[r0:r0 + 64, c, t0:t0 + 128], Cb[r0:r0 + 64, c, :], start=False, stop=True)
            pu = nps(k)
            for h in range(4):
                c, r0 = h // 2, (h % 2) * 64
                cx.mm(pu[r0:r0 + 64, c * 129:(c + 1) * 129], kg[:, h * 64:(h + 1) * 64], vaug.s(tt)[:, tt, h, :], start=True, stop=True)
            for c in range(2):
                po = pos[c]
                den = V(po, po.t[:, 0:258].rearrange("p (a b) -> p a b", b=129)[:, :, 128], None)
                cx.op(DVE, "tensor_tensor", out=dn[:, 0:2], in0=den, in1=fa[:, d, tt, 2 * c:2 * c + 2], op=ALU.mult)
                cx.act(dn[:, 0:2], dn[:, 0:2], AF.Abs)
                cx.op(DVE, "tensor_scalar", out=dn[:, 0:2], in0=dn[:, 0:2], scalar1=1.0, scalar2=None, op0=ALU.max)
                cx.op(DVE, "reciprocal", out=dn[:, 0:2], in_=dn[:, 0:2])
                cx.op(DVE, "tensor_tensor", out=dn[:, 2:4], in0=dn[:, 0:2], in1=fa[:, d, tt, 2 * c:2 * c + 2], op=ALU.mult)
                for hh in range(2):
                    h = 2 * c + hh
                    dst = hml.s(tt)[:, tt, h * 128:(h + 1) * 128]
                    if d == 0:
                        cx.op(DVE, "tensor_scalar", out=dst, in0=po[:, hh * 129:hh * 129 + 128], scalar1=dn[:, 2 + hh:3 + hh], scalar2=None, op0=ALU.mult)
                    else:
                        cx.op(DVE, "scalar_tensor_tensor", out=dst, in0=po[:, hh * 129:hh * 129 + 128], scalar=dn[:, 2 + hh:3 + hh], in1=dst, op0=ALU.mult, op1=ALU.add)
            for h in range(4):
                c, r0 = h // 2, (h % 2) * 64
                cx.op(DVE, "scalar_tensor_tensor", out=C32[r0:r0 + 64, c, :], in0=C32[r0:r0 + 64, c, :], scalar=fdec[r0:r0 + 64, d, tt, h:h + 1], in1=pu[r0:r0 + 64, c * 129:(c + 1) * 129], op0=ALU.mult, op1=ALU.add)
            cx.act(Cb[:], C32[:], AF.Copy)

        prep(order[0], 0)
        for i, tt in enumerate(order):
            if i + 1 < len(order):
                prep(order[i + 1], (i + 1) % 2)
            body(tt, i % 2)
    cx.pop()
    y = cx.sb((128, 4, T), BF16, NT)
    cx.push()
    wo = wload(k, W[:, OFF["ml_o"]:OFF["ml_o"] + 512], 8, 512)
    sgo = [cx.sb((128, 512), BF16) for _ in range(2)]

    def og(tt, p):
        cx.act(sgo[tt % 2][:], p[:, :], AF.Sigmoid)
        cx.op(DVE, "tensor_tensor", out=hml.s(tt)[:, tt, :], in0=hml.s(tt)[:, tt, :], in1=sgo[tt % 2][:], op=ALU.mult)
    proj_tm(k, hT, wo, og)
    t1_ = [cx.sb((128, 128), F32) for _ in range(2)]
    t2_ = [cx.sb((128, 128), F32) for _ in range(2)]
    ej = [0]

    def extra(tt, h, pv, szv):
        j = ej[0] % 2; ej[0] += 1
        t0 = tt * 128
        cx.act(t1_[j][:], pv, AF.Identity, scale=dc[:, DC_MLOG + h:DC_MLOG + h + 1])
        cx.op(DVE, "scalar_tensor_tensor", out=t2_[j][:], in0=xcT.s(tt)[:, h, t0:t0 + 128], scalar=k.pc[:, l, PC_MLSKIP + h:PC_MLSKIP + h + 1], in1=t1_[j][:], op0=ALU.mult, op1=ALU.add)
        cx.op(DVE, "tensor_tensor", out=y.s(tt)[:, h, t0:t0 + 128], in0=t2_[j][:], in1=szv, op=ALU.mult)
    out_stage(k, l, 2, hT, lambda tt: hml.s(tt)[:, tt, :], y, dc, DC_MLOG, OFF["ml_z"], last, extra)
    cx.pop()
    if k.dbg and s == 0 and l == 0:
        dump(k, y, k.dbg_y[2], 4)
    merge(k, l, 2, y, hT, acc, BLKS[1:] if last else BLKS)
    cx.pop()


def dumpv(k, name, v, ncols):
    cx = k.cx
    dst = k.nc.dram_tensor(name, [128, ncols], F32, kind="ExternalOutput").ap()
    cx.push()
    f = cx.sb((128, 256), F32)
    for j in range(0, ncols, 256):
        w = min(256, ncols - j)
        cx.op(DVE, "tensor_copy", out=f[:, 0:w], in_=V(v.tile, v.ap[:, j:j + w], v.subs))
        cx.dma(SP, U(dst[:, j:j + w]), f[:, 0:w])
    cx.pop()


def dump(k, t, dst, nch):
    cx = k.cx
    cx.push()
    f = cx.sb((128, 256), F32)
    for c in range(nch):
        for j in range(T // 256):
            cx.op(DVE, "tensor_copy", out=f[:], in_=t[:, c, j * 256:(j + 1) * 256])
            cx.dma(SP, U(dst[:, c, j * 256:(j + 1) * 256]), f[:])
    cx.pop()


def layer(k, s, l, last):
    cx = k.cx
    cx.push()
    hT = cx.sb((128, 8, T), BF16, NT)
    acc = cx.sb((128, 8, T), BF16, NT)
    stg = int(os.environ.get("KSTAGE", "9"))
    if stg < 1:
        cx.pop(); return
    dc, pr = layer_params(k, l)
    if stg < 2:
        cx.pop(); return
    stage_h(k, s, l, hT)
    if stg < 3:
        if k.dbg:
            dump(k, hT, k.dbg_h, 8)
        cx.pop(); return
    if k.dbg and s == 0 and l == 0:
        dump(k, hT, k.dbg_h, 8)
    mla(k, s, l, hT, acc, dc, last)
    if stg < 7:
        cx.pop(); return
    mix = os.environ.get("KMIX", "123")
    if "1" in mix:
        gla(k, s, l, hT, acc, dc, last)
    if "2" in mix:
        mlstm(k, s, l, hT, acc, dc, pr, last)
    if "3" in mix:
        diffattn(k, s, l, hT, acc, dc, last)
    if k.dbg and s == 0 and l == 0:
        dump(k, acc, k.dbg_acc, 8)
    final(k, s, l, acc, last)
    cx.pop()


def _consts():
    ident = np.eye(128, dtype=np.float32)
    bd = np.zeros((128, 128), np.float32); bd[:64, :64] = 1; bd[64:, 64:] = 1
    rm = np.zeros((64, 64), np.float32)
    for i in range(16):
        rm[16 + i, i] = -1; rm[i, 16 + i] = 1; rm[48 + i, 32 + i] = -1; rm[32 + i, 48 + i] = 1
    rm2 = np.zeros((128, 128), np.float32); rm2[:64, :64] = rm; rm2[64:, 64:] = rm
    si, ti = np.meshgrid(np.arange(128), np.arange(128), indexing="ij")
    trif = (si <= ti).astype(np.float32); trib = (si >= ti).astype(np.float32)
    quarter = 16
    inv_freq = (10000.0 ** (-np.arange(quarter, dtype=np.float32) / quarter)).astype(np.float32)
    row = np.repeat(np.arange(32, dtype=np.float32), 64); col = np.tile(np.arange(64, dtype=np.float32), 32)
    ar = row[:, None] * inv_freq; ac = col[:, None] * inv_freq
    ang = np.concatenate([ar, ar, ac, ac], axis=-1).astype(np.float32)
    cos = np.concatenate([np.ones((NCTX, 64), np.float32), np.cos(ang)], 0)
    sin = np.concatenate([np.zeros((NCTX, 64), np.float32), np.sin(ang)], 0)
    cosT = np.ascontiguousarray(np.concatenate([cos.T, cos.T], 0)); sinT = np.ascontiguousarray(np.concatenate([sin.T, sin.T], 0))
    return dict(cident=ident, cbd64=bd, crm2=rm2, ctrif=trif, ctrib=trib, ccos=cosT.astype(np.float32), csin=sinT.astype(np.float32))


def _cols(v):
    v = np.asarray(v, np.float32).reshape(-1)
    return v.reshape(-1, 128).T


def _pack(inp):
    pcols = np.zeros((128, NL, NPC), np.float32)
    prow = np.zeros((NL, NPR), np.float32)
    for l in range(NL):
        P = pcols[:, l]
        P[:, PC_NORMG:PC_NORMG + 8] = _cols(inp["norm_g"][l])
        P[:, PC_CQG:PC_CQG + 3] = _cols(inp["mla_cq_g"][l]); P[:, PC_CKVG:PC_CKVG + 2] = _cols(inp["mla_ckv_g"][l])
        qg, kg = inp["mla_q_g"][l], inp["mla_k_g"][l]
        P[:, PC_QGN] = qg[:128]; P[:, PC_QGR] = np.concatenate([qg[128:], qg[128:]])
        P[:, PC_KGN] = kg[:128]; P[:, PC_KGR] = np.concatenate([kg[128:], kg[128:]])
        P[:, PC_DFQG] = np.concatenate([inp["df_qk_g"][l, 0]] * 2); P[:, PC_DFKG] = np.concatenate([inp["df_qk_g"][l, 1]] * 2)
        P[:, PC_DFOG] = inp["df_out_g"][l]
        P[:, PC_GLAOG:PC_GLAOG + 4] = _cols(inp["gla_out_g"][l]); P[:, PC_MLOG:PC_MLOG + 4] = _cols(inp["ml_out_g"][l])
        P[:, PC_MLSKIP:PC_MLSKIP + 4] = _cols(inp["ml_skip"][l])
        for j in range(3):
            P[:, PC_CONVW + 4 * j:PC_CONVW + 4 * j + 4] = _cols(inp["ml_conv_w"][l, j])
        P[:, PC_CONVB:PC_CONVB + 4] = _cols(inp["ml_conv_b"][l])
        for d_ in range(2):
            P[:, PC_GLAAB + 2 * d_:PC_GLAAB + 2 * d_ + 2] = _cols(inp["gla_a_b"][l, d_])
        prow[l, 0:16] = inp["ml_gate_b"][l].reshape(-1)
        prow[l, 16:272] = inp["df_lambda"][l].reshape(-1)
    return pcols, prow


def _perm_wuq(w):
    w = w.reshape(NL, 384, 4, 192)
    return np.ascontiguousarray(np.concatenate([w[..., :128].reshape(NL, 384, 512), w[..., 128:].reshape(NL, 384, 256)], -1))


def _perm_wukv(w):
    w = w.reshape(NL, 256, 4, 256)
    return np.ascontiguousarray(np.concatenate([w[..., :128].reshape(NL, 256, 512), w[..., 128:].reshape(NL, 256, 512)], -1))


_CACHE = {}


def host_maps(inp, ncores=8):
    inp = {k_: np.asarray(v, np.float32) for k_, v in inp.items()}
    pcols, prow = _pack(inp)
    shared = dict(ada_w=inp["ada_w"], ada_b=inp["ada_b"], w_in=inp["w_in"], wuq=_perm_wuq(inp["mla_wuq"]),
                  wukv=_perm_wukv(inp["mla_wukv"]), gla_a_w=inp["gla_a_w"], ml_wq=inp["ml_wq"], ml_wk=inp["ml_wk"],
                  br_w=inp["br_w"], w_out=inp["w_out"], pcols=pcols, prow=prow, **_consts())
    maps = []
    for c in range(ncores):
        m = dict(shared)
        m["x"] = np.ascontiguousarray(inp["x"][4 * c:4 * c + 4]); m["ctx"] = np.ascontiguousarray(inp["ctx"][4 * c:4 * c + 4])
        m["c5"] = np.ascontiguousarray(np.concatenate([inp["c"][4 * c:4 * c + 4], inp["c_ctx"][None]], 0))
        maps.append(m)
    return maps


def kernel(**inputs):
    if "nc" not in _CACHE:
        _CACHE["nc"] = build(4, NL)[0]
    nc = _CACHE["nc"]
    maps = host_maps(inputs)
    res = run_bass_kernel_spmd(nc, maps, core_ids=list(range(8)))
    return np.concatenate([np.asarray(r["out"], np.float32) for r in res.results], 0)
```

```python
import math
import contextlib
import numpy as np
import concourse.bass as bass
import concourse.mybir as mybir
from concourse.bass_utils import run_bass_kernel_spmd

F32 = mybir.dt.float32
BF16 = mybir.dt.bfloat16
AF = mybir.ActivationFunctionType
ALU = mybir.AluOpType
AX = mybir.AxisListType

T = 2304
NCTX = 256
NT = 18
D = 1024
DIN = 10992
NL = 4
EPS = 1e-6
BLKS = [(0, 256), (256, 512), (768, 512), (1280, 512), (1792, 512)]
OFF = dict(mla_cq=0, mla_ckv=384, mla_kr=640, mla_z=704, gla_q=1216, gla_k=1472, gla_v=1728, gla_af=2240,
           gla_ab=2256, gla_z=2272, ml_x=2784, ml_v=3296, ml_o=3808, ml_if=4320, ml_z=4336, df_q=4848,
           df_k=5360, df_v=5872, df_z=6384, merge=6896)
PC_NORMG, PC_CQG, PC_CKVG, PC_QGN, PC_QGR, PC_KGN, PC_KGR, PC_DFQG, PC_DFKG, PC_DFOG = 0, 8, 11, 13, 14, 15, 16, 17, 18, 19
PC_GLAOG, PC_MLOG, PC_MLSKIP, PC_CONVW, PC_CONVB, PC_GLAAB, NPC = 20, 24, 28, 32, 44, 48, 52
NPR = 16 + 256
DC_CQG, DC_CKVG, DC_QGN, DC_QGR, DC_KGN, DC_KGR, DC_DFQG, DC_DFKG, DC_DFOG, DC_GLAOG, DC_MLOG, DC_NAB, DC_LAM, NDC = 0, 3, 5, 6, 7, 8, 9, 10, 11, 12, 16, 20, 24, 28

import os
PE, ACT, DVE, POOL, SP = range(5)
SERIAL = os.environ.get("SERIAL", "0") == "1"
PEL = DVE if os.environ.get("NOPOOL", "0") == "1" else POOL
NDS = 24


class V:
    __slots__ = ("tile", "ap", "subs")

    def __init__(self, tile, ap, subs):
        self.tile, self.ap, self.subs = tile, ap, subs


class _Sel:
    def __init__(self, tile, subs):
        self.tile, self.subs = tile, subs

    def __getitem__(self, idx):
        return V(self.tile, self.tile.t[idx], self.subs)


class Tile:
    def __init__(self, t, nsub=1):
        self.t = t
        self.nsub = nsub
        self.w = [None] * nsub
        self.r = [dict() for _ in range(nsub)]
        self.excl = False

    def __getitem__(self, idx):
        return V(self, self.t[idx], None)

    def s(self, subs):
        if isinstance(subs, int):
            subs = [subs]
        return _Sel(self, list(subs))


def tk(a, b):
    return list(range(a // 128, (b + 127) // 128))


class Ctx:
    def __init__(self, nc):
        self.nc = nc
        self.engs = [nc.tensor, nc.scalar, nc.vector, nc.gpsimd, nc.sync]
        self.es = contextlib.ExitStack()
        self.esem = [self.es.enter_context(nc.semaphore(f"e{i}")) for i in range(5)]
        self.dsem = [self.es.enter_context(nc.semaphore(f"d{i}")) for i in range(NDS)]
        self.cnt = [0] * 5
        self.dval = [0] * NDS
        self.dk = 0
        self.dkp = 0
        self.seen = [dict() for _ in range(5)]
        self.scopes = [self.es]
        self.scope_tiles = [[]]
        self.freed = {}
        self.uid = 0
        self.ninstr = 0
        self.last = None

    def push(self):
        st = contextlib.ExitStack()
        self.scopes.append(st)
        self.scope_tiles.append([])
        return st

    def pop(self):
        for tl in self.scope_tiles.pop():
            for s in range(tl.nsub):
                w = tl.w[s]
                if w is not None and self.freed.get(w[0], 0) < w[1]:
                    self.freed[w[0]] = w[1]
                for key, val in tl.r[s].items():
                    if self.freed.get(key, 0) < val:
                        self.freed[key] = val
        self.scopes.pop().close()

    def sb(self, shape, dt, nsub=1, name=None):
        self.uid += 1
        t = self.scopes[-1].enter_context(self.nc.sbuf_tensor(f"{name or 't'}{self.uid}", list(shape), dt))
        tl = Tile(t, nsub)
        if self.freed:
            for s in range(nsub):
                tl.r[s] = dict(self.freed)
        self.scope_tiles[-1].append(tl)
        return tl

    def psum(self, shape, dt):
        self.uid += 1
        t = self.scopes[-1].enter_context(self.nc.psum_tensor(f"ps{self.uid}", list(shape), dt))
        tl = Tile(t, 1)
        tl.excl = True
        return tl

    def _sync(self, eng, reads, writes, extra=None):
        need = {}

        def add(key, val):
            if key[0] == "E" and key[1] == eng and eng == PE:
                return
            if need.get(key, 0) < val:
                need[key] = val

        for v in reads:
            if v.tile is None:
                continue
            for s in (v.subs if v.subs is not None else range(v.tile.nsub)):
                w = v.tile.w[s]
                if w is not None:
                    add(w[0], w[1])
                if v.tile.excl:
                    for key, val in v.tile.r[s].items():
                        if not (key[0] == "E" and key[1] == eng):
                            add(key, val)
        for v in writes:
            if v.tile is None:
                continue
            for s in (v.subs if v.subs is not None else range(v.tile.nsub)):
                w = v.tile.w[s]
                if w is not None:
                    add(w[0], w[1])
                for key, val in v.tile.r[s].items():
                    add(key, val)
        if extra is not None and extra[1] > 0:
            add(extra[0], extra[1])
        if SERIAL and self.last is not None:
            if not (self.last[0][0] == "E" and self.last[0][1] == eng and eng == PE):
                if need.get(self.last[0], 0) < self.last[1]:
                    need[self.last[0]] = self.last[1]
        seen = self.seen[eng]
        for key, val in need.items():
            if seen.get(key, 0) >= val:
                continue
            seen[key] = val
            sem = self.esem[key[1]] if key[0] == "E" else self.dsem[key[1]]
            self.engs[eng].wait_ge(sem, val)
            self.ninstr += 1

    def _commit(self, tok, reads, writes):
        key, val = tok
        self.last = tok
        for v in reads:
            if v.tile is None:
                continue
            for s in (v.subs if v.subs is not None else range(v.tile.nsub)):
                r = v.tile.r[s]
                if r.get(key, 0) < val:
                    r[key] = val
        for v in writes:
            if v.tile is None:
                continue
            for s in (v.subs if v.subs is not None else range(v.tile.nsub)):
                v.tile.w[s] = tok
                v.tile.r[s] = {}

    def op(self, eng, name, **kw):
        reads, writes, args = [], [], {}
        for k, v in kw.items():
            if isinstance(v, V):
                (writes if k in ("out", "accum_out") else reads).append(v)
                args[k] = v.ap
            else:
                args[k] = v
        self._sync(eng, reads, writes)
        ins = getattr(self.engs[eng], name)(**args)
        self.cnt[eng] += 1
        ins.then_inc(self.esem[eng], 1)
        self.ninstr += 1
        self._commit((("E", eng), self.cnt[eng]), reads, writes)

    def dma(self, q, out, in_, **kw):
        if q == POOL:
            k = 16 + self.dkp
            self.dkp = (self.dkp + 1) % (NDS - 16)
        else:
            k = self.dk
            self.dk = (self.dk + 1) % 16
        prev = self.dval[k]
        self._sync(q, [in_], [out], extra=(("D", k), prev))
        ins = self.engs[q].dma_start(out=out.ap, in_=in_.ap, **kw)
        ins.then_inc(self.dsem[k], 16)
        self.ninstr += 1
        self.dval[k] = prev + 16
        self._commit((("D", k), prev + 16), [in_], [out])

    def finish(self):
        for k in range(NDS):
            if self.dval[k] > 0:
                self.nc.sync.wait_ge(self.dsem[k], self.dval[k])
        for e in range(4):
            if self.cnt[e] > 0:
                self.nc.sync.wait_ge(self.esem[e], self.cnt[e])

    def mm(self, out, lhsT, rhs, start=True, stop=True):
        self.op(PE, "matmul", out=out, lhsT=lhsT, rhs=rhs, start=start, stop=stop, skip_group_check=True)

    def tr(self, out, in_, identity):
        self.op(PE, "transpose", out=out, in_=in_, identity=identity)

    def act(self, out, in_, func, **kw):
        self.op(ACT, "activation", out=out, in_=in_, func=func, **kw)


def U(ap):
    return V(None, ap, None)


class K:
    pass


def build(nseq, nlayers, dbg=False):
    nc = bass.Bass("TRN2", target_bir_lowering=False)
    cx = Ctx(nc)
    k = K()
    k.nc, k.cx, k.dbg = nc, cx, dbg

    def din(name, shape, dt=F32):
        return nc.dram_tensor(name, list(shape), dt, kind="ExternalInput").ap()

    k.x = din("x", [4, 2048, D]); k.ctxin = din("ctx", [4, NCTX, D]); k.c5 = din("c5", [5, D])
    k.ada_w = din("ada_w", [NL, D, 3 * D]); k.ada_b = din("ada_b", [NL, 3 * D])
    k.w_in = din("w_in", [NL, D, DIN]); k.wuq = din("wuq", [NL, 384, 768]); k.wukv = din("wukv", [NL, 256, 1024])
    k.gla_a_w = din("gla_a_w", [NL, 2, 16, 256]); k.ml_wq = din("ml_wq", [NL, 4, 128, 64]); k.ml_wk = din("ml_wk", [NL, 4, 128, 64])
    k.br_w = din("br_w", [NL, 4, 512, D]); k.w_out = din("w_out", [NL, D, D])
    k.pcols = din("pcols", [128, NL, NPC]); k.prow = din("prow", [NL, NPR])
    k.cident = din("cident", [128, 128]); k.cbd64 = din("cbd64", [128, 128]); k.crm2 = din("crm2", [128, 128])
    k.ctrif = din("ctrif", [128, 128]); k.ctrib = din("ctrib", [128, 128])
    k.ccos = din("ccos", [128, T]); k.csin = din("csin", [128, T])
    k.out = nc.dram_tensor("out", [4, 2048, D], F32, kind="ExternalOutput").ap()
    k.xres_ap = nc.dram_tensor("xres", [4, T, D], F32, kind="Internal").ap()
    k.gscr_ap = nc.dram_tensor("gscr", [NL, 5, D], F32, kind="Internal").ap()
    k.xres = [Tile(k.xres_ap[s], NT) for s in range(4)]
    k.gscr = Tile(k.gscr_ap, NL)
    if dbg:
        k.dbg_y = nc.dram_tensor("dbg_y", [4, 128, 4, T], F32, kind="ExternalOutput").ap()
        k.dbg_acc = nc.dram_tensor("dbg_acc", [128, 8, T], F32, kind="ExternalOutput").ap()
        k.dbg_h = nc.dram_tensor("dbg_h", [128, 8, T], F32, kind="ExternalOutput").ap()
        k.dbg_x = nc.dram_tensor("dbg_x", [T, D], F32, kind="ExternalOutput").ap()

    def cload(src, dt, shape=(128, 128), q=POOL):
        t = cx.sb(shape, dt)
        cx.dma(q, t[:], U(src))
        return t

    k.identb = cload(k.cident, BF16); k.identf = cload(k.cident, F32, q=SP)
    k.bd64 = cload(k.cbd64, BF16); k.rm2 = cload(k.crm2, BF16)
    k.trif = cload(k.ctrif, F32, q=SP); k.trib = cload(k.ctrib, F32, q=SP)
    k.trifb = cload(k.ctrif, BF16); k.tribb = cload(k.ctrib, BF16)
    k.cos = cload(k.ccos, BF16, (128, T)); k.sin = cload(k.csin, BF16, (128, T))
    k.onesb = cx.sb((128, 128), BF16)
    k.onesf = cx.sb((128, 128), F32)
    _memset(cx, k.onesb[:], 1.0); _memset(cx, k.onesf[:], 1.0)
    k.epsc = cx.sb((128, 1), F32); _memset(cx, k.epsc[:], EPS)
    k.onec = cx.sb((128, 1), F32); _memset(cx, k.onec[:], 1.0)
    k.pc = cx.sb((128, NL, NPC), F32)
    cx.dma(SP, k.pc[:], U(k.pcols))
    k.AB = cx.sb((128, NL, 5, 16), F32)
    k.ps = [cx.psum((128, 512), F32) for _ in range(8)]
    k.psi = 0
    k.wpool = [cx.sb((128, 8, 512), BF16) for _ in range(3)]
    k.wi = 0

    for s in range(nseq):
        cx.dma(SP, k.xres[s].s([0, 1])[0:NCTX, :], U(k.ctxin[s]))
        for j in range(4):
            cx.dma(SP, k.xres[s].s(tk(NCTX + j * 512, NCTX + (j + 1) * 512))[NCTX + j * 512:NCTX + (j + 1) * 512, :],
                   U(k.x[s, j * 512:(j + 1) * 512, :]))

    prologue(k)
    for s in range(nseq):
        for l in range(nlayers):
            layer(k, s, l, last=(l == NL - 1))
    cx.finish()
    cx.es.close()
    return nc, cx


def _memset(cx, v, val, eng=DVE):
    cx._sync(eng, [], [v])
    ins = cx.engs[eng].memset(v.ap, val)
    cx.cnt[eng] += 1
    ins.then_inc(cx.esem[eng], 1)
    cx._commit((("E", eng), cx.cnt[eng]), [], [v])


def nps(k):
    p = k.ps[k.psi]
    k.psi = (k.psi + 1) % 8
    return p


def wload(k, src2d, kc, ncols):
    wt = k.wpool[k.wi]
    k.wi = (k.wi + 1) % len(k.wpool)
    k.cx.dma(POOL, wt[:, 0:kc, 0:ncols], U(src2d.rearrange("(kc p) n -> p kc n", p=128)))
    return wt


def prologue(k):
    cx, nc = k.cx, k.nc
    cx.push()
    c5t = cx.sb((5, D), F32)
    cx.dma(SP, c5t[:], U(k.c5))
    cx.act(c5t[:], c5t[:], AF.Silu)
    scT = cx.sb((128, 8, 5), F32)
    p = nps(k)
    for kc in range(8):
        cx.tr(p[:, kc * 8:kc * 8 + 5], c5t[0:5, kc * 128:(kc + 1) * 128], k.identf[0:5, 0:5])
    cx.op(DVE, "tensor_copy", out=scT[:], in_=V(p, p.t[:, 0:64].rearrange("p (a b) -> p a b", b=8)[:, :, 0:5], None))
    wf = [cx.sb((128, 8, 512), F32) for _ in range(2)]
    adab = cx.sb((5, 3 * D), F32)
    modrow = cx.sb((5, 3 * D), F32)
    modcol = cx.sb((128, 16, 5), F32)
    for l in range(NL):
        cx.dma(SP, adab[:], U(k.ada_b[l].partition_broadcast(5)))
        for nb in range(6):
            w = wf[nb % 2]
            cx.dma(SP, w[:], U(k.ada_w[l][:, nb * 512:(nb + 1) * 512].rearrange("(kc p) n -> p kc n", p=128)))
            p = nps(k)
            for kc in range(8):
                cx.mm(p[0:5, :], scT[:, kc, :], w[:, kc, :], start=(kc == 0), stop=(kc == 7))
            cx.op(DVE, "tensor_tensor", out=modrow[:, nb * 512:(nb + 1) * 512], in0=p[0:5, :], in1=adab[:, nb * 512:(nb + 1) * 512], op=ALU.add)
        p = nps(k)
        for j in range(16):
            cx.tr(p[:, j * 8:j * 8 + 5], modrow[0:5, j * 128:(j + 1) * 128], k.identf[0:5, 0:5])
        cx.op(DVE, "tensor_copy", out=modcol[:], in_=V(p, p.t[:, 0:128].rearrange("p (a b) -> p a b", b=8)[:, :, 0:5], None))
        for r in range(5):
            cx.op(DVE, "scalar_tensor_tensor", out=k.AB[:, l, r, 0:8], in0=modcol[:, 8:16, r], scalar=1.0, in1=k.pc[:, l, PC_NORMG:PC_NORMG + 8], op0=ALU.add, op1=ALU.mult)
            cx.op(DVE, "tensor_copy", out=k.AB[:, l, r, 8:16], in_=modcol[:, 0:8, r])
        cx.dma(SP, k.gscr.s(l)[l], modrow[0:5, 2 * D:3 * D])
    cx.pop()


def layer_params(k, l):
    cx = k.cx
    dc = cx.sb((128, NDC), F32)
    pc = k.pc

    def sc(dst, src, n, f):
        cx.op(DVE, "tensor_scalar", out=dc[:, dst:dst + n], in0=pc[:, l, src:src + n], scalar1=float(f), scalar2=None, op0=ALU.mult)

    lam_init = 0.8 - 0.6 * math.exp(-0.3 * l)
    sc(DC_CQG, PC_CQG, 3, 1.0); sc(DC_CKVG, PC_CKVG, 2, 1.0)
    sc(DC_QGN, PC_QGN, 1, 192 ** -0.5); sc(DC_QGR, PC_QGR, 1, 192 ** -0.5)
    sc(DC_KGN, PC_KGN, 1, 1.0); sc(DC_KGR, PC_KGR, 1, 1.0)
    sc(DC_DFQG, PC_DFQG, 1, 0.125); sc(DC_DFKG, PC_DFKG, 1, 1.0)
    sc(DC_DFOG, PC_DFOG, 1, (1.0 - lam_init))
    sc(DC_GLAOG, PC_GLAOG, 4, 1.0); sc(DC_MLOG, PC_MLOG, 4, 1.0)
    sc(DC_NAB, PC_GLAAB, 4, -1.0)
    pr = cx.sb((128, NPR), F32)
    cx.dma(SP, pr[:], U(k.prow[l].partition_broadcast(128)))
    junk = cx.sb((128, 64), F32)
    s2 = cx.sb((128, 2), F32)
    for i in range(2):
        cx.op(DVE, "tensor_tensor", out=junk[:], in0=pr[:, 16 + 128 * i:16 + 128 * i + 64], in1=pr[:, 16 + 128 * i + 64:16 + 128 * i + 128], op=ALU.mult)
        cx.op(DVE, "reduce_sum", out=s2[:, i:i + 1], in_=junk[:], axis=AX.X)
    cx.act(s2[:], s2[:], AF.Exp)
    cx.op(DVE, "scalar_tensor_tensor", out=dc[:, DC_LAM:DC_LAM + 1], in0=s2[:, 1:2], scalar=-lam_init, in1=s2[:, 0:1], op0=ALU.add, op1=ALU.subtract)
    return dc, pr


def stage_h(k, s, l, hT):
    cx = k.cx
    cx.push()
    xts = [cx.sb((128, D), F32) for _ in range(2)]
    xns = [cx.sb((128, D), BF16) for _ in range(2)]
    junk = cx.sb((128, D), BF16)
    ss = cx.sb((128, NT), F32)
    rs = cx.sb((128, NT), F32)
    for tt in range(NT):
        xt, xn = xts[tt % 2], xns[tt % 2]
        cx.dma(SP, xt[:], k.xres[s].s(tt)[tt * 128:(tt + 1) * 128, :])
        cx.act(junk[:], xt[:], AF.Square, accum_out=ss[:, tt:tt + 1])
        cx.act(rs[:, tt:tt + 1], ss[:, tt:tt + 1], AF.Sqrt, scale=1.0 / D, bias=k.epsc[:, 0:1])
        cx.op(DVE, "reciprocal", out=rs[:, tt:tt + 1], in_=rs[:, tt:tt + 1])
        cx.op(DVE, "tensor_scalar", out=xn[:], in0=xt[:], scalar1=rs[:, tt:tt + 1], scalar2=None, op0=ALU.mult)
        p = nps(k)
        pb = V(p, p.t[:].bitcast(BF16), None)
        for kc in range(8):
            cx.tr(V(p, pb.ap[:, kc * 128:(kc + 1) * 128], None), xn[:, kc * 128:(kc + 1) * 128], k.identb[:])
        r = 4 if tt < 2 else s
        for kc in range(8):
            src = V(p, pb.ap[:, kc * 128:(kc + 1) * 128], None)
            dst = hT.s(tt)[:, kc, tt * 128:(tt + 1) * 128]
            if kc % 2 == 0:
                cx.act(dst, src, AF.Identity, scale=k.AB[:, l, r, kc:kc + 1], bias=k.AB[:, l, r, 8 + kc:9 + kc])
            else:
                cx.op(DVE, "tensor_scalar", out=dst, in0=src, scalar1=k.AB[:, l, r, kc:kc + 1], scalar2=k.AB[:, l, r, 8 + kc:9 + kc], op0=ALU.mult, op1=ALU.add)
    cx.pop()


def proj_fm(k, src, wt, chunks, evac, blks, kcs=8):
    cx = k.cx
    for (b0, n) in blks:
        for ci, grp in enumerate(chunks):
            p = nps(k)
            for (co, m, r0) in grp:
                for kc in range(kcs):
                    cx.mm(p[r0:r0 + m, 0:n], wt[:, kc, co:co + m], src.s(tk(b0, b0 + n))[:, kc, b0:b0 + n], start=(kc == 0), stop=(kc == kcs - 1))
            evac(ci, p, b0, n)


def norm_group(k, pss, n, gmat, neps, gcols, dsts, tmp):
    cx = k.cx
    nchunk = len(pss)
    for c, p in enumerate(pss):
        cx.act(tmp["sq"][:, c, 0:n], p[:, 0:n], AF.Square)
        cx.op(DVE, "tensor_copy", out=tmp["raw"][:, c, 0:n], in_=p[:, 0:n])
    pq = nps(k)
    for c in range(nchunk):
        cx.mm(pq[:, 0:n], gmat[:], tmp["sq"][:, c, 0:n], start=(c == 0), stop=(c == nchunk - 1))
    cx.act(tmp["rs"][:, 0:n], pq[:, 0:n], AF.Ln, scale=EPS / float(neps), bias=k.epsc[:, 0:1])
    cx.act(tmp["rs"][:, 0:n], tmp["rs"][:, 0:n], AF.Exp, scale=-0.5)
    for c in range(nchunk):
        cx.op(DVE, "scalar_tensor_tensor", out=dsts[c], in0=tmp["raw"][:, c, 0:n], scalar=gcols[c], in1=tmp["rs"][:, 0:n], op0=ALU.mult, op1=ALU.mult)


def rope(k, xn, dst, b0, n, tmp):
    cx = k.cx
    p = nps(k)
    cx.mm(p[:, 0:n], k.rm2[:], xn)
    cx.op(DVE, "tensor_tensor", out=tmp["t1"][:, 0:n], in0=p[:, 0:n], in1=k.sin[:, b0:b0 + n], op=ALU.mult)
    cx.op(PEL, "tensor_tensor", out=tmp["t2"][:, 0:n], in0=xn, in1=k.cos[:, b0:b0 + n], op=ALU.mult)
    cx.op(DVE, "tensor_tensor", out=dst, in0=tmp["t1"][:, 0:n], in1=tmp["t2"][:, 0:n], op=ALU.add)


def mk_tmp(cx, nchunk=3):
    return dict(raw=cx.sb((128, nchunk, 512), BF16), sq=cx.sb((128, nchunk, 512), BF16), rs=cx.sb((128, 512), F32),
                t1=cx.sb((128, 512), F32), t2=cx.sb((128, 512), F32), xn=cx.sb((128, 512), BF16))


def merge(k, l, i, y, hT, acc, blks):
    cx = k.cx
    cx.push()
    sg = [cx.sb((128, 512), BF16) for _ in range(2)]
    tmps = [cx.sb((128, 512), BF16) for _ in range(2)]
    j = 0
    for mg in range(2):
        c0 = OFF["merge"] + i * D + mg * 512
        wg = wload(k, k.w_in[l][:, c0:c0 + 512], 8, 512)
        wb = wload(k, k.br_w[l, i][:, mg * 512:(mg + 1) * 512], 4, 512)
        for mc4 in range(4):
            mc = mg * 4 + mc4
            for (b0, n) in blks:
                sb_ = tk(b0, b0 + n)
                pg = nps(k)
                for kc in range(8):
                    cx.mm(pg[:, 0:n], wg[:, kc, mc4 * 128:(mc4 + 1) * 128], hT.s(sb_)[:, kc, b0:b0 + n], start=(kc == 0), stop=(kc == 7))
                g = sg[j % 2]
                cx.act(g[:, 0:n], pg[:, 0:n], AF.Sigmoid)
                py = nps(k)
                for kc in range(4):
                    cx.mm(py[:, 0:n], wb[:, kc, mc4 * 128:(mc4 + 1) * 128], y.s(sb_)[:, kc, b0:b0 + n], start=(kc == 0), stop=(kc == 3))
                dst = acc.s(sb_)[:, mc, b0:b0 + n]
                if i == 0:
                    cx.op(DVE, "tensor_tensor", out=dst, in0=py[:, 0:n], in1=g[:, 0:n], op=ALU.mult)
                else:
                    t = tmps[j % 2]
                    cx.op(DVE, "tensor_tensor", out=t[:, 0:n], in0=py[:, 0:n], in1=g[:, 0:n], op=ALU.mult)
                    cx.op(PEL, "tensor_tensor", out=dst, in0=acc.s(sb_)[:, mc, b0:b0 + n], in1=t[:, 0:n], op=ALU.add)
                j += 1
    cx.pop()


def final(k, s, l, acc, last):
    cx = k.cx
    cx.push()
    w0 = wload(k, k.w_out[l][:, 0:512], 8, 512)
    w1 = wload(k, k.w_out[l][:, 512:1024], 8, 512)
    gb = [cx.sb((128, D), F32) for _ in range(2)]
    cx.dma(SP, gb[0][:], V(k.gscr, k.gscr.t[l, 4].partition_broadcast(128), [l]))
    cx.dma(SP, gb[1][:], V(k.gscr, k.gscr.t[l, s].partition_broadcast(128), [l]))
    xo = [cx.sb((128, D), F32) for _ in range(2)]
    tm = [cx.sb((128, 512), F32) for _ in range(2)]
    for tt in range(2 if last else 0, NT):
        x_ = xo[tt % 2]
        cx.dma(SP, x_[:], k.xres[s].s(tt)[tt * 128:(tt + 1) * 128, :])
        g = gb[0] if tt < 2 else gb[1]
        for nh, w in enumerate((w0, w1)):
            p = nps(k)
            for kc in range(8):
                cx.mm(p[:, :], acc.s(tt)[:, kc, tt * 128:(tt + 1) * 128], w[:, kc, :], start=(kc == 0), stop=(kc == 7))
            t = tm[nh]
            cx.op(DVE, "tensor_tensor", out=t[:], in0=p[:, :], in1=g[:, nh * 512:(nh + 1) * 512], op=ALU.mult)
            cx.op(PEL if nh else DVE, "tensor_tensor", out=x_[:, nh * 512:(nh + 1) * 512], in0=x_[:, nh * 512:(nh + 1) * 512], in1=t[:], op=ALU.add)
        if k.dbg and s == 0 and l == 0:
            cx.dma(SP, U(k.dbg_x[tt * 128:(tt + 1) * 128, :]), x_[:])
        if last:
            cx.dma(SP, U(k.out[s, (tt - 2) * 128:(tt - 1) * 128, :]), x_[:])
        else:
            cx.dma(SP, k.xres[s].s(tt)[tt * 128:(tt + 1) * 128, :], x_[:])
    cx.pop()


def attn_core(k, nheads, nmaps, qk_fn, v_fn, out_fn, last, blk_fn=None):
    cx = k.cx
    cx.push()
    pts = [cx.sb((128, 512), BF16) for _ in range(4)]
    stage = [cx.sb((128, 129), F32) for _ in range(4 * nmaps)] if nmaps == 2 else None
    pj = 0
    sbank = [k.ps[0], k.ps[1]]
    abanks = [k.ps[2], k.ps[3], k.ps[4], k.ps[5]]
    sj = 0
    for qb, (q0, nq) in enumerate(BLKS):
        if last and qb == 0:
            continue
        kts = [0, 1] if qb == 0 else list(range(NT))
        nqt = nq // 128
        if blk_fn is not None:
            blk_fn(qb, q0, nq)
        for h in range(nheads):
            def accv(m, qi):
                idx = m * 4 + qi
                b = abanks[idx // 2] if nmaps == 2 else abanks[qi // 2]
                o = (idx % 2) * 129
                return b, V(b, b.t[:, o:o + 129], None)
            started = set()
            pend = []

            def do_pv(kt_, items):
                vv = v_fn(h, kt_)
                for (m, pt) in items:
                    for qi in range(nqt):
                        b, av = accv(m, qi)
                        first = id(b) not in started
                        started.add(id(b))
                        cx.mm(av, pt[:, qi * 128:(qi + 1) * 128], vv, start=first, stop=(kt_ == kts[-1]))

            for kt in kts:
                items = []
                for m in range(nmaps):
                    sp_ = sbank[sj % 2]; sj += 1
                    qk_fn(h, m, kt, q0, nq, sp_)
                    pt = pts[pj % 4]; pj += 1
                    cx.act(pt[:, 0:nq], sp_[:, 0:nq], AF.Exp)
                    items.append((m, pt))
                if pend:
                    do_pv(*pend.pop())
                pend.append((kt, items))
            do_pv(*pend.pop())
            stg = []
            for qi in range(nqt):
                row = []
                for m in range(nmaps):
                    if stage is None:
                        row.append(accv(m, qi)[1])
                        continue
                    st_ = stage[m * 4 + qi]
                    cx.act(st_[:, :], accv(m, qi)[1], AF.Copy)
                    row.append(st_[:, :])
                stg.append(row)
            for qi in range(nqt):
                out_fn(h, qb, q0 + qi * 128, stg[qi])
    cx.pop()


def mla(k, s, l, hT, acc, dc, last):
    cx = k.cx
    cx.push()
    qn = cx.sb((128, 4, T), BF16, NT); qr = cx.sb((128, 2, T), BF16, NT)
    kn = cx.sb((128, 4, T), BF16, NT); kr2 = cx.sb((128, T), BF16, NT)
    vaug = cx.sb((128, NT, 4, 129), BF16, NT)
    _memset(cx, vaug[:], 1.0)
    W = k.w_in[l]
    cx.push()
    cqn = cx.sb((128, 3, T), BF16, NT)
    tmp = mk_tmp(cx)
    wA = wload(k, W[:, 0:384], 8, 384)
    wq1 = wload(k, k.wuq[l][:, 0:512], 3, 512)
    wq2 = wload(k, k.wuq[l][:, 512:768], 3, 256)
    for (b0, n) in BLKS:
        sb_ = tk(b0, b0 + n)
        pss = []
        for c in range(3):
            p = nps(k)
            for kc in range(8):
                cx.mm(p[:, 0:n], wA[:, kc, c * 128:(c + 1) * 128], hT.s(sb_)[:, kc, b0:b0 + n], start=(kc == 0), stop=(kc == 7))
            pss.append(p)
        ksub = int(os.environ.get("KSUB", "9"))
        if ksub < 1:
            for c in range(3):
                cx.op(DVE, "tensor_copy", out=cqn.s(sb_)[:, c, b0:b0 + n], in_=pss[c][:, 0:n])
            continue
        norm_group(k, pss, n, k.onesb, 384 * EPS, [dc[:, DC_CQG + c:DC_CQG + c + 1] for c in range(3)],
                   [cqn.s(sb_)[:, c, b0:b0 + n] for c in range(3)], tmp)
        if ksub < 2:
            continue
        for h in range(4):
            p = nps(k)
            for kc in range(3):
                kv = os.environ.get("KV", "0")
                lw = wA if kv == "2" else wq1
                rr = hT if kv == "1" else cqn
                cx.mm(p[:, 0:n], lw[:, kc, h * 128:(h + 1) * 128], rr.s(sb_)[:, kc, b0:b0 + n], start=(kc == 0), stop=(kc == 2))
            if os.environ.get("KQ", "1") == "0":
                cx.op(DVE, "tensor_copy", out=qn.s(sb_)[:, h, b0:b0 + n], in_=p[:, 0:n])
            else:
                norm_group(k, [p], n, k.onesb, 128 * EPS, [dc[:, DC_QGN:DC_QGN + 1]], [qn.s(sb_)[:, h, b0:b0 + n]], tmp)
        if ksub < 3:
            continue
        for c in range(2):
            p = nps(k)
            for kc in range(3):
                cx.mm(p[:, 0:n], wq2[:, kc, c * 128:(c + 1) * 128], cqn.s(sb_)[:, kc, b0:b0 + n], start=(kc == 0), stop=(kc == 2))
            norm_group(k, [p], n, k.bd64, 64 * EPS, [dc[:, DC_QGR:DC_QGR + 1]], [tmp["xn"][:, 0:n]], tmp)
            rope(k, tmp["xn"][:, 0:n], qr.s(sb_)[:, c, b0:b0 + n], b0, n, tmp)
    cx.pop()
    stg = int(os.environ.get("KSTAGE", "9"))
    if stg < 4:
        cx.pop(); return
    cx.push()
    ckvn = cx.sb((128, 2, T), BF16, NT)
    tmp = mk_tmp(cx)
    wB = wload(k, W[:, 384:704], 8, 320)
    wk1 = wload(k, k.wukv[l][:, 0:512], 2, 512)
    for (b0, n) in BLKS:
        sb_ = tk(b0, b0 + n)
        pss = []
        for c in range(2):
            p = nps(k)
            for kc in range(8):
                cx.mm(p[:, 0:n], wB[:, kc, c * 128:(c + 1) * 128], hT.s(sb_)[:, kc, b0:b0 + n], start=(kc == 0), stop=(kc == 7))
            pss.append(p)
        norm_group(k, pss, n, k.onesb, 256 * EPS, [dc[:, DC_CKVG + c:DC_CKVG + c + 1] for c in range(2)],
                   [ckvn.s(sb_)[:, c, b0:b0 + n] for c in range(2)], tmp)
        p = nps(k)
        for r0 in (0, 64):
            for kc in range(8):
                cx.mm(p[r0:r0 + 64, 0:n], wB[:, kc, 256:320], hT.s(sb_)[:, kc, b0:b0 + n], start=(kc == 0), stop=(kc == 7))
        norm_group(k, [p], n, k.bd64, 64 * EPS, [dc[:, DC_KGR:DC_KGR + 1]], [tmp["xn"][:, 0:n]], tmp)
        rope(k, tmp["xn"][:, 0:n], kr2.s(sb_)[:, b0:b0 + n], b0, n, tmp)
        for h in range(4):
            p = nps(k)
            for kc in range(2):
                cx.mm(p[:, 0:n], wk1[:, kc, h * 128:(h + 1) * 128], ckvn.s(sb_)[:, kc, b0:b0 + n], start=(kc == 0), stop=(kc == 1))
            norm_group(k, [p], n, k.onesb, 128 * EPS, [dc[:, DC_KGN:DC_KGN + 1]], [kn.s(sb_)[:, h, b0:b0 + n]], tmp)
    wv1 = wload(k, k.wukv[l][:, 512:1024], 2, 512)
    for tt in range(NT):
        p = nps(k)
        for kc in range(2):
            cx.mm(p[:, :], ckvn.s(tt)[:, kc, tt * 128:(tt + 1) * 128], wv1[:, kc, :], start=(kc == 0), stop=(kc == 1))
        cx.op(DVE, "tensor_copy", out=vaug.s(tt)[:, tt, :, 0:128], in_=V(p, p.t[:, :].rearrange("p (h e) -> p h e", e=128), None))
    cx.pop()
    if stg < 5:
        cx.pop(); return
    if k.dbg and s == 0 and l == 0:
        dumpv(k, "d_kr2", kr2[:, :], T)
        dumpv(k, "d_qr0", qr[:, 0, :], T)
        dumpv(k, "d_qn1", qn[:, 1, :], T)
        dumpv(k, "d_kn1", kn[:, 1, :], T)
    y = cx.sb((128, 4, T), BF16, NT)
    sz = cx.sb((128, 4, 512), BF16)
    wz = wload(k, W[:, OFF["mla_z"]:OFF["mla_z"] + 512], 8, 512)

    def blk_fn(qb, b0, n):
        for c in range(4):
            p = k.ps[6 + c % 2]
            for kc in range(8):
                cx.mm(p[:, 0:n], wz[:, kc, c * 128:(c + 1) * 128], hT.s(tk(b0, b0 + n))[:, kc, b0:b0 + n], start=(kc == 0), stop=(kc == 7))
            cx.act(sz[:, c, 0:n], p[:, 0:n], AF.Silu)

    ob = [cx.sb((128, 128), BF16) for _ in range(2)]
    rc = [cx.sb((128, 1), F32) for _ in range(2)]
    oj = [0]

    def qk_fn(h, m, kt, q0, nq, sp_):
        r0 = (h % 2) * 64
        cx.mm(sp_[:, 0:nq], kn.s(kt)[:, h, kt * 128:(kt + 1) * 128], qn.s(tk(q0, q0 + nq))[:, h, q0:q0 + nq], start=True, stop=False)
        cx.mm(sp_[:, 0:nq], kr2.s(kt)[r0:r0 + 64, kt * 128:(kt + 1) * 128], qr.s(tk(q0, q0 + nq))[r0:r0 + 64, h // 2, q0:q0 + nq], start=False, stop=True)

    def v_fn(h, kt):
        return vaug.s(kt)[:, kt, h, :]

    def out_fn(h, qb, q0, accs):
        a = accs[0]
        j = oj[0]; oj[0] += 1
        o_, r_ = ob[j % 2], rc[j % 2]
        cx.op(DVE, "reciprocal", out=r_[:], in_=V(a.tile, a.ap[:, 128:129], None))
        cx.op(DVE, "tensor_scalar", out=o_[:], in0=V(a.tile, a.ap[:, 0:128], None), scalar1=r_[:, 0:1], scalar2=None, op0=ALU.mult)
        p = k.ps[6 + j % 2]
        pb = V(p, p.t[:].bitcast(BF16)[:, 0:128], None)
        cx.tr(pb, o_[:], k.identb[:])
        tt = q0 // 128
        qoff = q0 - BLKS[qb][0]
        cx.op(DVE, "tensor_tensor", out=y.s(tt)[:, h, q0:q0 + 128], in0=pb, in1=sz[:, h, qoff:qoff + 128], op=ALU.mult)

    attn_core(k, 4, 1, qk_fn, v_fn, out_fn, last, blk_fn)
    if k.dbg and s == 0 and l == 0:
        dump(k, y, k.dbg_y[0], 4)
    if stg < 6:
        cx.pop(); return
    merge(k, l, 0, y, hT, acc, BLKS[1:] if last else BLKS)
    cx.pop()


def sz_block(k, wz, hT, sz, b0, n):
    cx = k.cx
    for c in range(4):
        p = k.ps[6 + c % 2]
        for kc in range(8):
            cx.mm(p[:, 0:n], wz[:, kc, c * 128:(c + 1) * 128], hT.s(tk(b0, b0 + n))[:, kc, b0:b0 + n], start=(kc == 0), stop=(kc == 7))
        cx.act(sz[:, c, 0:n], p[:, 0:n], AF.Silu)


def proj_tm(k, hT, w, dst_fn, ncols=512):
    cx = k.cx
    for tt in range(NT):
        p = nps(k)
        for kc in range(8):
            cx.mm(p[:, 0:ncols], hT.s(tt)[:, kc, tt * 128:(tt + 1) * 128], w[:, kc, 0:ncols], start=(kc == 0), stop=(kc == 7))
        dst_fn(tt, p)


def diffattn(k, s, l, hT, acc, dc, last):
    cx = k.cx
    cx.push()
    qd = cx.sb((128, 4, T), BF16, NT); kd = cx.sb((128, 4, T), BF16, NT)
    vaug = cx.sb((128, NT, 4, 129), BF16, NT)
    _memset(cx, vaug[:], 1.0)
    W = k.w_in[l]
    cx.push()
    tmp = mk_tmp(cx, 1)
    for (dst, off, gcol) in ((qd, OFF["df_q"], DC_DFQG), (kd, OFF["df_k"], DC_DFKG)):
        w = wload(k, W[:, off:off + 512], 8, 512)
        for (b0, n) in BLKS:
            sb_ = tk(b0, b0 + n)
            for h in range(4):
                p = nps(k)
                for kc in range(8):
                    cx.mm(p[:, 0:n], w[:, kc, h * 128:(h + 1) * 128], hT.s(sb_)[:, kc, b0:b0 + n], start=(kc == 0), stop=(kc == 7))
                norm_group(k, [p], n, k.bd64, 64 * EPS, [dc[:, gcol:gcol + 1]], [tmp["xn"][:, 0:n]], tmp)
                rope(k, tmp["xn"][:, 0:n], dst.s(sb_)[:, h, b0:b0 + n], b0, n, tmp)
    wv = wload(k, W[:, OFF["df_v"]:OFF["df_v"] + 512], 8, 512)
    proj_tm(k, hT, wv, lambda tt, p: cx.op(DVE, "tensor_copy", out=vaug.s(tt)[:, tt, :, 0:128],
                                             in_=V(p, p.t[:, :].rearrange("p (h e) -> p h e", e=128), None)))
    cx.pop()
    y = cx.sb((128, 4, T), BF16, NT)
    sz = cx.sb((128, 4, 512), BF16)
    wz = wload(k, W[:, OFF["df_z"]:OFF["df_z"] + 512], 8, 512)
    o1 = [cx.sb((128, 128), F32) for _ in range(2)]
    o2 = [cx.sb((128, 128), F32) for _ in range(2)]
    ob = [cx.sb((128, 128), BF16) for _ in range(2)]
    junk = cx.sb((128, 128), BF16)
    rc = [cx.sb((128, 4), F32) for _ in range(2)]
    oj = [0]

    def qk_fn(h, m, kt, q0, nq, sp_):
        r0 = m * 64
        cx.mm(sp_[:, 0:nq], kd.s(kt)[r0:r0 + 64, h, kt * 128:(kt + 1) * 128], qd.s(tk(q0, q0 + nq))[r0:r0 + 64, h, q0:q0 + nq], start=True, stop=True)

    def v_fn(h, kt):
        return vaug.s(kt)[:, kt, h, :]

    def out_fn(h, qb, q0, accs):
        a0, a1 = accs
        j = oj[0]; oj[0] += 1
        r_ = rc[j % 2]
        cx.op(DVE, "reciprocal", out=r_[:, 0:1], in_=V(a0.tile, a0.ap[:, 128:129], None))
        cx.op(DVE, "reciprocal", out=r_[:, 1:2], in_=V(a1.tile, a1.ap[:, 128:129], None))
        cx.op(DVE, "tensor_tensor", out=r_[:, 1:2], in0=r_[:, 1:2], in1=dc[:, DC_LAM:DC_LAM + 1], op=ALU.mult)
        cx.op(DVE, "tensor_scalar", out=o1[j % 2][:], in0=V(a0.tile, a0.ap[:, 0:128], None), scalar1=r_[:, 0:1], scalar2=None, op0=ALU.mult)
        cx.op(DVE, "scalar_tensor_tensor", out=o2[j % 2][:], in0=V(a1.tile, a1.ap[:, 0:128], None), scalar=r_[:, 1:2], in1=o1[j % 2][:], op0=ALU.mult, op1=ALU.add)
        cx.act(junk[:], o2[j % 2][:], AF.Square, accum_out=r_[:, 2:3])
        cx.act(r_[:, 3:4], r_[:, 2:3], AF.Sqrt, scale=1.0 / 128, bias=k.epsc[:, 0:1])
        cx.op(DVE, "reciprocal", out=r_[:, 3:4], in_=r_[:, 3:4])
        cx.op(DVE, "tensor_scalar", out=ob[j % 2][:], in0=o2[j % 2][:], scalar1=r_[:, 3:4], scalar2=None, op0=ALU.mult)
        p = k.ps[6 + j % 2]
        pb = V(p, p.t[:].bitcast(BF16)[:, 0:128], None)
        cx.tr(pb, ob[j % 2][:], k.identb[:])
        tt = q0 // 128
        qoff = q0 - BLKS[qb][0]
        cx.op(DVE, "scalar_tensor_tensor", out=y.s(tt)[:, h, q0:q0 + 128], in0=pb, scalar=dc[:, DC_DFOG:DC_DFOG + 1], in1=sz[:, h, qoff:qoff + 128], op0=ALU.mult, op1=ALU.mult)

    attn_core(k, 4, 2, qk_fn, v_fn, out_fn, last, lambda qb, b0, n: sz_block(k, wz, hT, sz, b0, n))
    if k.dbg and s == 0 and l == 0:
        dump(k, y, k.dbg_y[3], 4)
    merge(k, l, 3, y, hT, acc, BLKS[1:] if last else BLKS)
    cx.pop()


def out_stage(k, l, i, hT, osrc_fn, y, dc, gcol0, zoff, last, extra_fn=None):
    cx = k.cx
    cx.push()
    sz = cx.sb((128, 4, 512), BF16)
    wz = wload(k, k.w_in[l][:, zoff:zoff + 512], 8, 512)
    on = [cx.sb((128, 512), BF16) for _ in range(2)]
    junk = cx.sb((128, 128), BF16)
    ss = [cx.sb((128, 4), F32) for _ in range(2)]
    for (b0, n) in (BLKS[1:] if last else BLKS):
        sz_block(k, wz, hT, sz, b0, n)
        for tt in tk(b0, b0 + n):
            src = osrc_fn(tt)
            s_, o_ = ss[tt % 2], on[tt % 2]
            for h in range(4):
                cx.act(junk[:], V(src.tile, src.ap[:, h * 128:(h + 1) * 128], src.subs), AF.Square, accum_out=s_[:, h:h + 1])
            cx.act(s_[:], s_[:], AF.Sqrt, scale=1.0 / 128, bias=k.epsc[:, 0:1])
            cx.op(DVE, "reciprocal", out=s_[:], in_=s_[:])
            for h in range(4):
                cx.op(DVE, "tensor_scalar", out=o_[:, h * 128:(h + 1) * 128], in0=V(src.tile, src.ap[:, h * 128:(h + 1) * 128], src.subs), scalar1=s_[:, h:h + 1], scalar2=None, op0=ALU.mult)
            p = nps(k)
            pb = p.t[:].bitcast(BF16)
            for h in range(4):
                cx.tr(V(p, pb[:, h * 128:(h + 1) * 128], None), o_[:, h * 128:(h + 1) * 128], k.identb[:])
            t0 = tt * 128
            for h in range(4):
                pv = V(p, pb[:, h * 128:(h + 1) * 128], None)
                if extra_fn is None:
                    cx.op(DVE, "scalar_tensor_tensor", out=y.s(tt)[:, h, t0:t0 + 128], in0=pv, scalar=dc[:, gcol0 + h:gcol0 + h + 1], in1=sz[:, h, t0 - b0:t0 - b0 + 128], op0=ALU.mult, op1=ALU.mult)
                else:
                    extra_fn(tt, h, pv, sz[:, h, t0 - b0:t0 - b0 + 128])
    cx.pop()


def gla(k, s, l, hT, acc, dc, last):
    cx = k.cx
    cx.push()
    W = k.w_in[l]
    ogl = cx.sb((128, NT, 512), BF16, NT)
    cx.push()
    qT = cx.sb((128, 2, T), BF16, NT); kT = cx.sb((128, 2, T), BF16, NT)
    afT = cx.sb((16, 2, T), BF16, NT)
    vtm = cx.sb((128, NT, 512), BF16, NT)
    aw = cx.sb((16, 2, 256), BF16)
    cx.dma(POOL, aw[:], U(k.gla_a_w[l].rearrange("d r n -> r d n")))
    wqk = wload(k, W[:, OFF["gla_q"]:OFF["gla_q"] + 512], 8, 512)
    waf = wload(k, W[:, OFF["gla_af"]:OFF["gla_af"] + 32], 8, 32)
    for (b0, n) in BLKS:
        sb_ = tk(b0, b0 + n)
        for c in range(4):
            p = nps(k)
            for kc in range(8):
                cx.mm(p[:, 0:n], wqk[:, kc, c * 128:(c + 1) * 128], hT.s(sb_)[:, kc, b0:b0 + n], start=(kc == 0), stop=(kc == 7))
            if c < 2:
                cx.act(qT.s(sb_)[:, c, b0:b0 + n], p[:, 0:n], AF.Copy, scale=0.125)
            else:
                cx.op(DVE, "tensor_copy", out=kT.s(sb_)[:, c - 2, b0:b0 + n], in_=p[:, 0:n])
        for d in range(2):
            p = nps(k)
            for kc in range(8):
                cx.mm(p[0:16, 0:n], waf[:, kc, d * 16:(d + 1) * 16], hT.s(sb_)[:, kc, b0:b0 + n], start=(kc == 0), stop=(kc == 7))
            cx.op(DVE, "tensor_copy", out=afT.s(sb_)[0:16, d, b0:b0 + n], in_=p[0:16, 0:n])
    wv = wload(k, W[:, OFF["gla_v"]:OFF["gla_v"] + 512], 8, 512)
    proj_tm(k, hT, wv, lambda tt, p: cx.act(vtm.s(tt)[:, tt, :], p[:, :], AF.Copy))
    S32_ = [cx.sb((128, 2, 128), F32) for _ in range(2)]; Sb_ = [cx.sb((128, 2, 128), BF16) for _ in range(2)]
    R = lambda shape, dt, nb=4: [cx.sb(shape, dt) for _ in range(nb)]
    e_ = R((128, 2, 128), F32); lp_ = R((128, 2, 128), F32); Bp_ = R((128, 2, 128), F32); tR_ = R((128, 2, 128), F32)
    E_ = R((128, 2, 128), BF16, 6); qe_ = R((128, 2, 128), BF16); ke_ = R((128, 2, 128), BF16); kl_ = R((128, 2, 128), BF16)
    bt_ = R((128, 2, 4), F32); kltm_ = R((128, 256), BF16); AT_ = R((128, 128), BF16, 4)
    orders = [list(range(NT)), [1, 0] + list(range(NT - 1, 1, -1))]
    masks = [k.trifb, k.tribb]
    written = set()
    if True:
        def prep(d, tt, j):
            t0 = tt * 128
            j = 2 * d + j
            e, lp, Bp, tR, qe, ke, kl, bt, kltm = e_[j], lp_[j], Bp_[j], tR_[j], qe_[j], ke_[j], kl_[j], bt_[j], kltm_[j]
            pl = nps(k)
            for c in range(2):
                cx.mm(pl[:, c * 128:(c + 1) * 128], aw[0:16, d, c * 128:(c + 1) * 128], afT.s(tt)[0:16, d, t0:t0 + 128])
            for c in range(2):
                cx.act(e[:, c, :], pl[:, c * 128:(c + 1) * 128], AF.Exp, scale=-1.0, bias=dc[:, DC_NAB + 2 * d + c:DC_NAB + 2 * d + c + 1])
            cx.act(lp[:], e[:], AF.Ln, bias=k.onec[:, 0:1])
            for c in range(2):
                cx.op(DVE, "tensor_tensor_scan", out=Bp[:, c, :], data0=k.onesf[:, 0:128], data1=lp[:, c, :], initial=0.0, op0=ALU.mult, op1=ALU.add)
            E1, E2, E3 = E_[3 * d], E_[3 * d + 1], E_[3 * d + 2]
            if d == 0:
                cx.op(DVE, "tensor_scalar", out=bt[:, :, 0:1], in0=Bp[:, :, 127:128], scalar1=-1.0 / 16, scalar2=None, op0=ALU.mult)
                cx.act(E1[:], Bp[:], AF.Exp, scale=-1.0 / 16)
                cx.act(E2[:], Bp[:], AF.Exp, scale=1.0 / 16)
                for c in range(2):
                    cx.act(E3[:, c, :], Bp[:, c, :], AF.Exp, scale=1.0 / 16, bias=bt[:, c, 0:1])
            else:
                cx.op(DVE, "tensor_tensor", out=tR[:], in0=lp[:], in1=Bp[:], op=ALU.subtract)
                cx.op(DVE, "tensor_scalar", out=bt[:, :, 0:1], in0=Bp[:, :, 127:128], scalar1=-1.0 / 16, scalar2=None, op0=ALU.mult)
                cx.op(DVE, "tensor_scalar", out=bt[:, :, 1:2], in0=Bp[:, :, 127:128], scalar1=1.0 / 16, scalar2=None, op0=ALU.mult)
                for c in range(2):
                    cx.act(E1[:, c, :], tR[:, c, :], AF.Exp, scale=-1.0 / 16, bias=bt[:, c, 0:1])
                    cx.act(E2[:, c, :], tR[:, c, :], AF.Exp, scale=1.0 / 16, bias=bt[:, c, 1:2])
                cx.act(E3[:], tR[:], AF.Exp, scale=1.0 / 16)
            cx.act(bt[:, :, 2:3], bt[:, :, 0:1], AF.Exp)
            cx.op(DVE, "tensor_tensor", out=qe[:], in0=qT.s(tt)[:, :, t0:t0 + 128], in1=E1[:], op=ALU.mult)
            cx.op(DVE, "tensor_tensor", out=ke[:], in0=kT.s(tt)[:, :, t0:t0 + 128], in1=E2[:], op=ALU.mult)
            cx.op(DVE, "tensor_tensor", out=kl[:], in0=kT.s(tt)[:, :, t0:t0 + 128], in1=E3[:], op=ALU.mult)
            ptr = nps(k)
            pb = ptr.t[:].bitcast(BF16)
            for c in range(2):
                cx.tr(V(ptr, pb[:, c * 128:(c + 1) * 128], None), kl[:, c, :], k.identb[:])
            cx.op(DVE, "tensor_copy", out=kltm[:], in_=V(ptr, pb[:, 0:256], None))

        def body(d, tt, j):
            j = 2 * d + j
            mask, S32, Sb = masks[d], S32_[d], Sb_[d]
            qe, ke, bt, kltm = qe_[j], ke_[j], bt_[j], kltm_[j]
            po = nps(k)
            for h in range(4):
                c, r0 = h // 2, (h % 2) * 64
                pa = nps(k)
                cx.mm(pa[:, 0:128], ke[r0:r0 + 64, c, :], qe[r0:r0 + 64, c, :])
                AT = AT_[h % 4]
                cx.op(DVE, "tensor_tensor", out=AT[:], in0=pa[:, 0:128], in1=mask[:], op=ALU.mult)
                cx.mm(po[:, h * 128:(h + 1) * 128], AT[:], vtm.s(tt)[:, tt, h * 128:(h + 1) * 128], start=True, stop=False)
                cx.mm(po[:, h * 128:(h + 1) * 128], qe[r0:r0 + 64, c, :], Sb[r0:r0 + 64, c, :], start=False, stop=True)
            pu = nps(k)
            for h in range(4):
                c, r0 = h // 2, (h % 2) * 64
                cx.mm(pu[r0:r0 + 64, c * 128:(c + 1) * 128], kltm[:, h * 64:(h + 1) * 64], vtm.s(tt)[:, tt, h * 128:(h + 1) * 128], start=True, stop=True)
            if tt not in written:
                written.add(tt)
                cx.act(ogl.s(tt)[:, tt, :], po[:, :], AF.Copy)
            else:
                cx.op(DVE, "tensor_tensor", out=ogl.s(tt)[:, tt, :], in0=po[:, :], in1=ogl.s(tt)[:, tt, :], op=ALU.add)
            for c in range(2):
                cx.op(DVE, "scalar_tensor_tensor", out=S32[:, c, :], in0=S32[:, c, :], scalar=bt[:, c, 2:3], in1=pu[:, c * 128:(c + 1) * 128], op0=ALU.mult, op1=ALU.add)
            cx.act(Sb[:], S32[:], AF.Copy)

        for d in range(2):
            _memset(cx, S32_[d][:], 0.0); _memset(cx, Sb_[d][:], 0.0)
            prep(d, orders[d][0], 0)
        for i in range(NT):
            for d in range(2):
                if i + 1 < NT:
                    prep(d, orders[d][i + 1], (i + 1) % 2)
                body(d, orders[d][i], i % 2)
    cx.pop()
    y = cx.sb((128, 4, T), BF16, NT)
    out_stage(k, l, 1, hT, lambda tt: ogl.s(tt)[:, tt, :], y, dc, DC_GLAOG, OFF["gla_z"], last)
    if k.dbg and s == 0 and l == 0:
        dump(k, y, k.dbg_y[1], 4)
    merge(k, l, 1, y, hT, acc, BLKS[1:] if last else BLKS)
    cx.pop()


def mlstm(k, s, l, hT, acc, dc, pr, last):
    cx = k.cx
    cx.push()
    W = k.w_in[l]
    xcT = cx.sb((128, 4, T), BF16, NT)
    hml = cx.sb((128, NT, 512), BF16, NT)
    cx.push()
    xraw = cx.sb((128, 4, T), BF16, NT)
    cacc = cx.sb((128, T), F32)
    wx = wload(k, W[:, OFF["ml_x"]:OFF["ml_x"] + 512], 8, 512)
    for (b0, n) in BLKS:
        sb_ = tk(b0, b0 + n)
        for c in range(4):
            p = nps(k)
            for kc in range(8):
                cx.mm(p[:, 0:n], wx[:, kc, c * 128:(c + 1) * 128], hT.s(sb_)[:, kc, b0:b0 + n], start=(kc == 0), stop=(kc == 7))
            if c % 2:
                cx.act(xraw.s(sb_)[:, c, b0:b0 + n], p[:, 0:n], AF.Copy)
            else:
                cx.op(DVE, "tensor_copy", out=xraw.s(sb_)[:, c, b0:b0 + n], in_=p[:, 0:n])
    pc = k.pc
    for c in range(4):
        w0, w1, w2 = [pc[:, l, PC_CONVW + 4 * j + c:PC_CONVW + 4 * j + c + 1] for j in range(3)]
        cx.op(DVE, "tensor_scalar", out=cacc[:], in0=xraw[:, c, :], scalar1=w1, scalar2=pc[:, l, PC_CONVB + c:PC_CONVB + c + 1], op0=ALU.mult, op1=ALU.add)
        for (a_, b_) in ((0, NCTX), (NCTX, T)):
            cx.op(DVE, "scalar_tensor_tensor", out=cacc[:, a_ + 1:b_], in0=xraw[:, c, a_:b_ - 1], scalar=w0, in1=cacc[:, a_ + 1:b_], op0=ALU.mult, op1=ALU.add)
            cx.op(DVE, "scalar_tensor_tensor", out=cacc[:, a_:b_ - 1], in0=xraw[:, c, a_ + 1:b_], scalar=w2, in1=cacc[:, a_:b_ - 1], op0=ALU.mult, op1=ALU.add)
        cx.act(xcT[:, c, :], cacc[:], AF.Silu)
    cx.pop()
    cx.push()
    qT = cx.sb((128, 2, T), BF16, NT); kT = cx.sb((128, 2, T), BF16, NT)
    vaug = cx.sb((128, NT, 4, 129), BF16, NT)
    _memset(cx, vaug[:], 1.0)
    gt = cx.sb((128, NT, 16), F32); lpt = cx.sb((128, NT, 16), F32)
    wqb = cx.sb((128, 4, 64), BF16); wkb = cx.sb((128, 4, 64), BF16)
    cx.dma(POOL, wqb[:], U(k.ml_wq[l].rearrange("h c d -> c h d")))
    cx.dma(POOL, wkb[:], U(k.ml_wk[l].rearrange("h c d -> c h d")))
    for (b0, n) in BLKS:
        sb_ = tk(b0, b0 + n)
        for (dst, wb, scl) in ((qT, wqb, 1.0), (kT, wkb, 0.125)):
            for c in range(2):
                p = nps(k)
                for hh in range(2):
                    h = 2 * c + hh
                    cx.mm(p[hh * 64:(hh + 1) * 64, 0:n], wb[:, h, :], xcT.s(sb_)[:, h, b0:b0 + n], start=True, stop=True)
                cx.act(dst.s(sb_)[:, c, b0:b0 + n], p[:, 0:n], AF.Copy, scale=scl)
    wv = wload(k, W[:, OFF["ml_v"]:OFF["ml_v"] + 512], 8, 512)
    proj_tm(k, hT, wv, lambda tt, p: cx.op(DVE, "tensor_copy", out=vaug.s(tt)[:, tt, :, 0:128],
                                             in_=V(p, p.t[:, :].rearrange("p (h e) -> p h e", e=128), None)))
    wif = wload(k, W[:, OFF["ml_if"]:OFF["ml_if"] + 16], 8, 16)
    proj_tm(k, hT, wif, lambda tt, p: cx.op(DVE, "tensor_tensor", out=gt[:, tt, :], in0=p[:, 0:16], in1=pr[:, 0:16], op=ALU.add), ncols=16)
    cx.act(lpt[:], gt[:], AF.Exp, scale=-1.0)
    cx.act(lpt[:], lpt[:], AF.Ln, bias=k.onec[:, 0:1])
    Bp = cx.sb((128, 2, NT, 4), F32); Bt = cx.sb((128, 2, NT, 4), F32)
    fa = cx.sb((128, 2, NT, 4), F32); fg = cx.sb((128, 2, NT, 4), F32); fgk = cx.sb((128, 2, NT, 4), F32); fdec = cx.sb((128, 2, NT, 4), F32)
    for d in range(2):
        g0 = (1 + 2 * d) * 4
        rhs = lpt[:, :, g0:g0 + 4]
        p = nps(k)
        cx.mm(p[:, 0:72], (k.trif if d == 0 else k.trib)[:], rhs)
        cx.op(DVE, "tensor_copy", out=Bp[:, d], in_=V(p, p.t[:, 0:72].rearrange("p (a b) -> p a b", b=4), None))
        p = nps(k)
        cx.mm(p[:, 0:72], k.onesf[:], rhs)
        cx.op(DVE, "tensor_copy", out=Bt[:, d], in_=V(p, p.t[:, 0:72].rearrange("p (a b) -> p a b", b=4), None))
        li = gt[:, :, 8 * d:8 * d + 4]
        cx.act(fa[:, d], Bp[:, d], AF.Exp, scale=-1.0)
        cx.act(fdec[:, d], Bt[:, d], AF.Exp, scale=-1.0)
        cx.op(DVE, "tensor_tensor", out=Bp[:, d], in0=Bp[:, d], in1=li, op=ALU.add)
        cx.act(fg[:, d], Bp[:, d], AF.Exp)
        cx.op(DVE, "tensor_tensor", out=Bp[:, d], in0=Bp[:, d], in1=Bt[:, d], op=ALU.subtract)
        cx.act(fgk[:, d], Bp[:, d], AF.Exp)
    C32_ = [cx.sb((128, 2, 129), F32) for _ in range(2)]; Cb_ = [cx.sb((128, 2, 129), BF16) for _ in range(2)]
    PT_ = [cx.sb((128, 128), BF16) for _ in range(4)]
    kg_ = [cx.sb((128, 256), BF16) for _ in range(4)]
    dn_ = [cx.sb((128, 4), F32) for _ in range(4)]
    orders = [list(range(NT)), [1, 0] + list(range(NT - 1, 1, -1))]
    masks = [k.trifb, k.tribb]
    written = set()
    if True:
        def prep(d, tt, j):
            t0 = tt * 128
            kg = kg_[2 * d + j]
            pk = nps(k)
            for h in range(4):
                cx.mm(pk[:, h * 64:(h + 1) * 64], xcT.s(tt)[:, h, t0:t0 + 128], wkb[:, h, :], start=True, stop=True)
            for h in range(4):
                cx.op(DVE, "tensor_scalar", out=kg[:, h * 64:(h + 1) * 64], in0=pk[:, h * 64:(h + 1) * 64], scalar1=fgk[:, d, tt, h:h + 1], scalar2=0.125, op0=ALU.mult, op1=ALU.mult)

        def body(d, tt, j):
            t0 = tt * 128
            mask, C32, Cb = masks[d], C32_[d], Cb_[d]
            kg, dn = kg_[2 * d + j], dn_[2 * d + j]
            first = tt not in written
            written.add(tt)
            pos = [nps(k), nps(k)]
            for h in range(4):
                c, r0 = h // 2, (h % 2) * 64
                pa = nps(k)
                cx.mm(pa[:, 0:128], kT.s(tt)[r0:r0 + 64, c, t0:t0 + 128], qT.s(tt)[r0:r0 + 64, c, t0:t0 + 128])
                PT = PT_[h % 4]
                cx.op(DVE, "scalar_tensor_tensor", out=PT[:], in0=pa[:, 0:128], scalar=fg[:, d, tt, h:h + 1], in1=mask[:], op0=ALU.mult, op1=ALU.mult)
                o = (h % 2) * 129
                cx.mm(pos[c][:, o:o + 129], PT[:], vaug.s(tt)[:, tt, h, :], start=True, stop=False)
                cx.mm(pos[c][:, o:o + 129], qT.s(tt)[r0:r0 + 64, c, t0:t0 + 128], Cb[r0:r0 + 64, c, :], start=False, stop=True)
            pu = nps(k)
            for h in range(4):
                c, r0 = h // 2, (h % 2) * 64
                cx.mm(pu[r0:r0 + 64, c * 129:(c + 1) * 129], kg[:, h * 64:(h + 1) * 64], vaug.s(tt)[:, tt, h, :], start=True, stop=True)
            for c in range(2):
                po = pos[c]
                den = V(po, po.t[:, 0:258].rearrange("p (a b) -> p a b", b=129)[:, :, 128], None)
                cx.op(DVE, "tensor_tensor", out=dn[:, 0:2], in0=den, in1=fa[:, d, tt, 2 * c:2 * c + 2], op=ALU.mult)
                cx.act(dn[:, 0:2], dn[:, 0:2], AF.Abs)
                cx.op(DVE, "tensor_scalar", out=dn[:, 0:2], in0=dn[:, 0:2], scalar1=1.0, scalar2=None, op0=ALU.max)
                cx.op(DVE, "reciprocal", out=dn[:, 0:2], in_=dn[:, 0:2])
                cx.op(DVE, "tensor_tensor", out=dn[:, 2:4], in0=dn[:, 0:2], in1=fa[:, d, tt, 2 * c:2 * c + 2], op=ALU.mult)
                for hh in range(2):
                    h = 2 * c + hh
                    dst = hml.s(tt)[:, tt, h * 128:(h + 1) * 128]
                    if first:
                        cx.op(DVE, "tensor_scalar", out=dst, in0=po[:, hh * 129:hh * 129 + 128], scalar1=dn[:, 2 + hh:3 + hh], scalar2=None, op0=ALU.mult)
                    else:
                        cx.op(DVE, "scalar_tensor_tensor", out=dst, in0=po[:, hh * 129:hh * 129 + 128], scalar=dn[:, 2 + hh:3 + hh], in1=dst, op0=ALU.mult, op1=ALU.add)
            for h in range(4):
                c, r0 = h // 2, (h % 2) * 64
                cx.op(DVE, "scalar_tensor_tensor", out=C32[r0:r0 + 64, c, :], in0=C32[r0:r0 + 64, c, :], scalar=fdec[r0:r0 + 64, d, tt, h:h + 1], in1=pu[r0:r0 + 64, c * 129:(c + 1) * 129], op0=ALU.mult, op1=ALU.add)
            cx.act(Cb[:], C32[:], AF.Copy)

        for d in range(2):
            _memset(cx, C32_[d][:], 0.0); _memset(cx, Cb_[d][:], 0.0)
            prep(d, orders[d][0], 0)
        for i in range(NT):
            for d in range(2):
                if i + 1 < NT:
                    prep(d, orders[d][i + 1], (i + 1) % 2)
                body(d, orders[d][i], i % 2)
    cx.pop()
    y = cx.sb((128, 4, T), BF16, NT)
    cx.push()
    wo = wload(k, W[:, OFF["ml_o"]:OFF["ml_o"] + 512], 8, 512)
    sgo = [cx.sb((128, 512), BF16) for _ in range(2)]

    def og(tt, p):
        cx.act(sgo[tt % 2][:], p[:, :], AF.Sigmoid)
        cx.op(DVE, "tensor_tensor", out=hml.s(tt)[:, tt, :], in0=hml.s(tt)[:, tt, :], in1=sgo[tt % 2][:], op=ALU.mult)
    proj_tm(k, hT, wo, og)
    t1_ = [cx.sb((128, 128), F32) for _ in range(2)]
    t2_ = [cx.sb((128, 128), F32) for _ in range(2)]
    ej = [0]

    def extra(tt, h, pv, szv):
        j = ej[0] % 2; ej[0] += 1
        t0 = tt * 128
        cx.act(t1_[j][:], pv, AF.Identity, scale=dc[:, DC_MLOG + h:DC_MLOG + h + 1])
        cx.op(DVE, "scalar_tensor_tensor", out=t2_[j][:], in0=xcT.s(tt)[:, h, t0:t0 + 128], scalar=k.pc[:, l, PC_MLSKIP + h:PC_MLSKIP + h + 1], in1=t1_[j][:], op0=ALU.mult, op1=ALU.add)
        cx.op(DVE, "tensor_tensor", out=y.s(tt)[:, h, t0:t0 + 128], in0=t2_[j][:], in1=szv, op=ALU.mult)
    out_stage(k, l, 2, hT, lambda tt: hml.s(tt)[:, tt, :], y, dc, DC_MLOG, OFF["ml_z"], last, extra)
    cx.pop()
    if k.dbg and s == 0 and l == 0:
        dump(k, y, k.dbg_y[2], 4)
    merge(k, l, 2, y, hT, acc, BLKS[1:] if last else BLKS)
    cx.pop()


def dumpv(k, name, v, ncols):
    cx = k.cx
    dst = k.nc.dram_tensor(name, [128, ncols], F32, kind="ExternalOutput").ap()
    cx.push()
    f = cx.sb((128, 256), F32)
    for j in range(0, ncols, 256):
        w = min(256, ncols - j)
        cx.op(DVE, "tensor_copy", out=f[:, 0:w], in_=V(v.tile, v.ap[:, j:j + w], v.subs))
        cx.dma(SP, U(dst[:, j:j + w]), f[:, 0:w])
    cx.pop()


def dump(k, t, dst, nch):
    cx = k.cx
    cx.push()
    f = cx.sb((128, 256), F32)
    for c in range(nch):
        for j in range(T // 256):
            cx.op(DVE, "tensor_copy", out=f[:], in_=t[:, c, j * 256:(j + 1) * 256])
            cx.dma(SP, U(dst[:, c, j * 256:(j + 1) * 256]), f[:])
    cx.pop()


def layer(k, s, l, last):
    cx = k.cx
    cx.push()
    hT = cx.sb((128, 8, T), BF16, NT)
    acc = cx.sb((128, 8, T), BF16, NT)
    stg = int(os.environ.get("KSTAGE", "9"))
    if stg < 1:
        cx.pop(); return
    dc, pr = layer_params(k, l)
    if stg < 2:
        cx.pop(); return
    stage_h(k, s, l, hT)
    if stg < 3:
        if k.dbg:
            dump(k, hT, k.dbg_h, 8)
        cx.pop(); return
    if k.dbg and s == 0 and l == 0:
        dump(k, hT, k.dbg_h, 8)
    mla(k, s, l, hT, acc, dc, last)
    if stg < 7:
        cx.pop(); return
    mix = os.environ.get("KMIX", "123")
    if "1" in mix:
        gla(k, s, l, hT, acc, dc, last)
    if "2" in mix:
        mlstm(k, s, l, hT, acc, dc, pr, last)
    if "3" in mix:
        diffattn(k, s, l, hT, acc, dc, last)
    if k.dbg and s == 0 and l == 0:
        dump(k, acc, k.dbg_acc, 8)
    final(k, s, l, acc, last)
    cx.pop()


def _consts():
    ident = np.eye(128, dtype=np.float32)
    bd = np.zeros((128, 128), np.float32); bd[:64, :64] = 1; bd[64:, 64:] = 1
    rm = np.zeros((64, 64), np.float32)
    for i in range(16):
        rm[16 + i, i] = -1; rm[i, 16 + i] = 1; rm[48 + i, 32 + i] = -1; rm[32 + i, 48 + i] = 1
    rm2 = np.zeros((128, 128), np.float32); rm2[:64, :64] = rm; rm2[64:, 64:] = rm
    si, ti = np.meshgrid(np.arange(128), np.arange(128), indexing="ij")
    trif = (si <= ti).astype(np.float32); trib = (si >= ti).astype(np.float32)
    quarter = 16
    inv_freq = (10000.0 ** (-np.arange(quarter, dtype=np.float32) / quarter)).astype(np.float32)
    row = np.repeat(np.arange(32, dtype=np.float32), 64); col = np.tile(np.arange(64, dtype=np.float32), 32)
    ar = row[:, None] * inv_freq; ac = col[:, None] * inv_freq
    ang = np.concatenate([ar, ar, ac, ac], axis=-1).astype(np.float32)
    cos = np.concatenate([np.ones((NCTX, 64), np.float32), np.cos(ang)], 0)
    sin = np.concatenate([np.zeros((NCTX, 64), np.float32), np.sin(ang)], 0)
    cosT = np.ascontiguousarray(np.concatenate([cos.T, cos.T], 0)); sinT = np.ascontiguousarray(np.concatenate([sin.T, sin.T], 0))
    return dict(cident=ident, cbd64=bd, crm2=rm2, ctrif=trif, ctrib=trib, ccos=cosT.astype(np.float32), csin=sinT.astype(np.float32))


def _cols(v):
    v = np.asarray(v, np.float32).reshape(-1)
    return v.reshape(-1, 128).T


def _pack(inp):
    pcols = np.zeros((128, NL, NPC), np.float32)
    prow = np.zeros((NL, NPR), np.float32)
    for l in range(NL):
        P = pcols[:, l]
        P[:, PC_NORMG:PC_NORMG + 8] = _cols(inp["norm_g"][l])
        P[:, PC_CQG:PC_CQG + 3] = _cols(inp["mla_cq_g"][l]); P[:, PC_CKVG:PC_CKVG + 2] = _cols(inp["mla_ckv_g"][l])
        qg, kg = inp["mla_q_g"][l], inp["mla_k_g"][l]
        P[:, PC_QGN] = qg[:128]; P[:, PC_QGR] = np.concatenate([qg[128:], qg[128:]])
        P[:, PC_KGN] = kg[:128]; P[:, PC_KGR] = np.concatenate([kg[128:], kg[128:]])
        P[:, PC_DFQG] = np.concatenate([inp["df_qk_g"][l, 0]] * 2); P[:, PC_DFKG] = np.concatenate([inp["df_qk_g"][l, 1]] * 2)
        P[:, PC_DFOG] = inp["df_out_g"][l]
        P[:, PC_GLAOG:PC_GLAOG + 4] = _cols(inp["gla_out_g"][l]); P[:, PC_MLOG:PC_MLOG + 4] = _cols(inp["ml_out_g"][l])
        P[:, PC_MLSKIP:PC_MLSKIP + 4] = _cols(inp["ml_skip"][l])
        for j in range(3):
            P[:, PC_CONVW + 4 * j:PC_CONVW + 4 * j + 4] = _cols(inp["ml_conv_w"][l, j])
        P[:, PC_CONVB:PC_CONVB + 4] = _cols(inp["ml_conv_b"][l])
        for d_ in range(2):
            P[:, PC_GLAAB + 2 * d_:PC_GLAAB + 2 * d_ + 2] = _cols(inp["gla_a_b"][l, d_])
        prow[l, 0:16] = inp["ml_gate_b"][l].reshape(-1)
        prow[l, 16:272] = inp["df_lambda"][l].reshape(-1)
    return pcols, prow


def _perm_wuq(w):
    w = w.reshape(NL, 384, 4, 192)
    return np.ascontiguousarray(np.concatenate([w[..., :128].reshape(NL, 384, 512), w[..., 128:].reshape(NL, 384, 256)], -1))


def _perm_wukv(w):
    w = w.reshape(NL, 256, 4, 256)
    return np.ascontiguousarray(np.concatenate([w[..., :128].reshape(NL, 256, 512), w[..., 128:].reshape(NL, 256, 512)], -1))


_CACHE = {}


def host_maps(inp, ncores=8):
    inp = {k_: np.asarray(v, np.float32) for k_, v in inp.items()}
    pcols, prow = _pack(inp)
    shared = dict(ada_w=inp["ada_w"], ada_b=inp["ada_b"], w_in=inp["w_in"], wuq=_perm_wuq(inp["mla_wuq"]),
                  wukv=_perm_wukv(inp["mla_wukv"]), gla_a_w=inp["gla_a_w"], ml_wq=inp["ml_wq"], ml_wk=inp["ml_wk"],
                  br_w=inp["br_w"], w_out=inp["w_out"], pcols=pcols, prow=prow, **_consts())
    maps = []
    for c in range(ncores):
        m = dict(shared)
        m["x"] = np.ascontiguousarray(inp["x"][4 * c:4 * c + 4]); m["ctx"] = np.ascontiguousarray(inp["ctx"][4 * c:4 * c + 4])
        m["c5"] = np.ascontiguousarray(np.concatenate([inp["c"][4 * c:4 * c + 4], inp["c_ctx"][None]], 0))
        maps.append(m)
    return maps


def kernel(**inputs):
    if "nc" not in _CACHE:
        _CACHE["nc"] = build(4, NL)[0]
    nc = _CACHE["nc"]
    maps = host_maps(inputs)
    res = run_bass_kernel_spmd(nc, maps, core_ids=list(range(8)))
    return np.concatenate([np.asarray(r["out"], np.float32) for r in res.results], 0)
```

```python
import math
import contextlib
import numpy as np
import concourse.bass as bass
import concourse.mybir as mybir
from concourse.bass_utils import run_bass_kernel_spmd

F32 = mybir.dt.float32
BF16 = mybir.dt.bfloat16
AF = mybir.ActivationFunctionType
ALU = mybir.AluOpType
AX = mybir.AxisListType

T = 2304
NCTX = 256
NT = 18
D = 1024
DIN = 10992
NL = 4
EPS = 1e-6
BLKS = [(0, 256), (256, 512), (768, 512), (1280, 512), (1792, 512)]
OFF = dict(mla_cq=0, mla_ckv=384, mla_kr=640, mla_z=704, gla_q=1216, gla_k=1472, gla_v=1728, gla_af=2240,
           gla_ab=2256, gla_z=2272, ml_x=2784, ml_v=3296, ml_o=3808, ml_if=4320, ml_z=4336, df_q=4848,
           df_k=5360, df_v=5872, df_z=6384, merge=6896)
PC_NORMG, PC_CQG, PC_CKVG, PC_QGN, PC_QGR, PC_KGN, PC_KGR, PC_DFQG, PC_DFKG, PC_DFOG = 0, 8, 11, 13, 14, 15, 16, 17, 18, 19
PC_GLAOG, PC_MLOG, PC_MLSKIP, PC_CONVW, PC_CONVB, PC_GLAAB, NPC = 20, 24, 28, 32, 44, 48, 52
NPR = 16 + 256
DC_CQG, DC_CKVG, DC_QGN, DC_QGR, DC_KGN, DC_KGR, DC_DFQG, DC_DFKG, DC_DFOG, DC_GLAOG, DC_MLOG, DC_NAB, DC_LAM, NDC = 0, 3, 5, 6, 7, 8, 9, 10, 11, 12, 16, 20, 24, 28

import os
PE, ACT, DVE, POOL, SP = range(5)
SERIAL = os.environ.get("SERIAL", "0") == "1"
PEL = DVE if os.environ.get("NOPOOL", "0") == "1" else POOL
NDS = 24


class V:
    __slots__ = ("tile", "ap", "subs")

    def __init__(self, tile, ap, subs):
        self.tile, self.ap, self.subs = tile, ap, subs


class _Sel:
    def __init__(self, tile, subs):
        self.tile, self.subs = tile, subs

    def __getitem__(self, idx):
        return V(self.tile, self.tile.t[idx], self.subs)


class Tile:
    def __init__(self, t, nsub=1):
        self.t = t
        self.nsub = nsub
        self.w = [None] * nsub
        self.r = [dict() for _ in range(nsub)]
        self.excl = False

    def __getitem__(self, idx):
        return V(self, self.t[idx], None)

    def s(self, subs):
        if isinstance(subs, int):
            subs = [subs]
        return _Sel(self, list(subs))


FASTSE = os.environ.get("FASTSE", "1") == "1"


def _fsize(ap):
    n = 1
    for d_ in ap.shape[1:]:
        n *= d_
    return n


def tk(a, b):
    return list(range(a // 128, (b + 127) // 128))


class Ctx:
    def __init__(self, nc):
        self.nc = nc
        self.engs = [nc.tensor, nc.scalar, nc.vector, nc.gpsimd, nc.sync]
        self.es = contextlib.ExitStack()
        self.esem = [self.es.enter_context(nc.semaphore(f"e{i}")) for i in range(5)]
        self.dsem = [self.es.enter_context(nc.semaphore(f"d{i}")) for i in range(NDS)]
        self.cnt = [0] * 5
        self.dval = [0] * NDS
        self.dk = 0
        self.dkp = 0
        self.seen = [dict() for _ in range(5)]
        self.scopes = [self.es]
        self.scope_tiles = [[]]
        self.freed = {}
        self.uid = 0
        self.ninstr = 0
        self.last = None

    def push(self):
        st = contextlib.ExitStack()
        self.scopes.append(st)
        self.scope_tiles.append([])
        return st

    def pop(self):
        for tl in self.scope_tiles.pop():
            for s in range(tl.nsub):
                w = tl.w[s]
                if w is not None and self.freed.get(w[0], 0) < w[1]:
                    self.freed[w[0]] = w[1]
                for key, val in tl.r[s].items():
                    if self.freed.get(key, 0) < val:
                        self.freed[key] = val
        self.scopes.pop().close()

    def sb(self, shape, dt, nsub=1, name=None):
        self.uid += 1
        t = self.scopes[-1].enter_context(self.nc.sbuf_tensor(f"{name or 't'}{self.uid}", list(shape), dt))
        tl = Tile(t, nsub)
        if self.freed:
            for s in range(nsub):
                tl.r[s] = dict(self.freed)
        self.scope_tiles[-1].append(tl)
        return tl

    def psum(self, shape, dt):
        self.uid += 1
        t = self.scopes[-1].enter_context(self.nc.psum_tensor(f"ps{self.uid}", list(shape), dt))
        tl = Tile(t, 1)
        tl.excl = True
        return tl

    def _sync(self, eng, reads, writes, extra=None):
        need = {}

        def add(key, val):
            if key[0] == "E" and key[1] == eng and eng == PE:
                return
            if need.get(key, 0) < val:
                need[key] = val

        same = ("E", eng)
        fast = FASTSE and eng in (ACT, DVE)
        for v in reads:
            if v.tile is None:
                continue
            big = fast and _fsize(v.ap) >= 256
            for s in (v.subs if v.subs is not None else range(v.tile.nsub)):
                w = v.tile.w[s]
                if w is not None and not (big and w[0] == same):
                    add(w[0], w[1])
                if v.tile.excl:
                    for key, val in v.tile.r[s].items():
                        if not (key[0] == "E" and key[1] == eng):
                            add(key, val)
        for v in writes:
            if v.tile is None:
                continue
            for s in (v.subs if v.subs is not None else range(v.tile.nsub)):
                w = v.tile.w[s]
                if w is not None and not (fast and w[0] == same):
                    add(w[0], w[1])
                for key, val in v.tile.r[s].items():
                    if not (fast and key == same):
                        add(key, val)
        if extra is not None and extra[1] > 0:
            add(extra[0], extra[1])
        if SERIAL and self.last is not None:
            if not (self.last[0][0] == "E" and self.last[0][1] == eng and eng == PE):
                if need.get(self.last[0], 0) < self.last[1]:
                    need[self.last[0]] = self.last[1]
        seen = self.seen[eng]
        for key, val in need.items():
            if seen.get(key, 0) >= val:
                continue
            seen[key] = val
            sem = self.esem[key[1]] if key[0] == "E" else self.dsem[key[1]]
            self.engs[eng].wait_ge(sem, val)
            self.ninstr += 1

    def _commit(self, tok, reads, writes):
        key, val = tok
        self.last = tok
        for v in reads:
            if v.tile is None:
                continue
            for s in (v.subs if v.subs is not None else range(v.tile.nsub)):
                r = v.tile.r[s]
                if r.get(key, 0) < val:
                    r[key] = val
        for v in writes:
            if v.tile is None:
                continue
            for s in (v.subs if v.subs is not None else range(v.tile.nsub)):
                v.tile.w[s] = tok
                v.tile.r[s] = {}

    def op(self, eng, name, **kw):
        reads, writes, args = [], [], {}
        for k, v in kw.items():
            if isinstance(v, V):
                (writes if k in ("out", "accum_out") else reads).append(v)
                args[k] = v.ap
            else:
                args[k] = v
        self._sync(eng, reads, writes)
        ins = getattr(self.engs[eng], name)(**args)
        self.cnt[eng] += 1
        ins.then_inc(self.esem[eng], 1)
        self.ninstr += 1
        self._commit((("E", eng), self.cnt[eng]), reads, writes)

    def dma(self, q, out, in_, **kw):
        if q == POOL:
            k = 16 + self.dkp
            self.dkp = (self.dkp + 1) % (NDS - 16)
        else:
            k = self.dk
            self.dk = (self.dk + 1) % 16
        prev = self.dval[k]
        self._sync(q, [in_], [out], extra=(("D", k), prev))
        ins = self.engs[q].dma_start(out=out.ap, in_=in_.ap, **kw)
        ins.then_inc(self.dsem[k], 16)
        self.ninstr += 1
        self.dval[k] = prev + 16
        self._commit((("D", k), prev + 16), [in_], [out])

    def finish(self):
        for k in range(NDS):
            if self.dval[k] > 0:
                self.nc.sync.wait_ge(self.dsem[k], self.dval[k])
        for e in range(4):
            if self.cnt[e] > 0:
                self.nc.sync.wait_ge(self.esem[e], self.cnt[e])

    def mm(self, out, lhsT, rhs, start=True, stop=True):
        self.op(PE, "matmul", out=out, lhsT=lhsT, rhs=rhs, start=start, stop=stop, skip_group_check=True)

    def tr(self, out, in_, identity):
        self.op(PE, "transpose", out=out, in_=in_, identity=identity)

    def act(self, out, in_, func, **kw):
        self.op(ACT, "activation", out=out, in_=in_, func=func, **kw)


def U(ap):
    return V(None, ap, None)


class K:
    pass


def build(nseq, nlayers, dbg=False):
    nc = bass.Bass("TRN2", target_bir_lowering=False)
    cx = Ctx(nc)
    k = K()
    k.nc, k.cx, k.dbg = nc, cx, dbg

    def din(name, shape, dt=F32):
        return nc.dram_tensor(name, list(shape), dt, kind="ExternalInput").ap()

    k.x = din("x", [4, 2048, D]); k.ctxin = din("ctx", [4, NCTX, D]); k.c5 = din("c5", [5, D])
    k.ada_w = din("ada_w", [NL, D, 3 * D]); k.ada_b = din("ada_b", [NL, 3 * D])
    k.w_in = din("w_in", [NL, D, DIN]); k.wuq = din("wuq", [NL, 384, 768]); k.wukv = din("wukv", [NL, 256, 1024])
    k.gla_a_w = din("gla_a_w", [NL, 2, 16, 256]); k.ml_wq = din("ml_wq", [NL, 4, 128, 64]); k.ml_wk = din("ml_wk", [NL, 4, 128, 64])
    k.br_w = din("br_w", [NL, 4, 512, D]); k.w_out = din("w_out", [NL, D, D])
    k.pcols = din("pcols", [128, NL, NPC]); k.prow = din("prow", [NL, NPR])
    k.cident = din("cident", [128, 128]); k.cbd64 = din("cbd64", [128, 128]); k.crm2 = din("crm2", [128, 128])
    k.ctrif = din("ctrif", [128, 128]); k.ctrib = din("ctrib", [128, 128])
    k.ccos = din("ccos", [128, T]); k.csin = din("csin", [128, T])
    k.out = nc.dram_tensor("out", [4, 2048, D], F32, kind="ExternalOutput").ap()
    k.xres_ap = nc.dram_tensor("xres", [4, T, D], F32, kind="Internal").ap()
    k.gscr_ap = nc.dram_tensor("gscr", [NL, 5, D], F32, kind="Internal").ap()
    k.xres = [Tile(k.xres_ap[s], NT) for s in range(4)]
    k.gscr = Tile(k.gscr_ap, NL)
    if dbg:
        k.dbg_y = nc.dram_tensor("dbg_y", [4, 128, 4, T], F32, kind="ExternalOutput").ap()
        k.dbg_acc = nc.dram_tensor("dbg_acc", [128, 8, T], F32, kind="ExternalOutput").ap()
        k.dbg_h = nc.dram_tensor("dbg_h", [128, 8, T], F32, kind="ExternalOutput").ap()
        k.dbg_x = nc.dram_tensor("dbg_x", [T, D], F32, kind="ExternalOutput").ap()

    def cload(src, dt, shape=(128, 128), q=POOL):
        t = cx.sb(shape, dt)
        cx.dma(q, t[:], U(src))
        return t

    k.identb = cload(k.cident, BF16); k.identf = cload(k.cident, F32, q=SP)
    k.bd64 = cload(k.cbd64, BF16); k.rm2 = cload(k.crm2, BF16)
    k.trif = cload(k.ctrif, F32, q=SP); k.trib = cload(k.ctrib, F32, q=SP)
    k.trifb = cload(k.ctrif, BF16); k.tribb = cload(k.ctrib, BF16)
    k.cos = cload(k.ccos, BF16, (128, T)); k.sin = cload(k.csin, BF16, (128, T))
    k.onesb = cx.sb((128, 128), BF16)
    k.onesf = cx.sb((128, 128), F32)
    _memset(cx, k.onesb[:], 1.0); _memset(cx, k.onesf[:], 1.0)
    k.epsc = cx.sb((128, 1), F32); _memset(cx, k.epsc[:], EPS)
    k.onec = cx.sb((128, 1), F32); _memset(cx, k.onec[:], 1.0)
    k.pc = cx.sb((128, NL, NPC), F32)
    cx.dma(SP, k.pc[:], U(k.pcols))
    k.AB = cx.sb((128, NL, 5, 16), F32)
    k.ps = [cx.psum((128, 512), F32) for _ in range(8)]
    k.psi = 0
    k.wpool = [cx.sb((128, 8, 512), BF16) for _ in range(3)]
    k.wi = 0

    for s in range(nseq):
        cx.dma(SP, k.xres[s].s([0, 1])[0:NCTX, :], U(k.ctxin[s]))
        for j in range(4):
            cx.dma(SP, k.xres[s].s(tk(NCTX + j * 512, NCTX + (j + 1) * 512))[NCTX + j * 512:NCTX + (j + 1) * 512, :],
                   U(k.x[s, j * 512:(j + 1) * 512, :]))

    prologue(k)
    for s in range(nseq):
        for l in range(nlayers):
            layer(k, s, l, last=(l == NL - 1))
    cx.finish()
    cx.es.close()
    return nc, cx


def _memset(cx, v, val, eng=DVE):
    cx._sync(eng, [], [v])
    ins = cx.engs[eng].memset(v.ap, val)
    cx.cnt[eng] += 1
    ins.then_inc(cx.esem[eng], 1)
    cx._commit((("E", eng), cx.cnt[eng]), [], [v])


def nps(k):
    p = k.ps[k.psi]
    k.psi = (k.psi + 1) % 8
    return p


def wload(k, src2d, kc, ncols):
    wt = k.wpool[k.wi]
    k.wi = (k.wi + 1) % len(k.wpool)
    k.cx.dma(POOL, wt[:, 0:kc, 0:ncols], U(src2d.rearrange("(kc p) n -> p kc n", p=128)))
    return wt


def prologue(k):
    cx, nc = k.cx, k.nc
    cx.push()
    c5t = cx.sb((5, D), F32)
    cx.dma(SP, c5t[:], U(k.c5))
    cx.act(c5t[:], c5t[:], AF.Silu)
    scT = cx.sb((128, 8, 5), F32)
    p = nps(k)
    for kc in range(8):
        cx.tr(p[:, kc * 8:kc * 8 + 5], c5t[0:5, kc * 128:(kc + 1) * 128], k.identf[0:5, 0:5])
    cx.op(DVE, "tensor_copy", out=scT[:], in_=V(p, p.t[:, 0:64].rearrange("p (a b) -> p a b", b=8)[:, :, 0:5], None))
    wf = [cx.sb((128, 8, 512), F32) for _ in range(2)]
    adab = cx.sb((5, 3 * D), F32)
    modrow = cx.sb((5, 3 * D), F32)
    modcol = cx.sb((128, 16, 5), F32)
    for l in range(NL):
        cx.dma(SP, adab[:], U(k.ada_b[l].partition_broadcast(5)))
        for nb in range(6):
            w = wf[nb % 2]
            cx.dma(SP, w[:], U(k.ada_w[l][:, nb * 512:(nb + 1) * 512].rearrange("(kc p) n -> p kc n", p=128)))
            p = nps(k)
            for kc in range(8):
                cx.mm(p[0:5, :], scT[:, kc, :], w[:, kc, :], start=(kc == 0), stop=(kc == 7))
            cx.op(DVE, "tensor_tensor", out=modrow[:, nb * 512:(nb + 1) * 512], in0=p[0:5, :], in1=adab[:, nb * 512:(nb + 1) * 512], op=ALU.add)
        p = nps(k)
        for j in range(16):
            cx.tr(p[:, j * 8:j * 8 + 5], modrow[0:5, j * 128:(j + 1) * 128], k.identf[0:5, 0:5])
        cx.op(DVE, "tensor_copy", out=modcol[:], in_=V(p, p.t[:, 0:128].rearrange("p (a b) -> p a b", b=8)[:, :, 0:5], None))
        for r in range(5):
            cx.op(DVE, "scalar_tensor_tensor", out=k.AB[:, l, r, 0:8], in0=modcol[:, 8:16, r], scalar=1.0, in1=k.pc[:, l, PC_NORMG:PC_NORMG + 8], op0=ALU.add, op1=ALU.mult)
            cx.op(DVE, "tensor_copy", out=k.AB[:, l, r, 8:16], in_=modcol[:, 0:8, r])
        cx.dma(SP, k.gscr.s(l)[l], modrow[0:5, 2 * D:3 * D])
    cx.pop()


def layer_params(k, l):
    cx = k.cx
    dc = cx.sb((128, NDC), F32)
    pc = k.pc

    def sc(dst, src, n, f):
        cx.op(DVE, "tensor_scalar", out=dc[:, dst:dst + n], in0=pc[:, l, src:src + n], scalar1=float(f), scalar2=None, op0=ALU.mult)

    lam_init = 0.8 - 0.6 * math.exp(-0.3 * l)
    sc(DC_CQG, PC_CQG, 3, 1.0); sc(DC_CKVG, PC_CKVG, 2, 1.0)
    sc(DC_QGN, PC_QGN, 1, 192 ** -0.5); sc(DC_QGR, PC_QGR, 1, 192 ** -0.5)
    sc(DC_KGN, PC_KGN, 1, 1.0); sc(DC_KGR, PC_KGR, 1, 1.0)
    sc(DC_DFQG, PC_DFQG, 1, 0.125); sc(DC_DFKG, PC_DFKG, 1, 1.0)
    sc(DC_DFOG, PC_DFOG, 1, (1.0 - lam_init))
    sc(DC_GLAOG, PC_GLAOG, 4, 1.0); sc(DC_MLOG, PC_MLOG, 4, 1.0)
    sc(DC_NAB, PC_GLAAB, 4, -1.0)
    pr = cx.sb((128, NPR), F32)
    cx.dma(SP, pr[:], U(k.prow[l].partition_broadcast(128)))
    junk = cx.sb((128, 64), F32)
    s2 = cx.sb((128, 2), F32)
    for i in range(2):
        cx.op(DVE, "tensor_tensor", out=junk[:], in0=pr[:, 16 + 128 * i:16 + 128 * i + 64], in1=pr[:, 16 + 128 * i + 64:16 + 128 * i + 128], op=ALU.mult)
        cx.op(DVE, "reduce_sum", out=s2[:, i:i + 1], in_=junk[:], axis=AX.X)
    cx.act(s2[:], s2[:], AF.Exp)
    cx.op(DVE, "scalar_tensor_tensor", out=dc[:, DC_LAM:DC_LAM + 1], in0=s2[:, 1:2], scalar=-lam_init, in1=s2[:, 0:1], op0=ALU.add, op1=ALU.subtract)
    return dc, pr


def stage_h(k, s, l, hT):
    cx = k.cx
    cx.push()
    xts = [cx.sb((128, D), F32) for _ in range(2)]
    xns = [cx.sb((128, D), BF16) for _ in range(2)]
    junk = cx.sb((128, D), BF16)
    ss = cx.sb((128, NT), F32)
    rs = cx.sb((128, NT), F32)
    for tt in range(NT):
        xt, xn = xts[tt % 2], xns[tt % 2]
        cx.dma(SP, xt[:], k.xres[s].s(tt)[tt * 128:(tt + 1) * 128, :])
        cx.act(junk[:], xt[:], AF.Square, accum_out=ss[:, tt:tt + 1])
        cx.act(rs[:, tt:tt + 1], ss[:, tt:tt + 1], AF.Sqrt, scale=1.0 / D, bias=k.epsc[:, 0:1])
        cx.op(DVE, "reciprocal", out=rs[:, tt:tt + 1], in_=rs[:, tt:tt + 1])
        cx.op(DVE, "tensor_scalar", out=xn[:], in0=xt[:], scalar1=rs[:, tt:tt + 1], scalar2=None, op0=ALU.mult)
        p = nps(k)
        pb = V(p, p.t[:].bitcast(BF16), None)
        for kc in range(8):
            cx.tr(V(p, pb.ap[:, kc * 128:(kc + 1) * 128], None), xn[:, kc * 128:(kc + 1) * 128], k.identb[:])
        r = 4 if tt < 2 else s
        for kc in range(8):
            src = V(p, pb.ap[:, kc * 128:(kc + 1) * 128], None)
            dst = hT.s(tt)[:, kc, tt * 128:(tt + 1) * 128]
            if kc % 2 == 0:
                cx.act(dst, src, AF.Identity, scale=k.AB[:, l, r, kc:kc + 1], bias=k.AB[:, l, r, 8 + kc:9 + kc])
            else:
                cx.op(DVE, "tensor_scalar", out=dst, in0=src, scalar1=k.AB[:, l, r, kc:kc + 1], scalar2=k.AB[:, l, r, 8 + kc:9 + kc], op0=ALU.mult, op1=ALU.add)
    cx.pop()


def proj_fm(k, src, wt, chunks, evac, blks, kcs=8):
    cx = k.cx
    for (b0, n) in blks:
        for ci, grp in enumerate(chunks):
            p = nps(k)
            for (co, m, r0) in grp:
                for kc in range(kcs):
                    cx.mm(p[r0:r0 + m, 0:n], wt[:, kc, co:co + m], src.s(tk(b0, b0 + n))[:, kc, b0:b0 + n], start=(kc == 0), stop=(kc == kcs - 1))
            evac(ci, p, b0, n)


def norm_group(k, pss, n, gmat, neps, gcols, dsts, tmp):
    cx = k.cx
    nchunk = len(pss)
    for c, p in enumerate(pss):
        cx.act(tmp["sq"][:, c, 0:n], p[:, 0:n], AF.Square)
        cx.op(DVE, "tensor_copy", out=tmp["raw"][:, c, 0:n], in_=p[:, 0:n])
    pq = nps(k)
    for c in range(nchunk):
        cx.mm(pq[:, 0:n], gmat[:], tmp["sq"][:, c, 0:n], start=(c == 0), stop=(c == nchunk - 1))
    cx.act(tmp["rs"][:, 0:n], pq[:, 0:n], AF.Ln, scale=EPS / float(neps), bias=k.epsc[:, 0:1])
    cx.act(tmp["rs"][:, 0:n], tmp["rs"][:, 0:n], AF.Exp, scale=-0.5)
    for c in range(nchunk):
        cx.op(DVE, "scalar_tensor_tensor", out=dsts[c], in0=tmp["raw"][:, c, 0:n], scalar=gcols[c], in1=tmp["rs"][:, 0:n], op0=ALU.mult, op1=ALU.mult)


def rope(k, xn, dst, b0, n, tmp):
    cx = k.cx
    p = nps(k)
    cx.mm(p[:, 0:n], k.rm2[:], xn)
    cx.op(DVE, "tensor_tensor", out=tmp["t1"][:, 0:n], in0=p[:, 0:n], in1=k.sin[:, b0:b0 + n], op=ALU.mult)
    cx.op(PEL, "tensor_tensor", out=tmp["t2"][:, 0:n], in0=xn, in1=k.cos[:, b0:b0 + n], op=ALU.mult)
    cx.op(DVE, "tensor_tensor", out=dst, in0=tmp["t1"][:, 0:n], in1=tmp["t2"][:, 0:n], op=ALU.add)


def mk_tmp(cx, nchunk=3):
    return dict(raw=cx.sb((128, nchunk, 512), BF16), sq=cx.sb((128, nchunk, 512), BF16), rs=cx.sb((128, 512), F32),
                t1=cx.sb((128, 512), F32), t2=cx.sb((128, 512), F32), xn=cx.sb((128, 512), BF16))


def merge(k, l, i, y, hT, acc, blks):
    cx = k.cx
    cx.push()
    sg = [cx.sb((128, 512), BF16) for _ in range(2)]
    tmps = [cx.sb((128, 512), BF16) for _ in range(2)]
    j = 0
    for mg in range(2):
        c0 = OFF["merge"] + i * D + mg * 512
        wg = wload(k, k.w_in[l][:, c0:c0 + 512], 8, 512)
        wb = wload(k, k.br_w[l, i][:, mg * 512:(mg + 1) * 512], 4, 512)
        for mc4 in range(4):
            mc = mg * 4 + mc4
            for (b0, n) in blks:
                sb_ = tk(b0, b0 + n)
                pg = nps(k)
                for kc in range(8):
                    cx.mm(pg[:, 0:n], wg[:, kc, mc4 * 128:(mc4 + 1) * 128], hT.s(sb_)[:, kc, b0:b0 + n], start=(kc == 0), stop=(kc == 7))
                g = sg[j % 2]
                cx.act(g[:, 0:n], pg[:, 0:n], AF.Sigmoid)
                py = nps(k)
                for kc in range(4):
                    cx.mm(py[:, 0:n], wb[:, kc, mc4 * 128:(mc4 + 1) * 128], y.s(sb_)[:, kc, b0:b0 + n], start=(kc == 0), stop=(kc == 3))
                dst = acc.s(sb_)[:, mc, b0:b0 + n]
                if i == 0:
                    cx.op(DVE, "tensor_tensor", out=dst, in0=py[:, 0:n], in1=g[:, 0:n], op=ALU.mult)
                else:
                    t = tmps[j % 2]
                    cx.op(DVE, "tensor_tensor", out=t[:, 0:n], in0=py[:, 0:n], in1=g[:, 0:n], op=ALU.mult)
                    cx.op(PEL, "tensor_tensor", out=dst, in0=acc.s(sb_)[:, mc, b0:b0 + n], in1=t[:, 0:n], op=ALU.add)
                j += 1
    cx.pop()


def final(k, s, l, acc, last):
    cx = k.cx
    cx.push()
    w0 = wload(k, k.w_out[l][:, 0:512], 8, 512)
    w1 = wload(k, k.w_out[l][:, 512:1024], 8, 512)
    gb = [cx.sb((128, D), F32) for _ in range(2)]
    cx.dma(SP, gb[0][:], V(k.gscr, k.gscr.t[l, 4].partition_broadcast(128), [l]))
    cx.dma(SP, gb[1][:], V(k.gscr, k.gscr.t[l, s].partition_broadcast(128), [l]))
    xo = [cx.sb((128, D), F32) for _ in range(2)]
    tm = [cx.sb((128, 512), F32) for _ in range(2)]
    for tt in range(2 if last else 0, NT):
        x_ = xo[tt % 2]
        cx.dma(SP, x_[:], k.xres[s].s(tt)[tt * 128:(tt + 1) * 128, :])
        g = gb[0] if tt < 2 else gb[1]
        for nh, w in enumerate((w0, w1)):
            p = nps(k)
            for kc in range(8):
                cx.mm(p[:, :], acc.s(tt)[:, kc, tt * 128:(tt + 1) * 128], w[:, kc, :], start=(kc == 0), stop=(kc == 7))
            t = tm[nh]
            cx.op(DVE, "tensor_tensor", out=t[:], in0=p[:, :], in1=g[:, nh * 512:(nh + 1) * 512], op=ALU.mult)
            cx.op(PEL if nh else DVE, "tensor_tensor", out=x_[:, nh * 512:(nh + 1) * 512], in0=x_[:, nh * 512:(nh + 1) * 512], in1=t[:], op=ALU.add)
        if k.dbg and s == 0 and l == 0:
            cx.dma(SP, U(k.dbg_x[tt * 128:(tt + 1) * 128, :]), x_[:])
        if last:
            cx.dma(SP, U(k.out[s, (tt - 2) * 128:(tt - 1) * 128, :]), x_[:])
        else:
            cx.dma(SP, k.xres[s].s(tt)[tt * 128:(tt + 1) * 128, :], x_[:])
    cx.pop()


def attn_core(k, nheads, nmaps, qk_fn, v_fn, out_fn, last, blk_fn=None):
    cx = k.cx
    cx.push()
    pts = [cx.sb((128, 512), BF16) for _ in range(4)]
    stage = [cx.sb((128, 129), F32) for _ in range(4 * nmaps)] if nmaps == 2 else None
    pj = 0
    sbank = [k.ps[0], k.ps[1]]
    abanks = [k.ps[2], k.ps[3], k.ps[4], k.ps[5]]
    sj = 0
    for qb, (q0, nq) in enumerate(BLKS):
        if last and qb == 0:
            continue
        kts = [0, 1] if qb == 0 else list(range(NT))
        nqt = nq // 128
        if blk_fn is not None:
            blk_fn(qb, q0, nq)
        for h in range(nheads):
            def accv(m, qi):
                idx = m * 4 + qi
                b = abanks[idx // 2] if nmaps == 2 else abanks[qi // 2]
                o = (idx % 2) * 129
                return b, V(b, b.t[:, o:o + 129], None)
            started = set()
            pend = []

            def do_pv(kt_, items):
                vv = v_fn(h, kt_)
                for (m, pt) in items:
                    for qi in range(nqt):
                        b, av = accv(m, qi)
                        first = id(b) not in started
                        started.add(id(b))
                        cx.mm(av, pt[:, qi * 128:(qi + 1) * 128], vv, start=first, stop=(kt_ == kts[-1]))

            for kt in kts:
                items = []
                for m in range(nmaps):
                    sp_ = sbank[sj % 2]; sj += 1
                    qk_fn(h, m, kt, q0, nq, sp_)
                    pt = pts[pj % 4]; pj += 1
                    cx.act(pt[:, 0:nq], sp_[:, 0:nq], AF.Exp)
                    items.append((m, pt))
                if pend:
                    do_pv(*pend.pop())
                pend.append((kt, items))
            do_pv(*pend.pop())
            stg = []
            for qi in range(nqt):
                row = []
                for m in range(nmaps):
                    if stage is None:
                        row.append(accv(m, qi)[1])
                        continue
                    st_ = stage[m * 4 + qi]
                    cx.act(st_[:, :], accv(m, qi)[1], AF.Copy)
                    row.append(st_[:, :])
                stg.append(row)
            for qi in range(nqt):
                out_fn(h, qb, q0 + qi * 128, stg[qi])
    cx.pop()


def mla(k, s, l, hT, acc, dc, last):
    cx = k.cx
    cx.push()
    qn = cx.sb((128, 4, T), BF16, NT); qr = cx.sb((128, 2, T), BF16, NT)
    kn = cx.sb((128, 4, T), BF16, NT); kr2 = cx.sb((128, T), BF16, NT)
    vaug = cx.sb((128, NT, 4, 129), BF16, NT)
    _memset(cx, vaug[:], 1.0)
    W = k.w_in[l]
    cx.push()
    cqn = cx.sb((128, 3, T), BF16, NT)
    tmp = mk_tmp(cx)
    wA = wload(k, W[:, 0:384], 8, 384)
    wq1 = wload(k, k.wuq[l][:, 0:512], 3, 512)
    wq2 = wload(k, k.wuq[l][:, 512:768], 3, 256)
    for (b0, n) in BLKS:
        sb_ = tk(b0, b0 + n)
        pss = []
        for c in range(3):
            p = nps(k)
            for kc in range(8):
                cx.mm(p[:, 0:n], wA[:, kc, c * 128:(c + 1) * 128], hT.s(sb_)[:, kc, b0:b0 + n], start=(kc == 0), stop=(kc == 7))
            pss.append(p)
        ksub = int(os.environ.get("KSUB", "9"))
        if ksub < 1:
            for c in range(3):
                cx.op(DVE, "tensor_copy", out=cqn.s(sb_)[:, c, b0:b0 + n], in_=pss[c][:, 0:n])
            continue
        norm_group(k, pss, n, k.onesb, 384 * EPS, [dc[:, DC_CQG + c:DC_CQG + c + 1] for c in range(3)],
                   [cqn.s(sb_)[:, c, b0:b0 + n] for c in range(3)], tmp)
        if ksub < 2:
            continue
        for h in range(4):
            p = nps(k)
            for kc in range(3):
                kv = os.environ.get("KV", "0")
                lw = wA if kv == "2" else wq1
                rr = hT if kv == "1" else cqn
                cx.mm(p[:, 0:n], lw[:, kc, h * 128:(h + 1) * 128], rr.s(sb_)[:, kc, b0:b0 + n], start=(kc == 0), stop=(kc == 2))
            if os.environ.get("KQ", "1") == "0":
                cx.op(DVE, "tensor_copy", out=qn.s(sb_)[:, h, b0:b0 + n], in_=p[:, 0:n])
            else:
                norm_group(k, [p], n, k.onesb, 128 * EPS, [dc[:, DC_QGN:DC_QGN + 1]], [qn.s(sb_)[:, h, b0:b0 + n]], tmp)
        if ksub < 3:
            continue
        for c in range(2):
            p = nps(k)
            for kc in range(3):
                cx.mm(p[:, 0:n], wq2[:, kc, c * 128:(c + 1) * 128], cqn.s(sb_)[:, kc, b0:b0 + n], start=(kc == 0), stop=(kc == 2))
            norm_group(k, [p], n, k.bd64, 64 * EPS, [dc[:, DC_QGR:DC_QGR + 1]], [tmp["xn"][:, 0:n]], tmp)
            rope(k, tmp["xn"][:, 0:n], qr.s(sb_)[:, c, b0:b0 + n], b0, n, tmp)
    cx.pop()
    stg = int(os.environ.get("KSTAGE", "9"))
    if stg < 4:
        cx.pop(); return
    cx.push()
    ckvn = cx.sb((128, 2, T), BF16, NT)
    tmp = mk_tmp(cx)
    wB = wload(k, W[:, 384:704], 8, 320)
    wk1 = wload(k, k.wukv[l][:, 0:512], 2, 512)
    for (b0, n) in BLKS:
        sb_ = tk(b0, b0 + n)
        pss = []
        for c in range(2):
            p = nps(k)
            for kc in range(8):
                cx.mm(p[:, 0:n], wB[:, kc, c * 128:(c + 1) * 128], hT.s(sb_)[:, kc, b0:b0 + n], start=(kc == 0), stop=(kc == 7))
            pss.append(p)
        norm_group(k, pss, n, k.onesb, 256 * EPS, [dc[:, DC_CKVG + c:DC_CKVG + c + 1] for c in range(2)],
                   [ckvn.s(sb_)[:, c, b0:b0 + n] for c in range(2)], tmp)
        p = nps(k)
        for r0 in (0, 64):
            for kc in range(8):
                cx.mm(p[r0:r0 + 64, 0:n], wB[:, kc, 256:320], hT.s(sb_)[:, kc, b0:b0 + n], start=(kc == 0), stop=(kc == 7))
        norm_group(k, [p], n, k.bd64, 64 * EPS, [dc[:, DC_KGR:DC_KGR + 1]], [tmp["xn"][:, 0:n]], tmp)
        rope(k, tmp["xn"][:, 0:n], kr2.s(sb_)[:, b0:b0 + n], b0, n, tmp)
        for h in range(4):
            p = nps(k)
            for kc in range(2):
                cx.mm(p[:, 0:n], wk1[:, kc, h * 128:(h + 1) * 128], ckvn.s(sb_)[:, kc, b0:b0 + n], start=(kc == 0), stop=(kc == 1))
            norm_group(k, [p], n, k.onesb, 128 * EPS, [dc[:, DC_KGN:DC_KGN + 1]], [kn.s(sb_)[:, h, b0:b0 + n]], tmp)
    wv1 = wload(k, k.wukv[l][:, 512:1024], 2, 512)
    for tt in range(NT):
        p = nps(k)
        for kc in range(2):
            cx.mm(p[:, :], ckvn.s(tt)[:, kc, tt * 128:(tt + 1) * 128], wv1[:, kc, :], start=(kc == 0), stop=(kc == 1))
        cx.op(DVE, "tensor_copy", out=vaug.s(tt)[:, tt, :, 0:128], in_=V(p, p.t[:, :].rearrange("p (h e) -> p h e", e=128), None))
    cx.pop()
    if stg < 5:
        cx.pop(); return
    if k.dbg and s == 0 and l == 0:
        dumpv(k, "d_kr2", kr2[:, :], T)
        dumpv(k, "d_qr0", qr[:, 0, :], T)
        dumpv(k, "d_qn1", qn[:, 1, :], T)
        dumpv(k, "d_kn1", kn[:, 1, :], T)
    y = cx.sb((128, 4, T), BF16, NT)
    sz = cx.sb((128, 4, 512), BF16)
    wz = wload(k, W[:, OFF["mla_z"]:OFF["mla_z"] + 512], 8, 512)

    def blk_fn(qb, b0, n):
        for c in range(4):
            p = k.ps[6 + c % 2]
            for kc in range(8):
                cx.mm(p[:, 0:n], wz[:, kc, c * 128:(c + 1) * 128], hT.s(tk(b0, b0 + n))[:, kc, b0:b0 + n], start=(kc == 0), stop=(kc == 7))
            cx.act(sz[:, c, 0:n], p[:, 0:n], AF.Silu)

    ob = [cx.sb((128, 128), BF16) for _ in range(2)]
    rc = [cx.sb((128, 1), F32) for _ in range(2)]
    oj = [0]

    def qk_fn(h, m, kt, q0, nq, sp_):
        r0 = (h % 2) * 64
        cx.mm(sp_[:, 0:nq], kn.s(kt)[:, h, kt * 128:(kt + 1) * 128], qn.s(tk(q0, q0 + nq))[:, h, q0:q0 + nq], start=True, stop=False)
        cx.mm(sp_[:, 0:nq], kr2.s(kt)[r0:r0 + 64, kt * 128:(kt + 1) * 128], qr.s(tk(q0, q0 + nq))[r0:r0 + 64, h // 2, q0:q0 + nq], start=False, stop=True)

    def v_fn(h, kt):
        return vaug.s(kt)[:, kt, h, :]

    def out_fn(h, qb, q0, accs):
        a = accs[0]
        j = oj[0]; oj[0] += 1
        o_, r_ = ob[j % 2], rc[j % 2]
        cx.op(DVE, "reciprocal", out=r_[:], in_=V(a.tile, a.ap[:, 128:129], None))
        cx.op(DVE, "tensor_scalar", out=o_[:], in0=V(a.tile, a.ap[:, 0:128], None), scalar1=r_[:, 0:1], scalar2=None, op0=ALU.mult)
        p = k.ps[6 + j % 2]
        pb = V(p, p.t[:].bitcast(BF16)[:, 0:128], None)
        cx.tr(pb, o_[:], k.identb[:])
        tt = q0 // 128
        qoff = q0 - BLKS[qb][0]
        cx.op(DVE, "tensor_tensor", out=y.s(tt)[:, h, q0:q0 + 128], in0=pb, in1=sz[:, h, qoff:qoff + 128], op=ALU.mult)

    attn_core(k, 4, 1, qk_fn, v_fn, out_fn, last, blk_fn)
    if k.dbg and s == 0 and l == 0:
        dump(k, y, k.dbg_y[0], 4)
    if stg < 6:
        cx.pop(); return
    merge(k, l, 0, y, hT, acc, BLKS[1:] if last else BLKS)
    cx.pop()


def sz_block(k, wz, hT, sz, b0, n):
    cx = k.cx
    for c in range(4):
        p = k.ps[6 + c % 2]
        for kc in range(8):
            cx.mm(p[:, 0:n], wz[:, kc, c * 128:(c + 1) * 128], hT.s(tk(b0, b0 + n))[:, kc, b0:b0 + n], start=(kc == 0), stop=(kc == 7))
        cx.act(sz[:, c, 0:n], p[:, 0:n], AF.Silu)


def proj_tm(k, hT, w, dst_fn, ncols=512):
    cx = k.cx
    for tt in range(NT):
        p = nps(k)
        for kc in range(8):
            cx.mm(p[:, 0:ncols], hT.s(tt)[:, kc, tt * 128:(tt + 1) * 128], w[:, kc, 0:ncols], start=(kc == 0), stop=(kc == 7))
        dst_fn(tt, p)


def diffattn(k, s, l, hT, acc, dc, last):
    cx = k.cx
    cx.push()
    qd = cx.sb((128, 4, T), BF16, NT); kd = cx.sb((128, 4, T), BF16, NT)
    vaug = cx.sb((128, NT, 4, 129), BF16, NT)
    _memset(cx, vaug[:], 1.0)
    W = k.w_in[l]
    cx.push()
    tmp = mk_tmp(cx, 1)
    for (dst, off, gcol) in ((qd, OFF["df_q"], DC_DFQG), (kd, OFF["df_k"], DC_DFKG)):
        w = wload(k, W[:, off:off + 512], 8, 512)
        for (b0, n) in BLKS:
            sb_ = tk(b0, b0 + n)
            for h in range(4):
                p = nps(k)
                for kc in range(8):
                    cx.mm(p[:, 0:n], w[:, kc, h * 128:(h + 1) * 128], hT.s(sb_)[:, kc, b0:b0 + n], start=(kc == 0), stop=(kc == 7))
                norm_group(k, [p], n, k.bd64, 64 * EPS, [dc[:, gcol:gcol + 1]], [tmp["xn"][:, 0:n]], tmp)
                rope(k, tmp["xn"][:, 0:n], dst.s(sb_)[:, h, b0:b0 + n], b0, n, tmp)
    wv = wload(k, W[:, OFF["df_v"]:OFF["df_v"] + 512], 8, 512)
    proj_tm(k, hT, wv, lambda tt, p: cx.op(DVE, "tensor_copy", out=vaug.s(tt)[:, tt, :, 0:128],
                                             in_=V(p, p.t[:, :].rearrange("p (h e) -> p h e", e=128), None)))
    cx.pop()
    y = cx.sb((128, 4, T), BF16, NT)
    sz = cx.sb((128, 4, 512), BF16)
    wz = wload(k, W[:, OFF["df_z"]:OFF["df_z"] + 512], 8, 512)
    o1 = [cx.sb((128, 128), F32) for _ in range(2)]
    o2 = [cx.sb((128, 128), F32) for _ in range(2)]
    ob = [cx.sb((128, 128), BF16) for _ in range(2)]
    junk = cx.sb((128, 128), BF16)
    rc = [cx.sb((128, 4), F32) for _ in range(2)]
    oj = [0]

    def qk_fn(h, m, kt, q0, nq, sp_):
        r0 = m * 64
        cx.mm(sp_[:, 0:nq], kd.s(kt)[r0:r0 + 64, h, kt * 128:(kt + 1) * 128], qd.s(tk(q0, q0 + nq))[r0:r0 + 64, h, q0:q0 + nq], start=True, stop=True)

    def v_fn(h, kt):
        return vaug.s(kt)[:, kt, h, :]

    def out_fn(h, qb, q0, accs):
        a0, a1 = accs
        j = oj[0]; oj[0] += 1
        r_ = rc[j % 2]
        cx.op(DVE, "reciprocal", out=r_[:, 0:1], in_=V(a0.tile, a0.ap[:, 128:129], None))
        cx.op(DVE, "reciprocal", out=r_[:, 1:2], in_=V(a1.tile, a1.ap[:, 128:129], None))
        cx.op(DVE, "tensor_tensor", out=r_[:, 1:2], in0=r_[:, 1:2], in1=dc[:, DC_LAM:DC_LAM + 1], op=ALU.mult)
        cx.op(DVE, "tensor_scalar", out=o1[j % 2][:], in0=V(a0.tile, a0.ap[:, 0:128], None), scalar1=r_[:, 0:1], scalar2=None, op0=ALU.mult)
        cx.op(DVE, "scalar_tensor_tensor", out=o2[j % 2][:], in0=V(a1.tile, a1.ap[:, 0:128], None), scalar=r_[:, 1:2], in1=o1[j % 2][:], op0=ALU.mult, op1=ALU.add)
        cx.act(junk[:], o2[j % 2][:], AF.Square, accum_out=r_[:, 2:3])
        cx.act(r_[:, 3:4], r_[:, 2:3], AF.Sqrt, scale=1.0 / 128, bias=k.epsc[:, 0:1])
        cx.op(DVE, "reciprocal", out=r_[:, 3:4], in_=r_[:, 3:4])
        cx.op(DVE, "tensor_scalar", out=ob[j % 2][:], in0=o2[j % 2][:], scalar1=r_[:, 3:4], scalar2=None, op0=ALU.mult)
        p = k.ps[6 + j % 2]
        pb = V(p, p.t[:].bitcast(BF16)[:, 0:128], None)
        cx.tr(pb, ob[j % 2][:], k.identb[:])
        tt = q0 // 128
        qoff = q0 - BLKS[qb][0]
        cx.op(DVE, "scalar_tensor_tensor", out=y.s(tt)[:, h, q0:q0 + 128], in0=pb, scalar=dc[:, DC_DFOG:DC_DFOG + 1], in1=sz[:, h, qoff:qoff + 128], op0=ALU.mult, op1=ALU.mult)

    attn_core(k, 4, 2, qk_fn, v_fn, out_fn, last, lambda qb, b0, n: sz_block(k, wz, hT, sz, b0, n))
    if k.dbg and s == 0 and l == 0:
        dump(k, y, k.dbg_y[3], 4)
    merge(k, l, 3, y, hT, acc, BLKS[1:] if last else BLKS)
    cx.pop()


def out_stage(k, l, i, hT, osrc_fn, y, dc, gcol0, zoff, last, extra_fn=None):
    cx = k.cx
    cx.push()
    sz = cx.sb((128, 4, 512), BF16)
    wz = wload(k, k.w_in[l][:, zoff:zoff + 512], 8, 512)
    on = [cx.sb((128, 512), BF16) for _ in range(2)]
    junk = cx.sb((128, 128), BF16)
    ss = [cx.sb((128, 4), F32) for _ in range(2)]
    for (b0, n) in (BLKS[1:] if last else BLKS):
        sz_block(k, wz, hT, sz, b0, n)
        for tt in tk(b0, b0 + n):
            src = osrc_fn(tt)
            s_, o_ = ss[tt % 2], on[tt % 2]
            for h in range(4):
                cx.act(junk[:], V(src.tile, src.ap[:, h * 128:(h + 1) * 128], src.subs), AF.Square, accum_out=s_[:, h:h + 1])
            cx.act(s_[:], s_[:], AF.Sqrt, scale=1.0 / 128, bias=k.epsc[:, 0:1])
            cx.op(DVE, "reciprocal", out=s_[:], in_=s_[:])
            for h in range(4):
                cx.op(DVE, "tensor_scalar", out=o_[:, h * 128:(h + 1) * 128], in0=V(src.tile, src.ap[:, h * 128:(h + 1) * 128], src.subs), scalar1=s_[:, h:h + 1], scalar2=None, op0=ALU.mult)
            p = nps(k)
            pb = p.t[:].bitcast(BF16)
            for h in range(4):
                cx.tr(V(p, pb[:, h * 128:(h + 1) * 128], None), o_[:, h * 128:(h + 1) * 128], k.identb[:])
            t0 = tt * 128
            for h in range(4):
                pv = V(p, pb[:, h * 128:(h + 1) * 128], None)
                if extra_fn is None:
                    cx.op(DVE, "scalar_tensor_tensor", out=y.s(tt)[:, h, t0:t0 + 128], in0=pv, scalar=dc[:, gcol0 + h:gcol0 + h + 1], in1=sz[:, h, t0 - b0:t0 - b0 + 128], op0=ALU.mult, op1=ALU.mult)
                else:
                    extra_fn(tt, h, pv, sz[:, h, t0 - b0:t0 - b0 + 128])
    cx.pop()


def gla(k, s, l, hT, acc, dc, last):
    cx = k.cx
    cx.push()
    W = k.w_in[l]
    ogl = cx.sb((128, NT, 512), BF16, NT)
    cx.push()
    qT = cx.sb((128, 2, T), BF16, NT); kT = cx.sb((128, 2, T), BF16, NT)
    afT = cx.sb((16, 2, T), BF16, NT)
    vtm = cx.sb((128, NT, 512), BF16, NT)
    aw = cx.sb((16, 2, 256), BF16)
    cx.dma(POOL, aw[:], U(k.gla_a_w[l].rearrange("d r n -> r d n")))
    wqk = wload(k, W[:, OFF["gla_q"]:OFF["gla_q"] + 512], 8, 512)
    waf = wload(k, W[:, OFF["gla_af"]:OFF["gla_af"] + 32], 8, 32)
    for (b0, n) in BLKS:
        sb_ = tk(b0, b0 + n)
        for c in range(4):
            p = nps(k)
            for kc in range(8):
                cx.mm(p[:, 0:n], wqk[:, kc, c * 128:(c + 1) * 128], hT.s(sb_)[:, kc, b0:b0 + n], start=(kc == 0), stop=(kc == 7))
            if c < 2:
                cx.act(qT.s(sb_)[:, c, b0:b0 + n], p[:, 0:n], AF.Copy, scale=0.125)
            else:
                cx.op(DVE, "tensor_copy", out=kT.s(sb_)[:, c - 2, b0:b0 + n], in_=p[:, 0:n])
        for d in range(2):
            p = nps(k)
            for kc in range(8):
                cx.mm(p[0:16, 0:n], waf[:, kc, d * 16:(d + 1) * 16], hT.s(sb_)[:, kc, b0:b0 + n], start=(kc == 0), stop=(kc == 7))
            cx.op(DVE, "tensor_copy", out=afT.s(sb_)[0:16, d, b0:b0 + n], in_=p[0:16, 0:n])
    wv = wload(k, W[:, OFF["gla_v"]:OFF["gla_v"] + 512], 8, 512)
    proj_tm(k, hT, wv, lambda tt, p: cx.act(vtm.s(tt)[:, tt, :], p[:, :], AF.Copy))
    S32_ = [cx.sb((128, 2, 128), F32) for _ in range(2)]; Sb_ = [cx.sb((128, 2, 128), BF16) for _ in range(2)]
    R = lambda shape, dt, nb=4: [cx.sb(shape, dt) for _ in range(nb)]
    e_ = R((128, 2, 128), F32); lp_ = R((128, 2, 128), F32); Bp_ = R((128, 2, 128), F32); tR_ = R((128, 2, 128), F32)
    E_ = R((128, 2, 128), BF16, 6); qe_ = R((128, 2, 128), BF16); ke_ = R((128, 2, 128), BF16); kl_ = R((128, 2, 128), BF16)
    bt_ = R((128, 2, 4), F32); kltm_ = R((128, 256), BF16); AT_ = R((128, 128), BF16, 4)
    orders = [list(range(NT)), [1, 0] + list(range(NT - 1, 1, -1))]
    masks = [k.trifb, k.tribb]
    written = set()
    if True:
        def prep(d, tt, j):
            t0 = tt * 128
            j = 2 * d + j
            e, lp, Bp, tR, qe, ke, kl, bt, kltm = e_[j], lp_[j], Bp_[j], tR_[j], qe_[j], ke_[j], kl_[j], bt_[j], kltm_[j]
            pl = nps(k)
            for c in range(2):
                cx.mm(pl[:, c * 128:(c + 1) * 128], aw[0:16, d, c * 128:(c + 1) * 128], afT.s(tt)[0:16, d, t0:t0 + 128])
            for c in range(2):
                cx.act(e[:, c, :], pl[:, c * 128:(c + 1) * 128], AF.Exp, scale=-1.0, bias=dc[:, DC_NAB + 2 * d + c:DC_NAB + 2 * d + c + 1])
            cx.act(lp[:], e[:], AF.Ln, bias=k.onec[:, 0:1])
            for c in range(2):
                cx.op(DVE, "tensor_tensor_scan", out=Bp[:, c, :], data0=k.onesf[:, 0:128], data1=lp[:, c, :], initial=0.0, op0=ALU.mult, op1=ALU.add)
            E1, E2, E3 = E_[3 * d], E_[3 * d + 1], E_[3 * d + 2]
            if d == 0:
                cx.op(DVE, "tensor_scalar", out=bt[:, :, 0:1], in0=Bp[:, :, 127:128], scalar1=-1.0 / 16, scalar2=None, op0=ALU.mult)
                cx.act(E1[:], Bp[:], AF.Exp, scale=-1.0 / 16)
                cx.act(E2[:], Bp[:], AF.Exp, scale=1.0 / 16)
                for c in range(2):
                    cx.act(E3[:, c, :], Bp[:, c, :], AF.Exp, scale=1.0 / 16, bias=bt[:, c, 0:1])
            else:
                cx.op(DVE, "tensor_tensor", out=tR[:], in0=lp[:], in1=Bp[:], op=ALU.subtract)
                cx.op(DVE, "tensor_scalar", out=bt[:, :, 0:1], in0=Bp[:, :, 127:128], scalar1=-1.0 / 16, scalar2=None, op0=ALU.mult)
                cx.op(DVE, "tensor_scalar", out=bt[:, :, 1:2], in0=Bp[:, :, 127:128], scalar1=1.0 / 16, scalar2=None, op0=ALU.mult)
                for c in range(2):
                    cx.act(E1[:, c, :], tR[:, c, :], AF.Exp, scale=-1.0 / 16, bias=bt[:, c, 0:1])
                    cx.act(E2[:, c, :], tR[:, c, :], AF.Exp, scale=1.0 / 16, bias=bt[:, c, 1:2])
                cx.act(E3[:], tR[:], AF.Exp, scale=1.0 / 16)
            cx.act(bt[:, :, 2:3], bt[:, :, 0:1], AF.Exp)
            cx.op(DVE, "tensor_tensor", out=qe[:], in0=qT.s(tt)[:, :, t0:t0 + 128], in1=E1[:], op=ALU.mult)
            cx.op(DVE, "tensor_tensor", out=ke[:], in0=kT.s(tt)[:, :, t0:t0 + 128], in1=E2[:], op=ALU.mult)
            cx.op(DVE, "tensor_tensor", out=kl[:], in0=kT.s(tt)[:, :, t0:t0 + 128], in1=E3[:], op=ALU.mult)
            ptr = nps(k)
            pb = ptr.t[:].bitcast(BF16)
            for c in range(2):
                cx.tr(V(ptr, pb[:, c * 128:(c + 1) * 128], None), kl[:, c, :], k.identb[:])
            cx.op(DVE, "tensor_copy", out=kltm[:], in_=V(ptr, pb[:, 0:256], None))

        def body(d, tt, j):
            j = 2 * d + j
            mask, S32, Sb = masks[d], S32_[d], Sb_[d]
            qe, ke, bt, kltm = qe_[j], ke_[j], bt_[j], kltm_[j]
            po = nps(k)
            for h in range(4):
                c, r0 = h // 2, (h % 2) * 64
                pa = nps(k)
                cx.mm(pa[:, 0:128], ke[r0:r0 + 64, c, :], qe[r0:r0 + 64, c, :])
                AT = AT_[h % 4]
                cx.op(DVE, "tensor_tensor", out=AT[:], in0=pa[:, 0:128], in1=mask[:], op=ALU.mult)
                cx.mm(po[:, h * 128:(h + 1) * 128], AT[:], vtm.s(tt)[:, tt, h * 128:(h + 1) * 128], start=True, stop=False)
                cx.mm(po[:, h * 128:(h + 1) * 128], qe[r0:r0 + 64, c, :], Sb[r0:r0 + 64, c, :], start=False, stop=True)
            pu = nps(k)
            for h in range(4):
                c, r0 = h // 2, (h % 2) * 64
                cx.mm(pu[r0:r0 + 64, c * 128:(c + 1) * 128], kltm[:, h * 64:(h + 1) * 64], vtm.s(tt)[:, tt, h * 128:(h + 1) * 128], start=True, stop=True)
            if tt not in written:
                written.add(tt)
                cx.act(ogl.s(tt)[:, tt, :], po[:, :], AF.Copy)
            else:
                cx.op(DVE, "tensor_tensor", out=ogl.s(tt)[:, tt, :], in0=po[:, :], in1=ogl.s(tt)[:, tt, :], op=ALU.add)
            for c in range(2):
                cx.op(DVE, "scalar_tensor_tensor", out=S32[:, c, :], in0=S32[:, c, :], scalar=bt[:, c, 2:3], in1=pu[:, c * 128:(c + 1) * 128], op0=ALU.mult, op1=ALU.add)
            cx.act(Sb[:], S32[:], AF.Copy)

        for d in range(2):
            _memset(cx, S32_[d][:], 0.0); _memset(cx, Sb_[d][:], 0.0)
            prep(d, orders[d][0], 0)
        for i in range(NT):
            for d in range(2):
                if i + 1 < NT:
                    prep(d, orders[d][i + 1], (i + 1) % 2)
                body(d, orders[d][i], i % 2)
    cx.pop()
    y = cx.sb((128, 4, T), BF16, NT)
    out_stage(k, l, 1, hT, lambda tt: ogl.s(tt)[:, tt, :], y, dc, DC_GLAOG, OFF["gla_z"], last)
    if k.dbg and s == 0 and l == 0:
        dump(k, y, k.dbg_y[1], 4)
    merge(k, l, 1, y, hT, acc, BLKS[1:] if last else BLKS)
    cx.pop()


def mlstm(k, s, l, hT, acc, dc, pr, last):
    cx = k.cx
    cx.push()
    W = k.w_in[l]
    xcT = cx.sb((128, 4, T), BF16, NT)
    hml = cx.sb((128, NT, 512), BF16, NT)
    cx.push()
    xraw = cx.sb((128, 4, T), BF16, NT)
    cacc = cx.sb((128, T), F32)
    wx = wload(k, W[:, OFF["ml_x"]:OFF["ml_x"] + 512], 8, 512)
    for (b0, n) in BLKS:
        sb_ = tk(b0, b0 + n)
        for c in range(4):
            p = nps(k)
            for kc in range(8):
                cx.mm(p[:, 0:n], wx[:, kc, c * 128:(c + 1) * 128], hT.s(sb_)[:, kc, b0:b0 + n], start=(kc == 0), stop=(kc == 7))
            if c % 2:
                cx.act(xraw.s(sb_)[:, c, b0:b0 + n], p[:, 0:n], AF.Copy)
            else:
                cx.op(DVE, "tensor_copy", out=xraw.s(sb_)[:, c, b0:b0 + n], in_=p[:, 0:n])
    pc = k.pc
    for c in range(4):
        w0, w1, w2 = [pc[:, l, PC_CONVW + 4 * j + c:PC_CONVW + 4 * j + c + 1] for j in range(3)]
        cx.op(DVE, "tensor_scalar", out=cacc[:], in0=xraw[:, c, :], scalar1=w1, scalar2=pc[:, l, PC_CONVB + c:PC_CONVB + c + 1], op0=ALU.mult, op1=ALU.add)
        for (a_, b_) in ((0, NCTX), (NCTX, T)):
            cx.op(DVE, "scalar_tensor_tensor", out=cacc[:, a_ + 1:b_], in0=xraw[:, c, a_:b_ - 1], scalar=w0, in1=cacc[:, a_ + 1:b_], op0=ALU.mult, op1=ALU.add)
            cx.op(DVE, "scalar_tensor_tensor", out=cacc[:, a_:b_ - 1], in0=xraw[:, c, a_ + 1:b_], scalar=w2, in1=cacc[:, a_:b_ - 1], op0=ALU.mult, op1=ALU.add)
        cx.act(xcT[:, c, :], cacc[:], AF.Silu)
    cx.pop()
    cx.push()
    qT = cx.sb((128, 2, T), BF16, NT); kT = cx.sb((128, 2, T), BF16, NT)
    vaug = cx.sb((128, NT, 4, 129), BF16, NT)
    _memset(cx, vaug[:], 1.0)
    gt = cx.sb((128, NT, 16), F32); lpt = cx.sb((128, NT, 16), F32)
    wqb = cx.sb((128, 4, 64), BF16); wkb = cx.sb((128, 4, 64), BF16)
    cx.dma(POOL, wqb[:], U(k.ml_wq[l].rearrange("h c d -> c h d")))
    cx.dma(POOL, wkb[:], U(k.ml_wk[l].rearrange("h c d -> c h d")))
    for (b0, n) in BLKS:
        sb_ = tk(b0, b0 + n)
        for (dst, wb, scl) in ((qT, wqb, 1.0), (kT, wkb, 0.125)):
            for c in range(2):
                p = nps(k)
                for hh in range(2):
                    h = 2 * c + hh
                    cx.mm(p[hh * 64:(hh + 1) * 64, 0:n], wb[:, h, :], xcT.s(sb_)[:, h, b0:b0 + n], start=True, stop=True)
                cx.act(dst.s(sb_)[:, c, b0:b0 + n], p[:, 0:n], AF.Copy, scale=scl)
    wv = wload(k, W[:, OFF["ml_v"]:OFF["ml_v"] + 512], 8, 512)
    proj_tm(k, hT, wv, lambda tt, p: cx.op(DVE, "tensor_copy", out=vaug.s(tt)[:, tt, :, 0:128],
                                             in_=V(p, p.t[:, :].rearrange("p (h e) -> p h e", e=128), None)))
    wif = wload(k, W[:, OFF["ml_if"]:OFF["ml_if"] + 16], 8, 16)
    proj_tm(k, hT, wif, lambda tt, p: cx.op(DVE, "tensor_tensor", out=gt[:, tt, :], in0=p[:, 0:16], in1=pr[:, 0:16], op=ALU.add), ncols=16)
    cx.act(lpt[:], gt[:], AF.Exp, scale=-1.0)
    cx.act(lpt[:], lpt[:], AF.Ln, bias=k.onec[:, 0:1])
    Bp = cx.sb((128, 2, NT, 4), F32); Bt = cx.sb((128, 2, NT, 4), F32)
    fa = cx.sb((128, 2, NT, 4), F32); fg = cx.sb((128, 2, NT, 4), F32); fgk = cx.sb((128, 2, NT, 4), F32); fdec = cx.sb((128, 2, NT, 4), F32)
    for d in range(2):
        g0 = (1 + 2 * d) * 4
        rhs = lpt[:, :, g0:g0 + 4]
        p = nps(k)
        cx.mm(p[:, 0:72], (k.trif if d == 0 else k.trib)[:], rhs)
        cx.op(DVE, "tensor_copy", out=Bp[:, d], in_=V(p, p.t[:, 0:72].rearrange("p (a b) -> p a b", b=4), None))
        p = nps(k)
        cx.mm(p[:, 0:72], k.onesf[:], rhs)
        cx.op(DVE, "tensor_copy", out=Bt[:, d], in_=V(p, p.t[:, 0:72].rearrange("p (a b) -> p a b", b=4), None))
        li = gt[:, :, 8 * d:8 * d + 4]
        cx.act(fa[:, d], Bp[:, d], AF.Exp, scale=-1.0)
        cx.act(fdec[:, d], Bt[:, d], AF.Exp, scale=-1.0)
        cx.op(DVE, "tensor_tensor", out=Bp[:, d], in0=Bp[:, d], in1=li, op=ALU.add)
        cx.act(fg[:, d], Bp[:, d], AF.Exp)
        cx.op(DVE, "tensor_tensor", out=Bp[:, d], in0=Bp[:, d], in1=Bt[:, d], op=ALU.subtract)
        cx.act(fgk[:, d], Bp[:, d], AF.Exp)
    C32_ = [cx.sb((128, 2, 129), F32) for _ in range(2)]; Cb_ = [cx.sb((128, 2, 129), BF16) for _ in range(2)]
    PT_ = [cx.sb((128, 128), BF16) for _ in range(4)]
    kg_ = [cx.sb((128, 256), BF16) for _ in range(4)]
    dn_ = [cx.sb((128, 4), F32) for _ in range(4)]
    orders = [list(range(NT)), [1, 0] + list(range(NT - 1, 1, -1))]
    masks = [k.trifb, k.tribb]
    written = set()
    if True:
        def prep(d, tt, j):
            t0 = tt * 128
            kg = kg_[2 * d + j]
            pk = nps(k)
            for h in range(4):
                cx.mm(pk[:, h * 64:(h + 1) * 64], xcT.s(tt)[:, h, t0:t0 + 128], wkb[:, h, :], start=True, stop=True)
            for h in range(4):
                cx.op(DVE, "tensor_scalar", out=kg[:, h * 64:(h + 1) * 64], in0=pk[:, h * 64:(h + 1) * 64], scalar1=fgk[:, d, tt, h:h + 1], scalar2=0.125, op0=ALU.mult, op1=ALU.mult)

        def body(d, tt, j):
            t0 = tt * 128
            mask, C32, Cb = masks[d], C32_[d], Cb_[d]
            kg, dn = kg_[2 * d + j], dn_[2 * d + j]
            first = tt not in written
            written.add(tt)
            pos = [nps(k), nps(k)]
            for h in range(4):
                c, r0 = h // 2, (h % 2) * 64
                pa = nps(k)
                cx.mm(pa[:, 0:128], kT.s(tt)[r0:r0 + 64, c, t0:t0 + 128], qT.s(tt)[r0:r0 + 64, c, t0:t0 + 128])
                PT = PT_[h % 4]
                cx.op(DVE, "scalar_tensor_tensor", out=PT[:], in0=pa[:, 0:128], scalar=fg[:, d, tt, h:h + 1], in1=mask[:], op0=ALU.mult, op1=ALU.mult)
                o = (h % 2) * 129
                cx.mm(pos[c][:, o:o + 129], PT[:], vaug.s(tt)[:, tt, h, :], start=True, stop=False)
                cx.mm(pos[c][:, o:o + 129], qT.s(tt)[r0:r0 + 64, c, t0:t0 + 128], Cb[r0:r0 + 64, c, :], start=False, stop=True)
            pu = nps(k)
            for h in range(4):
                c, r0 = h // 2, (h % 2) * 64
                cx.mm(pu[r0:r0 + 64, c * 129:(c + 1) * 129], kg[:, h * 64:(h + 1) * 64], vaug.s(tt)[:, tt, h, :], start=True, stop=True)
            for c in range(2):
                po = pos[c]
                den = V(po, po.t[:, 0:258].rearrange("p (a b) -> p a b", b=129)[:, :, 128], None)
                cx.op(DVE, "tensor_tensor", out=dn[:, 0:2], in0=den, in1=fa[:, d, tt, 2 * c:2 * c + 2], op=ALU.mult)
                cx.act(dn[:, 0:2], dn[:, 0:2], AF.Abs)
                cx.op(DVE, "tensor_scalar", out=dn[:, 0:2], in0=dn[:, 0:2], scalar1=1.0, scalar2=None, op0=ALU.max)
                cx.op(DVE, "reciprocal", out=dn[:, 0:2], in_=dn[:, 0:2])
                cx.op(DVE, "tensor_tensor", out=dn[:, 2:4], in0=dn[:, 0:2], in1=fa[:, d, tt, 2 * c:2 * c + 2], op=ALU.mult)
                for hh in range(2):
                    h = 2 * c + hh
                    dst = hml.s(tt)[:, tt, h * 128:(h + 1) * 128]
                    if first:
                        cx.op(DVE, "tensor_scalar", out=dst, in0=po[:, hh * 129:hh * 129 + 128], scalar1=dn[:, 2 + hh:3 + hh], scalar2=None, op0=ALU.mult)
                    else:
                        cx.op(DVE, "scalar_tensor_tensor", out=dst, in0=po[:, hh * 129:hh * 129 + 128], scalar=dn[:, 2 + hh:3 + hh], in1=dst, op0=ALU.mult, op1=ALU.add)
            for h in range(4):
                c, r0 = h // 2, (h % 2) * 64
                cx.op(DVE, "scalar_tensor_tensor", out=C32[r0:r0 + 64, c, :], in0=C32[r0:r0 + 64, c, :], scalar=fdec[r0:r0 + 64, d, tt, h:h + 1], in1=pu[r0:r0 + 64, c * 129:(c + 1) * 129], op0=ALU.mult, op1=ALU.add)
            cx.act(Cb[:], C32[:], AF.Copy)

        for d in range(2):
            _memset(cx, C32_[d][:], 0.0); _memset(cx, Cb_[d][:], 0.0)
            prep(d, orders[d][0], 0)
        for i in range(NT):
            for d in range(2):
                if i + 1 < NT:
                    prep(d, orders[d][i + 1], (i + 1) % 2)
                body(d, orders[d][i], i % 2)
    cx.pop()
    y = cx.sb((128, 4, T), BF16, NT)
    cx.push()
    wo = wload(k, W[:, OFF["ml_o"]:OFF["ml_o"] + 512], 8, 512)
    sgo = [cx.sb((128, 512), BF16) for _ in range(2)]

    def og(tt, p):
        cx.act(sgo[tt % 2][:], p[:, :], AF.Sigmoid)
        cx.op(DVE, "tensor_tensor", out=hml.s(tt)[:, tt, :], in0=hml.s(tt)[:, tt, :], in1=sgo[tt % 2][:], op=ALU.mult)
    proj_tm(k, hT, wo, og)
    t1_ = [cx.sb((128, 128), F32) for _ in range(2)]
    t2_ = [cx.sb((128, 128), F32) for _ in range(2)]
    ej = [0]

    def extra(tt, h, pv, szv):
        j = ej[0] % 2; ej[0] += 1
        t0 = tt * 128
        cx.act(t1_[j][:], pv, AF.Identity, scale=dc[:, DC_MLOG + h:DC_MLOG + h + 1])
        cx.op(DVE, "scalar_tensor_tensor", out=t2_[j][:], in0=xcT.s(tt)[:, h, t0:t0 + 128], scalar=k.pc[:, l, PC_MLSKIP + h:PC_MLSKIP + h + 1], in1=t1_[j][:], op0=ALU.mult, op1=ALU.add)
        cx.op(DVE, "tensor_tensor", out=y.s(tt)[:, h, t0:t0 + 128], in0=t2_[j][:], in1=szv, op=ALU.mult)
    out_stage(k, l, 2, hT, lambda tt: hml.s(tt)[:, tt, :], y, dc, DC_MLOG, OFF["ml_z"], last, extra)
    cx.pop()
    if k.dbg and s == 0 and l == 0:
        dump(k, y, k.dbg_y[2], 4)
    merge(k, l, 2, y, hT, acc, BLKS[1:] if last else BLKS)
    cx.pop()


def dumpv(k, name, v, ncols):
    cx = k.cx
    dst = k.nc.dram_tensor(name, [128, ncols], F32, kind="ExternalOutput").ap()
    cx.push()
    f = cx.sb((128, 256), F32)
    for j in range(0, ncols, 256):
        w = min(256, ncols - j)
        cx.op(DVE, "tensor_copy", out=f[:, 0:w], in_=V(v.tile, v.ap[:, j:j + w], v.subs))
        cx.dma(SP, U(dst[:, j:j + w]), f[:, 0:w])
    cx.pop()


def dump(k, t, dst, nch):
    cx = k.cx
    cx.push()
    f = cx.sb((128, 256), F32)
    for c in range(nch):
        for j in range(T // 256):
            cx.op(DVE, "tensor_copy", out=f[:], in_=t[:, c, j * 256:(j + 1) * 256])
            cx.dma(SP, U(dst[:, c, j * 256:(j + 1) * 256]), f[:])
    cx.pop()


def layer(k, s, l, last):
    cx = k.cx
    cx.push()
    hT = cx.sb((128, 8, T), BF16, NT)
    acc = cx.sb((128, 8, T), BF16, NT)
    stg = int(os.environ.get("KSTAGE", "9"))
    if stg < 1:
        cx.pop(); return
    dc, pr = layer_params(k, l)
    if stg < 2:
        cx.pop(); return
    stage_h(k, s, l, hT)
    if stg < 3:
        if k.dbg:
            dump(k, hT, k.dbg_h, 8)
        cx.pop(); return
    if k.dbg and s == 0 and l == 0:
        dump(k, hT, k.dbg_h, 8)
    mla(k, s, l, hT, acc, dc, last)
    if stg < 7:
        cx.pop(); return
    mix = os.environ.get("KMIX", "123")
    if "1" in mix:
        gla(k, s, l, hT, acc, dc, last)
    if "2" in mix:
        mlstm(k, s, l, hT, acc, dc, pr, last)
    if "3" in mix:
        diffattn(k, s, l, hT, acc, dc, last)
    if k.dbg and s == 0 and l == 0:
        dump(k, acc, k.dbg_acc, 8)
    final(k, s, l, acc, last)
    cx.pop()


def _consts():
    ident = np.eye(128, dtype=np.float32)
    bd = np.zeros((128, 128), np.float32); bd[:64, :64] = 1; bd[64:, 64:] = 1
    rm = np.zeros((64, 64), np.float32)
    for i in range(16):
        rm[16 + i, i] = -1; rm[i, 16 + i] = 1; rm[48 + i, 32 + i] = -1; rm[32 + i, 48 + i] = 1
    rm2 = np.zeros((128, 128), np.float32); rm2[:64, :64] = rm; rm2[64:, 64:] = rm
    si, ti = np.meshgrid(np.arange(128), np.arange(128), indexing="ij")
    trif = (si <= ti).astype(np.float32); trib = (si >= ti).astype(np.float32)
    quarter = 16
    inv_freq = (10000.0 ** (-np.arange(quarter, dtype=np.float32) / quarter)).astype(np.float32)
    row = np.repeat(np.arange(32, dtype=np.float32), 64); col = np.tile(np.arange(64, dtype=np.float32), 32)
    ar = row[:, None] * inv_freq; ac = col[:, None] * inv_freq
    ang = np.concatenate([ar, ar, ac, ac], axis=-1).astype(np.float32)
    cos = np.concatenate([np.ones((NCTX, 64), np.float32), np.cos(ang)], 0)
    sin = np.concatenate([np.zeros((NCTX, 64), np.float32), np.sin(ang)], 0)
    cosT = np.ascontiguousarray(np.concatenate([cos.T, cos.T], 0)); sinT = np.ascontiguousarray(np.concatenate([sin.T, sin.T], 0))
    return dict(cident=ident, cbd64=bd, crm2=rm2, ctrif=trif, ctrib=trib, ccos=cosT.astype(np.float32), csin=sinT.astype(np.float32))


def _cols(v):
    v = np.asarray(v, np.float32).reshape(-1)
    return v.reshape(-1, 128).T


def _pack(inp):
    pcols = np.zeros((128, NL, NPC), np.float32)
    prow = np.zeros((NL, NPR), np.float32)
    for l in range(NL):
        P = pcols[:, l]
        P[:, PC_NORMG:PC_NORMG + 8] = _cols(inp["norm_g"][l])
        P[:, PC_CQG:PC_CQG + 3] = _cols(inp["mla_cq_g"][l]); P[:, PC_CKVG:PC_CKVG + 2] = _cols(inp["mla_ckv_g"][l])
        qg, kg = inp["mla_q_g"][l], inp["mla_k_g"][l]
        P[:, PC_QGN] = qg[:128]; P[:, PC_QGR] = np.concatenate([qg[128:], qg[128:]])
        P[:, PC_KGN] = kg[:128]; P[:, PC_KGR] = np.concatenate([kg[128:], kg[128:]])
        P[:, PC_DFQG] = np.concatenate([inp["df_qk_g"][l, 0]] * 2); P[:, PC_DFKG] = np.concatenate([inp["df_qk_g"][l, 1]] * 2)
        P[:, PC_DFOG] = inp["df_out_g"][l]
        P[:, PC_GLAOG:PC_GLAOG + 4] = _cols(inp["gla_out_g"][l]); P[:, PC_MLOG:PC_MLOG + 4] = _cols(inp["ml_out_g"][l])
        P[:, PC_MLSKIP:PC_MLSKIP + 4] = _cols(inp["ml_skip"][l])
        for j in range(3):
            P[:, PC_CONVW + 4 * j:PC_CONVW + 4 * j + 4] = _cols(inp["ml_conv_w"][l, j])
        P[:, PC_CONVB:PC_CONVB + 4] = _cols(inp["ml_conv_b"][l])
        for d_ in range(2):
            P[:, PC_GLAAB + 2 * d_:PC_GLAAB + 2 * d_ + 2] = _cols(inp["gla_a_b"][l, d_])
        prow[l, 0:16] = inp["ml_gate_b"][l].reshape(-1)
        prow[l, 16:272] = inp["df_lambda"][l].reshape(-1)
    return pcols, prow


def _perm_wuq(w):
    w = w.reshape(NL, 384, 4, 192)
    return np.ascontiguousarray(np.concatenate([w[..., :128].reshape(NL, 384, 512), w[..., 128:].reshape(NL, 384, 256)], -1))


def _perm_wukv(w):
    w = w.reshape(NL, 256, 4, 256)
    return np.ascontiguousarray(np.concatenate([w[..., :128].reshape(NL, 256, 512), w[..., 128:].reshape(NL, 256, 512)], -1))


_CACHE = {}


def host_maps(inp, ncores=8):
    inp = {k_: np.asarray(v, np.float32) for k_, v in inp.items()}
    pcols, prow = _pack(inp)
    shared = dict(ada_w=inp["ada_w"], ada_b=inp["ada_b"], w_in=inp["w_in"], wuq=_perm_wuq(inp["mla_wuq"]),
                  wukv=_perm_wukv(inp["mla_wukv"]), gla_a_w=inp["gla_a_w"], ml_wq=inp["ml_wq"], ml_wk=inp["ml_wk"],
                  br_w=inp["br_w"], w_out=inp["w_out"], pcols=pcols, prow=prow, **_consts())
    maps = []
    for c in range(ncores):
        m = dict(shared)
        m["x"] = np.ascontiguousarray(inp["x"][4 * c:4 * c + 4]); m["ctx"] = np.ascontiguousarray(inp["ctx"][4 * c:4 * c + 4])
        m["c5"] = np.ascontiguousarray(np.concatenate([inp["c"][4 * c:4 * c + 4], inp["c_ctx"][None]], 0))
        maps.append(m)
    return maps


def kernel(**inputs):
    if "nc" not in _CACHE:
        _CACHE["nc"] = build(4, NL)[0]
    nc = _CACHE["nc"]
    maps = host_maps(inputs)
    res = run_bass_kernel_spmd(nc, maps, core_ids=list(range(8)))
    return np.concatenate([np.asarray(r["out"], np.float32) for r in res.results], 0)
```

```python
import math
import contextlib
import numpy as np
import concourse.bass as bass
import concourse.mybir as mybir
from concourse.bass_utils import run_bass_kernel_spmd

F32 = mybir.dt.float32
BF16 = mybir.dt.bfloat16
AF = mybir.ActivationFunctionType
ALU = mybir.AluOpType
AX = mybir.AxisListType

T = 2304
NCTX = 256
NT = 18
D = 1024
DIN = 10992
NL = 4
EPS = 1e-6
BLKS = [(0, 256), (256, 512), (768, 512), (1280, 512), (1792, 512)]
OFF = dict(mla_cq=0, mla_ckv=384, mla_kr=640, mla_z=704, gla_q=1216, gla_k=1472, gla_v=1728, gla_af=2240,
           gla_ab=2256, gla_z=2272, ml_x=2784, ml_v=3296, ml_o=3808, ml_if=4320, ml_z=4336, df_q=4848,
           df_k=5360, df_v=5872, df_z=6384, merge=6896)
PC_NORMG, PC_CQG, PC_CKVG, PC_QGN, PC_QGR, PC_KGN, PC_KGR, PC_DFQG, PC_DFKG, PC_DFOG = 0, 8, 11, 13, 14, 15, 16, 17, 18, 19
PC_GLAOG, PC_MLOG, PC_MLSKIP, PC_CONVW, PC_CONVB, PC_GLAAB, NPC = 20, 24, 28, 32, 44, 48, 52
NPR = 16 + 256
DC_CQG, DC_CKVG, DC_QGN, DC_QGR, DC_KGN, DC_KGR, DC_DFQG, DC_DFKG, DC_DFOG, DC_GLAOG, DC_MLOG, DC_NAB, DC_LAM, NDC = 0, 3, 5, 6, 7, 8, 9, 10, 11, 12, 16, 20, 24, 28

import os
PE, ACT, DVE, POOL, SP = range(5)
SERIAL = os.environ.get("SERIAL", "0") == "1"
PEL = DVE if os.environ.get("NOPOOL", "0") == "1" else POOL
NDS = 24


class V:
    __slots__ = ("tile", "ap", "subs")

    def __init__(self, tile, ap, subs):
        self.tile, self.ap, self.subs = tile, ap, subs


class _Sel:
    def __init__(self, tile, subs):
        self.tile, self.subs = tile, subs

    def __getitem__(self, idx):
        return V(self.tile, self.tile.t[idx], self.subs)


class Tile:
    def __init__(self, t, nsub=1):
        self.t = t
        self.nsub = nsub
        self.w = [None] * nsub
        self.r = [dict() for _ in range(nsub)]
        self.excl = False

    def __getitem__(self, idx):
        return V(self, self.t[idx], None)

    def s(self, subs):
        if isinstance(subs, int):
            subs = [subs]
        return _Sel(self, list(subs))


FASTSE = os.environ.get("FASTSE", "1") == "1"


def _fsize(ap):
    n = 1
    for d_ in ap.shape[1:]:
        n *= d_
    return n


def tk(a, b):
    return list(range(a // 128, (b + 127) // 128))


class Ctx:
    def __init__(self, nc):
        self.nc = nc
        self.engs = [nc.tensor, nc.scalar, nc.vector, nc.gpsimd, nc.sync]
        self.es = contextlib.ExitStack()
        self.esem = [self.es.enter_context(nc.semaphore(f"e{i}")) for i in range(5)]
        self.dsem = [self.es.enter_context(nc.semaphore(f"d{i}")) for i in range(NDS)]
        self.cnt = [0] * 5
        self.dval = [0] * NDS
        self.dk = 0
        self.dkp = 0
        self.seen = [dict() for _ in range(5)]
        self.scopes = [self.es]
        self.scope_tiles = [[]]
        self.freed = {}
        self.uid = 0
        self.ninstr = 0
        self.last = None

    def push(self):
        st = contextlib.ExitStack()
        self.scopes.append(st)
        self.scope_tiles.append([])
        return st

    def pop(self):
        for tl in self.scope_tiles.pop():
            for s in range(tl.nsub):
                w = tl.w[s]
                if w is not None and self.freed.get(w[0], 0) < w[1]:
                    self.freed[w[0]] = w[1]
                for key, val in tl.r[s].items():
                    if self.freed.get(key, 0) < val:
                        self.freed[key] = val
        self.scopes.pop().close()

    def sb(self, shape, dt, nsub=1, name=None):
        self.uid += 1
        t = self.scopes[-1].enter_context(self.nc.sbuf_tensor(f"{name or 't'}{self.uid}", list(shape), dt))
        tl = Tile(t, nsub)
        if self.freed:
            for s in range(nsub):
                tl.r[s] = dict(self.freed)
        self.scope_tiles[-1].append(tl)
        return tl

    def psum(self, shape, dt):
        self.uid += 1
        t = self.scopes[-1].enter_context(self.nc.psum_tensor(f"ps{self.uid}", list(shape), dt))
        tl = Tile(t, 1)
        tl.excl = True
        return tl

    def _sync(self, eng, reads, writes, extra=None):
        need = {}

        def add(key, val):
            if key[0] == "E" and key[1] == eng and eng == PE:
                return
            if need.get(key, 0) < val:
                need[key] = val

        same = ("E", eng)
        fast = FASTSE and eng in (ACT, DVE)
        for v in reads:
            if v.tile is None:
                continue
            big = fast and _fsize(v.ap) >= 128
            for s in (v.subs if v.subs is not None else range(v.tile.nsub)):
                w = v.tile.w[s]
                if w is not None and not (big and w[0] == same):
                    add(w[0], w[1])
                if v.tile.excl:
                    for key, val in v.tile.r[s].items():
                        if not (key[0] == "E" and key[1] == eng):
                            add(key, val)
        for v in writes:
            if v.tile is None:
                continue
            for s in (v.subs if v.subs is not None else range(v.tile.nsub)):
                w = v.tile.w[s]
                if w is not None and not (fast and w[0] == same):
                    add(w[0], w[1])
                for key, val in v.tile.r[s].items():
                    if not (fast and key == same):
                        add(key, val)
        if extra is not None and extra[1] > 0:
            add(extra[0], extra[1])
        if SERIAL and self.last is not None:
            if not (self.last[0][0] == "E" and self.last[0][1] == eng and eng == PE):
                if need.get(self.last[0], 0) < self.last[1]:
                    need[self.last[0]] = self.last[1]
        seen = self.seen[eng]
        for key, val in need.items():
            if seen.get(key, 0) >= val:
                continue
            seen[key] = val
            sem = self.esem[key[1]] if key[0] == "E" else self.dsem[key[1]]
            self.engs[eng].wait_ge(sem, val)
            self.ninstr += 1

    def _commit(self, tok, reads, writes):
        key, val = tok
        self.last = tok
        for v in reads:
            if v.tile is None:
                continue
            for s in (v.subs if v.subs is not None else range(v.tile.nsub)):
                r = v.tile.r[s]
                if r.get(key, 0) < val:
                    r[key] = val
        for v in writes:
            if v.tile is None:
                continue
            for s in (v.subs if v.subs is not None else range(v.tile.nsub)):
                v.tile.w[s] = tok
                v.tile.r[s] = {}

    def op(self, eng, name, **kw):
        reads, writes, args = [], [], {}
        for k, v in kw.items():
            if isinstance(v, V):
                (writes if k in ("out", "accum_out") else reads).append(v)
                args[k] = v.ap
            else:
                args[k] = v
        self._sync(eng, reads, writes)
        ins = getattr(self.engs[eng], name)(**args)
        self.cnt[eng] += 1
        ins.then_inc(self.esem[eng], 1)
        self.ninstr += 1
        self._commit((("E", eng), self.cnt[eng]), reads, writes)

    def dma(self, q, out, in_, **kw):
        if q == POOL:
            k = 16 + self.dkp
            self.dkp = (self.dkp + 1) % (NDS - 16)
        else:
            k = self.dk
            self.dk = (self.dk + 1) % 16
        prev = self.dval[k]
        self._sync(q, [in_], [out], extra=(("D", k), prev))
        ins = self.engs[q].dma_start(out=out.ap, in_=in_.ap, **kw)
        ins.then_inc(self.dsem[k], 16)
        self.ninstr += 1
        self.dval[k] = prev + 16
        self._commit((("D", k), prev + 16), [in_], [out])

    def finish(self):
        for k in range(NDS):
            if self.dval[k] > 0:
                self.nc.sync.wait_ge(self.dsem[k], self.dval[k])
        for e in range(4):
            if self.cnt[e] > 0:
                self.nc.sync.wait_ge(self.esem[e], self.cnt[e])

    def mm(self, out, lhsT, rhs, start=True, stop=True):
        self.op(PE, "matmul", out=out, lhsT=lhsT, rhs=rhs, start=start, stop=stop, skip_group_check=True)

    def tr(self, out, in_, identity):
        self.op(PE, "transpose", out=out, in_=in_, identity=identity)

    def act(self, out, in_, func, **kw):
        self.op(ACT, "activation", out=out, in_=in_, func=func, **kw)


def U(ap):
    return V(None, ap, None)


class K:
    pass


def build(nseq, nlayers, dbg=False):
    nc = bass.Bass("TRN2", target_bir_lowering=False)
    cx = Ctx(nc)
    k = K()
    k.nc, k.cx, k.dbg = nc, cx, dbg

    def din(name, shape, dt=F32):
        return nc.dram_tensor(name, list(shape), dt, kind="ExternalInput").ap()

    k.x = din("x", [4, 2048, D]); k.ctxin = din("ctx", [4, NCTX, D]); k.c5 = din("c5", [5, D])
    k.ada_w = din("ada_w", [NL, D, 3 * D]); k.ada_b = din("ada_b", [NL, 3 * D])
    k.w_in = din("w_in", [NL, D, DIN]); k.wuq = din("wuq", [NL, 384, 768]); k.wukv = din("wukv", [NL, 256, 1024])
    k.gla_a_w = din("gla_a_w", [NL, 2, 16, 256]); k.ml_wq = din("ml_wq", [NL, 4, 128, 64]); k.ml_wk = din("ml_wk", [NL, 4, 128, 64])
    k.br_w = din("br_w", [NL, 4, 512, D]); k.w_out = din("w_out", [NL, D, D])
    k.pcols = din("pcols", [128, NL, NPC]); k.prow = din("prow", [NL, NPR])
    k.cident = din("cident", [128, 128]); k.cbd64 = din("cbd64", [128, 128]); k.crm2 = din("crm2", [128, 128])
    k.ctrif = din("ctrif", [128, 128]); k.ctrib = din("ctrib", [128, 128])
    k.ccos = din("ccos", [128, T]); k.csin = din("csin", [128, T])
    k.out = nc.dram_tensor("out", [4, 2048, D], F32, kind="ExternalOutput").ap()
    k.xres_ap = nc.dram_tensor("xres", [4, T, D], F32, kind="Internal").ap()
    k.gscr_ap = nc.dram_tensor("gscr", [NL, 5, D], F32, kind="Internal").ap()
    k.xres = [Tile(k.xres_ap[s], NT) for s in range(4)]
    k.gscr = Tile(k.gscr_ap, NL)
    if dbg:
        k.dbg_y = nc.dram_tensor("dbg_y", [4, 128, 4, T], F32, kind="ExternalOutput").ap()
        k.dbg_acc = nc.dram_tensor("dbg_acc", [128, 8, T], F32, kind="ExternalOutput").ap()
        k.dbg_h = nc.dram_tensor("dbg_h", [128, 8, T], F32, kind="ExternalOutput").ap()
        k.dbg_x = nc.dram_tensor("dbg_x", [T, D], F32, kind="ExternalOutput").ap()

    def cload(src, dt, shape=(128, 128), q=POOL):
        t = cx.sb(shape, dt)
        cx.dma(q, t[:], U(src))
        return t

    k.identb = cload(k.cident, BF16); k.identf = cload(k.cident, F32, q=SP)
    k.bd64 = cload(k.cbd64, BF16); k.rm2 = cload(k.crm2, BF16)
    k.trif = cload(k.ctrif, F32, q=SP); k.trib = cload(k.ctrib, F32, q=SP)
    k.trifb = cload(k.ctrif, BF16); k.tribb = cload(k.ctrib, BF16)
    k.cos = cload(k.ccos, BF16, (128, T)); k.sin = cload(k.csin, BF16, (128, T))
    k.onesb = cx.sb((128, 128), BF16)
    k.onesf = cx.sb((128, 128), F32)
    _memset(cx, k.onesb[:], 1.0); _memset(cx, k.onesf[:], 1.0)
    k.epsc = cx.sb((128, 1), F32); _memset(cx, k.epsc[:], EPS)
    k.onec = cx.sb((128, 1), F32); _memset(cx, k.onec[:], 1.0)
    k.pc = cx.sb((128, NL, NPC), F32)
    cx.dma(SP, k.pc[:], U(k.pcols))
    k.AB = cx.sb((128, NL, 5, 16), F32)
    k.ps = [cx.psum((128, 512), F32) for _ in range(8)]
    k.psi = 0
    k.wpool = [cx.sb((128, 8, 512), BF16) for _ in range(3)]
    k.wi = 0

    for s in range(nseq):
        cx.dma(SP, k.xres[s].s([0, 1])[0:NCTX, :], U(k.ctxin[s]))
        for j in range(4):
            cx.dma(SP, k.xres[s].s(tk(NCTX + j * 512, NCTX + (j + 1) * 512))[NCTX + j * 512:NCTX + (j + 1) * 512, :],
                   U(k.x[s, j * 512:(j + 1) * 512, :]))

    prologue(k)
    for s in range(nseq):
        for l in range(nlayers):
            layer(k, s, l, last=(l == NL - 1))
    cx.finish()
    cx.es.close()
    return nc, cx


def _memset(cx, v, val, eng=DVE):
    cx._sync(eng, [], [v])
    ins = cx.engs[eng].memset(v.ap, val)
    cx.cnt[eng] += 1
    ins.then_inc(cx.esem[eng], 1)
    cx._commit((("E", eng), cx.cnt[eng]), [], [v])


def nps(k):
    p = k.ps[k.psi]
    k.psi = (k.psi + 1) % 8
    return p


def wload(k, src2d, kc, ncols):
    wt = k.wpool[k.wi]
    k.wi = (k.wi + 1) % len(k.wpool)
    k.cx.dma(POOL, wt[:, 0:kc, 0:ncols], U(src2d.rearrange("(kc p) n -> p kc n", p=128)))
    return wt


def prologue(k):
    cx, nc = k.cx, k.nc
    cx.push()
    c5t = cx.sb((5, D), F32)
    cx.dma(SP, c5t[:], U(k.c5))
    cx.act(c5t[:], c5t[:], AF.Silu)
    scT = cx.sb((128, 8, 5), F32)
    p = nps(k)
    for kc in range(8):
        cx.tr(p[:, kc * 8:kc * 8 + 5], c5t[0:5, kc * 128:(kc + 1) * 128], k.identf[0:5, 0:5])
    cx.op(DVE, "tensor_copy", out=scT[:], in_=V(p, p.t[:, 0:64].rearrange("p (a b) -> p a b", b=8)[:, :, 0:5], None))
    wf = [cx.sb((128, 8, 512), F32) for _ in range(2)]
    adab = cx.sb((5, 3 * D), F32)
    modrow = cx.sb((5, 3 * D), F32)
    modcol = cx.sb((128, 16, 5), F32)
    for l in range(NL):
        cx.dma(SP, adab[:], U(k.ada_b[l].partition_broadcast(5)))
        for nb in range(6):
            w = wf[nb % 2]
            cx.dma(SP, w[:], U(k.ada_w[l][:, nb * 512:(nb + 1) * 512].rearrange("(kc p) n -> p kc n", p=128)))
            p = nps(k)
            for kc in range(8):
                cx.mm(p[0:5, :], scT[:, kc, :], w[:, kc, :], start=(kc == 0), stop=(kc == 7))
            cx.op(DVE, "tensor_tensor", out=modrow[:, nb * 512:(nb + 1) * 512], in0=p[0:5, :], in1=adab[:, nb * 512:(nb + 1) * 512], op=ALU.add)
        p = nps(k)
        for j in range(16):
            cx.tr(p[:, j * 8:j * 8 + 5], modrow[0:5, j * 128:(j + 1) * 128], k.identf[0:5, 0:5])
        cx.op(DVE, "tensor_copy", out=modcol[:], in_=V(p, p.t[:, 0:128].rearrange("p (a b) -> p a b", b=8)[:, :, 0:5], None))
        for r in range(5):
            cx.op(DVE, "scalar_tensor_tensor", out=k.AB[:, l, r, 0:8], in0=modcol[:, 8:16, r], scalar=1.0, in1=k.pc[:, l, PC_NORMG:PC_NORMG + 8], op0=ALU.add, op1=ALU.mult)
            cx.op(DVE, "tensor_copy", out=k.AB[:, l, r, 8:16], in_=modcol[:, 0:8, r])
        cx.dma(SP, k.gscr.s(l)[l], modrow[0:5, 2 * D:3 * D])
    cx.pop()


def layer_params(k, l):
    cx = k.cx
    dc = cx.sb((128, NDC), F32)
    pc = k.pc

    def sc(dst, src, n, f):
        cx.op(DVE, "tensor_scalar", out=dc[:, dst:dst + n], in0=pc[:, l, src:src + n], scalar1=float(f), scalar2=None, op0=ALU.mult)

    lam_init = 0.8 - 0.6 * math.exp(-0.3 * l)
    sc(DC_CQG, PC_CQG, 3, 1.0); sc(DC_CKVG, PC_CKVG, 2, 1.0)
    sc(DC_QGN, PC_QGN, 1, 192 ** -0.5); sc(DC_QGR, PC_QGR, 1, 192 ** -0.5)
    sc(DC_KGN, PC_KGN, 1, 1.0); sc(DC_KGR, PC_KGR, 1, 1.0)
    sc(DC_DFQG, PC_DFQG, 1, 0.125); sc(DC_DFKG, PC_DFKG, 1, 1.0)
    sc(DC_DFOG, PC_DFOG, 1, (1.0 - lam_init))
    sc(DC_GLAOG, PC_GLAOG, 4, 1.0); sc(DC_MLOG, PC_MLOG, 4, 1.0)
    sc(DC_NAB, PC_GLAAB, 4, -1.0)
    pr = cx.sb((128, NPR), F32)
    cx.dma(SP, pr[:], U(k.prow[l].partition_broadcast(128)))
    junk = cx.sb((128, 64), F32)
    s2 = cx.sb((128, 2), F32)
    for i in range(2):
        cx.op(DVE, "tensor_tensor", out=junk[:], in0=pr[:, 16 + 128 * i:16 + 128 * i + 64], in1=pr[:, 16 + 128 * i + 64:16 + 128 * i + 128], op=ALU.mult)
        cx.op(DVE, "reduce_sum", out=s2[:, i:i + 1], in_=junk[:], axis=AX.X)
    cx.act(s2[:], s2[:], AF.Exp)
    cx.op(DVE, "scalar_tensor_tensor", out=dc[:, DC_LAM:DC_LAM + 1], in0=s2[:, 1:2], scalar=-lam_init, in1=s2[:, 0:1], op0=ALU.add, op1=ALU.subtract)
    return dc, pr


def stage_h(k, s, l, hT):
    cx = k.cx
    cx.push()
    xts = [cx.sb((128, D), F32) for _ in range(2)]
    xns = [cx.sb((128, D), BF16) for _ in range(2)]
    junk = cx.sb((128, D), BF16)
    ss = cx.sb((128, NT), F32)
    rs = cx.sb((128, NT), F32)
    for tt in range(NT):
        xt, xn = xts[tt % 2], xns[tt % 2]
        cx.dma(SP, xt[:], k.xres[s].s(tt)[tt * 128:(tt + 1) * 128, :])
        cx.act(junk[:], xt[:], AF.Square, accum_out=ss[:, tt:tt + 1])
        cx.act(rs[:, tt:tt + 1], ss[:, tt:tt + 1], AF.Sqrt, scale=1.0 / D, bias=k.epsc[:, 0:1])
        cx.op(DVE, "reciprocal", out=rs[:, tt:tt + 1], in_=rs[:, tt:tt + 1])
        cx.op(DVE, "tensor_scalar", out=xn[:], in0=xt[:], scalar1=rs[:, tt:tt + 1], scalar2=None, op0=ALU.mult)
        p = nps(k)
        pb = V(p, p.t[:].bitcast(BF16), None)
        for kc in range(8):
            cx.tr(V(p, pb.ap[:, kc * 128:(kc + 1) * 128], None), xn[:, kc * 128:(kc + 1) * 128], k.identb[:])
        r = 4 if tt < 2 else s
        for kc in range(8):
            src = V(p, pb.ap[:, kc * 128:(kc + 1) * 128], None)
            dst = hT.s(tt)[:, kc, tt * 128:(tt + 1) * 128]
            if kc % 2 == 0:
                cx.act(dst, src, AF.Identity, scale=k.AB[:, l, r, kc:kc + 1], bias=k.AB[:, l, r, 8 + kc:9 + kc])
            else:
                cx.op(DVE, "tensor_scalar", out=dst, in0=src, scalar1=k.AB[:, l, r, kc:kc + 1], scalar2=k.AB[:, l, r, 8 + kc:9 + kc], op0=ALU.mult, op1=ALU.add)
    cx.pop()


def proj_fm(k, src, wt, chunks, evac, blks, kcs=8):
    cx = k.cx
    for (b0, n) in blks:
        for ci, grp in enumerate(chunks):
            p = nps(k)
            for (co, m, r0) in grp:
                for kc in range(kcs):
                    cx.mm(p[r0:r0 + m, 0:n], wt[:, kc, co:co + m], src.s(tk(b0, b0 + n))[:, kc, b0:b0 + n], start=(kc == 0), stop=(kc == kcs - 1))
            evac(ci, p, b0, n)


def norm_group(k, pss, n, gmat, neps, gcols, dsts, tmp):
    cx = k.cx
    nchunk = len(pss)
    for c, p in enumerate(pss):
        cx.act(tmp["sq"][:, c, 0:n], p[:, 0:n], AF.Square)
        cx.op(DVE, "tensor_copy", out=tmp["raw"][:, c, 0:n], in_=p[:, 0:n])
    pq = nps(k)
    for c in range(nchunk):
        cx.mm(pq[:, 0:n], gmat[:], tmp["sq"][:, c, 0:n], start=(c == 0), stop=(c == nchunk - 1))
    cx.act(tmp["rs"][:, 0:n], pq[:, 0:n], AF.Ln, scale=EPS / float(neps), bias=k.epsc[:, 0:1])
    cx.act(tmp["rs"][:, 0:n], tmp["rs"][:, 0:n], AF.Exp, scale=-0.5)
    for c in range(nchunk):
        cx.op(DVE, "scalar_tensor_tensor", out=dsts[c], in0=tmp["raw"][:, c, 0:n], scalar=gcols[c], in1=tmp["rs"][:, 0:n], op0=ALU.mult, op1=ALU.mult)


def rope(k, xn, dst, b0, n, tmp):
    cx = k.cx
    p = nps(k)
    cx.mm(p[:, 0:n], k.rm2[:], xn)
    cx.op(DVE, "tensor_tensor", out=tmp["t1"][:, 0:n], in0=p[:, 0:n], in1=k.sin[:, b0:b0 + n], op=ALU.mult)
    cx.op(PEL, "tensor_tensor", out=tmp["t2"][:, 0:n], in0=xn, in1=k.cos[:, b0:b0 + n], op=ALU.mult)
    cx.op(DVE, "tensor_tensor", out=dst, in0=tmp["t1"][:, 0:n], in1=tmp["t2"][:, 0:n], op=ALU.add)


def mk_tmp(cx, nchunk=3):
    return dict(raw=cx.sb((128, nchunk, 512), BF16), sq=cx.sb((128, nchunk, 512), BF16), rs=cx.sb((128, 512), F32),
                t1=cx.sb((128, 512), F32), t2=cx.sb((128, 512), F32), xn=cx.sb((128, 512), BF16))


def merge(k, l, i, y, hT, acc, blks):
    cx = k.cx
    cx.push()
    sg = [cx.sb((128, 512), BF16) for _ in range(2)]
    tmps = [cx.sb((128, 512), BF16) for _ in range(2)]
    j = 0
    for mg in range(2):
        c0 = OFF["merge"] + i * D + mg * 512
        wg = wload(k, k.w_in[l][:, c0:c0 + 512], 8, 512)
        wb = wload(k, k.br_w[l, i][:, mg * 512:(mg + 1) * 512], 4, 512)
        for mc4 in range(4):
            mc = mg * 4 + mc4
            for (b0, n) in blks:
                sb_ = tk(b0, b0 + n)
                pg = nps(k)
                for kc in range(8):
                    cx.mm(pg[:, 0:n], wg[:, kc, mc4 * 128:(mc4 + 1) * 128], hT.s(sb_)[:, kc, b0:b0 + n], start=(kc == 0), stop=(kc == 7))
                g = sg[j % 2]
                cx.act(g[:, 0:n], pg[:, 0:n], AF.Sigmoid)
                py = nps(k)
                for kc in range(4):
                    cx.mm(py[:, 0:n], wb[:, kc, mc4 * 128:(mc4 + 1) * 128], y.s(sb_)[:, kc, b0:b0 + n], start=(kc == 0), stop=(kc == 3))
                dst = acc.s(sb_)[:, mc, b0:b0 + n]
                if i == 0:
                    cx.op(DVE, "tensor_tensor", out=dst, in0=py[:, 0:n], in1=g[:, 0:n], op=ALU.mult)
                else:
                    t = tmps[j % 2]
                    cx.op(DVE, "tensor_tensor", out=t[:, 0:n], in0=py[:, 0:n], in1=g[:, 0:n], op=ALU.mult)
                    cx.op(PEL, "tensor_tensor", out=dst, in0=acc.s(sb_)[:, mc, b0:b0 + n], in1=t[:, 0:n], op=ALU.add)
                j += 1
    cx.pop()


def final(k, s, l, acc, last):
    cx = k.cx
    cx.push()
    w0 = wload(k, k.w_out[l][:, 0:512], 8, 512)
    w1 = wload(k, k.w_out[l][:, 512:1024], 8, 512)
    gb = [cx.sb((128, D), F32) for _ in range(2)]
    cx.dma(SP, gb[0][:], V(k.gscr, k.gscr.t[l, 4].partition_broadcast(128), [l]))
    cx.dma(SP, gb[1][:], V(k.gscr, k.gscr.t[l, s].partition_broadcast(128), [l]))
    xo = [cx.sb((128, D), F32) for _ in range(2)]
    tm = [cx.sb((128, 512), F32) for _ in range(2)]
    for tt in range(2 if last else 0, NT):
        x_ = xo[tt % 2]
        cx.dma(SP, x_[:], k.xres[s].s(tt)[tt * 128:(tt + 1) * 128, :])
        g = gb[0] if tt < 2 else gb[1]
        for nh, w in enumerate((w0, w1)):
            p = nps(k)
            for kc in range(8):
                cx.mm(p[:, :], acc.s(tt)[:, kc, tt * 128:(tt + 1) * 128], w[:, kc, :], start=(kc == 0), stop=(kc == 7))
            t = tm[nh]
            cx.op(DVE, "tensor_tensor", out=t[:], in0=p[:, :], in1=g[:, nh * 512:(nh + 1) * 512], op=ALU.mult)
            cx.op(PEL if nh else DVE, "tensor_tensor", out=x_[:, nh * 512:(nh + 1) * 512], in0=x_[:, nh * 512:(nh + 1) * 512], in1=t[:], op=ALU.add)
        if k.dbg and s == 0 and l == 0:
            cx.dma(SP, U(k.dbg_x[tt * 128:(tt + 1) * 128, :]), x_[:])
        if last:
            cx.dma(SP, U(k.out[s, (tt - 2) * 128:(tt - 1) * 128, :]), x_[:])
        else:
            cx.dma(SP, k.xres[s].s(tt)[tt * 128:(tt + 1) * 128, :], x_[:])
    cx.pop()


def attn_core(k, nheads, nmaps, qk_fn, v_fn, out_fn, last, blk_fn=None):
    cx = k.cx
    cx.push()
    pts = [cx.sb((128, 512), BF16) for _ in range(4)]
    stage = [cx.sb((128, 129), F32) for _ in range(4 * nmaps)] if nmaps == 2 else None
    pj = 0
    sbank = [k.ps[0], k.ps[1]]
    abanks = [k.ps[2], k.ps[3], k.ps[4], k.ps[5]]
    sj = 0
    for qb, (q0, nq) in enumerate(BLKS):
        if last and qb == 0:
            continue
        kts = [0, 1] if qb == 0 else list(range(NT))
        nqt = nq // 128
        if blk_fn is not None:
            blk_fn(qb, q0, nq)
        for h in range(nheads):
            def accv(m, qi):
                idx = m * 4 + qi
                b = abanks[idx // 2] if nmaps == 2 else abanks[qi // 2]
                o = (idx % 2) * 129
                return b, V(b, b.t[:, o:o + 129], None)
            started = set()
            pend = []

            def do_pv(kt_, items):
                vv = v_fn(h, kt_)
                for (m, pt) in items:
                    for qi in range(nqt):
                        b, av = accv(m, qi)
                        first = id(b) not in started
                        started.add(id(b))
                        cx.mm(av, pt[:, qi * 128:(qi + 1) * 128], vv, start=first, stop=(kt_ == kts[-1]))

            for kt in kts:
                items = []
                for m in range(nmaps):
                    sp_ = sbank[sj % 2]; sj += 1
                    qk_fn(h, m, kt, q0, nq, sp_)
                    pt = pts[pj % 4]; pj += 1
                    cx.act(pt[:, 0:nq], sp_[:, 0:nq], AF.Exp)
                    items.append((m, pt))
                if pend:
                    do_pv(*pend.pop())
                pend.append((kt, items))
            do_pv(*pend.pop())
            stg = []
            for qi in range(nqt):
                row = []
                for m in range(nmaps):
                    if stage is None:
                        row.append(accv(m, qi)[1])
                        continue
                    st_ = stage[m * 4 + qi]
                    cx.act(st_[:, :], accv(m, qi)[1], AF.Copy)
                    row.append(st_[:, :])
                stg.append(row)
            for qi in range(nqt):
                out_fn(h, qb, q0 + qi * 128, stg[qi])
    cx.pop()


def mla(k, s, l, hT, acc, dc, last):
    cx = k.cx
    cx.push()
    qn = cx.sb((128, 4, T), BF16, NT); qr = cx.sb((128, 2, T), BF16, NT)
    kn = cx.sb((128, 4, T), BF16, NT); kr2 = cx.sb((128, T), BF16, NT)
    vaug = cx.sb((128, NT, 4, 129), BF16, NT)
    _memset(cx, vaug[:], 1.0)
    W = k.w_in[l]
    cx.push()
    cqn = cx.sb((128, 3, T), BF16, NT)
    tmp = mk_tmp(cx)
    wA = wload(k, W[:, 0:384], 8, 384)
    wq1 = wload(k, k.wuq[l][:, 0:512], 3, 512)
    wq2 = wload(k, k.wuq[l][:, 512:768], 3, 256)
    for (b0, n) in BLKS:
        sb_ = tk(b0, b0 + n)
        pss = []
        for c in range(3):
            p = nps(k)
            for kc in range(8):
                cx.mm(p[:, 0:n], wA[:, kc, c * 128:(c + 1) * 128], hT.s(sb_)[:, kc, b0:b0 + n], start=(kc == 0), stop=(kc == 7))
            pss.append(p)
        ksub = int(os.environ.get("KSUB", "9"))
        if ksub < 1:
            for c in range(3):
                cx.op(DVE, "tensor_copy", out=cqn.s(sb_)[:, c, b0:b0 + n], in_=pss[c][:, 0:n])
            continue
        norm_group(k, pss, n, k.onesb, 384 * EPS, [dc[:, DC_CQG + c:DC_CQG + c + 1] for c in range(3)],
                   [cqn.s(sb_)[:, c, b0:b0 + n] for c in range(3)], tmp)
        if ksub < 2:
            continue
        for h in range(4):
            p = nps(k)
            for kc in range(3):
                kv = os.environ.get("KV", "0")
                lw = wA if kv == "2" else wq1
                rr = hT if kv == "1" else cqn
                cx.mm(p[:, 0:n], lw[:, kc, h * 128:(h + 1) * 128], rr.s(sb_)[:, kc, b0:b0 + n], start=(kc == 0), stop=(kc == 2))
            if os.environ.get("KQ", "1") == "0":
                cx.op(DVE, "tensor_copy", out=qn.s(sb_)[:, h, b0:b0 + n], in_=p[:, 0:n])
            else:
                norm_group(k, [p], n, k.onesb, 128 * EPS, [dc[:, DC_QGN:DC_QGN + 1]], [qn.s(sb_)[:, h, b0:b0 + n]], tmp)
        if ksub < 3:
            continue
        for c in range(2):
            p = nps(k)
            for kc in range(3):
                cx.mm(p[:, 0:n], wq2[:, kc, c * 128:(c + 1) * 128], cqn.s(sb_)[:, kc, b0:b0 + n], start=(kc == 0), stop=(kc == 2))
            norm_group(k, [p], n, k.bd64, 64 * EPS, [dc[:, DC_QGR:DC_QGR + 1]], [tmp["xn"][:, 0:n]], tmp)
            rope(k, tmp["xn"][:, 0:n], qr.s(sb_)[:, c, b0:b0 + n], b0, n, tmp)
    cx.pop()
    stg = int(os.environ.get("KSTAGE", "9"))
    if stg < 4:
        cx.pop(); return
    cx.push()
    ckvn = cx.sb((128, 2, T), BF16, NT)
    tmp = mk_tmp(cx)
    wB = wload(k, W[:, 384:704], 8, 320)
    wk1 = wload(k, k.wukv[l][:, 0:512], 2, 512)
    for (b0, n) in BLKS:
        sb_ = tk(b0, b0 + n)
        pss = []
        for c in range(2):
            p = nps(k)
            for kc in range(8):
                cx.mm(p[:, 0:n], wB[:, kc, c * 128:(c + 1) * 128], hT.s(sb_)[:, kc, b0:b0 + n], start=(kc == 0), stop=(kc == 7))
            pss.append(p)
        norm_group(k, pss, n, k.onesb, 256 * EPS, [dc[:, DC_CKVG + c:DC_CKVG + c + 1] for c in range(2)],
                   [ckvn.s(sb_)[:, c, b0:b0 + n] for c in range(2)], tmp)
        p = nps(k)
        for r0 in (0, 64):
            for kc in range(8):
                cx.mm(p[r0:r0 + 64, 0:n], wB[:, kc, 256:320], hT.s(sb_)[:, kc, b0:b0 + n], start=(kc == 0), stop=(kc == 7))
        norm_group(k, [p], n, k.bd64, 64 * EPS, [dc[:, DC_KGR:DC_KGR + 1]], [tmp["xn"][:, 0:n]], tmp)
        rope(k, tmp["xn"][:, 0:n], kr2.s(sb_)[:, b0:b0 + n], b0, n, tmp)
        for h in range(4):
            p = nps(k)
            for kc in range(2):
                cx.mm(p[:, 0:n], wk1[:, kc, h * 128:(h + 1) * 128], ckvn.s(sb_)[:, kc, b0:b0 + n], start=(kc == 0), stop=(kc == 1))
            norm_group(k, [p], n, k.onesb, 128 * EPS, [dc[:, DC_KGN:DC_KGN + 1]], [kn.s(sb_)[:, h, b0:b0 + n]], tmp)
    wv1 = wload(k, k.wukv[l][:, 512:1024], 2, 512)
    for tt in range(NT):
        p = nps(k)
        for kc in range(2):
            cx.mm(p[:, :], ckvn.s(tt)[:, kc, tt * 128:(tt + 1) * 128], wv1[:, kc, :], start=(kc == 0), stop=(kc == 1))
        cx.op(DVE, "tensor_copy", out=vaug.s(tt)[:, tt, :, 0:128], in_=V(p, p.t[:, :].rearrange("p (h e) -> p h e", e=128), None))
    cx.pop()
    if stg < 5:
        cx.pop(); return
    if k.dbg and s == 0 and l == 0:
        dumpv(k, "d_kr2", kr2[:, :], T)
        dumpv(k, "d_qr0", qr[:, 0, :], T)
        dumpv(k, "d_qn1", qn[:, 1, :], T)
        dumpv(k, "d_kn1", kn[:, 1, :], T)
    y = cx.sb((128, 4, T), BF16, NT)
    sz = cx.sb((128, 4, 512), BF16)
    wz = wload(k, W[:, OFF["mla_z"]:OFF["mla_z"] + 512], 8, 512)

    def blk_fn(qb, b0, n):
        for c in range(4):
            p = k.ps[6 + c % 2]
            for kc in range(8):
                cx.mm(p[:, 0:n], wz[:, kc, c * 128:(c + 1) * 128], hT.s(tk(b0, b0 + n))[:, kc, b0:b0 + n], start=(kc == 0), stop=(kc == 7))
            cx.act(sz[:, c, 0:n], p[:, 0:n], AF.Silu)

    ob = [cx.sb((128, 128), BF16) for _ in range(2)]
    rc = [cx.sb((128, 1), F32) for _ in range(2)]
    oj = [0]

    def qk_fn(h, m, kt, q0, nq, sp_):
        r0 = (h % 2) * 64
        cx.mm(sp_[:, 0:nq], kn.s(kt)[:, h, kt * 128:(kt + 1) * 128], qn.s(tk(q0, q0 + nq))[:, h, q0:q0 + nq], start=True, stop=False)
        cx.mm(sp_[:, 0:nq], kr2.s(kt)[r0:r0 + 64, kt * 128:(kt + 1) * 128], qr.s(tk(q0, q0 + nq))[r0:r0 + 64, h // 2, q0:q0 + nq], start=False, stop=True)

    def v_fn(h, kt):
        return vaug.s(kt)[:, kt, h, :]

    def out_fn(h, qb, q0, accs):
        a = accs[0]
        j = oj[0]; oj[0] += 1
        o_, r_ = ob[j % 2], rc[j % 2]
        cx.op(DVE, "reciprocal", out=r_[:], in_=V(a.tile, a.ap[:, 128:129], None))
        cx.op(DVE, "tensor_scalar", out=o_[:], in0=V(a.tile, a.ap[:, 0:128], None), scalar1=r_[:, 0:1], scalar2=None, op0=ALU.mult)
        p = k.ps[6 + j % 2]
        pb = V(p, p.t[:].bitcast(BF16)[:, 0:128], None)
        cx.tr(pb, o_[:], k.identb[:])
        tt = q0 // 128
        qoff = q0 - BLKS[qb][0]
        cx.op(DVE, "tensor_tensor", out=y.s(tt)[:, h, q0:q0 + 128], in0=pb, in1=sz[:, h, qoff:qoff + 128], op=ALU.mult)

    attn_core(k, 4, 1, qk_fn, v_fn, out_fn, last, blk_fn)
    if k.dbg and s == 0 and l == 0:
        dump(k, y, k.dbg_y[0], 4)
    if stg < 6:
        cx.pop(); return
    merge(k, l, 0, y, hT, acc, BLKS[1:] if last else BLKS)
    cx.pop()


def sz_block(k, wz, hT, sz, b0, n):
    cx = k.cx
    for c in range(4):
        p = k.ps[6 + c % 2]
        for kc in range(8):
            cx.mm(p[:, 0:n], wz[:, kc, c * 128:(c + 1) * 128], hT.s(tk(b0, b0 + n))[:, kc, b0:b0 + n], start=(kc == 0), stop=(kc == 7))
        cx.act(sz[:, c, 0:n], p[:, 0:n], AF.Silu)


def proj_tm(k, hT, w, dst_fn, ncols=512):
    cx = k.cx
    for tt in range(NT):
        p = nps(k)
        for kc in range(8):
            cx.mm(p[:, 0:ncols], hT.s(tt)[:, kc, tt * 128:(tt + 1) * 128], w[:, kc, 0:ncols], start=(kc == 0), stop=(kc == 7))
        dst_fn(tt, p)


def diffattn(k, s, l, hT, acc, dc, last):
    cx = k.cx
    cx.push()
    qd = cx.sb((128, 4, T), BF16, NT); kd = cx.sb((128, 4, T), BF16, NT)
    vaug = cx.sb((128, NT, 4, 129), BF16, NT)
    _memset(cx, vaug[:], 1.0)
    W = k.w_in[l]
    cx.push()
    tmp = mk_tmp(cx, 1)
    for (dst, off, gcol) in ((qd, OFF["df_q"], DC_DFQG), (kd, OFF["df_k"], DC_DFKG)):
        w = wload(k, W[:, off:off + 512], 8, 512)
        for (b0, n) in BLKS:
            sb_ = tk(b0, b0 + n)
            for h in range(4):
                p = nps(k)
                for kc in range(8):
                    cx.mm(p[:, 0:n], w[:, kc, h * 128:(h + 1) * 128], hT.s(sb_)[:, kc, b0:b0 + n], start=(kc == 0), stop=(kc == 7))
                norm_group(k, [p], n, k.bd64, 64 * EPS, [dc[:, gcol:gcol + 1]], [tmp["xn"][:, 0:n]], tmp)
                rope(k, tmp["xn"][:, 0:n], dst.s(sb_)[:, h, b0:b0 + n], b0, n, tmp)
    wv = wload(k, W[:, OFF["df_v"]:OFF["df_v"] + 512], 8, 512)
    proj_tm(k, hT, wv, lambda tt, p: cx.op(DVE, "tensor_copy", out=vaug.s(tt)[:, tt, :, 0:128],
                                             in_=V(p, p.t[:, :].rearrange("p (h e) -> p h e", e=128), None)))
    cx.pop()
    y = cx.sb((128, 4, T), BF16, NT)
    sz = cx.sb((128, 4, 512), BF16)
    wz = wload(k, W[:, OFF["df_z"]:OFF["df_z"] + 512], 8, 512)
    o1 = [cx.sb((128, 128), F32) for _ in range(2)]
    o2 = [cx.sb((128, 128), F32) for _ in range(2)]
    ob = [cx.sb((128, 128), BF16) for _ in range(2)]
    junk = cx.sb((128, 128), BF16)
    rc = [cx.sb((128, 4), F32) for _ in range(2)]
    oj = [0]

    def qk_fn(h, m, kt, q0, nq, sp_):
        r0 = m * 64
        cx.mm(sp_[:, 0:nq], kd.s(kt)[r0:r0 + 64, h, kt * 128:(kt + 1) * 128], qd.s(tk(q0, q0 + nq))[r0:r0 + 64, h, q0:q0 + nq], start=True, stop=True)

    def v_fn(h, kt):
        return vaug.s(kt)[:, kt, h, :]

    def out_fn(h, qb, q0, accs):
        a0, a1 = accs
        j = oj[0]; oj[0] += 1
        r_ = rc[j % 2]
        cx.op(DVE, "reciprocal", out=r_[:, 0:1], in_=V(a0.tile, a0.ap[:, 128:129], None))
        cx.op(DVE, "reciprocal", out=r_[:, 1:2], in_=V(a1.tile, a1.ap[:, 128:129], None))
        cx.op(DVE, "tensor_tensor", out=r_[:, 1:2], in0=r_[:, 1:2], in1=dc[:, DC_LAM:DC_LAM + 1], op=ALU.mult)
        cx.op(DVE, "tensor_scalar", out=o1[j % 2][:], in0=V(a0.tile, a0.ap[:, 0:128], None), scalar1=r_[:, 0:1], scalar2=None, op0=ALU.mult)
        cx.op(DVE, "scalar_tensor_tensor", out=o2[j % 2][:], in0=V(a1.tile, a1.ap[:, 0:128], None), scalar=r_[:, 1:2], in1=o1[j % 2][:], op0=ALU.mult, op1=ALU.add)
        cx.act(junk[:], o2[j % 2][:], AF.Square, accum_out=r_[:, 2:3])
        cx.act(r_[:, 3:4], r_[:, 2:3], AF.Sqrt, scale=1.0 / 128, bias=k.epsc[:, 0:1])
        cx.op(DVE, "reciprocal", out=r_[:, 3:4], in_=r_[:, 3:4])
        cx.op(DVE, "tensor_scalar", out=ob[j % 2][:], in0=o2[j % 2][:], scalar1=r_[:, 3:4], scalar2=None, op0=ALU.mult)
        p = k.ps[6 + j % 2]
        pb = V(p, p.t[:].bitcast(BF16)[:, 0:128], None)
        cx.tr(pb, ob[j % 2][:], k.identb[:])
        tt = q0 // 128
        qoff = q0 - BLKS[qb][0]
        cx.op(DVE, "scalar_tensor_tensor", out=y.s(tt)[:, h, q0:q0 + 128], in0=pb, scalar=dc[:, DC_DFOG:DC_DFOG + 1], in1=sz[:, h, qoff:qoff + 128], op0=ALU.mult, op1=ALU.mult)

    attn_core(k, 4, 2, qk_fn, v_fn, out_fn, last, lambda qb, b0, n: sz_block(k, wz, hT, sz, b0, n))
    if k.dbg and s == 0 and l == 0:
        dump(k, y, k.dbg_y[3], 4)
    merge(k, l, 3, y, hT, acc, BLKS[1:] if last else BLKS)
    cx.pop()


def out_stage(k, l, i, hT, osrc_fn, y, dc, gcol0, zoff, last, extra_fn=None):
    cx = k.cx
    cx.push()
    sz = cx.sb((128, 4, 512), BF16)
    wz = wload(k, k.w_in[l][:, zoff:zoff + 512], 8, 512)
    on = [cx.sb((128, 512), BF16) for _ in range(2)]
    junk = cx.sb((128, 128), BF16)
    ss = [cx.sb((128, 4), F32) for _ in range(2)]
    for (b0, n) in (BLKS[1:] if last else BLKS):
        sz_block(k, wz, hT, sz, b0, n)
        for tt in tk(b0, b0 + n):
            src = osrc_fn(tt)
            s_, o_ = ss[tt % 2], on[tt % 2]
            for h in range(4):
                cx.act(junk[:], V(src.tile, src.ap[:, h * 128:(h + 1) * 128], src.subs), AF.Square, accum_out=s_[:, h:h + 1])
            cx.act(s_[:], s_[:], AF.Sqrt, scale=1.0 / 128, bias=k.epsc[:, 0:1])
            cx.op(DVE, "reciprocal", out=s_[:], in_=s_[:])
            for h in range(4):
                cx.op(DVE, "tensor_scalar", out=o_[:, h * 128:(h + 1) * 128], in0=V(src.tile, src.ap[:, h * 128:(h + 1) * 128], src.subs), scalar1=s_[:, h:h + 1], scalar2=None, op0=ALU.mult)
            p = nps(k)
            pb = p.t[:].bitcast(BF16)
            for h in range(4):
                cx.tr(V(p, pb[:, h * 128:(h + 1) * 128], None), o_[:, h * 128:(h + 1) * 128], k.identb[:])
            t0 = tt * 128
            for h in range(4):
                pv = V(p, pb[:, h * 128:(h + 1) * 128], None)
                if extra_fn is None:
                    cx.op(DVE, "scalar_tensor_tensor", out=y.s(tt)[:, h, t0:t0 + 128], in0=pv, scalar=dc[:, gcol0 + h:gcol0 + h + 1], in1=sz[:, h, t0 - b0:t0 - b0 + 128], op0=ALU.mult, op1=ALU.mult)
                else:
                    extra_fn(tt, h, pv, sz[:, h, t0 - b0:t0 - b0 + 128])
    cx.pop()


def gla(k, s, l, hT, acc, dc, last):
    cx = k.cx
    cx.push()
    W = k.w_in[l]
    ogl = cx.sb((128, NT, 512), BF16, NT)
    cx.push()
    qT = cx.sb((128, 2, T), BF16, NT); kT = cx.sb((128, 2, T), BF16, NT)
    afT = cx.sb((16, 2, T), BF16, NT)
    vtm = cx.sb((128, NT, 512), BF16, NT)
    aw = cx.sb((16, 2, 256), BF16)
    cx.dma(POOL, aw[:], U(k.gla_a_w[l].rearrange("d r n -> r d n")))
    wqk = wload(k, W[:, OFF["gla_q"]:OFF["gla_q"] + 512], 8, 512)
    waf = wload(k, W[:, OFF["gla_af"]:OFF["gla_af"] + 32], 8, 32)
    for (b0, n) in BLKS:
        sb_ = tk(b0, b0 + n)
        for c in range(4):
            p = nps(k)
            for kc in range(8):
                cx.mm(p[:, 0:n], wqk[:, kc, c * 128:(c + 1) * 128], hT.s(sb_)[:, kc, b0:b0 + n], start=(kc == 0), stop=(kc == 7))
            if c < 2:
                cx.act(qT.s(sb_)[:, c, b0:b0 + n], p[:, 0:n], AF.Copy, scale=0.125)
            else:
                cx.op(DVE, "tensor_copy", out=kT.s(sb_)[:, c - 2, b0:b0 + n], in_=p[:, 0:n])
        for d in range(2):
            p = nps(k)
            for kc in range(8):
                cx.mm(p[0:16, 0:n], waf[:, kc, d * 16:(d + 1) * 16], hT.s(sb_)[:, kc, b0:b0 + n], start=(kc == 0), stop=(kc == 7))
            cx.op(DVE, "tensor_copy", out=afT.s(sb_)[0:16, d, b0:b0 + n], in_=p[0:16, 0:n])
    wv = wload(k, W[:, OFF["gla_v"]:OFF["gla_v"] + 512], 8, 512)
    proj_tm(k, hT, wv, lambda tt, p: cx.act(vtm.s(tt)[:, tt, :], p[:, :], AF.Copy))
    S32_ = [cx.sb((128, 2, 128), F32) for _ in range(2)]; Sb_ = [cx.sb((128, 2, 128), BF16) for _ in range(2)]
    R = lambda shape, dt, nb=4: [cx.sb(shape, dt) for _ in range(nb)]
    e_ = R((128, 2, 128), F32); lp_ = R((128, 2, 128), F32); Bp_ = R((128, 2, 128), F32); tR_ = R((128, 2, 128), F32)
    E_ = R((128, 2, 128), BF16, 6); qe_ = R((128, 2, 128), BF16); ke_ = R((128, 2, 128), BF16); kl_ = R((128, 2, 128), BF16)
    bt_ = R((128, 2, 4), F32); kltm_ = R((128, 256), BF16); AT_ = R((128, 128), BF16, 4)
    orders = [list(range(NT)), [1, 0] + list(range(NT - 1, 1, -1))]
    masks = [k.trifb, k.tribb]
    written = set()
    if True:
        def prep(d, tt, j):
            t0 = tt * 128
            j = 2 * d + j
            e, lp, Bp, tR, qe, ke, kl, bt, kltm = e_[j], lp_[j], Bp_[j], tR_[j], qe_[j], ke_[j], kl_[j], bt_[j], kltm_[j]
            pl = nps(k)
            for c in range(2):
                cx.mm(pl[:, c * 128:(c + 1) * 128], aw[0:16, d, c * 128:(c + 1) * 128], afT.s(tt)[0:16, d, t0:t0 + 128])
            for c in range(2):
                cx.act(e[:, c, :], pl[:, c * 128:(c + 1) * 128], AF.Exp, scale=-1.0, bias=dc[:, DC_NAB + 2 * d + c:DC_NAB + 2 * d + c + 1])
            cx.act(lp[:], e[:], AF.Ln, bias=k.onec[:, 0:1])
            for c in range(2):
                cx.op(DVE, "tensor_tensor_scan", out=Bp[:, c, :], data0=k.onesf[:, 0:128], data1=lp[:, c, :], initial=0.0, op0=ALU.mult, op1=ALU.add)
            E1, E2, E3 = E_[3 * d], E_[3 * d + 1], E_[3 * d + 2]
            if d == 0:
                cx.op(DVE, "tensor_scalar", out=bt[:, :, 0:1], in0=Bp[:, :, 127:128], scalar1=-1.0 / 16, scalar2=None, op0=ALU.mult)
                cx.act(E1[:], Bp[:], AF.Exp, scale=-1.0 / 16)
                cx.act(E2[:], Bp[:], AF.Exp, scale=1.0 / 16)
                for c in range(2):
                    cx.act(E3[:, c, :], Bp[:, c, :], AF.Exp, scale=1.0 / 16, bias=bt[:, c, 0:1])
            else:
                cx.op(DVE, "tensor_tensor", out=tR[:], in0=lp[:], in1=Bp[:], op=ALU.subtract)
                cx.op(DVE, "tensor_scalar", out=bt[:, :, 0:1], in0=Bp[:, :, 127:128], scalar1=-1.0 / 16, scalar2=None, op0=ALU.mult)
                cx.op(DVE, "tensor_scalar", out=bt[:, :, 1:2], in0=Bp[:, :, 127:128], scalar1=1.0 / 16, scalar2=None, op0=ALU.mult)
                for c in range(2):
                    cx.act(E1[:, c, :], tR[:, c, :], AF.Exp, scale=-1.0 / 16, bias=bt[:, c, 0:1])
                    cx.act(E2[:, c, :], tR[:, c, :], AF.Exp, scale=1.0 / 16, bias=bt[:, c, 1:2])
                cx.act(E3[:], tR[:], AF.Exp, scale=1.0 / 16)
            cx.act(bt[:, :, 2:3], bt[:, :, 0:1], AF.Exp)
            cx.op(DVE, "tensor_tensor", out=qe[:], in0=qT.s(tt)[:, :, t0:t0 + 128], in1=E1[:], op=ALU.mult)
            cx.op(DVE, "tensor_tensor", out=ke[:], in0=kT.s(tt)[:, :, t0:t0 + 128], in1=E2[:], op=ALU.mult)
            cx.op(DVE, "tensor_tensor", out=kl[:], in0=kT.s(tt)[:, :, t0:t0 + 128], in1=E3[:], op=ALU.mult)
            ptr = nps(k)
            pb = ptr.t[:].bitcast(BF16)
            for c in range(2):
                cx.tr(V(ptr, pb[:, c * 128:(c + 1) * 128], None), kl[:, c, :], k.identb[:])
            cx.op(DVE, "tensor_copy", out=kltm[:], in_=V(ptr, pb[:, 0:256], None))

        def body(d, tt, j):
            j = 2 * d + j
            mask, S32, Sb = masks[d], S32_[d], Sb_[d]
            qe, ke, bt, kltm = qe_[j], ke_[j], bt_[j], kltm_[j]
            po = nps(k)
            for h in range(4):
                c, r0 = h // 2, (h % 2) * 64
                pa = nps(k)
                cx.mm(pa[:, 0:128], ke[r0:r0 + 64, c, :], qe[r0:r0 + 64, c, :])
                AT = AT_[h % 4]
                cx.op(DVE, "tensor_tensor", out=AT[:], in0=pa[:, 0:128], in1=mask[:], op=ALU.mult)
                cx.mm(po[:, h * 128:(h + 1) * 128], AT[:], vtm.s(tt)[:, tt, h * 128:(h + 1) * 128], start=True, stop=False)
                cx.mm(po[:, h * 128:(h + 1) * 128], qe[r0:r0 + 64, c, :], Sb[r0:r0 + 64, c, :], start=False, stop=True)
            pu = nps(k)
            for h in range(4):
                c, r0 = h // 2, (h % 2) * 64
                cx.mm(pu[r0:r0 + 64, c * 128:(c + 1) * 128], kltm[:, h * 64:(h + 1) * 64], vtm.s(tt)[:, tt, h * 128:(h + 1) * 128], start=True, stop=True)
            if tt not in written:
                written.add(tt)
                cx.act(ogl.s(tt)[:, tt, :], po[:, :], AF.Copy)
            else:
                cx.op(DVE, "tensor_tensor", out=ogl.s(tt)[:, tt, :], in0=po[:, :], in1=ogl.s(tt)[:, tt, :], op=ALU.add)
            for c in range(2):
                cx.op(DVE, "scalar_tensor_tensor", out=S32[:, c, :], in0=S32[:, c, :], scalar=bt[:, c, 2:3], in1=pu[:, c * 128:(c + 1) * 128], op0=ALU.mult, op1=ALU.add)
            cx.act(Sb[:], S32[:], AF.Copy)

        for d in range(2):
            _memset(cx, S32_[d][:], 0.0); _memset(cx, Sb_[d][:], 0.0)
            prep(d, orders[d][0], 0)
        for i in range(NT):
            for d in range(2):
                if i + 1 < NT:
                    prep(d, orders[d][i + 1], (i + 1) % 2)
                body(d, orders[d][i], i % 2)
    cx.pop()
    y = cx.sb((128, 4, T), BF16, NT)
    out_stage(k, l, 1, hT, lambda tt: ogl.s(tt)[:, tt, :], y, dc, DC_GLAOG, OFF["gla_z"], last)
    if k.dbg and s == 0 and l == 0:
        dump(k, y, k.dbg_y[1], 4)
    merge(k, l, 1, y, hT, acc, BLKS[1:] if last else BLKS)
    cx.pop()


def mlstm(k, s, l, hT, acc, dc, pr, last):
    cx = k.cx
    cx.push()
    W = k.w_in[l]
    xcT = cx.sb((128, 4, T), BF16, NT)
    hml = cx.sb((128, NT, 512), BF16, NT)
    cx.push()
    xraw = cx.sb((128, 4, T), BF16, NT)
    cacc = cx.sb((128, T), F32)
    wx = wload(k, W[:, OFF["ml_x"]:OFF["ml_x"] + 512], 8, 512)
    for (b0, n) in BLKS:
        sb_ = tk(b0, b0 + n)
        for c in range(4):
            p = nps(k)
            for kc in range(8):
                cx.mm(p[:, 0:n], wx[:, kc, c * 128:(c + 1) * 128], hT.s(sb_)[:, kc, b0:b0 + n], start=(kc == 0), stop=(kc == 7))
            if c % 2:
                cx.act(xraw.s(sb_)[:, c, b0:b0 + n], p[:, 0:n], AF.Copy)
            else:
                cx.op(DVE, "tensor_copy", out=xraw.s(sb_)[:, c, b0:b0 + n], in_=p[:, 0:n])
    pc = k.pc
    for c in range(4):
        w0, w1, w2 = [pc[:, l, PC_CONVW + 4 * j + c:PC_CONVW + 4 * j + c + 1] for j in range(3)]
        cx.op(DVE, "tensor_scalar", out=cacc[:], in0=xraw[:, c, :], scalar1=w1, scalar2=pc[:, l, PC_CONVB + c:PC_CONVB + c + 1], op0=ALU.mult, op1=ALU.add)
        for (a_, b_) in ((0, NCTX), (NCTX, T)):
            cx.op(DVE, "scalar_tensor_tensor", out=cacc[:, a_ + 1:b_], in0=xraw[:, c, a_:b_ - 1], scalar=w0, in1=cacc[:, a_ + 1:b_], op0=ALU.mult, op1=ALU.add)
            cx.op(DVE, "scalar_tensor_tensor", out=cacc[:, a_:b_ - 1], in0=xraw[:, c, a_ + 1:b_], scalar=w2, in1=cacc[:, a_:b_ - 1], op0=ALU.mult, op1=ALU.add)
        cx.act(xcT[:, c, :], cacc[:], AF.Silu)
    cx.pop()
    cx.push()
    qT = cx.sb((128, 2, T), BF16, NT); kT = cx.sb((128, 2, T), BF16, NT)
    vaug = cx.sb((128, NT, 4, 129), BF16, NT)
    _memset(cx, vaug[:], 1.0)
    gt = cx.sb((128, NT, 16), F32); lpt = cx.sb((128, NT, 16), F32)
    wqb = cx.sb((128, 4, 64), BF16); wkb = cx.sb((128, 4, 64), BF16)
    cx.dma(POOL, wqb[:], U(k.ml_wq[l].rearrange("h c d -> c h d")))
    cx.dma(POOL, wkb[:], U(k.ml_wk[l].rearrange("h c d -> c h d")))
    for (b0, n) in BLKS:
        sb_ = tk(b0, b0 + n)
        for (dst, wb, scl) in ((qT, wqb, 1.0), (kT, wkb, 0.125)):
            for c in range(2):
                p = nps(k)
                for hh in range(2):
                    h = 2 * c + hh
                    cx.mm(p[hh * 64:(hh + 1) * 64, 0:n], wb[:, h, :], xcT.s(sb_)[:, h, b0:b0 + n], start=True, stop=True)
                cx.act(dst.s(sb_)[:, c, b0:b0 + n], p[:, 0:n], AF.Copy, scale=scl)
    wv = wload(k, W[:, OFF["ml_v"]:OFF["ml_v"] + 512], 8, 512)
    proj_tm(k, hT, wv, lambda tt, p: cx.op(DVE, "tensor_copy", out=vaug.s(tt)[:, tt, :, 0:128],
                                             in_=V(p, p.t[:, :].rearrange("p (h e) -> p h e", e=128), None)))
    wif = wload(k, W[:, OFF["ml_if"]:OFF["ml_if"] + 16], 8, 16)
    proj_tm(k, hT, wif, lambda tt, p: cx.op(DVE, "tensor_tensor", out=gt[:, tt, :], in0=p[:, 0:16], in1=pr[:, 0:16], op=ALU.add), ncols=16)
    cx.act(lpt[:], gt[:], AF.Exp, scale=-1.0)
    cx.act(lpt[:], lpt[:], AF.Ln, bias=k.onec[:, 0:1])
    Bp = cx.sb((128, 2, NT, 4), F32); Bt = cx.sb((128, 2, NT, 4), F32)
    fa = cx.sb((128, 2, NT, 4), F32); fg = cx.sb((128, 2, NT, 4), F32); fgk = cx.sb((128, 2, NT, 4), F32); fdec = cx.sb((128, 2, NT, 4), F32)
    for d in range(2):
        g0 = (1 + 2 * d) * 4
        rhs = lpt[:, :, g0:g0 + 4]
        p = nps(k)
        cx.mm(p[:, 0:72], (k.trif if d == 0 else k.trib)[:], rhs)
        cx.op(DVE, "tensor_copy", out=Bp[:, d], in_=V(p, p.t[:, 0:72].rearrange("p (a b) -> p a b", b=4), None))
        p = nps(k)
        cx.mm(p[:, 0:72], k.onesf[:], rhs)
        cx.op(DVE, "tensor_copy", out=Bt[:, d], in_=V(p, p.t[:, 0:72].rearrange("p (a b) -> p a b", b=4), None))
        li = gt[:, :, 8 * d:8 * d + 4]
        cx.act(fa[:, d], Bp[:, d], AF.Exp, scale=-1.0)
        cx.act(fdec[:, d], Bt[:, d], AF.Exp, scale=-1.0)
        cx.op(DVE, "tensor_tensor", out=Bp[:, d], in0=Bp[:, d], in1=li, op=ALU.add)
        cx.act(fg[:, d], Bp[:, d], AF.Exp)
        cx.op(DVE, "tensor_tensor", out=Bp[:, d], in0=Bp[:, d], in1=Bt[:, d], op=ALU.subtract)
        cx.act(fgk[:, d], Bp[:, d], AF.Exp)
    C32_ = [cx.sb((128, 2, 129), F32) for _ in range(2)]; Cb_ = [cx.sb((128, 2, 129), BF16) for _ in range(2)]
    PT_ = [cx.sb((128, 128), BF16) for _ in range(4)]
    kg_ = [cx.sb((128, 256), BF16) for _ in range(4)]
    dn_ = [cx.sb((128, 4), F32) for _ in range(4)]
    orders = [list(range(NT)), [1, 0] + list(range(NT - 1, 1, -1))]
    masks = [k.trifb, k.tribb]
    written = set()
    if True:
        def prep(d, tt, j):
            t0 = tt * 128
            kg = kg_[2 * d + j]
            pk = nps(k)
            for h in range(4):
                cx.mm(pk[:, h * 64:(h + 1) * 64], xcT.s(tt)[:, h, t0:t0 + 128], wkb[:, h, :], start=True, stop=True)
            for h in range(4):
                cx.op(DVE, "tensor_scalar", out=kg[:, h * 64:(h + 1) * 64], in0=pk[:, h * 64:(h + 1) * 64], scalar1=fgk[:, d, tt, h:h + 1], scalar2=0.125, op0=ALU.mult, op1=ALU.mult)

        def body(d, tt, j):
            t0 = tt * 128
            mask, C32, Cb = masks[d], C32_[d], Cb_[d]
            kg, dn = kg_[2 * d + j], dn_[2 * d + j]
            first = tt not in written
            written.add(tt)
            pos = [nps(k), nps(k)]
            for h in range(4):
                c, r0 = h // 2, (h % 2) * 64
                pa = nps(k)
                cx.mm(pa[:, 0:128], kT.s(tt)[r0:r0 + 64, c, t0:t0 + 128], qT.s(tt)[r0:r0 + 64, c, t0:t0 + 128])
                PT = PT_[h % 4]
                cx.op(DVE, "scalar_tensor_tensor", out=PT[:], in0=pa[:, 0:128], scalar=fg[:, d, tt, h:h + 1], in1=mask[:], op0=ALU.mult, op1=ALU.mult)
                o = (h % 2) * 129
                cx.mm(pos[c][:, o:o + 129], PT[:], vaug.s(tt)[:, tt, h, :], start=True, stop=False)
                cx.mm(pos[c][:, o:o + 129], qT.s(tt)[r0:r0 + 64, c, t0:t0 + 128], Cb[r0:r0 + 64, c, :], start=False, stop=True)
            pu = nps(k)
            for h in range(4):
                c, r0 = h // 2, (h % 2) * 64
                cx.mm(pu[r0:r0 + 64, c * 129:(c + 1) * 129], kg[:, h * 64:(h + 1) * 64], vaug.s(tt)[:, tt, h, :], start=True, stop=True)
            for c in range(2):
                po = pos[c]
                den = V(po, po.t[:, 0:258].rearrange("p (a b) -> p a b", b=129)[:, :, 128], None)
                cx.op(DVE, "tensor_tensor", out=dn[:, 0:2], in0=den, in1=fa[:, d, tt, 2 * c:2 * c + 2], op=ALU.mult)
                cx.act(dn[:, 0:2], dn[:, 0:2], AF.Abs)
                cx.op(DVE, "tensor_scalar", out=dn[:, 0:2], in0=dn[:, 0:2], scalar1=1.0, scalar2=None, op0=ALU.max)
                cx.op(DVE, "reciprocal", out=dn[:, 0:2], in_=dn[:, 0:2])
                cx.op(DVE, "tensor_tensor", out=dn[:, 2:4], in0=dn[:, 0:2], in1=fa[:, d, tt, 2 * c:2 * c + 2], op=ALU.mult)
                for hh in range(2):
                    h = 2 * c + hh
                    dst = hml.s(tt)[:, tt, h * 128:(h + 1) * 128]
                    if first:
                        cx.op(DVE, "tensor_scalar", out=dst, in0=po[:, hh * 129:hh * 129 + 128], scalar1=dn[:, 2 + hh:3 + hh], scalar2=None, op0=ALU.mult)
                    else:
                        cx.op(DVE, "scalar_tensor_tensor", out=dst, in0=po[:, hh * 129:hh * 129 + 128], scalar=dn[:, 2 + hh:3 + hh], in1=dst, op0=ALU.mult, op1=ALU.add)
            for h in range(4):
                c, r0 = h // 2, (h % 2) * 64
                cx.op(DVE, "scalar_tensor_tensor", out=C32[r0:r0 + 64, c, :], in0=C32[r0:r0 + 64, c, :], scalar=fdec[r0:r0 + 64, d, tt, h:h + 1], in1=pu[r0:r0 + 64, c * 129:(c + 1) * 129], op0=ALU.mult, op1=ALU.add)
            cx.act(Cb[:], C32[:], AF.Copy)

        for d in range(2):
            _memset(cx, C32_[d][:], 0.0); _memset(cx, Cb_[d][:], 0.0)
            prep(d, orders[d][0], 0)
        for i in range(NT):
            for d in range(2):
                if i + 1 < NT:
                    prep(d, orders[d][i + 1], (i + 1) % 2)
                body(d, orders[d][i], i % 2)
    cx.pop()
    y = cx.sb((128, 4, T), BF16, NT)
    cx.push()
    wo = wload(k, W[:, OFF["ml_o"]:OFF["ml_o"] + 512], 8, 512)
    sgo = [cx.sb((128, 512), BF16) for _ in range(2)]

    def og(tt, p):
        cx.act(sgo[tt % 2][:], p[:, :], AF.Sigmoid)
        cx.op(DVE, "tensor_tensor", out=hml.s(tt)[:, tt, :], in0=hml.s(tt)[:, tt, :], in1=sgo[tt % 2][:], op=ALU.mult)
    proj_tm(k, hT, wo, og)
    t1_ = [cx.sb((128, 128), F32) for _ in range(2)]
    t2_ = [cx.sb((128, 128), F32) for _ in range(2)]
    ej = [0]

    def extra(tt, h, pv, szv):
        j = ej[0] % 2; ej[0] += 1
        t0 = tt * 128
        cx.act(t1_[j][:], pv, AF.Identity, scale=dc[:, DC_MLOG + h:DC_MLOG + h + 1])
        cx.op(DVE, "scalar_tensor_tensor", out=t2_[j][:], in0=xcT.s(tt)[:, h, t0:t0 + 128], scalar=k.pc[:, l, PC_MLSKIP + h:PC_MLSKIP + h + 1], in1=t1_[j][:], op0=ALU.mult, op1=ALU.add)
        cx.op(DVE, "tensor_tensor", out=y.s(tt)[:, h, t0:t0 + 128], in0=t2_[j][:], in1=szv, op=ALU.mult)
    out_stage(k, l, 2, hT, lambda tt: hml.s(tt)[:, tt, :], y, dc, DC_MLOG, OFF["ml_z"], last, extra)
    cx.pop()
    if k.dbg and s == 0 and l == 0:
        dump(k, y, k.dbg_y[2], 4)
    merge(k, l, 2, y, hT, acc, BLKS[1:] if last else BLKS)
    cx.pop()


def dumpv(k, name, v, ncols):
    cx = k.cx
    dst = k.nc.dram_tensor(name, [128, ncols], F32, kind="ExternalOutput").ap()
    cx.push()
    f = cx.sb((128, 256), F32)
    for j in range(0, ncols, 256):
        w = min(256, ncols - j)
        cx.op(DVE, "tensor_copy", out=f[:, 0:w], in_=V(v.tile, v.ap[:, j:j + w], v.subs))
        cx.dma(SP, U(dst[:, j:j + w]), f[:, 0:w])
    cx.pop()


def dump(k, t, dst, nch):
    cx = k.cx
    cx.push()
    f = cx.sb((128, 256), F32)
    for c in range(nch):
        for j in range(T // 256):
            cx.op(DVE, "tensor_copy", out=f[:], in_=t[:, c, j * 256:(j + 1) * 256])
            cx.dma(SP, U(dst[:, c, j * 256:(j + 1) * 256]), f[:])
    cx.pop()


def layer(k, s, l, last):
    cx = k.cx
    cx.push()
    hT = cx.sb((128, 8, T), BF16, NT)
    acc = cx.sb((128, 8, T), BF16, NT)
    stg = int(os.environ.get("KSTAGE", "9"))
    if stg < 1:
        cx.pop(); return
    dc, pr = layer_params(k, l)
    if stg < 2:
        cx.pop(); return
    stage_h(k, s, l, hT)
    if stg < 3:
        if k.dbg:
            dump(k, hT, k.dbg_h, 8)
        cx.pop(); return
    if k.dbg and s == 0 and l == 0:
        dump(k, hT, k.dbg_h, 8)
    mla(k, s, l, hT, acc, dc, last)
    if stg < 7:
        cx.pop(); return
    mix = os.environ.get("KMIX", "123")
    if "1" in mix:
        gla(k, s, l, hT, acc, dc, last)
    if "2" in mix:
        mlstm(k, s, l, hT, acc, dc, pr, last)
    if "3" in mix:
        diffattn(k, s, l, hT, acc, dc, last)
    if k.dbg and s == 0 and l == 0:
        dump(k, acc, k.dbg_acc, 8)
    final(k, s, l, acc, last)
    cx.pop()


def _consts():
    ident = np.eye(128, dtype=np.float32)
    bd = np.zeros((128, 128), np.float32); bd[:64, :64] = 1; bd[64:, 64:] = 1
    rm = np.zeros((64, 64), np.float32)
    for i in range(16):
        rm[16 + i, i] = -1; rm[i, 16 + i] = 1; rm[48 + i, 32 + i] = -1; rm[32 + i, 48 + i] = 1
    rm2 = np.zeros((128, 128), np.float32); rm2[:64, :64] = rm; rm2[64:, 64:] = rm
    si, ti = np.meshgrid(np.arange(128), np.arange(128), indexing="ij")
    trif = (si <= ti).astype(np.float32); trib = (si >= ti).astype(np.float32)
    quarter = 16
    inv_freq = (10000.0 ** (-np.arange(quarter, dtype=np.float32) / quarter)).astype(np.float32)
    row = np.repeat(np.arange(32, dtype=np.float32), 64); col = np.tile(np.arange(64, dtype=np.float32), 32)
    ar = row[:, None] * inv_freq; ac = col[:, None] * inv_freq
    ang = np.concatenate([ar, ar, ac, ac], axis=-1).astype(np.float32)
    cos = np.concatenate([np.ones((NCTX, 64), np.float32), np.cos(ang)], 0)
    sin = np.concatenate([np.zeros((NCTX, 64), np.float32), np.sin(ang)], 0)
    cosT = np.ascontiguousarray(np.concatenate([cos.T, cos.T], 0)); sinT = np.ascontiguousarray(np.concatenate([sin.T, sin.T], 0))
    return dict(cident=ident, cbd64=bd, crm2=rm2, ctrif=trif, ctrib=trib, ccos=cosT.astype(np.float32), csin=sinT.astype(np.float32))


def _cols(v):
    v = np.asarray(v, np.float32).reshape(-1)
    return v.reshape(-1, 128).T


def _pack(inp):
    pcols = np.zeros((128, NL, NPC), np.float32)
    prow = np.zeros((NL, NPR), np.float32)
    for l in range(NL):
        P = pcols[:, l]
        P[:, PC_NORMG:PC_NORMG + 8] = _cols(inp["norm_g"][l])
        P[:, PC_CQG:PC_CQG + 3] = _cols(inp["mla_cq_g"][l]); P[:, PC_CKVG:PC_CKVG + 2] = _cols(inp["mla_ckv_g"][l])
        qg, kg = inp["mla_q_g"][l], inp["mla_k_g"][l]
        P[:, PC_QGN] = qg[:128]; P[:, PC_QGR] = np.concatenate([qg[128:], qg[128:]])
        P[:, PC_KGN] = kg[:128]; P[:, PC_KGR] = np.concatenate([kg[128:], kg[128:]])
        P[:, PC_DFQG] = np.concatenate([inp["df_qk_g"][l, 0]] * 2); P[:, PC_DFKG] = np.concatenate([inp["df_qk_g"][l, 1]] * 2)
        P[:, PC_DFOG] = inp["df_out_g"][l]
        P[:, PC_GLAOG:PC_GLAOG + 4] = _cols(inp["gla_out_g"][l]); P[:, PC_MLOG:PC_MLOG + 4] = _cols(inp["ml_out_g"][l])
        P[:, PC_MLSKIP:PC_MLSKIP + 4] = _cols(inp["ml_skip"][l])
        for j in range(3):
            P[:, PC_CONVW + 4 * j:PC_CONVW + 4 * j + 4] = _cols(inp["ml_conv_w"][l, j])
        P[:, PC_CONVB:PC_CONVB + 4] = _cols(inp["ml_conv_b"][l])
        for d_ in range(2):
            P[:, PC_GLAAB + 2 * d_:PC_GLAAB + 2 * d_ + 2] = _cols(inp["gla_a_b"][l, d_])
        prow[l, 0:16] = inp["ml_gate_b"][l].reshape(-1)
        prow[l, 16:272] = inp["df_lambda"][l].reshape(-1)
    return pcols, prow


def _perm_wuq(w):
    w = w.reshape(NL, 384, 4, 192)
    return np.ascontiguousarray(np.concatenate([w[..., :128].reshape(NL, 384, 512), w[..., 128:].reshape(NL, 384, 256)], -1))


def _perm_wukv(w):
    w = w.reshape(NL, 256, 4, 256)
    return np.ascontiguousarray(np.concatenate([w[..., :128].reshape(NL, 256, 512), w[..., 128:].reshape(NL, 256, 512)], -1))


_CACHE = {}


def host_maps(inp, ncores=8):
    inp = {k_: np.asarray(v, np.float32) for k_, v in inp.items()}
    pcols, prow = _pack(inp)
    shared = dict(ada_w=inp["ada_w"], ada_b=inp["ada_b"], w_in=inp["w_in"], wuq=_perm_wuq(inp["mla_wuq"]),
                  wukv=_perm_wukv(inp["mla_wukv"]), gla_a_w=inp["gla_a_w"], ml_wq=inp["ml_wq"], ml_wk=inp["ml_wk"],
                  br_w=inp["br_w"], w_out=inp["w_out"], pcols=pcols, prow=prow, **_consts())
    maps = []
    for c in range(ncores):
        m = dict(shared)
        m["x"] = np.ascontiguousarray(inp["x"][4 * c:4 * c + 4]); m["ctx"] = np.ascontiguousarray(inp["ctx"][4 * c:4 * c + 4])
        m["c5"] = np.ascontiguousarray(np.concatenate([inp["c"][4 * c:4 * c + 4], inp["c_ctx"][None]], 0))
        maps.append(m)
    return maps


def kernel(**inputs):
    if "nc" not in _CACHE:
        _CACHE["nc"] = build(4, NL)[0]
    nc = _CACHE["nc"]
    maps = host_maps(inputs)
    res = run_bass_kernel_spmd(nc, maps, core_ids=list(range(8)))
    return np.concatenate([np.asarray(r["out"], np.float32) for r in res.results], 0)
```

```python
import math
import contextlib
import numpy as np
import concourse.bass as bass
import concourse.mybir as mybir
from concourse.bass_utils import run_bass_kernel_spmd

F32 = mybir.dt.float32
BF16 = mybir.dt.bfloat16
AF = mybir.ActivationFunctionType
ALU = mybir.AluOpType
AX = mybir.AxisListType

T = 2304
NCTX = 256
NT = 18
D = 1024
DIN = 10992
NL = 4
EPS = 1e-6
BLKS = [(0, 256), (256, 512), (768, 512), (1280, 512), (1792, 512)]
OFF = dict(mla_cq=0, mla_ckv=384, mla_kr=640, mla_z=704, gla_q=1216, gla_k=1472, gla_v=1728, gla_af=2240,
           gla_ab=2256, gla_z=2272, ml_x=2784, ml_v=3296, ml_o=3808, ml_if=4320, ml_z=4336, df_q=4848,
           df_k=5360, df_v=5872, df_z=6384, merge=6896)
PC_NORMG, PC_CQG, PC_CKVG, PC_QGN, PC_QGR, PC_KGN, PC_KGR, PC_DFQG, PC_DFKG, PC_DFOG = 0, 8, 11, 13, 14, 15, 16, 17, 18, 19
PC_GLAOG, PC_MLOG, PC_MLSKIP, PC_CONVW, PC_CONVB, PC_GLAAB, NPC = 20, 24, 28, 32, 44, 48, 52
NPR = 16 + 256
DC_CQG, DC_CKVG, DC_QGN, DC_QGR, DC_KGN, DC_KGR, DC_DFQG, DC_DFKG, DC_DFOG, DC_GLAOG, DC_MLOG, DC_NAB, DC_LAM, NDC = 0, 3, 5, 6, 7, 8, 9, 10, 11, 12, 16, 20, 24, 28

import os
PE, ACT, DVE, POOL, SP = range(5)
SERIAL = os.environ.get("SERIAL", "0") == "1"
PEL = DVE if os.environ.get("NOPOOL", "0") == "1" else POOL
NDS = 24


class V:
    __slots__ = ("tile", "ap", "subs")

    def __init__(self, tile, ap, subs):
        self.tile, self.ap, self.subs = tile, ap, subs


class _Sel:
    def __init__(self, tile, subs):
        self.tile, self.subs = tile, subs

    def __getitem__(self, idx):
        return V(self.tile, self.tile.t[idx], self.subs)


class Tile:
    def __init__(self, t, nsub=1):
        self.t = t
        self.nsub = nsub
        self.w = [None] * nsub
        self.r = [dict() for _ in range(nsub)]
        self.excl = False

    def __getitem__(self, idx):
        return V(self, self.t[idx], None)

    def s(self, subs):
        if isinstance(subs, int):
            subs = [subs]
        return _Sel(self, list(subs))


FASTSE = os.environ.get("FASTSE", "1") == "1"


def _fsize(ap):
    n = 1
    for d_ in ap.shape[1:]:
        n *= d_
    return n


def tk(a, b):
    return list(range(a // 128, (b + 127) // 128))


class Ctx:
    def __init__(self, nc):
        self.nc = nc
        self.engs = [nc.tensor, nc.scalar, nc.vector, nc.gpsimd, nc.sync]
        self.es = contextlib.ExitStack()
        self.esem = [self.es.enter_context(nc.semaphore(f"e{i}")) for i in range(5)]
        self.dsem = [self.es.enter_context(nc.semaphore(f"d{i}")) for i in range(NDS)]
        self.cnt = [0] * 5
        self.dval = [0] * NDS
        self.dk = 0
        self.dkp = 0
        self.seen = [dict() for _ in range(5)]
        self.scopes = [self.es]
        self.scope_tiles = [[]]
        self.freed = {}
        self.uid = 0
        self.ninstr = 0
        self.last = None

    def push(self):
        st = contextlib.ExitStack()
        self.scopes.append(st)
        self.scope_tiles.append([])
        return st

    def pop(self):
        for tl in self.scope_tiles.pop():
            for s in range(tl.nsub):
                w = tl.w[s]
                if w is not None and self.freed.get(w[0], 0) < w[1]:
                    self.freed[w[0]] = w[1]
                for key, val in tl.r[s].items():
                    if self.freed.get(key, 0) < val:
                        self.freed[key] = val
        self.scopes.pop().close()

    def sb(self, shape, dt, nsub=1, name=None):
        self.uid += 1
        t = self.scopes[-1].enter_context(self.nc.sbuf_tensor(f"{name or 't'}{self.uid}", list(shape), dt))
        tl = Tile(t, nsub)
        if self.freed:
            for s in range(nsub):
                tl.r[s] = dict(self.freed)
        self.scope_tiles[-1].append(tl)
        return tl

    def psum(self, shape, dt):
        self.uid += 1
        t = self.scopes[-1].enter_context(self.nc.psum_tensor(f"ps{self.uid}", list(shape), dt))
        tl = Tile(t, 1)
        tl.excl = True
        return tl

    def _sync(self, eng, reads, writes, extra=None):
        need = {}

        def add(key, val):
            if key[0] == "E" and key[1] == eng and eng == PE:
                return
            if need.get(key, 0) < val:
                need[key] = val

        same = ("E", eng)
        fast = FASTSE and eng in (ACT, DVE)
        for v in reads:
            if v.tile is None:
                continue
            big = fast and _fsize(v.ap) >= 32
            for s in (v.subs if v.subs is not None else range(v.tile.nsub)):
                w = v.tile.w[s]
                if w is not None and not (big and w[0] == same):
                    add(w[0], w[1])
                if v.tile.excl:
                    for key, val in v.tile.r[s].items():
                        if not (key[0] == "E" and key[1] == eng):
                            add(key, val)
        for v in writes:
            if v.tile is None:
                continue
            for s in (v.subs if v.subs is not None else range(v.tile.nsub)):
                w = v.tile.w[s]
                if w is not None and not (fast and w[0] == same):
                    add(w[0], w[1])
                for key, val in v.tile.r[s].items():
                    if not (fast and key == same):
                        add(key, val)
        if extra is not None and extra[1] > 0:
            add(extra[0], extra[1])
        if SERIAL and self.last is not None:
            if not (self.last[0][0] == "E" and self.last[0][1] == eng and eng == PE):
                if need.get(self.last[0], 0) < self.last[1]:
                    need[self.last[0]] = self.last[1]
        seen = self.seen[eng]
        for key, val in need.items():
            if seen.get(key, 0) >= val:
                continue
            seen[key] = val
            sem = self.esem[key[1]] if key[0] == "E" else self.dsem[key[1]]
            self.engs[eng].wait_ge(sem, val)
            self.ninstr += 1

    def _commit(self, tok, reads, writes):
        key, val = tok
        self.last = tok
        for v in reads:
            if v.tile is None:
                continue
            for s in (v.subs if v.subs is not None else range(v.tile.nsub)):
                r = v.tile.r[s]
                if r.get(key, 0) < val:
                    r[key] = val
        for v in writes:
            if v.tile is None:
                continue
            for s in (v.subs if v.subs is not None else range(v.tile.nsub)):
                v.tile.w[s] = tok
                v.tile.r[s] = {}

    def op(self, eng, name, **kw):
        reads, writes, args = [], [], {}
        for k, v in kw.items():
            if isinstance(v, V):
                (writes if k in ("out", "accum_out") else reads).append(v)
                args[k] = v.ap
            else:
                args[k] = v
        self._sync(eng, reads, writes)
        ins = getattr(self.engs[eng], name)(**args)
        self.cnt[eng] += 1
        ins.then_inc(self.esem[eng], 1)
        self.ninstr += 1
        self._commit((("E", eng), self.cnt[eng]), reads, writes)

    def dma(self, q, out, in_, **kw):
        if q == POOL:
            k = 16 + self.dkp
            self.dkp = (self.dkp + 1) % (NDS - 16)
        else:
            k = self.dk
            self.dk = (self.dk + 1) % 16
        prev = self.dval[k]
        self._sync(q, [in_], [out], extra=(("D", k), prev))
        ins = self.engs[q].dma_start(out=out.ap, in_=in_.ap, **kw)
        ins.then_inc(self.dsem[k], 16)
        self.ninstr += 1
        self.dval[k] = prev + 16
        self._commit((("D", k), prev + 16), [in_], [out])

    def finish(self):
        for k in range(NDS):
            if self.dval[k] > 0:
                self.nc.sync.wait_ge(self.dsem[k], self.dval[k])
        for e in range(4):
            if self.cnt[e] > 0:
                self.nc.sync.wait_ge(self.esem[e], self.cnt[e])

    def mm(self, out, lhsT, rhs, start=True, stop=True):
        self.op(PE, "matmul", out=out, lhsT=lhsT, rhs=rhs, start=start, stop=stop, skip_group_check=True)

    def tr(self, out, in_, identity):
        self.op(PE, "transpose", out=out, in_=in_, identity=identity)

    def act(self, out, in_, func, **kw):
        self.op(ACT, "activation", out=out, in_=in_, func=func, **kw)


def U(ap):
    return V(None, ap, None)


class K:
    pass


def build(nseq, nlayers, dbg=False):
    nc = bass.Bass("TRN2", target_bir_lowering=False)
    cx = Ctx(nc)
    k = K()
    k.nc, k.cx, k.dbg = nc, cx, dbg

    def din(name, shape, dt=F32):
        return nc.dram_tensor(name, list(shape), dt, kind="ExternalInput").ap()

    k.x = din("x", [4, 2048, D]); k.ctxin = din("ctx", [4, NCTX, D]); k.c5 = din("c5", [5, D])
    k.ada_w = din("ada_w", [NL, D, 3 * D]); k.ada_b = din("ada_b", [NL, 3 * D])
    k.w_in = din("w_in", [NL, D, DIN]); k.wuq = din("wuq", [NL, 384, 768]); k.wukv = din("wukv", [NL, 256, 1024])
    k.gla_a_w = din("gla_a_w", [NL, 2, 16, 256]); k.ml_wq = din("ml_wq", [NL, 4, 128, 64]); k.ml_wk = din("ml_wk", [NL, 4, 128, 64])
    k.br_w = din("br_w", [NL, 4, 512, D]); k.w_out = din("w_out", [NL, D, D])
    k.pcols = din("pcols", [128, NL, NPC]); k.prow = din("prow", [NL, NPR])
    k.cident = din("cident", [128, 128]); k.cbd64 = din("cbd64", [128, 128]); k.crm2 = din("crm2", [128, 128])
    k.ctrif = din("ctrif", [128, 128]); k.ctrib = din("ctrib", [128, 128])
    k.ccos = din("ccos", [128, T]); k.csin = din("csin", [128, T])
    k.out = nc.dram_tensor("out", [4, 2048, D], F32, kind="ExternalOutput").ap()
    k.xres_ap = nc.dram_tensor("xres", [4, T, D], F32, kind="Internal").ap()
    k.gscr_ap = nc.dram_tensor("gscr", [NL, 5, D], F32, kind="Internal").ap()
    k.xres = [Tile(k.xres_ap[s], NT) for s in range(4)]
    k.gscr = Tile(k.gscr_ap, NL)
    if dbg:
        k.dbg_y = nc.dram_tensor("dbg_y", [4, 128, 4, T], F32, kind="ExternalOutput").ap()
        k.dbg_acc = nc.dram_tensor("dbg_acc", [128, 8, T], F32, kind="ExternalOutput").ap()
        k.dbg_h = nc.dram_tensor("dbg_h", [128, 8, T], F32, kind="ExternalOutput").ap()
        k.dbg_x = nc.dram_tensor("dbg_x", [T, D], F32, kind="ExternalOutput").ap()

    def cload(src, dt, shape=(128, 128), q=POOL):
        t = cx.sb(shape, dt)
        cx.dma(q, t[:], U(src))
        return t

    k.identb = cload(k.cident, BF16); k.identf = cload(k.cident, F32, q=SP)
    k.bd64 = cload(k.cbd64, BF16); k.rm2 = cload(k.crm2, BF16)
    k.trif = cload(k.ctrif, F32, q=SP); k.trib = cload(k.ctrib, F32, q=SP)
    k.trifb = cload(k.ctrif, BF16); k.tribb = cload(k.ctrib, BF16)
    k.cos = cload(k.ccos, BF16, (128, T)); k.sin = cload(k.csin, BF16, (128, T))
    k.onesb = cx.sb((128, 128), BF16)
    k.onesf = cx.sb((128, 128), F32)
    _memset(cx, k.onesb[:], 1.0); _memset(cx, k.onesf[:], 1.0)
    k.epsc = cx.sb((128, 1), F32); _memset(cx, k.epsc[:], EPS)
    k.onec = cx.sb((128, 1), F32); _memset(cx, k.onec[:], 1.0)
    k.pc = cx.sb((128, NL, NPC), F32)
    cx.dma(SP, k.pc[:], U(k.pcols))
    k.AB = cx.sb((128, NL, 5, 16), F32)
    k.ps = [cx.psum((128, 512), F32) for _ in range(8)]
    k.psi = 0
    k.wpool = [cx.sb((128, 8, 512), BF16) for _ in range(3)]
    k.wi = 0

    for s in range(nseq):
        cx.dma(SP, k.xres[s].s([0, 1])[0:NCTX, :], U(k.ctxin[s]))
        for j in range(4):
            cx.dma(SP, k.xres[s].s(tk(NCTX + j * 512, NCTX + (j + 1) * 512))[NCTX + j * 512:NCTX + (j + 1) * 512, :],
                   U(k.x[s, j * 512:(j + 1) * 512, :]))

    prologue(k)
    for s in range(nseq):
        for l in range(nlayers):
            layer(k, s, l, last=(l == NL - 1))
    cx.finish()
    cx.es.close()
    return nc, cx


def _memset(cx, v, val, eng=DVE):
    cx._sync(eng, [], [v])
    ins = cx.engs[eng].memset(v.ap, val)
    cx.cnt[eng] += 1
    ins.then_inc(cx.esem[eng], 1)
    cx._commit((("E", eng), cx.cnt[eng]), [], [v])


def nps(k):
    p = k.ps[k.psi]
    k.psi = (k.psi + 1) % 8
    return p


def wload(k, src2d, kc, ncols):
    wt = k.wpool[k.wi]
    k.wi = (k.wi + 1) % len(k.wpool)
    k.cx.dma(POOL, wt[:, 0:kc, 0:ncols], U(src2d.rearrange("(kc p) n -> p kc n", p=128)))
    return wt


def prologue(k):
    cx, nc = k.cx, k.nc
    cx.push()
    c5t = cx.sb((5, D), F32)
    cx.dma(SP, c5t[:], U(k.c5))
    cx.act(c5t[:], c5t[:], AF.Silu)
    scT = cx.sb((128, 8, 5), F32)
    p = nps(k)
    for kc in range(8):
        cx.tr(p[:, kc * 8:kc * 8 + 5], c5t[0:5, kc * 128:(kc + 1) * 128], k.identf[0:5, 0:5])
    cx.op(DVE, "tensor_copy", out=scT[:], in_=V(p, p.t[:, 0:64].rearrange("p (a b) -> p a b", b=8)[:, :, 0:5], None))
    wf = [cx.sb((128, 8, 512), F32) for _ in range(2)]
    adab = cx.sb((5, 3 * D), F32)
    modrow = cx.sb((5, 3 * D), F32)
    modcol = cx.sb((128, 16, 5), F32)
    for l in range(NL):
        cx.dma(SP, adab[:], U(k.ada_b[l].partition_broadcast(5)))
        for nb in range(6):
            w = wf[nb % 2]
            cx.dma(SP, w[:], U(k.ada_w[l][:, nb * 512:(nb + 1) * 512].rearrange("(kc p) n -> p kc n", p=128)))
            p = nps(k)
            for kc in range(8):
                cx.mm(p[0:5, :], scT[:, kc, :], w[:, kc, :], start=(kc == 0), stop=(kc == 7))
            cx.op(DVE, "tensor_tensor", out=modrow[:, nb * 512:(nb + 1) * 512], in0=p[0:5, :], in1=adab[:, nb * 512:(nb + 1) * 512], op=ALU.add)
        p = nps(k)
        for j in range(16):
            cx.tr(p[:, j * 8:j * 8 + 5], modrow[0:5, j * 128:(j + 1) * 128], k.identf[0:5, 0:5])
        cx.op(DVE, "tensor_copy", out=modcol[:], in_=V(p, p.t[:, 0:128].rearrange("p (a b) -> p a b", b=8)[:, :, 0:5], None))
        for r in range(5):
            cx.op(DVE, "scalar_tensor_tensor", out=k.AB[:, l, r, 0:8], in0=modcol[:, 8:16, r], scalar=1.0, in1=k.pc[:, l, PC_NORMG:PC_NORMG + 8], op0=ALU.add, op1=ALU.mult)
            cx.op(DVE, "tensor_copy", out=k.AB[:, l, r, 8:16], in_=modcol[:, 0:8, r])
        cx.dma(SP, k.gscr.s(l)[l], modrow[0:5, 2 * D:3 * D])
    cx.pop()


def layer_params(k, l):
    cx = k.cx
    dc = cx.sb((128, NDC), F32)
    pc = k.pc

    def sc(dst, src, n, f):
        cx.op(DVE, "tensor_scalar", out=dc[:, dst:dst + n], in0=pc[:, l, src:src + n], scalar1=float(f), scalar2=None, op0=ALU.mult)

    lam_init = 0.8 - 0.6 * math.exp(-0.3 * l)
    sc(DC_CQG, PC_CQG, 3, 1.0); sc(DC_CKVG, PC_CKVG, 2, 1.0)
    sc(DC_QGN, PC_QGN, 1, 192 ** -0.5); sc(DC_QGR, PC_QGR, 1, 192 ** -0.5)
    sc(DC_KGN, PC_KGN, 1, 1.0); sc(DC_KGR, PC_KGR, 1, 1.0)
    sc(DC_DFQG, PC_DFQG, 1, 0.125); sc(DC_DFKG, PC_DFKG, 1, 1.0)
    sc(DC_DFOG, PC_DFOG, 1, (1.0 - lam_init))
    sc(DC_GLAOG, PC_GLAOG, 4, 1.0); sc(DC_MLOG, PC_MLOG, 4, 1.0)
    sc(DC_NAB, PC_GLAAB, 4, -1.0)
    pr = cx.sb((128, NPR), F32)
    cx.dma(SP, pr[:], U(k.prow[l].partition_broadcast(128)))
    junk = cx.sb((128, 64), F32)
    s2 = cx.sb((128, 2), F32)
    for i in range(2):
        cx.op(DVE, "tensor_tensor", out=junk[:], in0=pr[:, 16 + 128 * i:16 + 128 * i + 64], in1=pr[:, 16 + 128 * i + 64:16 + 128 * i + 128], op=ALU.mult)
        cx.op(DVE, "reduce_sum", out=s2[:, i:i + 1], in_=junk[:], axis=AX.X)
    cx.act(s2[:], s2[:], AF.Exp)
    cx.op(DVE, "scalar_tensor_tensor", out=dc[:, DC_LAM:DC_LAM + 1], in0=s2[:, 1:2], scalar=-lam_init, in1=s2[:, 0:1], op0=ALU.add, op1=ALU.subtract)
    return dc, pr


def stage_h(k, s, l, hT):
    cx = k.cx
    cx.push()
    xts = [cx.sb((128, D), F32) for _ in range(2)]
    xns = [cx.sb((128, D), BF16) for _ in range(2)]
    junk = cx.sb((128, D), BF16)
    ss = cx.sb((128, NT), F32)
    rs = cx.sb((128, NT), F32)
    for tt in range(NT):
        xt, xn = xts[tt % 2], xns[tt % 2]
        cx.dma(SP, xt[:], k.xres[s].s(tt)[tt * 128:(tt + 1) * 128, :])
        cx.act(junk[:], xt[:], AF.Square, accum_out=ss[:, tt:tt + 1])
        cx.act(rs[:, tt:tt + 1], ss[:, tt:tt + 1], AF.Sqrt, scale=1.0 / D, bias=k.epsc[:, 0:1])
        cx.op(DVE, "reciprocal", out=rs[:, tt:tt + 1], in_=rs[:, tt:tt + 1])
        cx.op(DVE, "tensor_scalar", out=xn[:], in0=xt[:], scalar1=rs[:, tt:tt + 1], scalar2=None, op0=ALU.mult)
        p = nps(k)
        pb = V(p, p.t[:].bitcast(BF16), None)
        for kc in range(8):
            cx.tr(V(p, pb.ap[:, kc * 128:(kc + 1) * 128], None), xn[:, kc * 128:(kc + 1) * 128], k.identb[:])
        r = 4 if tt < 2 else s
        for kc in range(8):
            src = V(p, pb.ap[:, kc * 128:(kc + 1) * 128], None)
            dst = hT.s(tt)[:, kc, tt * 128:(tt + 1) * 128]
            if kc % 2 == 0:
                cx.act(dst, src, AF.Identity, scale=k.AB[:, l, r, kc:kc + 1], bias=k.AB[:, l, r, 8 + kc:9 + kc])
            else:
                cx.op(DVE, "tensor_scalar", out=dst, in0=src, scalar1=k.AB[:, l, r, kc:kc + 1], scalar2=k.AB[:, l, r, 8 + kc:9 + kc], op0=ALU.mult, op1=ALU.add)
    cx.pop()


def proj_fm(k, src, wt, chunks, evac, blks, kcs=8):
    cx = k.cx
    for (b0, n) in blks:
        for ci, grp in enumerate(chunks):
            p = nps(k)
            for (co, m, r0) in grp:
                for kc in range(kcs):
                    cx.mm(p[r0:r0 + m, 0:n], wt[:, kc, co:co + m], src.s(tk(b0, b0 + n))[:, kc, b0:b0 + n], start=(kc == 0), stop=(kc == kcs - 1))
            evac(ci, p, b0, n)


def norm_group(k, pss, n, gmat, neps, gcols, dsts, tmp):
    cx = k.cx
    nchunk = len(pss)
    for c, p in enumerate(pss):
        cx.act(tmp["sq"][:, c, 0:n], p[:, 0:n], AF.Square)
        cx.op(DVE, "tensor_copy", out=tmp["raw"][:, c, 0:n], in_=p[:, 0:n])
    pq = nps(k)
    for c in range(nchunk):
        cx.mm(pq[:, 0:n], gmat[:], tmp["sq"][:, c, 0:n], start=(c == 0), stop=(c == nchunk - 1))
    cx.act(tmp["rs"][:, 0:n], pq[:, 0:n], AF.Ln, scale=EPS / float(neps), bias=k.epsc[:, 0:1])
    cx.act(tmp["rs"][:, 0:n], tmp["rs"][:, 0:n], AF.Exp, scale=-0.5)
    for c in range(nchunk):
        cx.op(DVE, "scalar_tensor_tensor", out=dsts[c], in0=tmp["raw"][:, c, 0:n], scalar=gcols[c], in1=tmp["rs"][:, 0:n], op0=ALU.mult, op1=ALU.mult)


def rope(k, xn, dst, b0, n, tmp):
    cx = k.cx
    p = nps(k)
    cx.mm(p[:, 0:n], k.rm2[:], xn)
    cx.op(DVE, "tensor_tensor", out=tmp["t1"][:, 0:n], in0=p[:, 0:n], in1=k.sin[:, b0:b0 + n], op=ALU.mult)
    cx.op(PEL, "tensor_tensor", out=tmp["t2"][:, 0:n], in0=xn, in1=k.cos[:, b0:b0 + n], op=ALU.mult)
    cx.op(DVE, "tensor_tensor", out=dst, in0=tmp["t1"][:, 0:n], in1=tmp["t2"][:, 0:n], op=ALU.add)


def mk_tmp(cx, nchunk=3):
    return dict(raw=cx.sb((128, nchunk, 512), BF16), sq=cx.sb((128, nchunk, 512), BF16), rs=cx.sb((128, 512), F32),
                t1=cx.sb((128, 512), F32), t2=cx.sb((128, 512), F32), xn=cx.sb((128, 512), BF16))


def merge(k, l, i, y, hT, acc, blks):
    cx = k.cx
    cx.push()
    sg = [cx.sb((128, 512), BF16) for _ in range(2)]
    tmps = [cx.sb((128, 512), BF16) for _ in range(2)]
    j = 0
    for mg in range(2):
        c0 = OFF["merge"] + i * D + mg * 512
        wg = wload(k, k.w_in[l][:, c0:c0 + 512], 8, 512)
        wb = wload(k, k.br_w[l, i][:, mg * 512:(mg + 1) * 512], 4, 512)
        for mc4 in range(4):
            mc = mg * 4 + mc4
            for (b0, n) in blks:
                sb_ = tk(b0, b0 + n)
                pg = nps(k)
                for kc in range(8):
                    cx.mm(pg[:, 0:n], wg[:, kc, mc4 * 128:(mc4 + 1) * 128], hT.s(sb_)[:, kc, b0:b0 + n], start=(kc == 0), stop=(kc == 7))
                g = sg[j % 2]
                cx.act(g[:, 0:n], pg[:, 0:n], AF.Sigmoid)
                py = nps(k)
                for kc in range(4):
                    cx.mm(py[:, 0:n], wb[:, kc, mc4 * 128:(mc4 + 1) * 128], y.s(sb_)[:, kc, b0:b0 + n], start=(kc == 0), stop=(kc == 3))
                dst = acc.s(sb_)[:, mc, b0:b0 + n]
                if i == 0:
                    cx.op(DVE, "tensor_tensor", out=dst, in0=py[:, 0:n], in1=g[:, 0:n], op=ALU.mult)
                else:
                    t = tmps[j % 2]
                    cx.op(DVE, "tensor_tensor", out=t[:, 0:n], in0=py[:, 0:n], in1=g[:, 0:n], op=ALU.mult)
                    cx.op(PEL, "tensor_tensor", out=dst, in0=acc.s(sb_)[:, mc, b0:b0 + n], in1=t[:, 0:n], op=ALU.add)
                j += 1
    cx.pop()


def final(k, s, l, acc, last):
    cx = k.cx
    cx.push()
    w0 = wload(k, k.w_out[l][:, 0:512], 8, 512)
    w1 = wload(k, k.w_out[l][:, 512:1024], 8, 512)
    gb = [cx.sb((128, D), F32) for _ in range(2)]
    cx.dma(SP, gb[0][:], V(k.gscr, k.gscr.t[l, 4].partition_broadcast(128), [l]))
    cx.dma(SP, gb[1][:], V(k.gscr, k.gscr.t[l, s].partition_broadcast(128), [l]))
    xo = [cx.sb((128, D), F32) for _ in range(2)]
    tm = [cx.sb((128, 512), F32) for _ in range(2)]
    for tt in range(2 if last else 0, NT):
        x_ = xo[tt % 2]
        cx.dma(SP, x_[:], k.xres[s].s(tt)[tt * 128:(tt + 1) * 128, :])
        g = gb[0] if tt < 2 else gb[1]
        for nh, w in enumerate((w0, w1)):
            p = nps(k)
            for kc in range(8):
                cx.mm(p[:, :], acc.s(tt)[:, kc, tt * 128:(tt + 1) * 128], w[:, kc, :], start=(kc == 0), stop=(kc == 7))
            t = tm[nh]
            cx.op(DVE, "tensor_tensor", out=t[:], in0=p[:, :], in1=g[:, nh * 512:(nh + 1) * 512], op=ALU.mult)
            cx.op(PEL if nh else DVE, "tensor_tensor", out=x_[:, nh * 512:(nh + 1) * 512], in0=x_[:, nh * 512:(nh + 1) * 512], in1=t[:], op=ALU.add)
        if k.dbg and s == 0 and l == 0:
            cx.dma(SP, U(k.dbg_x[tt * 128:(tt + 1) * 128, :]), x_[:])
        if last:
            cx.dma(SP, U(k.out[s, (tt - 2) * 128:(tt - 1) * 128, :]), x_[:])
        else:
            cx.dma(SP, k.xres[s].s(tt)[tt * 128:(tt + 1) * 128, :], x_[:])
    cx.pop()


def attn_core(k, nheads, nmaps, qk_fn, v_fn, out_fn, last, blk_fn=None):
    cx = k.cx
    cx.push()
    pts = [cx.sb((128, 512), BF16) for _ in range(4)]
    stage = [cx.sb((128, 129), F32) for _ in range(4 * nmaps)] if nmaps == 2 else None
    pj = 0
    sbank = [k.ps[0], k.ps[1]]
    abanks = [k.ps[2], k.ps[3], k.ps[4], k.ps[5]]
    sj = 0
    for qb, (q0, nq) in enumerate(BLKS):
        if last and qb == 0:
            continue
        kts = [0, 1] if qb == 0 else list(range(NT))
        nqt = nq // 128
        if blk_fn is not None:
            blk_fn(qb, q0, nq)
        for h in range(nheads):
            def accv(m, qi):
                idx = m * 4 + qi
                b = abanks[idx // 2] if nmaps == 2 else abanks[qi // 2]
                o = (idx % 2) * 129
                return b, V(b, b.t[:, o:o + 129], None)
            started = set()
            pend = []

            def do_pv(kt_, items):
                vv = v_fn(h, kt_)
                for (m, pt) in items:
                    for qi in range(nqt):
                        b, av = accv(m, qi)
                        first = id(b) not in started
                        started.add(id(b))
                        cx.mm(av, pt[:, qi * 128:(qi + 1) * 128], vv, start=first, stop=(kt_ == kts[-1]))

            for kt in kts:
                items = []
                for m in range(nmaps):
                    sp_ = sbank[sj % 2]; sj += 1
                    qk_fn(h, m, kt, q0, nq, sp_)
                    pt = pts[pj % 4]; pj += 1
                    cx.act(pt[:, 0:nq], sp_[:, 0:nq], AF.Exp)
                    items.append((m, pt))
                if pend:
                    do_pv(*pend.pop())
                pend.append((kt, items))
            do_pv(*pend.pop())
            stg = []
            for qi in range(nqt):
                row = []
                for m in range(nmaps):
                    if stage is None:
                        row.append(accv(m, qi)[1])
                        continue
                    st_ = stage[m * 4 + qi]
                    cx.act(st_[:, :], accv(m, qi)[1], AF.Copy)
                    row.append(st_[:, :])
                stg.append(row)
            for qi in range(nqt):
                out_fn(h, qb, q0 + qi * 128, stg[qi])
    cx.pop()


def mla(k, s, l, hT, acc, dc, last):
    cx = k.cx
    cx.push()
    qn = cx.sb((128, 4, T), BF16, NT); qr = cx.sb((128, 2, T), BF16, NT)
    kn = cx.sb((128, 4, T), BF16, NT); kr2 = cx.sb((128, T), BF16, NT)
    vaug = cx.sb((128, NT, 4, 129), BF16, NT)
    _memset(cx, vaug[:], 1.0)
    W = k.w_in[l]
    cx.push()
    cqn = cx.sb((128, 3, T), BF16, NT)
    tmp = mk_tmp(cx)
    wA = wload(k, W[:, 0:384], 8, 384)
    wq1 = wload(k, k.wuq[l][:, 0:512], 3, 512)
    wq2 = wload(k, k.wuq[l][:, 512:768], 3, 256)
    for (b0, n) in BLKS:
        sb_ = tk(b0, b0 + n)
        pss = []
        for c in range(3):
            p = nps(k)
            for kc in range(8):
                cx.mm(p[:, 0:n], wA[:, kc, c * 128:(c + 1) * 128], hT.s(sb_)[:, kc, b0:b0 + n], start=(kc == 0), stop=(kc == 7))
            pss.append(p)
        ksub = int(os.environ.get("KSUB", "9"))
        if ksub < 1:
            for c in range(3):
                cx.op(DVE, "tensor_copy", out=cqn.s(sb_)[:, c, b0:b0 + n], in_=pss[c][:, 0:n])
            continue
        norm_group(k, pss, n, k.onesb, 384 * EPS, [dc[:, DC_CQG + c:DC_CQG + c + 1] for c in range(3)],
                   [cqn.s(sb_)[:, c, b0:b0 + n] for c in range(3)], tmp)
        if ksub < 2:
            continue
        for h in range(4):
            p = nps(k)
            for kc in range(3):
                kv = os.environ.get("KV", "0")
                lw = wA if kv == "2" else wq1
                rr = hT if kv == "1" else cqn
                cx.mm(p[:, 0:n], lw[:, kc, h * 128:(h + 1) * 128], rr.s(sb_)[:, kc, b0:b0 + n], start=(kc == 0), stop=(kc == 2))
            if os.environ.get("KQ", "1") == "0":
                cx.op(DVE, "tensor_copy", out=qn.s(sb_)[:, h, b0:b0 + n], in_=p[:, 0:n])
            else:
                norm_group(k, [p], n, k.onesb, 128 * EPS, [dc[:, DC_QGN:DC_QGN + 1]], [qn.s(sb_)[:, h, b0:b0 + n]], tmp)
        if ksub < 3:
            continue
        for c in range(2):
            p = nps(k)
            for kc in range(3):
                cx.mm(p[:, 0:n], wq2[:, kc, c * 128:(c + 1) * 128], cqn.s(sb_)[:, kc, b0:b0 + n], start=(kc == 0), stop=(kc == 2))
            norm_group(k, [p], n, k.bd64, 64 * EPS, [dc[:, DC_QGR:DC_QGR + 1]], [tmp["xn"][:, 0:n]], tmp)
            rope(k, tmp["xn"][:, 0:n], qr.s(sb_)[:, c, b0:b0 + n], b0, n, tmp)
    cx.pop()
    stg = int(os.environ.get("KSTAGE", "9"))
    if stg < 4:
        cx.pop(); return
    cx.push()
    ckvn = cx.sb((128, 2, T), BF16, NT)
    tmp = mk_tmp(cx)
    wB = wload(k, W[:, 384:704], 8, 320)
    wk1 = wload(k, k.wukv[l][:, 0:512], 2, 512)
    for (b0, n) in BLKS:
        sb_ = tk(b0, b0 + n)
        pss = []
        for c in range(2):
            p = nps(k)
            for kc in range(8):
                cx.mm(p[:, 0:n], wB[:, kc, c * 128:(c + 1) * 128], hT.s(sb_)[:, kc, b0:b0 + n], start=(kc == 0), stop=(kc == 7))
            pss.append(p)
        norm_group(k, pss, n, k.onesb, 256 * EPS, [dc[:, DC_CKVG + c:DC_CKVG + c + 1] for c in range(2)],
                   [ckvn.s(sb_)[:, c, b0:b0 + n] for c in range(2)], tmp)
        p = nps(k)
        for r0 in (0, 64):
            for kc in range(8):
                cx.mm(p[r0:r0 + 64, 0:n], wB[:, kc, 256:320], hT.s(sb_)[:, kc, b0:b0 + n], start=(kc == 0), stop=(kc == 7))
        norm_group(k, [p], n, k.bd64, 64 * EPS, [dc[:, DC_KGR:DC_KGR + 1]], [tmp["xn"][:, 0:n]], tmp)
        rope(k, tmp["xn"][:, 0:n], kr2.s(sb_)[:, b0:b0 + n], b0, n, tmp)
        for h in range(4):
            p = nps(k)
            for kc in range(2):
                cx.mm(p[:, 0:n], wk1[:, kc, h * 128:(h + 1) * 128], ckvn.s(sb_)[:, kc, b0:b0 + n], start=(kc == 0), stop=(kc == 1))
            norm_group(k, [p], n, k.onesb, 128 * EPS, [dc[:, DC_KGN:DC_KGN + 1]], [kn.s(sb_)[:, h, b0:b0 + n]], tmp)
    wv1 = wload(k, k.wukv[l][:, 512:1024], 2, 512)
    for tt in range(NT):
        p = nps(k)
        for kc in range(2):
            cx.mm(p[:, :], ckvn.s(tt)[:, kc, tt * 128:(tt + 1) * 128], wv1[:, kc, :], start=(kc == 0), stop=(kc == 1))
        cx.op(DVE, "tensor_copy", out=vaug.s(tt)[:, tt, :, 0:128], in_=V(p, p.t[:, :].rearrange("p (h e) -> p h e", e=128), None))
    cx.pop()
    if stg < 5:
        cx.pop(); return
    if k.dbg and s == 0 and l == 0:
        dumpv(k, "d_kr2", kr2[:, :], T)
        dumpv(k, "d_qr0", qr[:, 0, :], T)
        dumpv(k, "d_qn1", qn[:, 1, :], T)
        dumpv(k, "d_kn1", kn[:, 1, :], T)
    y = cx.sb((128, 4, T), BF16, NT)
    sz = cx.sb((128, 4, 512), BF16)
    wz = wload(k, W[:, OFF["mla_z"]:OFF["mla_z"] + 512], 8, 512)

    def blk_fn(qb, b0, n):
        for c in range(4):
            p = k.ps[6 + c % 2]
            for kc in range(8):
                cx.mm(p[:, 0:n], wz[:, kc, c * 128:(c + 1) * 128], hT.s(tk(b0, b0 + n))[:, kc, b0:b0 + n], start=(kc == 0), stop=(kc == 7))
            cx.act(sz[:, c, 0:n], p[:, 0:n], AF.Silu)

    ob = [cx.sb((128, 128), BF16) for _ in range(2)]
    rc = [cx.sb((128, 1), F32) for _ in range(2)]
    oj = [0]

    def qk_fn(h, m, kt, q0, nq, sp_):
        r0 = (h % 2) * 64
        cx.mm(sp_[:, 0:nq], kn.s(kt)[:, h, kt * 128:(kt + 1) * 128], qn.s(tk(q0, q0 + nq))[:, h, q0:q0 + nq], start=True, stop=False)
        cx.mm(sp_[:, 0:nq], kr2.s(kt)[r0:r0 + 64, kt * 128:(kt + 1) * 128], qr.s(tk(q0, q0 + nq))[r0:r0 + 64, h // 2, q0:q0 + nq], start=False, stop=True)

    def v_fn(h, kt):
        return vaug.s(kt)[:, kt, h, :]

    def out_fn(h, qb, q0, accs):
        a = accs[0]
        j = oj[0]; oj[0] += 1
        o_, r_ = ob[j % 2], rc[j % 2]
        cx.op(DVE, "reciprocal", out=r_[:], in_=V(a.tile, a.ap[:, 128:129], None))
        cx.op(DVE, "tensor_scalar", out=o_[:], in0=V(a.tile, a.ap[:, 0:128], None), scalar1=r_[:, 0:1], scalar2=None, op0=ALU.mult)
        p = k.ps[6 + j % 2]
        pb = V(p, p.t[:].bitcast(BF16)[:, 0:128], None)
        cx.tr(pb, o_[:], k.identb[:])
        tt = q0 // 128
        qoff = q0 - BLKS[qb][0]
        cx.op(DVE, "tensor_tensor", out=y.s(tt)[:, h, q0:q0 + 128], in0=pb, in1=sz[:, h, qoff:qoff + 128], op=ALU.mult)

    attn_core(k, 4, 1, qk_fn, v_fn, out_fn, last, blk_fn)
    if k.dbg and s == 0 and l == 0:
        dump(k, y, k.dbg_y[0], 4)
    if stg < 6:
        cx.pop(); return
    merge(k, l, 0, y, hT, acc, BLKS[1:] if last else BLKS)
    cx.pop()


def sz_block(k, wz, hT, sz, b0, n):
    cx = k.cx
    for c in range(4):
        p = k.ps[6 + c % 2]
        for kc in range(8):
            cx.mm(p[:, 0:n], wz[:, kc, c * 128:(c + 1) * 128], hT.s(tk(b0, b0 + n))[:, kc, b0:b0 + n], start=(kc == 0), stop=(kc == 7))
        cx.act(sz[:, c, 0:n], p[:, 0:n], AF.Silu)


def proj_tm(k, hT, w, dst_fn, ncols=512):
    cx = k.cx
    for tt in range(NT):
        p = nps(k)
        for kc in range(8):
            cx.mm(p[:, 0:ncols], hT.s(tt)[:, kc, tt * 128:(tt + 1) * 128], w[:, kc, 0:ncols], start=(kc == 0), stop=(kc == 7))
        dst_fn(tt, p)


def diffattn(k, s, l, hT, acc, dc, last):
    cx = k.cx
    cx.push()
    qd = cx.sb((128, 4, T), BF16, NT); kd = cx.sb((128, 4, T), BF16, NT)
    vaug = cx.sb((128, NT, 4, 129), BF16, NT)
    _memset(cx, vaug[:], 1.0)
    W = k.w_in[l]
    cx.push()
    tmp = mk_tmp(cx, 1)
    for (dst, off, gcol) in ((qd, OFF["df_q"], DC_DFQG), (kd, OFF["df_k"], DC_DFKG)):
        w = wload(k, W[:, off:off + 512], 8, 512)
        for (b0, n) in BLKS:
            sb_ = tk(b0, b0 + n)
            for h in range(4):
                p = nps(k)
                for kc in range(8):
                    cx.mm(p[:, 0:n], w[:, kc, h * 128:(h + 1) * 128], hT.s(sb_)[:, kc, b0:b0 + n], start=(kc == 0), stop=(kc == 7))
                norm_group(k, [p], n, k.bd64, 64 * EPS, [dc[:, gcol:gcol + 1]], [tmp["xn"][:, 0:n]], tmp)
                rope(k, tmp["xn"][:, 0:n], dst.s(sb_)[:, h, b0:b0 + n], b0, n, tmp)
    wv = wload(k, W[:, OFF["df_v"]:OFF["df_v"] + 512], 8, 512)
    proj_tm(k, hT, wv, lambda tt, p: cx.op(DVE, "tensor_copy", out=vaug.s(tt)[:, tt, :, 0:128],
                                             in_=V(p, p.t[:, :].rearrange("p (h e) -> p h e", e=128), None)))
    cx.pop()
    y = cx.sb((128, 4, T), BF16, NT)
    sz = cx.sb((128, 4, 512), BF16)
    wz = wload(k, W[:, OFF["df_z"]:OFF["df_z"] + 512], 8, 512)
    o1 = [cx.sb((128, 128), F32) for _ in range(2)]
    o2 = [cx.sb((128, 128), F32) for _ in range(2)]
    ob = [cx.sb((128, 128), BF16) for _ in range(2)]
    junk = cx.sb((128, 128), BF16)
    rc = [cx.sb((128, 4), F32) for _ in range(2)]
    oj = [0]

    def qk_fn(h, m, kt, q0, nq, sp_):
        r0 = m * 64
        cx.mm(sp_[:, 0:nq], kd.s(kt)[r0:r0 + 64, h, kt * 128:(kt + 1) * 128], qd.s(tk(q0, q0 + nq))[r0:r0 + 64, h, q0:q0 + nq], start=True, stop=True)

    def v_fn(h, kt):
        return vaug.s(kt)[:, kt, h, :]

    def out_fn(h, qb, q0, accs):
        a0, a1 = accs
        j = oj[0]; oj[0] += 1
        r_ = rc[j % 2]
        cx.op(DVE, "reciprocal", out=r_[:, 0:1], in_=V(a0.tile, a0.ap[:, 128:129], None))
        cx.op(DVE, "reciprocal", out=r_[:, 1:2], in_=V(a1.tile, a1.ap[:, 128:129], None))
        cx.op(DVE, "tensor_tensor", out=r_[:, 1:2], in0=r_[:, 1:2], in1=dc[:, DC_LAM:DC_LAM + 1], op=ALU.mult)
        cx.op(DVE, "tensor_scalar", out=o1[j % 2][:], in0=V(a0.tile, a0.ap[:, 0:128], None), scalar1=r_[:, 0:1], scalar2=None, op0=ALU.mult)
        cx.op(DVE, "scalar_tensor_tensor", out=o2[j % 2][:], in0=V(a1.tile, a1.ap[:, 0:128], None), scalar=r_[:, 1:2], in1=o1[j % 2][:], op0=ALU.mult, op1=ALU.add)
        cx.act(junk[:], o2[j % 2][:], AF.Square, accum_out=r_[:, 2:3])
        cx.act(r_[:, 3:4], r_[:, 2:3], AF.Sqrt, scale=1.0 / 128, bias=k.epsc[:, 0:1])
        cx.op(DVE, "reciprocal", out=r_[:, 3:4], in_=r_[:, 3:4])
        cx.op(DVE, "tensor_scalar", out=ob[j % 2][:], in0=o2[j % 2][:], scalar1=r_[:, 3:4], scalar2=None, op0=ALU.mult)
        p = k.ps[6 + j % 2]
        pb = V(p, p.t[:].bitcast(BF16)[:, 0:128], None)
        cx.tr(pb, ob[j % 2][:], k.identb[:])
        tt = q0 // 128
        qoff = q0 - BLKS[qb][0]
        cx.op(DVE, "scalar_tensor_tensor", out=y.s(tt)[:, h, q0:q0 + 128], in0=pb, scalar=dc[:, DC_DFOG:DC_DFOG + 1], in1=sz[:, h, qoff:qoff + 128], op0=ALU.mult, op1=ALU.mult)

    attn_core(k, 4, 2, qk_fn, v_fn, out_fn, last, lambda qb, b0, n: sz_block(k, wz, hT, sz, b0, n))
    if k.dbg and s == 0 and l == 0:
        dump(k, y, k.dbg_y[3], 4)
    merge(k, l, 3, y, hT, acc, BLKS[1:] if last else BLKS)
    cx.pop()


def out_stage(k, l, i, hT, osrc_fn, y, dc, gcol0, zoff, last, extra_fn=None):
    cx = k.cx
    cx.push()
    sz = cx.sb((128, 4, 512), BF16)
    wz = wload(k, k.w_in[l][:, zoff:zoff + 512], 8, 512)
    on = [cx.sb((128, 512), BF16) for _ in range(2)]
    junk = cx.sb((128, 128), BF16)
    ss = [cx.sb((128, 4), F32) for _ in range(2)]
    for (b0, n) in (BLKS[1:] if last else BLKS):
        sz_block(k, wz, hT, sz, b0, n)
        for tt in tk(b0, b0 + n):
            src = osrc_fn(tt)
            s_, o_ = ss[tt % 2], on[tt % 2]
            for h in range(4):
                cx.act(junk[:], V(src.tile, src.ap[:, h * 128:(h + 1) * 128], src.subs), AF.Square, accum_out=s_[:, h:h + 1])
            cx.act(s_[:], s_[:], AF.Sqrt, scale=1.0 / 128, bias=k.epsc[:, 0:1])
            cx.op(DVE, "reciprocal", out=s_[:], in_=s_[:])
            for h in range(4):
                cx.op(DVE, "tensor_scalar", out=o_[:, h * 128:(h + 1) * 128], in0=V(src.tile, src.ap[:, h * 128:(h + 1) * 128], src.subs), scalar1=s_[:, h:h + 1], scalar2=None, op0=ALU.mult)
            p = nps(k)
            pb = p.t[:].bitcast(BF16)
            for h in range(4):
                cx.tr(V(p, pb[:, h * 128:(h + 1) * 128], None), o_[:, h * 128:(h + 1) * 128], k.identb[:])
            t0 = tt * 128
            for h in range(4):
                pv = V(p, pb[:, h * 128:(h + 1) * 128], None)
                if extra_fn is None:
                    cx.op(DVE, "scalar_tensor_tensor", out=y.s(tt)[:, h, t0:t0 + 128], in0=pv, scalar=dc[:, gcol0 + h:gcol0 + h + 1], in1=sz[:, h, t0 - b0:t0 - b0 + 128], op0=ALU.mult, op1=ALU.mult)
                else:
                    extra_fn(tt, h, pv, sz[:, h, t0 - b0:t0 - b0 + 128])
    cx.pop()


def gla(k, s, l, hT, acc, dc, last):
    cx = k.cx
    cx.push()
    W = k.w_in[l]
    ogl = cx.sb((128, NT, 512), BF16, NT)
    cx.push()
    qT = cx.sb((128, 2, T), BF16, NT); kT = cx.sb((128, 2, T), BF16, NT)
    afT = cx.sb((16, 2, T), BF16, NT)
    vtm = cx.sb((128, NT, 512), BF16, NT)
    aw = cx.sb((16, 2, 256), BF16)
    cx.dma(POOL, aw[:], U(k.gla_a_w[l].rearrange("d r n -> r d n")))
    wqk = wload(k, W[:, OFF["gla_q"]:OFF["gla_q"] + 512], 8, 512)
    waf = wload(k, W[:, OFF["gla_af"]:OFF["gla_af"] + 32], 8, 32)
    for (b0, n) in BLKS:
        sb_ = tk(b0, b0 + n)
        for c in range(4):
            p = nps(k)
            for kc in range(8):
                cx.mm(p[:, 0:n], wqk[:, kc, c * 128:(c + 1) * 128], hT.s(sb_)[:, kc, b0:b0 + n], start=(kc == 0), stop=(kc == 7))
            if c < 2:
                cx.act(qT.s(sb_)[:, c, b0:b0 + n], p[:, 0:n], AF.Copy, scale=0.125)
            else:
                cx.op(DVE, "tensor_copy", out=kT.s(sb_)[:, c - 2, b0:b0 + n], in_=p[:, 0:n])
        for d in range(2):
            p = nps(k)
            for kc in range(8):
                cx.mm(p[0:16, 0:n], waf[:, kc, d * 16:(d + 1) * 16], hT.s(sb_)[:, kc, b0:b0 + n], start=(kc == 0), stop=(kc == 7))
            cx.op(DVE, "tensor_copy", out=afT.s(sb_)[0:16, d, b0:b0 + n], in_=p[0:16, 0:n])
    wv = wload(k, W[:, OFF["gla_v"]:OFF["gla_v"] + 512], 8, 512)
    proj_tm(k, hT, wv, lambda tt, p: cx.act(vtm.s(tt)[:, tt, :], p[:, :], AF.Copy))
    S32_ = [cx.sb((128, 2, 128), F32) for _ in range(2)]; Sb_ = [cx.sb((128, 2, 128), BF16) for _ in range(2)]
    R = lambda shape, dt, nb=4: [cx.sb(shape, dt) for _ in range(nb)]
    e_ = R((128, 2, 128), F32); lp_ = R((128, 2, 128), F32); Bp_ = R((128, 2, 128), F32); tR_ = R((128, 2, 128), F32)
    E_ = R((128, 2, 128), BF16, 6); qe_ = R((128, 2, 128), BF16); ke_ = R((128, 2, 128), BF16); kl_ = R((128, 2, 128), BF16)
    bt_ = R((128, 2, 4), F32); kltm_ = R((128, 256), BF16); AT_ = R((128, 128), BF16, 4)
    orders = [list(range(NT)), [1, 0] + list(range(NT - 1, 1, -1))]
    masks = [k.trifb, k.tribb]
    written = set()
    if True:
        def prep(d, tt, j):
            t0 = tt * 128
            j = 2 * d + j
            e, lp, Bp, tR, qe, ke, kl, bt, kltm = e_[j], lp_[j], Bp_[j], tR_[j], qe_[j], ke_[j], kl_[j], bt_[j], kltm_[j]
            pl = nps(k)
            for c in range(2):
                cx.mm(pl[:, c * 128:(c + 1) * 128], aw[0:16, d, c * 128:(c + 1) * 128], afT.s(tt)[0:16, d, t0:t0 + 128])
            for c in range(2):
                cx.act(e[:, c, :], pl[:, c * 128:(c + 1) * 128], AF.Exp, scale=-1.0, bias=dc[:, DC_NAB + 2 * d + c:DC_NAB + 2 * d + c + 1])
            cx.act(lp[:], e[:], AF.Ln, bias=k.onec[:, 0:1])
            for c in range(2):
                cx.op(DVE, "tensor_tensor_scan", out=Bp[:, c, :], data0=k.onesf[:, 0:128], data1=lp[:, c, :], initial=0.0, op0=ALU.mult, op1=ALU.add)
            E1, E2, E3 = E_[3 * d], E_[3 * d + 1], E_[3 * d + 2]
            if d == 0:
                cx.op(DVE, "tensor_scalar", out=bt[:, :, 0:1], in0=Bp[:, :, 127:128], scalar1=-1.0 / 16, scalar2=None, op0=ALU.mult)
                cx.act(E1[:], Bp[:], AF.Exp, scale=-1.0 / 16)
                cx.act(E2[:], Bp[:], AF.Exp, scale=1.0 / 16)
                for c in range(2):
                    cx.act(E3[:, c, :], Bp[:, c, :], AF.Exp, scale=1.0 / 16, bias=bt[:, c, 0:1])
            else:
                cx.op(DVE, "tensor_tensor", out=tR[:], in0=lp[:], in1=Bp[:], op=ALU.subtract)
                cx.op(DVE, "tensor_scalar", out=bt[:, :, 0:1], in0=Bp[:, :, 127:128], scalar1=-1.0 / 16, scalar2=None, op0=ALU.mult)
                cx.op(DVE, "tensor_scalar", out=bt[:, :, 1:2], in0=Bp[:, :, 127:128], scalar1=1.0 / 16, scalar2=None, op0=ALU.mult)
                for c in range(2):
                    cx.act(E1[:, c, :], tR[:, c, :], AF.Exp, scale=-1.0 / 16, bias=bt[:, c, 0:1])
                    cx.act(E2[:, c, :], tR[:, c, :], AF.Exp, scale=1.0 / 16, bias=bt[:, c, 1:2])
                cx.act(E3[:], tR[:], AF.Exp, scale=1.0 / 16)
            cx.act(bt[:, :, 2:3], bt[:, :, 0:1], AF.Exp)
            cx.op(DVE, "tensor_tensor", out=qe[:], in0=qT.s(tt)[:, :, t0:t0 + 128], in1=E1[:], op=ALU.mult)
            cx.op(DVE, "tensor_tensor", out=ke[:], in0=kT.s(tt)[:, :, t0:t0 + 128], in1=E2[:], op=ALU.mult)
            cx.op(DVE, "tensor_tensor", out=kl[:], in0=kT.s(tt)[:, :, t0:t0 + 128], in1=E3[:], op=ALU.mult)
            ptr = nps(k)
            pb = ptr.t[:].bitcast(BF16)
            for c in range(2):
                cx.tr(V(ptr, pb[:, c * 128:(c + 1) * 128], None), kl[:, c, :], k.identb[:])
            cx.op(DVE, "tensor_copy", out=kltm[:], in_=V(ptr, pb[:, 0:256], None))

        def body(d, tt, j):
            j = 2 * d + j
            mask, S32, Sb = masks[d], S32_[d], Sb_[d]
            qe, ke, bt, kltm = qe_[j], ke_[j], bt_[j], kltm_[j]
            po = nps(k)
            for h in range(4):
                c, r0 = h // 2, (h % 2) * 64
                pa = nps(k)
                cx.mm(pa[:, 0:128], ke[r0:r0 + 64, c, :], qe[r0:r0 + 64, c, :])
                AT = AT_[h % 4]
                cx.op(DVE, "tensor_tensor", out=AT[:], in0=pa[:, 0:128], in1=mask[:], op=ALU.mult)
                cx.mm(po[:, h * 128:(h + 1) * 128], AT[:], vtm.s(tt)[:, tt, h * 128:(h + 1) * 128], start=True, stop=False)
                cx.mm(po[:, h * 128:(h + 1) * 128], qe[r0:r0 + 64, c, :], Sb[r0:r0 + 64, c, :], start=False, stop=True)
            pu = nps(k)
            for h in range(4):
                c, r0 = h // 2, (h % 2) * 64
                cx.mm(pu[r0:r0 + 64, c * 128:(c + 1) * 128], kltm[:, h * 64:(h + 1) * 64], vtm.s(tt)[:, tt, h * 128:(h + 1) * 128], start=True, stop=True)
            if tt not in written:
                written.add(tt)
                cx.act(ogl.s(tt)[:, tt, :], po[:, :], AF.Copy)
            else:
                cx.op(DVE, "tensor_tensor", out=ogl.s(tt)[:, tt, :], in0=po[:, :], in1=ogl.s(tt)[:, tt, :], op=ALU.add)
            for c in range(2):
                cx.op(DVE, "scalar_tensor_tensor", out=S32[:, c, :], in0=S32[:, c, :], scalar=bt[:, c, 2:3], in1=pu[:, c * 128:(c + 1) * 128], op0=ALU.mult, op1=ALU.add)
            cx.act(Sb[:], S32[:], AF.Copy)

        for d in range(2):
            _memset(cx, S32_[d][:], 0.0); _memset(cx, Sb_[d][:], 0.0)
            prep(d, orders[d][0], 0)
        for i in range(NT):
            for d in range(2):
                if i + 1 < NT:
                    prep(d, orders[d][i + 1], (i + 1) % 2)
                body(d, orders[d][i], i % 2)
    cx.pop()
    y = cx.sb((128, 4, T), BF16, NT)
    out_stage(k, l, 1, hT, lambda tt: ogl.s(tt)[:, tt, :], y, dc, DC_GLAOG, OFF["gla_z"], last)
    if k.dbg and s == 0 and l == 0:
        dump(k, y, k.dbg_y[1], 4)
    merge(k, l, 1, y, hT, acc, BLKS[1:] if last else BLKS)
    cx.pop()


def mlstm(k, s, l, hT, acc, dc, pr, last):
    cx = k.cx
    cx.push()
    W = k.w_in[l]
    xcT = cx.sb((128, 4, T), BF16, NT)
    hml = cx.sb((128, NT, 512), BF16, NT)
    cx.push()
    xraw = cx.sb((128, 4, T), BF16, NT)
    cacc = cx.sb((128, T), F32)
    wx = wload(k, W[:, OFF["ml_x"]:OFF["ml_x"] + 512], 8, 512)
    for (b0, n) in BLKS:
        sb_ = tk(b0, b0 + n)
        for c in range(4):
            p = nps(k)
            for kc in range(8):
                cx.mm(p[:, 0:n], wx[:, kc, c * 128:(c + 1) * 128], hT.s(sb_)[:, kc, b0:b0 + n], start=(kc == 0), stop=(kc == 7))
            if c % 2:
                cx.act(xraw.s(sb_)[:, c, b0:b0 + n], p[:, 0:n], AF.Copy)
            else:
                cx.op(DVE, "tensor_copy", out=xraw.s(sb_)[:, c, b0:b0 + n], in_=p[:, 0:n])
    pc = k.pc
    for c in range(4):
        w0, w1, w2 = [pc[:, l, PC_CONVW + 4 * j + c:PC_CONVW + 4 * j + c + 1] for j in range(3)]
        cx.op(DVE, "tensor_scalar", out=cacc[:], in0=xraw[:, c, :], scalar1=w1, scalar2=pc[:, l, PC_CONVB + c:PC_CONVB + c + 1], op0=ALU.mult, op1=ALU.add)
        for (a_, b_) in ((0, NCTX), (NCTX, T)):
            cx.op(DVE, "scalar_tensor_tensor", out=cacc[:, a_ + 1:b_], in0=xraw[:, c, a_:b_ - 1], scalar=w0, in1=cacc[:, a_ + 1:b_], op0=ALU.mult, op1=ALU.add)
            cx.op(DVE, "scalar_tensor_tensor", out=cacc[:, a_:b_ - 1], in0=xraw[:, c, a_ + 1:b_], scalar=w2, in1=cacc[:, a_:b_ - 1], op0=ALU.mult, op1=ALU.add)
        cx.act(xcT[:, c, :], cacc[:], AF.Silu)
    cx.pop()
    cx.push()
    qT = cx.sb((128, 2, T), BF16, NT); kT = cx.sb((128, 2, T), BF16, NT)
    vaug = cx.sb((128, NT, 4, 129), BF16, NT)
    _memset(cx, vaug[:], 1.0)
    gt = cx.sb((128, NT, 16), F32); lpt = cx.sb((128, NT, 16), F32)
    wqb = cx.sb((128, 4, 64), BF16); wkb = cx.sb((128, 4, 64), BF16)
    cx.dma(POOL, wqb[:], U(k.ml_wq[l].rearrange("h c d -> c h d")))
    cx.dma(POOL, wkb[:], U(k.ml_wk[l].rearrange("h c d -> c h d")))
    for (b0, n) in BLKS:
        sb_ = tk(b0, b0 + n)
        for (dst, wb, scl) in ((qT, wqb, 1.0), (kT, wkb, 0.125)):
            for c in range(2):
                p = nps(k)
                for hh in range(2):
                    h = 2 * c + hh
                    cx.mm(p[hh * 64:(hh + 1) * 64, 0:n], wb[:, h, :], xcT.s(sb_)[:, h, b0:b0 + n], start=True, stop=True)
                cx.act(dst.s(sb_)[:, c, b0:b0 + n], p[:, 0:n], AF.Copy, scale=scl)
    wv = wload(k, W[:, OFF["ml_v"]:OFF["ml_v"] + 512], 8, 512)
    proj_tm(k, hT, wv, lambda tt, p: cx.op(DVE, "tensor_copy", out=vaug.s(tt)[:, tt, :, 0:128],
                                             in_=V(p, p.t[:, :].rearrange("p (h e) -> p h e", e=128), None)))
    wif = wload(k, W[:, OFF["ml_if"]:OFF["ml_if"] + 16], 8, 16)
    proj_tm(k, hT, wif, lambda tt, p: cx.op(DVE, "tensor_tensor", out=gt[:, tt, :], in0=p[:, 0:16], in1=pr[:, 0:16], op=ALU.add), ncols=16)
    cx.act(lpt[:], gt[:], AF.Exp, scale=-1.0)
    cx.act(lpt[:], lpt[:], AF.Ln, bias=k.onec[:, 0:1])
    Bp = cx.sb((128, 2, NT, 4), F32); Bt = cx.sb((128, 2, NT, 4), F32)
    fa = cx.sb((128, 2, NT, 4), F32); fg = cx.sb((128, 2, NT, 4), F32); fgk = cx.sb((128, 2, NT, 4), F32); fdec = cx.sb((128, 2, NT, 4), F32)
    for d in range(2):
        g0 = (1 + 2 * d) * 4
        rhs = lpt[:, :, g0:g0 + 4]
        p = nps(k)
        cx.mm(p[:, 0:72], (k.trif if d == 0 else k.trib)[:], rhs)
        cx.op(DVE, "tensor_copy", out=Bp[:, d], in_=V(p, p.t[:, 0:72].rearrange("p (a b) -> p a b", b=4), None))
        p = nps(k)
        cx.mm(p[:, 0:72], k.onesf[:], rhs)
        cx.op(DVE, "tensor_copy", out=Bt[:, d], in_=V(p, p.t[:, 0:72].rearrange("p (a b) -> p a b", b=4), None))
        li = gt[:, :, 8 * d:8 * d + 4]
        cx.act(fa[:, d], Bp[:, d], AF.Exp, scale=-1.0)
        cx.act(fdec[:, d], Bt[:, d], AF.Exp, scale=-1.0)
        cx.op(DVE, "tensor_tensor", out=Bp[:, d], in0=Bp[:, d], in1=li, op=ALU.add)
        cx.act(fg[:, d], Bp[:, d], AF.Exp)
        cx.op(DVE, "tensor_tensor", out=Bp[:, d], in0=Bp[:, d], in1=Bt[:, d], op=ALU.subtract)
        cx.act(fgk[:, d], Bp[:, d], AF.Exp)
    C32_ = [cx.sb((128, 2, 129), F32) for _ in range(2)]; Cb_ = [cx.sb((128, 2, 129), BF16) for _ in range(2)]
    PT_ = [cx.sb((128, 128), BF16) for _ in range(4)]
    kg_ = [cx.sb((128, 256), BF16) for _ in range(4)]
    dn_ = [cx.sb((128, 4), F32) for _ in range(4)]
    orders = [list(range(NT)), [1, 0] + list(range(NT - 1, 1, -1))]
    masks = [k.trifb, k.tribb]
    written = set()
    if True:
        def prep(d, tt, j):
            t0 = tt * 128
            kg = kg_[2 * d + j]
            pk = nps(k)
            for h in range(4):
                cx.mm(pk[:, h * 64:(h + 1) * 64], xcT.s(tt)[:, h, t0:t0 + 128], wkb[:, h, :], start=True, stop=True)
            for h in range(4):
                cx.op(DVE, "tensor_scalar", out=kg[:, h * 64:(h + 1) * 64], in0=pk[:, h * 64:(h + 1) * 64], scalar1=fgk[:, d, tt, h:h + 1], scalar2=0.125, op0=ALU.mult, op1=ALU.mult)

        def body(d, tt, j):
            t0 = tt * 128
            mask, C32, Cb = masks[d], C32_[d], Cb_[d]
            kg, dn = kg_[2 * d + j], dn_[2 * d + j]
            first = tt not in written
            written.add(tt)
            pos = [nps(k), nps(k)]
            for h in range(4):
                c, r0 = h // 2, (h % 2) * 64
                pa = nps(k)
                cx.mm(pa[:, 0:128], kT.s(tt)[r0:r0 + 64, c, t0:t0 + 128], qT.s(tt)[r0:r0 + 64, c, t0:t0 + 128])
                PT = PT_[h % 4]
                cx.op(DVE, "scalar_tensor_tensor", out=PT[:], in0=pa[:, 0:128], scalar=fg[:, d, tt, h:h + 1], in1=mask[:], op0=ALU.mult, op1=ALU.mult)
                o = (h % 2) * 129
                cx.mm(pos[c][:, o:o + 129], PT[:], vaug.s(tt)[:, tt, h, :], start=True, stop=False)
                cx.mm(pos[c][:, o:o + 129], qT.s(tt)[r0:r0 + 64, c, t0:t0 + 128], Cb[r0:r0 + 64, c, :], start=False, stop=True)
            pu = nps(k)
            for h in range(4):
                c, r0 = h // 2, (h % 2) * 64
                cx.mm(pu[r0:r0 + 64, c * 129:(c + 1) * 129], kg[:, h * 64:(h + 1) * 64], vaug.s(tt)[:, tt, h, :], start=True, stop=True)
            for c in range(2):
                po = pos[c]
                den = V(po, po.t[:, 0:258].rearrange("p (a b) -> p a b", b=129)[:, :, 128], None)
                cx.op(DVE, "tensor_tensor", out=dn[:, 0:2], in0=den, in1=fa[:, d, tt, 2 * c:2 * c + 2], op=ALU.mult)
                cx.act(dn[:, 0:2], dn[:, 0:2], AF.Abs)
                cx.op(DVE, "tensor_scalar", out=dn[:, 0:2], in0=dn[:, 0:2], scalar1=1.0, scalar2=None, op0=ALU.max)
                cx.op(DVE, "reciprocal", out=dn[:, 0:2], in_=dn[:, 0:2])
                cx.op(DVE, "tensor_tensor", out=dn[:, 2:4], in0=dn[:, 0:2], in1=fa[:, d, tt, 2 * c:2 * c + 2], op=ALU.mult)
                for hh in range(2):
                    h = 2 * c + hh
                    dst = hml.s(tt)[:, tt, h * 128:(h + 1) * 128]
                    if first:
                        cx.op(DVE, "tensor_scalar", out=dst, in0=po[:, hh * 129:hh * 129 + 128], scalar1=dn[:, 2 + hh:3 + hh], scalar2=None, op0=ALU.mult)
                    else:
                        cx.op(DVE, "scalar_tensor_tensor", out=dst, in0=po[:, hh * 129:hh * 129 + 128], scalar=dn[:, 2 + hh:3 + hh], in1=dst, op0=ALU.mult, op1=ALU.add)
            for h in range(4):
                c, r0 = h // 2, (h % 2) * 64
                cx.op(DVE, "scalar_tensor_tensor", out=C32[r0:r0 + 64, c, :], in0=C32[r0:r0 + 64, c, :], scalar=fdec[r0:r0 + 64, d, tt, h:h + 1], in1=pu[r0:r0 + 64, c * 129:(c + 1) * 129], op0=ALU.mult, op1=ALU.add)
            cx.act(Cb[:], C32[:], AF.Copy)

        for d in range(2):
            _memset(cx, C32_[d][:], 0.0); _memset(cx, Cb_[d][:], 0.0)
            prep(d, orders[d][0], 0)
        for i in range(NT):
            for d in range(2):
                if i + 1 < NT:
                    prep(d, orders[d][i + 1], (i + 1) % 2)
                body(d, orders[d][i], i % 2)
    cx.pop()
    y = cx.sb((128, 4, T), BF16, NT)
    cx.push()
    wo = wload(k, W[:, OFF["ml_o"]:OFF["ml_o"] + 512], 8, 512)
    sgo = [cx.sb((128, 512), BF16) for _ in range(2)]

    def og(tt, p):
        cx.act(sgo[tt % 2][:], p[:, :], AF.Sigmoid)
        cx.op(DVE, "tensor_tensor", out=hml.s(tt)[:, tt, :], in0=hml.s(tt)[:, tt, :], in1=sgo[tt % 2][:], op=ALU.mult)
    proj_tm(k, hT, wo, og)
    t1_ = [cx.sb((128, 128), F32) for _ in range(2)]
    t2_ = [cx.sb((128, 128), F32) for _ in range(2)]
    ej = [0]

    def extra(tt, h, pv, szv):
        j = ej[0] % 2; ej[0] += 1
        t0 = tt * 128
        cx.act(t1_[j][:], pv, AF.Identity, scale=dc[:, DC_MLOG + h:DC_MLOG + h + 1])
        cx.op(DVE, "scalar_tensor_tensor", out=t2_[j][:], in0=xcT.s(tt)[:, h, t0:t0 + 128], scalar=k.pc[:, l, PC_MLSKIP + h:PC_MLSKIP + h + 1], in1=t1_[j][:], op0=ALU.mult, op1=ALU.add)
        cx.op(DVE, "tensor_tensor", out=y.s(tt)[:, h, t0:t0 + 128], in0=t2_[j][:], in1=szv, op=ALU.mult)
    out_stage(k, l, 2, hT, lambda tt: hml.s(tt)[:, tt, :], y, dc, DC_MLOG, OFF["ml_z"], last, extra)
    cx.pop()
    if k.dbg and s == 0 and l == 0:
        dump(k, y, k.dbg_y[2], 4)
    merge(k, l, 2, y, hT, acc, BLKS[1:] if last else BLKS)
    cx.pop()


def dumpv(k, name, v, ncols):
    cx = k.cx
    dst = k.nc.dram_tensor(name, [128, ncols], F32, kind="ExternalOutput").ap()
    cx.push()
    f = cx.sb((128, 256), F32)
    for j in range(0, ncols, 256):
        w = min(256, ncols - j)
        cx.op(DVE, "tensor_copy", out=f[:, 0:w], in_=V(v.tile, v.ap[:, j:j + w], v.subs))
        cx.dma(SP, U(dst[:, j:j + w]), f[:, 0:w])
    cx.pop()


def dump(k, t, dst, nch):
    cx = k.cx
    cx.push()
    f = cx.sb((128, 256), F32)
    for c in range(nch):
        for j in range(T // 256):
            cx.op(DVE, "tensor_copy", out=f[:], in_=t[:, c, j * 256:(j + 1) * 256])
            cx.dma(SP, U(dst[:, c, j * 256:(j + 1) * 256]), f[:])
    cx.pop()


def layer(k, s, l, last):
    cx = k.cx
    cx.push()
    hT = cx.sb((128, 8, T), BF16, NT)
    acc = cx.sb((128, 8, T), BF16, NT)
    stg = int(os.environ.get("KSTAGE", "9"))
    if stg < 1:
        cx.pop(); return
    dc, pr = layer_params(k, l)
    if stg < 2:
        cx.pop(); return
    stage_h(k, s, l, hT)
    if stg < 3:
        if k.dbg:
            dump(k, hT, k.dbg_h, 8)
        cx.pop(); return
    if k.dbg and s == 0 and l == 0:
        dump(k, hT, k.dbg_h, 8)
    mla(k, s, l, hT, acc, dc, last)
    if stg < 7:
        cx.pop(); return
    mix = os.environ.get("KMIX", "123")
    if "1" in mix:
        gla(k, s, l, hT, acc, dc, last)
    if "2" in mix:
        mlstm(k, s, l, hT, acc, dc, pr, last)
    if "3" in mix:
        diffattn(k, s, l, hT, acc, dc, last)
    if k.dbg and s == 0 and l == 0:
        dump(k, acc, k.dbg_acc, 8)
    final(k, s, l, acc, last)
    cx.pop()


def _consts():
    ident = np.eye(128, dtype=np.float32)
    bd = np.zeros((128, 128), np.float32); bd[:64, :64] = 1; bd[64:, 64:] = 1
    rm = np.zeros((64, 64), np.float32)
    for i in range(16):
        rm[16 + i, i] = -1; rm[i, 16 + i] = 1; rm[48 + i, 32 + i] = -1; rm[32 + i, 48 + i] = 1
    rm2 = np.zeros((128, 128), np.float32); rm2[:64, :64] = rm; rm2[64:, 64:] = rm
    si, ti = np.meshgrid(np.arange(128), np.arange(128), indexing="ij")
    trif = (si <= ti).astype(np.float32); trib = (si >= ti).astype(np.float32)
    quarter = 16
    inv_freq = (10000.0 ** (-np.arange(quarter, dtype=np.float32) / quarter)).astype(np.float32)
    row = np.repeat(np.arange(32, dtype=np.float32), 64); col = np.tile(np.arange(64, dtype=np.float32), 32)
    ar = row[:, None] * inv_freq; ac = col[:, None] * inv_freq
    ang = np.concatenate([ar, ar, ac, ac], axis=-1).astype(np.float32)
    cos = np.concatenate([np.ones((NCTX, 64), np.float32), np.cos(ang)], 0)
    sin = np.concatenate([np.zeros((NCTX, 64), np.float32), np.sin(ang)], 0)
    cosT = np.ascontiguousarray(np.concatenate([cos.T, cos.T], 0)); sinT = np.ascontiguousarray(np.concatenate([sin.T, sin.T], 0))
    return dict(cident=ident, cbd64=bd, crm2=rm2, ctrif=trif, ctrib=trib, ccos=cosT.astype(np.float32), csin=sinT.astype(np.float32))


def _cols(v):
    v = np.asarray(v, np.float32).reshape(-1)
    return v.reshape(-1, 128).T


def _pack(inp):
    pcols = np.zeros((128, NL, NPC), np.float32)
    prow = np.zeros((NL, NPR), np.float32)
    for l in range(NL):
        P = pcols[:, l]
        P[:, PC_NORMG:PC_NORMG + 8] = _cols(inp["norm_g"][l])
        P[:, PC_CQG:PC_CQG + 3] = _cols(inp["mla_cq_g"][l]); P[:, PC_CKVG:PC_CKVG + 2] = _cols(inp["mla_ckv_g"][l])
        qg, kg = inp["mla_q_g"][l], inp["mla_k_g"][l]
        P[:, PC_QGN] = qg[:128]; P[:, PC_QGR] = np.concatenate([qg[128:], qg[128:]])
        P[:, PC_KGN] = kg[:128]; P[:, PC_KGR] = np.concatenate([kg[128:], kg[128:]])
        P[:, PC_DFQG] = np.concatenate([inp["df_qk_g"][l, 0]] * 2); P[:, PC_DFKG] = np.concatenate([inp["df_qk_g"][l, 1]] * 2)
        P[:, PC_DFOG] = inp["df_out_g"][l]
        P[:, PC_GLAOG:PC_GLAOG + 4] = _cols(inp["gla_out_g"][l]); P[:, PC_MLOG:PC_MLOG + 4] = _cols(inp["ml_out_g"][l])
        P[:, PC_MLSKIP:PC_MLSKIP + 4] = _cols(inp["ml_skip"][l])
        for j in range(3):
            P[:, PC_CONVW + 4 * j:PC_CONVW + 4 * j + 4] = _cols(inp["ml_conv_w"][l, j])
        P[:, PC_CONVB:PC_CONVB + 4] = _cols(inp["ml_conv_b"][l])
        for d_ in range(2):
            P[:, PC_GLAAB + 2 * d_:PC_GLAAB + 2 * d_ + 2] = _cols(inp["gla_a_b"][l, d_])
        prow[l, 0:16] = inp["ml_gate_b"][l].reshape(-1)
        prow[l, 16:272] = inp["df_lambda"][l].reshape(-1)
    return pcols, prow


def _perm_wuq(w):
    w = w.reshape(NL, 384, 4, 192)
    return np.ascontiguousarray(np.concatenate([w[..., :128].reshape(NL, 384, 512), w[..., 128:].reshape(NL, 384, 256)], -1))


def _perm_wukv(w):
    w = w.reshape(NL, 256, 4, 256)
    return np.ascontiguousarray(np.concatenate([w[..., :128].reshape(NL, 256, 512), w[..., 128:].reshape(NL, 256, 512)], -1))


_CACHE = {}


def host_maps(inp, ncores=8):
    inp = {k_: np.asarray(v, np.float32) for k_, v in inp.items()}
    pcols, prow = _pack(inp)
    shared = dict(ada_w=inp["ada_w"], ada_b=inp["ada_b"], w_in=inp["w_in"], wuq=_perm_wuq(inp["mla_wuq"]),
                  wukv=_perm_wukv(inp["mla_wukv"]), gla_a_w=inp["gla_a_w"], ml_wq=inp["ml_wq"], ml_wk=inp["ml_wk"],
                  br_w=inp["br_w"], w_out=inp["w_out"], pcols=pcols, prow=prow, **_consts())
    maps = []
    for c in range(ncores):
        m = dict(shared)
        m["x"] = np.ascontiguousarray(inp["x"][4 * c:4 * c + 4]); m["ctx"] = np.ascontiguousarray(inp["ctx"][4 * c:4 * c + 4])
        m["c5"] = np.ascontiguousarray(np.concatenate([inp["c"][4 * c:4 * c + 4], inp["c_ctx"][None]], 0))
        maps.append(m)
    return maps


def kernel(**inputs):
    if "nc" not in _CACHE:
        _CACHE["nc"] = build(4, NL)[0]
    nc = _CACHE["nc"]
    maps = host_maps(inputs)
    res = run_bass_kernel_spmd(nc, maps, core_ids=list(range(8)))
    return np.concatenate([np.asarray(r["out"], np.float32) for r in res.results], 0)
```
